# Optimizing a Trainium2 kernel written in Bass

```python
import math
import jax
import jax.numpy as jnp
from jax import lax
import numpy as np


D_MODEL = 2048
BATCH = 1
SEQ = 16384
DEPTH = 1

RWKV_HEADS = 16
RWKV_HEAD_DIM = 64
RWKV_WIDTH = RWKV_HEADS * RWKV_HEAD_DIM
DECAY_LORA = 96
ICLR_LORA = 96
GATE_LORA = 256
DIFF_HEADS = 8
DIFF_HEAD_DIM = 64
DIFF_V_DIM = 2 * DIFF_HEAD_DIM
DIFF_QK_COLS = DIFF_HEADS * 2 * DIFF_HEAD_DIM
DIFF_WIDTH = DIFF_HEADS * DIFF_V_DIM
Q_BLOCK = 128
N_BRANCHES = 2
RWKV_COLS = 3 * RWKV_WIDTH + DECAY_LORA + ICLR_LORA + GATE_LORA
DIFF_COLS = 2 * DIFF_QK_COLS + DIFF_WIDTH
GATE_COLS = N_BRANCHES * D_MODEL
IN_COLS = RWKV_COLS + DIFF_COLS + GATE_COLS
PEER_HEADS = 8
PEER_QUERY_DIM = 256
PEER_HALF = PEER_QUERY_DIM // 2
N_KEYS = 128
N_EXPERTS = N_KEYS * N_KEYS
PEER_TOPK = 16
PEER_CHUNK = 128
NORM_EPS = 1e-6
LN_X_EPS = 64e-5
SUBLN_EPS = 1e-5
NEG_INF = -1e30

kernel_name = 'hybrid_rwkv7_diffattn_peer'


def rmsnorm(x, g, eps=NORM_EPS):
    x32 = x.astype(jnp.float32)
    y = x32 * lax.rsqrt(jnp.mean(x32 * x32, axis=-1, keepdims=True) + eps)
    return (y * g.astype(jnp.float32)).astype(x.dtype)


def lambda_init_fn(layer):
    return 0.8 - 0.6 * math.exp(-0.3 * layer)


def wkv7_scan(r, decay, k, v, kk, a):
    B, S, H, N = r.shape

    def step(state, inp):
        r_t, w_t, k_t, v_t, kk_t, a_t = inp
        sa = jnp.einsum('bhvk,bhk->bhv', state, -kk_t)
        state = (state * w_t[:, :, None, :]
                 + sa[..., None] * (kk_t * a_t)[:, :, None, :]
                 + v_t[..., None] * k_t[:, :, None, :])
        y_t = jnp.einsum('bhvk,bhk->bhv', state, r_t)
        return state, y_t

    xs = (jnp.moveaxis(r, 1, 0), jnp.moveaxis(decay, 1, 0), jnp.moveaxis(k, 1, 0),
          jnp.moveaxis(v, 1, 0), jnp.moveaxis(kk, 1, 0), jnp.moveaxis(a, 1, 0))
    state0 = jnp.zeros((B, H, N, N), jnp.float32)
    _, ys = lax.scan(step, state0, xs)
    return jnp.moveaxis(ys, 0, 1)


def rwkv7_time_mix(p, mu, w0, w_decay_up, a0, w_iclr_up, w_gate_up, k_k, k_a, r_k, lnx_g, lnx_b):
    B, S, _ = p.shape
    H, N = RWKV_HEADS, RWKV_HEAD_DIM
    prev = jnp.pad(p, ((0, 0), (1, 0), (0, 0)))[:, :-1]
    p = p + (prev - p) * mu
    splits = [RWKV_WIDTH, 2 * RWKV_WIDTH, 3 * RWKV_WIDTH,
              3 * RWKV_WIDTH + DECAY_LORA, 3 * RWKV_WIDTH + DECAY_LORA + ICLR_LORA]
    r, k, v, xw, xa, xg = jnp.split(p, splits, axis=-1)
    w_log = -jax.nn.softplus(-(w0 + jnp.tanh(xw) @ w_decay_up)) - 0.5
    decay = jnp.exp(-jnp.exp(w_log.astype(jnp.float32)))
    a = jax.nn.sigmoid(a0 + xa @ w_iclr_up)
    g = jax.nn.sigmoid(xg) @ w_gate_up
    kk = (k * k_k).reshape(B, S, H, N).astype(jnp.float32)
    kk = kk / jnp.maximum(jnp.sqrt(jnp.sum(kk * kk, axis=-1, keepdims=True)), 1e-12)
    k = k * (1.0 + (a - 1.0) * k_a)
    r_h = r.reshape(B, S, H, N).astype(jnp.float32)
    k_h = k.reshape(B, S, H, N).astype(jnp.float32)
    v_h = v.reshape(B, S, H, N).astype(jnp.float32)
    a_h = a.reshape(B, S, H, N).astype(jnp.float32)
    y = wkv7_scan(r_h, decay.reshape(B, S, H, N), k_h, v_h, kk, a_h)
    mean = jnp.mean(y, axis=-1, keepdims=True)
    var = jnp.mean(jnp.square(y - mean), axis=-1, keepdims=True)
    y = ((y - mean) * lax.rsqrt(var + LN_X_EPS)).reshape(B, S, RWKV_WIDTH)
    y = y * lnx_g.astype(jnp.float32) + lnx_b.astype(jnp.float32)
    bonus = jnp.sum(r_h * k_h * r_k.astype(jnp.float32), axis=-1, keepdims=True) * v_h
    out = (y + bonus.reshape(B, S, RWKV_WIDTH)) * g.astype(jnp.float32)
    return out.astype(p.dtype)


def diff_attention(p, lam_q1, lam_k1, lam_q2, lam_k2, subln_g, lambda_init):
    B, S, _ = p.shape
    H, d = DIFF_HEADS, DIFF_HEAD_DIM
    q, k, v = jnp.split(p, [DIFF_QK_COLS, 2 * DIFF_QK_COLS], axis=-1)
    q = q.reshape(B, S, H, 2, d).transpose(3, 0, 2, 1, 4)
    k = k.reshape(B, S, H, 2, d).transpose(3, 0, 2, 1, 4)
    v = v.reshape(B, S, H, 2 * d).transpose(0, 2, 1, 3)
    lam = (jnp.exp(jnp.sum(lam_q1.astype(jnp.float32) * lam_k1.astype(jnp.float32)))
           - jnp.exp(jnp.sum(lam_q2.astype(jnp.float32) * lam_k2.astype(jnp.float32)))
           + lambda_init)
    scale = d ** -0.5
    n_blocks = S // Q_BLOCK
    q_blocks = q.reshape(2, B, H, n_blocks, Q_BLOCK, d).transpose(3, 0, 1, 2, 4, 5)
    key_pos = jnp.arange(S)

    def attend(args):
        q_blk, blk = args
        q_pos = blk * Q_BLOCK + jnp.arange(Q_BLOCK)
        causal = key_pos[None, :] <= q_pos[:, None]
        s = jnp.einsum('mbhqd,mbhkd->mbhqk', q_blk, k).astype(jnp.float32) * scale
        prob = jax.nn.softmax(jnp.where(causal, s, NEG_INF), axis=-1)
        attn = prob[0] - lam * prob[1]
        return jnp.einsum('bhqk,bhkd->bhqd', attn.astype(v.dtype), v)

    o = lax.map(attend, (q_blocks, jnp.arange(n_blocks)))
    o = o.transpose(1, 0, 3, 2, 4).reshape(B, S, H, 2 * d)
    o = rmsnorm(o, subln_g, SUBLN_EPS) * (1.0 - lambda_init)
    return o.reshape(B, S, DIFF_WIDTH)


def peer_ffn(h, wq, sub_keys, u_tab, v_tab):
    B, S, D = h.shape
    n_chunks = (B * S) // PEER_CHUNK
    xc = h.reshape(n_chunks, PEER_CHUNK, D)

    def chunk(xb):
        q = (xb @ wq).reshape(PEER_CHUNK, PEER_HEADS, 2, PEER_HALF)
        s = jnp.einsum('chpd,hpnd->chpn', q, sub_keys).astype(jnp.float32)
        top_s, top_i = lax.top_k(s, PEER_TOPK)
        cand_s = top_s[:, :, 0, :, None] + top_s[:, :, 1, None, :]
        cand_i = top_i[:, :, 0, :, None] * N_KEYS + top_i[:, :, 1, None, :]
        cand_s = cand_s.reshape(PEER_CHUNK, PEER_HEADS, PEER_TOPK * PEER_TOPK)
        cand_i = cand_i.reshape(PEER_CHUNK, PEER_HEADS, PEER_TOPK * PEER_TOPK)
        best_s, pos = lax.top_k(cand_s, PEER_TOPK)
        idx = jnp.take_along_axis(cand_i, pos, axis=-1)
        gate = jax.nn.softmax(best_s, axis=-1)
        u = jnp.take(u_tab, idx, axis=0)
        act = jax.nn.gelu(jnp.einsum('cd,chkd->chk', xb, u), approximate=False)
        vv = jnp.take(v_tab, idx, axis=0)
        return jnp.einsum('chk,chkd->cd', (gate * act.astype(jnp.float32)).astype(vv.dtype), vv)

    return lax.map(chunk, xc).reshape(B, S, D)


def setup_inputs(seed: int = 0) -> dict:
    key = jax.random.key(seed)
    ks = jax.random.split(key, 32)
    f32 = jnp.float32
    L = DEPTH

    def nrm(k, shape, scale):
        return jax.random.normal(k, shape, f32) * scale

    return {
        'x': nrm(ks[0], (BATCH, SEQ, D_MODEL), 1.0),
        'norm1_g': 1.0 + nrm(ks[1], (L, D_MODEL), 0.02),
        'w_in': nrm(ks[2], (L, D_MODEL, IN_COLS), D_MODEL ** -0.5),
        'shift_mu': jax.random.uniform(ks[3], (L, RWKV_COLS), f32),
        'rwkv_w0': jax.random.uniform(ks[4], (L, RWKV_WIDTH), f32, -5.5, -0.5),
        'w_decay_up': nrm(ks[5], (L, DECAY_LORA, RWKV_WIDTH), 0.5 * DECAY_LORA ** -0.5),
        'rwkv_a0': nrm(ks[6], (L, RWKV_WIDTH), 0.1),
        'w_iclr_up': nrm(ks[7], (L, ICLR_LORA, RWKV_WIDTH), ICLR_LORA ** -0.5),
        'w_gate_up': nrm(ks[8], (L, GATE_LORA, RWKV_WIDTH), GATE_LORA ** -0.5),
        'k_k': 0.85 + nrm(ks[9], (L, RWKV_WIDTH), 0.05),
        'k_a': 1.0 + nrm(ks[10], (L, RWKV_WIDTH), 0.05),
        'r_k': nrm(ks[11], (L, RWKV_HEADS, RWKV_HEAD_DIM), 0.1),
        'lnx_g': 1.0 + nrm(ks[12], (L, RWKV_WIDTH), 0.02),
        'lnx_b': nrm(ks[13], (L, RWKV_WIDTH), 0.02),
        'lam_q1': nrm(ks[14], (L, DIFF_HEAD_DIM), 0.1),
        'lam_k1': nrm(ks[15], (L, DIFF_HEAD_DIM), 0.1),
        'lam_q2': nrm(ks[16], (L, DIFF_HEAD_DIM), 0.1),
        'lam_k2': nrm(ks[17], (L, DIFF_HEAD_DIM), 0.1),
        'subln_g': 1.0 + nrm(ks[18], (L, DIFF_V_DIM), 0.02),
        'w_proj_a': nrm(ks[19], (L, RWKV_WIDTH, D_MODEL), RWKV_WIDTH ** -0.5),
        'w_proj_b': nrm(ks[20], (L, DIFF_WIDTH, D_MODEL), DIFF_WIDTH ** -0.5),
        'w_out': nrm(ks[21], (L, D_MODEL, D_MODEL), D_MODEL ** -0.5),
        'norm2_g': 1.0 + nrm(ks[22], (L, D_MODEL), 0.02),
        'peer_wq': nrm(ks[23], (L, D_MODEL, PEER_HEADS * PEER_QUERY_DIM), D_MODEL ** -0.5),
        'peer_sub_keys': nrm(ks[24], (L, PEER_HEADS, 2, N_KEYS, PEER_HALF), PEER_HALF ** -0.5),
        'peer_u': nrm(ks[25], (L, N_EXPERTS, D_MODEL), D_MODEL ** -0.5),
        'peer_v': nrm(ks[26], (L, N_EXPERTS, D_MODEL), PEER_HEADS ** -0.5),
        'final_g': 1.0 + nrm(ks[27], (D_MODEL,), 0.02),
    }


def reference(x, norm1_g, w_in, shift_mu, rwkv_w0, w_decay_up, rwkv_a0, w_iclr_up, w_gate_up,
              k_k, k_a, r_k, lnx_g, lnx_b, lam_q1, lam_k1, lam_q2, lam_k2, subln_g,
              w_proj_a, w_proj_b, w_out, norm2_g, peer_wq, peer_sub_keys, peer_u, peer_v,
              final_g):
    B, S, D = x.shape
    h = x
    for l in range(DEPTH):
        xn = rmsnorm(h, norm1_g[l])
        proj = xn @ w_in[l]
        p_rwkv, p_diff, p_gate = jnp.split(proj, [RWKV_COLS, RWKV_COLS + DIFF_COLS], axis=-1)
        y_a = rwkv7_time_mix(p_rwkv, shift_mu[l], rwkv_w0[l], w_decay_up[l], rwkv_a0[l],
                             w_iclr_up[l], w_gate_up[l], k_k[l], k_a[l], r_k[l],
                             lnx_g[l], lnx_b[l])
        y_b = diff_attention(p_diff, lam_q1[l], lam_k1[l], lam_q2[l], lam_k2[l], subln_g[l],
                             lambda_init_fn(l))
        gates = jax.nn.sigmoid(p_gate).reshape(B, S, N_BRANCHES, D)
        merged = gates[:, :, 0, :] * (y_a @ w_proj_a[l]) + gates[:, :, 1, :] * (y_b @ w_proj_b[l])
        h = h + merged @ w_out[l]
        h = h + peer_ffn(rmsnorm(h, norm2_g[l]), peer_wq[l], peer_sub_keys[l],
                         peer_u[l], peer_v[l])
    return rmsnorm(h, final_g)
```

```python
import numpy as np
import concourse.bass as bass
import concourse.mybir as mybir
from concourse.bass_utils import run_bass_kernel_spmd

F32 = mybir.dt.float32
BF16 = mybir.dt.bfloat16
AF = mybir.ActivationFunctionType
ALU = mybir.AluOpType
AX = mybir.AxisListType

NCORES = 8
D = 2048
SEQ = 16384
TOK = SEQ // NCORES
NEG = -1.0e30


class Tok:
    __slots__ = ("w", "r")

    def __init__(self):
        self.w = None
        self.r = []


class Sync:
    def __init__(self, nc):
        self.nc = nc
        self.engs = {"pe": nc.tensor, "act": nc.scalar, "dve": nc.vector, "pool": nc.gpsimd, "sp": nc.sync}
        self.sem = {k: nc.alloc_semaphore("s_" + k) for k in ("pe", "act", "dve", "pool")}
        self.cnt = {k: 0 for k in self.sem}
        self.waited = {e: {} for e in self.engs}
        self.dsem = {}
        self.ninst = 0

    def _deps(self, reads, writes):
        deps = {}
        for b in reads:
            if b.w is not None:
                k, v = b.w
                deps[k] = max(deps.get(k, 0), v)
        for b in writes:
            if b.w is not None:
                k, v = b.w
                deps[k] = max(deps.get(k, 0), v)
            for (k, v) in b.r:
                deps[k] = max(deps.get(k, 0), v)
        return deps

    def _wait(self, ek, deps):
        eng = self.engs[ek]
        wd = self.waited[ek]
        for k, v in deps.items():
            if k == ek and ek == "pe":
                continue
            if k.startswith("dma:"):
                v = self.dsem[k][1]
                s = self.dsem[k][0]
            else:
                s = self.sem[k]
            if wd.get(k, 0) >= v:
                continue
            eng.wait_ge(s, v)
            wd[k] = v
            self.ninst += 1

    def _mark(self, ev, reads, writes):
        for b in reads:
            b.r.append(ev)
        for b in writes:
            b.w = ev
            b.r = []

    def op(self, ek, fn, reads=(), writes=()):
        self._wait(ek, self._deps(reads, writes))
        inst = fn(self.engs[ek])
        self.cnt[ek] += 1
        inst.then_inc(self.sem[ek], 1)
        self.ninst += 1
        self._mark((ek, self.cnt[ek]), reads, writes)

    def dma(self, qk, stream, fn, reads=(), writes=()):
        self._wait(qk, self._deps(reads, writes))
        inst = fn(self.engs[qk])
        k = "dma:" + stream
        if k not in self.dsem:
            self.dsem[k] = [self.nc.alloc_semaphore("d_" + stream), 0]
        self.dsem[k][1] += 16
        inst.then_inc(self.dsem[k][0], 16)
        self.ninst += 1
        self._mark((k, self.dsem[k][1]), reads, writes)

    def finish(self, toks):
        deps = self._deps(toks, ())
        self._wait("sp", deps)


def _rms_rstd(S, ek_sq, src_ap, src_tok, junk, junk_tok, ss, ss_tok, rstd, rstd_tok, eps):
    S.op("act", lambda e: e.activation(out=junk, in_=src_ap, func=AF.Square, accum_out=ss),
         reads=[src_tok], writes=[junk_tok, ss_tok])
    S.op("act", lambda e: e.activation(out=ss, in_=ss, func=AF.Sqrt, scale=1.0 / D, bias=float(eps)),
         reads=[ss_tok], writes=[ss_tok])
    S.op("dve", lambda e: e.reciprocal(out=rstd, in_=ss), reads=[ss_tok], writes=[rstd_tok])


CH = 256
NT = CH // 128


def build_l2(nchunks=TOK // CH, peer_blocks=32, stage=99):
    nc = bass.Bass("TRN2", target_bir_lowering=False)
    ntok = nchunks * CH
    dr = lambda n, s, k="ExternalInput": nc.dram_tensor(n, s, F32, kind=k).ap()
    x = dr("x", [ntok, D])
    yaT = dr("yaT", [1024, ntok])
    ybT = dr("ybT", [1024, ntok])
    bfi = lambda n, nb: nc.dram_tensor(n, [nb, 128, 8192], BF16, kind="ExternalInput").ap()
    s_wg, s_wab, s_wo, s_wq, s_u, s_v = bfi("s_wg", 8), bfi("s_wab", 4), bfi("s_wo", 4), bfi("s_wq", 4), bfi("s_u", 32), bfi("s_v", 32)
    skT = dr("skT", [16, 128, 128])
    gv = dr("gv", [3, D])
    ident_d = dr("ident", [128, 128])
    y = dr("y", [ntok, D], "ExternalOutput")

    S = Sync(nc)
    sb = lambda n, s, dt=F32: nc.alloc_sbuf_tensor(n, s, dt)
    ps = [nc.alloc_psum_tensor("ps%d" % i, [128, 512], F32) for i in range(6)]
    psb = [nc.alloc_psum_tensor("psb%d" % i, [128, 1024], BF16) for i in range(2)]
    ps_t = [Tok() for _ in range(6)]
    psb_t = [Tok() for _ in range(2)]

    gvec = sb("gvec", [128, D]); gvec_t = Tok()
    xh = sb("xh", [128, NT, D]); xh_t = [Tok() for _ in range(NT)]
    R1 = sb("R1", [128, 16, CH], BF16); R1_t = Tok()
    R2 = sb("R2", [128, 16, CH], BF16); R2_t = Tok()
    R3 = sb("R3", [128, 16, CH], BF16); R3_t = Tok()
    WS = [sb("WS%d" % i, [128, 16, 512], BF16) for i in range(3)]; WS_t = [Tok() for _ in range(3)]
    VS = [sb("VS%d" % i, [128, 4, D], BF16) for i in range(2)]; VS_t = [Tok() for _ in range(2)]
    acc = sb("acc", [128, NT, D]); acc_t = [Tok() for _ in range(NT)]
    sc = sb("sc", [128, NT, 16, 128]); sc_t = [Tok() for _ in range(NT)]
    xnb = sb("xnb", [128, D], BF16); xnb_t = Tok()
    junk, junk_t = xnb, xnb_t
    ident = sb("identb", [128, 128], BF16); ident_t = Tok()
    identf = sb("identf", [128, 128]); identf_t = Tok()
    skb = sb("skb", [128, 16, 128], BF16); skb_t = Tok()
    ss = sb("ss", [128, 1]); ss_t = Tok()
    rstd = sb("rstd", [128, 1]); rstd_t = Tok()
    sg = [sb("sg%d" % i, [128, CH], BF16) for i in range(2)]; sg_t = [Tok() for _ in range(2)]
    mm = [sb("mm%d" % i, [128, CH], BF16) for i in range(2)]; mm_t = [Tok() for _ in range(2)]
    top = sb("top", [128, 1, 16, 16]); top_t = [Tok()] * NT
    tmpk = sb("tmpk", [128, 256]); tmpk_t = Tok()
    cand = sb("cand", [128, 256]); cand_t = Tok()
    best = sb("best", [128, NT, 8, 16]); best_t = [Tok() for _ in range(NT)]
    negmx = sb("negmx", [128, NT, 8]); negmx_t = [Tok() for _ in range(NT)]
    zz = sb("zz", [128, NT, 8]); zz_t = [Tok() for _ in range(NT)]
    nbias = sb("nbias", [128, NT, 8]); nbias_t = [Tok() for _ in range(NT)]
    ebuf = sb("ebuf", [128, 16]); ebuf_t = Tok()
    Ab = [sb("Ab%d" % i, [128, 512], BF16) for i in range(2)]; Ab_t = [Tok() for _ in range(2)]
    Wc = [sb("Wc%d" % i, [128, 512], BF16) for i in range(2)]; Wc_t = [Tok() for _ in range(2)]
    Tb = [sb("Tb%d" % i, [128, 512]) for i in range(3)]; Tb_t = [Tok() for _ in range(3)]
    Eb = [sb("Eb%d" % i, [128, 512]) for i in range(3)]; Eb_t = [Tok() for _ in range(3)]
    Wh = [sb("Wh%d" % i, [128, 512], BF16) for i in range(2)]; Wh_t = [Tok() for _ in range(2)]
    Wb = [sb("Wb%d" % i, [128, 512], BF16) for i in range(2)]; Wb_t = [Tok() for _ in range(2)]
    AW = [sb("AW%d" % i, [128, 512], BF16) for i in range(2)]; AW_t = [Tok() for _ in range(2)]
    AWT = [sb("AWT%d" % i, [128, 4, 128], BF16) for i in range(2)]; AWT_t = [Tok() for _ in range(2)]

    S.dma("sp", "c", lambda e: e.dma_start(out=identf[:], in_=ident_d[:, :]), writes=[identf_t])
    S.op("dve", lambda e: e.tensor_copy(out=ident[:], in_=identf[:]), reads=[identf_t], writes=[ident_t])
    S.dma("pool", "w", lambda e: e.dma_start(out=skb[:], in_=skT.rearrange("b d n -> d b n")), writes=[skb_t])

    def load_blk(dst_tile, dst_tok, name, b, scr):
        S.dma("sp", "ws", lambda e: e.dma_start(out=dst_tile[:].rearrange("p a b -> p (a b)"), in_=scr[b]),
              writes=[dst_tok])

    def norm_transpose(t, g_row, dstT, dstT_t, first):
        src = xh[:, t, :]
        _rms_rstd(S, "act", src, xh_t[t], junk[:], junk_t, ss[:], ss_t, rstd[:], rstd_t, 1e-6)
        S.op("dve", lambda e: e.scalar_tensor_tensor(out=xnb[:], in0=src, scalar=rstd[:, 0:1], in1=gvec[:],
                                                     op0=ALU.mult, op1=ALU.mult),
             reads=[xh_t[t], rstd_t, gvec_t], writes=[xnb_t])
        for half in range(2):
            pb = psb[half]
            for j in range(8):
                kc = half * 8 + j
                S.op("pe", lambda e, kc=kc, j=j, pb=pb: e.transpose(out=pb[:, j * 128:(j + 1) * 128],
                                                                    in_=xnb[:, kc * 128:(kc + 1) * 128], identity=ident[:]),
                     reads=[xnb_t, ident_t], writes=[psb_t[half]])
            S.op("act" if half == 0 else "dve",
                 (lambda e, half=half, pb=pb: e.activation(out=dstT[:, half * 8:(half + 1) * 8, t * 128:(t + 1) * 128],
                                                           in_=pb[:].rearrange("p (k t) -> p k t", k=8), func=AF.Copy))
                 if half == 0 else
                 (lambda e, half=half, pb=pb: e.tensor_copy(out=dstT[:, half * 8:(half + 1) * 8, t * 128:(t + 1) * 128],
                                                            in_=pb[:].rearrange("p (k t) -> p k t", k=8))),
                 reads=[psb_t[half]], writes=[dstT_t])

    for c in range(nchunks):
        t0 = c * CH
        for t in range(NT):
            S.dma("sp", "x", lambda e, t=t: e.dma_start(out=xh[:, t, :], in_=x[t0 + t * 128:t0 + (t + 1) * 128, :]),
                  writes=[xh_t[t]])
        S.dma("sp", "g", lambda e: e.dma_start(out=gvec[:], in_=gv[0:1, :].partition_broadcast(128)), writes=[gvec_t])
        S.dma("pool", "w", lambda e: e.dma_start(out=R2[:, 0:8, :], in_=yaT[:, t0:t0 + CH].rearrange("(k p) t -> p k t", p=128)),
              writes=[R2_t])
        S.dma("pool", "w", lambda e: e.dma_start(out=R2[:, 8:16, :], in_=ybT[:, t0:t0 + CH].rearrange("(k p) t -> p k t", p=128)),
              writes=[R2_t])
        for t in range(NT):
            norm_transpose(t, 0, R1, R1_t, True)
        for cg in range(4):
            cs = slice(cg * 512, (cg + 1) * 512)
            load_blk(WS[0], WS_t[0], "wg", cg, s_wg)
            load_blk(WS[1], WS_t[1], "wg", 4 + cg, s_wg)
            load_blk(WS[2], WS_t[2], "wab", cg, s_wab)
            for cb in range(4):
                cc = slice(cb * 128, (cb + 1) * 128)
                cidx = cg * 4 + cb
                for kc in range(16):
                    S.op("pe", lambda e, kc=kc, cc=cc: e.matmul(out=ps[0][:, 0:CH], lhsT=WS[0][:, kc, cc], rhs=R1[:, kc, :],
                                                                start=(kc == 0), stop=(kc == 15)),
                         reads=[WS_t[0], R1_t], writes=[ps_t[0]])
                for kc in range(16):
                    S.op("pe", lambda e, kc=kc, cc=cc: e.matmul(out=ps[1][:, 0:CH], lhsT=WS[1][:, kc, cc], rhs=R1[:, kc, :],
                                                                start=(kc == 0), stop=(kc == 15)),
                         reads=[WS_t[1], R1_t], writes=[ps_t[1]])
                for kc in range(8):
                    S.op("pe", lambda e, kc=kc, cc=cc: e.matmul(out=ps[2][:, 0:CH], lhsT=WS[2][:, kc, cc], rhs=R2[:, kc, :],
                                                                start=(kc == 0), stop=(kc == 7)),
                         reads=[WS_t[2], R2_t], writes=[ps_t[2]])
                for kc in range(8):
                    S.op("pe", lambda e, kc=kc, cc=cc: e.matmul(out=ps[3][:, 0:CH], lhsT=WS[2][:, 8 + kc, cc], rhs=R2[:, 8 + kc, :],
                                                                start=(kc == 0), stop=(kc == 7)),
                         reads=[WS_t[2], R2_t], writes=[ps_t[3]])
                for i in range(2):
                    S.op("act", lambda e, i=i: e.activation(out=sg[i][:], in_=ps[i][:, 0:CH], func=AF.Sigmoid),
                         reads=[ps_t[i]], writes=[sg_t[i]])
                for i in range(2):
                    S.op("dve", lambda e, i=i: e.tensor_tensor(out=mm[i][:], in0=sg[i][:], in1=ps[2 + i][:, 0:CH], op=ALU.mult),
                         reads=[sg_t[i], ps_t[2 + i]], writes=[mm_t[i]])
                S.op("pool", lambda e, cidx=cidx: e.tensor_tensor(out=R3[:, cidx, :], in0=mm[0][:], in1=mm[1][:], op=ALU.add),
                     reads=[mm_t[0], mm_t[1]], writes=[R3_t])
        for db in range(4):
            slot = db % 2
            load_blk(WS[slot], WS_t[slot], "wo", db, s_wo)
            for t in range(NT):
                pbk = 4 + (t % 2)
                for kc in range(16):
                    S.op("pe", lambda e, kc=kc, t=t, pbk=pbk, slot=slot: e.matmul(out=ps[pbk][:, :], lhsT=R3[:, kc, t * 128:(t + 1) * 128],
                                                                                 rhs=WS[slot][:, kc, :], start=(kc == 0), stop=(kc == 15)),
                         reads=[R3_t, WS_t[slot]], writes=[ps_t[pbk]])
                S.op("dve", lambda e, t=t, db=db, pbk=pbk: e.tensor_tensor(out=xh[:, t, db * 512:(db + 1) * 512],
                                                                         in0=xh[:, t, db * 512:(db + 1) * 512], in1=ps[pbk][:, :], op=ALU.add),
                     reads=[ps_t[pbk], xh_t[t]], writes=[xh_t[t]])
        if stage >= 2:
            S.dma("sp", "g", lambda e: e.dma_start(out=gvec[:], in_=gv[1:2, :].partition_broadcast(128)), writes=[gvec_t])
            for t in range(NT):
                norm_transpose(t, 1, R1, R1_t, False)
            for qg in range(4):
                slot = qg % 2
                load_blk(WS[slot], WS_t[slot], "wq", qg, s_wq)
                for qb in range(4):
                    blk = qg * 4 + qb
                    pbk = blk % 2
                    for kc in range(16):
                        S.op("pe", lambda e, kc=kc, qb=qb, pbk=pbk, slot=slot: e.matmul(out=ps[pbk][:, 0:CH], lhsT=WS[slot][:, kc, qb * 128:(qb + 1) * 128],
                                                                                       rhs=R1[:, kc, :], start=(kc == 0), stop=(kc == 15)),
                             reads=[WS_t[slot], R1_t], writes=[ps_t[pbk]])
                    S.op("act", lambda e, blk=blk, pbk=pbk: e.activation(out=R2[:, blk, :], in_=ps[pbk][:, 0:CH], func=AF.Copy),
                         reads=[ps_t[pbk]], writes=[R2_t])
            for t in range(NT):
                for g4 in range(4):
                    pbk = 2 + (g4 % 2)
                    for j in range(4):
                        blk = g4 * 4 + j
                        S.op("pe", lambda e, blk=blk, j=j, pbk=pbk, t=t: e.matmul(out=ps[pbk][:, j * 128:(j + 1) * 128],
                                                                                 lhsT=R2[:, blk, t * 128:(t + 1) * 128], rhs=skb[:, blk, :],
                                                                                 start=True, stop=True),
                             reads=[R2_t, skb_t], writes=[ps_t[pbk]])
                    S.op("act", lambda e, g4=g4, pbk=pbk, t=t: e.activation(out=sc[:, t, g4 * 4:(g4 + 1) * 4, :],
                                                                           in_=ps[pbk][:].rearrange("p (b n) -> p b n", b=4), func=AF.Copy),
                         reads=[ps_t[pbk]], writes=[sc_t[t]])
                for blk in range(16):
                    S.op("dve", lambda e, blk=blk, t=t: e.max(out=top[:, 0, blk, 0:8], in_=sc[:, t, blk, :]),
                         reads=[sc_t[t]], writes=[top_t[t]])
                    S.op("dve", lambda e, blk=blk, t=t: e.match_replace(out=tmpk[:, 0:128], in_to_replace=top[:, 0, blk, 0:8],
                                                                        in_values=sc[:, t, blk, :], imm_value=NEG),
                         reads=[sc_t[t], top_t[t]], writes=[tmpk_t])
                    S.op("dve", lambda e, blk=blk, t=t: e.max(out=top[:, 0, blk, 8:16], in_=tmpk[:, 0:128]),
                         reads=[tmpk_t], writes=[top_t[t]])
                for h in range(8):
                    S.op("dve", lambda e, h=h, t=t: e.tensor_tensor(
                        out=cand[:].rearrange("p (a b) -> p a b", a=16),
                        in0=top[:, 0, 2 * h, :].unsqueeze(2).broadcast_to([128, 16, 16]),
                        in1=top[:, 0, 2 * h + 1, :].unsqueeze(1).broadcast_to([128, 16, 16]), op=ALU.add),
                         reads=[top_t[t]], writes=[cand_t])
                    S.op("dve", lambda e, h=h, t=t: e.max(out=best[:, t, h, 0:8], in_=cand[:]),
                         reads=[cand_t], writes=[best_t[t]])
                    S.op("dve", lambda e, h=h, t=t: e.match_replace(out=tmpk[:], in_to_replace=best[:, t, h, 0:8],
                                                                    in_values=cand[:], imm_value=NEG),
                         reads=[cand_t, best_t[t]], writes=[tmpk_t])
                    S.op("dve", lambda e, h=h, t=t: e.max(out=best[:, t, h, 8:16], in_=tmpk[:]),
                         reads=[tmpk_t], writes=[best_t[t]])
                S.op("dve", lambda e, t=t: e.tensor_scalar(out=negmx[:, t, :], in0=best[:, t, :, 0], scalar1=-1.0, scalar2=None, op0=ALU.mult),
                     reads=[best_t[t]], writes=[negmx_t[t]])
                for h in range(8):
                    S.op("act", lambda e, h=h, t=t: e.activation(out=ebuf[:], in_=best[:, t, h, :], func=AF.Exp,
                                                                 bias=negmx[:, t, h:h + 1], accum_out=zz[:, t, h:h + 1]),
                         reads=[best_t[t], negmx_t[t]], writes=[ebuf_t, zz_t[t]])
                S.op("act", lambda e, t=t: e.activation(out=zz[:, t, :], in_=zz[:, t, :], func=AF.Ln),
                     reads=[zz_t[t]], writes=[zz_t[t]])
                S.op("dve", lambda e, t=t: e.tensor_tensor(out=nbias[:, t, :], in0=negmx[:, t, :], in1=zz[:, t, :], op=ALU.subtract),
                     reads=[negmx_t[t], zz_t[t]], writes=[nbias_t[t]])
            its = [(eb, t) for eb in range(peer_blocks) for t in range(NT)]
            nit = len(its)

            def st1(k):
                eb, t = its[k]
                slot = eb % 2
                if t == 0:
                    load_blk(WS[slot], WS_t[slot], "u", eb, s_u)
                    load_blk(VS[slot], VS_t[slot], "v", eb, s_v)
                pa = k % 2
                for kc in range(16):
                    S.op("pe", lambda e, kc=kc, t=t, slot=slot, pa=pa: e.matmul(out=ps[pa][:, :], lhsT=R1[:, kc, t * 128:(t + 1) * 128],
                                                                               rhs=WS[slot][:, kc, :], start=(kc == 0), stop=(kc == 15)),
                         reads=[R1_t, WS_t[slot]], writes=[ps_t[pa]])

            def st1g(k):
                pa = k % 2
                S.op("act", lambda e, pa=pa: e.activation(out=Ab[pa][:], in_=ps[pa][:, :], func=AF.Gelu),
                     reads=[ps_t[pa]], writes=[Ab_t[pa]])

            def st2(k):
                eb, t = its[k]
                pa = k % 2

                def emit_T(h):
                    i = h % 3
                    S.op("dve", lambda e, h=h, i=i: e.tensor_tensor(
                        out=Tb[i][:].rearrange("p (a b) -> p a b", a=4),
                        in0=sc[:, t, 2 * h, eb * 4:(eb + 1) * 4].unsqueeze(2).broadcast_to([128, 4, 128]),
                        in1=sc[:, t, 2 * h + 1, :].unsqueeze(1).broadcast_to([128, 4, 128]), op=ALU.add),
                         reads=[sc_t[t]], writes=[Tb_t[i]])
                    S.op("act", lambda e, h=h, i=i: e.activation(out=Eb[i][:], in_=Tb[i][:], func=AF.Exp, bias=nbias[:, t, h:h + 1]),
                         reads=[Tb_t[i], nbias_t[t]], writes=[Eb_t[i]])
                emit_T(0)
                emit_T(1)
                for h in range(8):
                    i = h % 3
                    if h + 2 < 8:
                        emit_T(h + 2)
                    accb, accb_t = (Wb[pa], Wb_t[pa]) if h % 2 == 0 else (Wc[pa], Wc_t[pa])
                    dst, dst_t = (accb, accb_t) if h < 2 else (Wh[h % 2], Wh_t[h % 2])
                    S.op("dve", lambda e, h=h, i=i, dst=dst: e.scalar_tensor_tensor(
                        out=dst[:], in0=Tb[i][:], scalar=best[:, t, h, 15:16], in1=Eb[i][:], op0=ALU.is_ge, op1=ALU.mult),
                         reads=[Tb_t[i], Eb_t[i], best_t[t]], writes=[dst_t])
                    if h >= 2:
                        S.op("pool" if h % 2 == 0 else "dve", lambda e, h=h, accb=accb: e.tensor_tensor(out=accb[:], in0=accb[:], in1=Wh[h % 2][:], op=ALU.add),
                             reads=[Wh_t[h % 2], accb_t], writes=[accb_t])
                S.op("dve", lambda e, pa=pa: e.tensor_tensor(out=Wb[pa][:], in0=Wb[pa][:], in1=Wc[pa][:], op=ALU.add),
                     reads=[Wb_t[pa], Wc_t[pa]], writes=[Wb_t[pa]])
                S.op("dve", lambda e, pa=pa: e.tensor_tensor(out=AW[pa][:], in0=Ab[pa][:], in1=Wb[pa][:], op=ALU.mult),
                     reads=[Ab_t[pa], Wb_t[pa]], writes=[AW_t[pa]])

            def st3a(k):
                pa = k % 2
                for es in range(4):
                    S.op("pe", lambda e, es=es, pa=pa: e.transpose(out=psb[pa][:, es * 128:(es + 1) * 128], in_=AW[pa][:, es * 128:(es + 1) * 128],
                                                                   identity=ident[:]),
                         reads=[AW_t[pa], ident_t], writes=[psb_t[pa]])
                S.op("act", lambda e, pa=pa: e.activation(out=AWT[pa][:], in_=psb[pa][:, 0:512].rearrange("p (s t) -> p s t", s=4), func=AF.Copy),
                     reads=[psb_t[pa]], writes=[AWT_t[pa]])

            def st3b(k):
                eb, t = its[k]
                slot = eb % 2
                pa = k % 2
                for db in range(4):
                    pbk = 2 + db
                    for es in range(4):
                        S.op("pe", lambda e, es=es, db=db, pbk=pbk, slot=slot, pa=pa: e.matmul(
                            out=ps[pbk][:, :], lhsT=AWT[pa][:, es, :], rhs=VS[slot][:, es, db * 512:(db + 1) * 512],
                            start=(es == 0), stop=(es == 3)),
                             reads=[AWT_t[pa], VS_t[slot]], writes=[ps_t[pbk]])

            def st3c(k):
                eb, t = its[k]
                for db in range(4):
                    pbk = 2 + db
                    if eb == 0:
                        S.op("dve", lambda e, db=db, t=t, pbk=pbk: e.tensor_copy(out=acc[:, t, db * 512:(db + 1) * 512], in_=ps[pbk][:, :]),
                             reads=[ps_t[pbk]], writes=[acc_t[t]])
                    else:
                        S.op("dve", lambda e, db=db, t=t, pbk=pbk: e.tensor_tensor(out=acc[:, t, db * 512:(db + 1) * 512],
                                                                                 in0=acc[:, t, db * 512:(db + 1) * 512], in1=ps[pbk][:, :], op=ALU.add),
                             reads=[ps_t[pbk], acc_t[t]], writes=[acc_t[t]])

            for k in range(nit + 2):
                if 0 <= k - 2 < nit:
                    st3a(k - 2)
                if k < nit:
                    st1(k)
                if 0 <= k - 2 < nit:
                    st3b(k - 2)
                if 0 <= k - 1 < nit:
                    st2(k - 1)
                if k < nit:
                    st1g(k)
                if 0 <= k - 2 < nit:
                    st3c(k - 2)
            for t in range(NT):
                S.op("pool", lambda e, t=t: e.tensor_tensor(out=xh[:, t, :], in0=xh[:, t, :], in1=acc[:, t, :], op=ALU.add),
                     reads=[acc_t[t], xh_t[t]], writes=[xh_t[t]])
        if stage >= 3:
            S.dma("sp", "g", lambda e: e.dma_start(out=gvec[:], in_=gv[2:3, :].partition_broadcast(128)), writes=[gvec_t])
        for t in range(NT):
            if stage >= 3:
                _rms_rstd(S, "act", xh[:, t, :], xh_t[t], junk[:], junk_t, ss[:], ss_t, rstd[:], rstd_t, 1e-6)
                S.op("dve", lambda e, t=t: e.scalar_tensor_tensor(out=acc[:, t, :], in0=xh[:, t, :], scalar=rstd[:, 0:1], in1=gvec[:],
                                                                 op0=ALU.mult, op1=ALU.mult),
                     reads=[xh_t[t], rstd_t, gvec_t], writes=[acc_t[t]])
            else:
                S.op("dve", lambda e, t=t: e.tensor_copy(out=acc[:, t, :], in_=xh[:, t, :]), reads=[xh_t[t]], writes=[acc_t[t]])
            S.dma("sp", "o", lambda e, t=t: e.dma_start(out=y[t0 + t * 128:t0 + (t + 1) * 128, :], in_=acc[:, t, :]),
                  reads=[acc_t[t]], writes=[])
    k = "dma:o"
    nc.sync.wait_ge(S.dsem[k][0], S.dsem[k][1])
    return nc, S


G1 = 256
NC1 = 1216
C_R, C_K, C_V, C_XW, C_XA, C_XG, C_QD, C_KD, C_VD = 0, 128, 256, 384, 480, 576, 832, 960, 1088
CHK = 32
NCAST = 11


def _roundrobin(gens):
    gens = list(gens)
    while gens:
        for g_ in list(gens):
            try:
                next(g_)
            except StopIteration:
                gens.remove(g_)


def build_l1(ngroups=SEQ // G1, do_rwkv=True, do_attn=True, rw_stage=9999):
    nc = bass.Bass("TRN2", target_bir_lowering=False)
    ntok = ngroups * G1
    ntile = ntok // 128
    dr = lambda n, s, k="ExternalInput": nc.dram_tensor(n, s, F32, kind=k).ap()
    x = dr("x", [ntok, D])
    w1 = dr("w1", [D, NC1])
    cvec = dr("cvec", [128, 20])
    wdu_d = dr("wdu", [96, 128]); wiu_d = dr("wiu", [96, 128]); wgu_d = dr("wgu", [256, 128])
    lamv = dr("lamv", [1, 256]); sublng = dr("sublng", [1, 128]); g1d = dr("g1", [1, D])
    consts = dr("consts", [7, 128, 128])
    rmask_d = dr("rmask", [1, G1])
    yaT = dr("yaT", [128, ntok], "ExternalOutput")
    yb = dr("yb", [ntok, 128], "ExternalOutput")
    castin = dr("castin", [NCAST, 128, 8192])
    castout = nc.dram_tensor("castout", [NCAST, 128, 8192], BF16, kind="ExternalOutput").ap()

    S = Sync(nc)
    for b in range(NCAST):
        S.dma("pool", "o", lambda e, b=b: e.dma_start(out=castout[b].rearrange("p (s e) -> p s e", e=2048),
                                                      in_=castin[b].rearrange("p (s e) -> p s e", e=2048)))
    sb = lambda n, s, dt=F32: nc.alloc_sbuf_tensor(n, s, dt)
    ps = [nc.alloc_psum_tensor("ps%d" % i, [128, 512], F32) for i in range(7)]
    psb = [nc.alloc_psum_tensor("psb%d" % i, [128, 1024], BF16) for i in range(1)]
    ps_t = [Tok() for _ in range(7)]
    psb_t = [Tok() for _ in range(1)]
    OB = [4, 6]
    scr_i = [0]

    def scr():
        scr_i[0] ^= 1
        return ps[scr_i[0]], ps_t[scr_i[0]]

    def T_(name, shape, dt=F32):
        return sb(name, shape, dt), Tok()

    W1, W1_t = T_("W1", [128, 16, NC1], BF16)
    KT, KT_t = T_("KT", [128, ntok], BF16)
    VA, VA_t = T_("VA", [128, ntile, 130], BF16)
    g1c, g1c_t = T_("g1c", [128, 16])
    xt = [T_("xt%d" % i, [128, D]) for i in range(2)]
    xnb, xnb_t = T_("xnb", [128, D], BF16)
    junk, junk_t = xnb, xnb_t
    junk2, junk2_t = T_("junk2", [128, 128], BF16)
    xnT, xnT_t = T_("xnT", [128, 16, G1], BF16)
    cst, cst_t = T_("cst", [128, 7, 128])
    identb, identb_t = T_("identb", [128, 128], BF16)
    trib, trib_t = T_("trib", [128, 128], BF16)
    rmask, rmask_t = T_("rmask_s", [128, G1])
    cv, cv_t = T_("cv_s", [128, 20])
    omm, omm_t = T_("omm", [128, 8])
    wdu, wdu_t = T_("wdus", [96, 128]); wiu, wiu_t = T_("wius", [96, 128]); wgu, wgu_t = T_("wgus", [128, 2, 128])
    lacc, lacc_t = T_("lacc", [128, 4])
    neglam, neglam_t = T_("neglam", [128, 1])
    sgv, sgv_t = T_("sgv", [128, 128])
    ss, ss_t = T_("ss", [128, 1]); rstd, rstd_t = T_("rstd", [128, 1])
    ss2, ss2_t = T_("ss2", [128, 1]); rstd2, rstd2_t = T_("rstd2", [128, 1])
    ident = cst[:, 0, :]; MU = cst[:, 1, :]; ML = cst[:, 2, :]; MUI = cst[:, 3, :]; onesblk = cst[:, 5, :]; I64 = cst[:, 6, 0:64]
    PB = [T_("PB%d" % i, [128, G1 + 1]) for i in range(7)]
    SH = [T_("SH%d" % i, [128, G1]) for i in range(7)]
    tmpA, tmpA_t = T_("tmpA", [128, G1]); tmpB, tmpB_t = T_("tmpB", [128, G1])
    logw, logw_t = T_("logw", [128, G1]); av, av_t = T_("av", [128, G1]); gg, gg_t = T_("gg", [128, G1])
    kkn, kkn_t = T_("kkn", [128, G1]); k2, k2_t = T_("k2", [128, G1]); bonus, bonus_t = T_("bonus", [128, G1])
    cum, cum_t = T_("cum", [128, G1]); Pm, Pm_t = T_("Pm", [128, G1]); Pinv, Pinv_t = T_("Pinv", [128, G1]); Pprev, Pprev_t = T_("Pprev", [128, G1])
    At, At_t = T_("At", [128, G1]); Bt, Bt_t = T_("Bt", [128, G1]); Kt, Kt_t = T_("Kt", [128, G1]); Rt, Rt_t = T_("Rt", [128, G1])
    yT, yT_t = T_("yT", [128, G1]); yo, yo_t = T_("yo", [128, G1])
    TOK, TOK_t = T_("TOK", [128, 4, 128])
    lam_s = TOK[:, 0:2, :].rearrange("p a (b c) -> p (a b) c", c=64); lam_t = TOK_t
    lamp = TOK[:, 2, :].rearrange("p (b c) -> p b c", c=64); lamp_t = TOK_t
    HB = []
    for h in range(2):
        HB.append(dict(
            XT=[T_("XT%d_%d" % (h, i), [128, 128]) for i in range(5)], XX=[T_("XX%d_%d" % (h, i), [128, 128]) for i in range(4)],
            LakT=T_("LakT%d" % h, [128, 128]), MrbT=T_("MrbT%d" % h, [128, 128]), MrkT=T_("MrkT%d" % h, [128, 128]),
            Z=[T_("Z%d_%d" % (h, i), [128, 128]) for i in range(2)], ZF=T_("ZF%d" % h, [128, 128]),
            MZ=T_("MZ%d" % h, [128, 4, 64]), MB=T_("MB%d" % h, [128, 4, 64]), MK=T_("MK%d" % h, [128, 4, 64])))
    RhT, RhT_t = T_("RhT", [128, 128]); YhT, YhT_t = T_("YhT", [128, 128])
    MT, MT_t = T_("MT", [128, 4, 64]); HP, HP_t = T_("HP", [128, 4, 64])
    Sst = [T_("Sst%d" % i, [128, 64]) for i in range(2)]
    QT, QT_t = T_("QT", [128, G1], BF16); QSQ, QSQ_t = T_("QSQ", [128, G1]); KSQ, KSQ_t = T_("KSQ", [128, G1])
    kmax2 = [T_("kmax2_%d" % i, [128, 1]) for i in range(2)]
    kred, kred_t = T_("kred", [128, 1]); sqq, sqq_t = T_("sqq", [128, 1])
    nshift = [T_("nshift%d" % i, [128, 1]) for i in range(2)]
    Pb = [T_("Pb%d" % i, [128, 512], BF16) for i in range(2)]
    om = [[T_("om%d_%d" % (i, j), [128, 128]) for j in range(2)] for i in range(2)]
    qmax2 = [T_("qmax2_%d" % i, [128, 1]) for i in range(2)]
    rs_, rs_t = T_("rs_", [128, 1]); attn, attn_t = T_("attn", [128, 128]); ybo, ybo_t = T_("ybo", [128, 128])
    ones128, ones_t = T_("ones128", [128, 128])

    def mm(out, lhsT, rhs, start=True, stop=True):
        return lambda e: e.matmul(out=out, lhsT=lhsT, rhs=rhs, start=start, stop=stop)

    cpy_i = [0]

    def copy_out(out, in_, reads, writes, eng=None):
        if eng is None:
            cpy_i[0] ^= 1
            eng = "act" if cpy_i[0] else "dve"
        if eng == "act":
            S.op("act", lambda e: e.activation(out=out, in_=in_, func=AF.Copy), reads=reads, writes=writes)
        else:
            S.op("dve", lambda e: e.tensor_copy(out=out, in_=in_), reads=reads, writes=writes)

    def dve_tt(out, in0, in1, op, reads, writes, eng="dve"):
        S.op(eng, lambda e: e.tensor_tensor(out=out, in0=in0, in1=in1, op=op), reads=reads, writes=writes)

    def dve_ts(out, in0, s1, s2, op0, op1, reads, writes, eng="dve"):
        if op1 is None:
            S.op(eng, lambda e: e.tensor_scalar(out=out, in0=in0, scalar1=s1, scalar2=None, op0=op0), reads=reads, writes=writes)
        else:
            S.op(eng, lambda e: e.tensor_scalar(out=out, in0=in0, scalar1=s1, scalar2=s2, op0=op0, op1=op1), reads=reads, writes=writes)

    def dve_stt(out, in0, scalar, in1, op0, op1, reads, writes):
        S.op("dve", lambda e: e.scalar_tensor_tensor(out=out, in0=in0, scalar=scalar, in1=in1, op0=op0, op1=op1), reads=reads, writes=writes)

    def act(out, in_, func, reads, writes, **kw):
        S.op("act", lambda e: e.activation(out=out, in_=in_, func=func, **kw), reads=reads, writes=writes)

    S.dma("sp", "c", lambda e: e.dma_start(out=cst[:], in_=consts.rearrange("c p n -> p c n")), writes=[cst_t])
    S.dma("sp", "c", lambda e: e.dma_start(out=cv[:], in_=cvec[:, :]), writes=[cv_t])
    S.dma("sp", "c", lambda e: e.dma_start(out=wdu[:], in_=wdu_d[:, :]), writes=[wdu_t])
    S.dma("sp", "c", lambda e: e.dma_start(out=wiu[:], in_=wiu_d[:, :]), writes=[wiu_t])
    S.dma("sp", "c", lambda e: e.dma_start(out=wgu[:], in_=wgu_d.rearrange("(k p) c -> p k c", p=128)), writes=[wgu_t])
    S.dma("sp", "c", lambda e: e.dma_start(out=TOK[:, 0:2, :].rearrange("p a b -> p (a b)"), in_=lamv[0:1, :].partition_broadcast(128)), writes=[lam_t])
    S.dma("sp", "c", lambda e: e.dma_start(out=sgv[:], in_=sublng[0:1, :].partition_broadcast(128)), writes=[sgv_t])
    S.dma("sp", "c", lambda e: e.dma_start(out=rmask[:], in_=rmask_d[0:1, :].partition_broadcast(128)), writes=[rmask_t])
    with nc.allow_non_contiguous_dma(reason="tiny gain vector"):
        S.dma("sp", "c", lambda e: e.dma_start(out=g1c[:], in_=g1d.rearrange("o (k p) -> p (o k)", p=128)), writes=[g1c_t])
    S.dma("pool", "w", lambda e: e.dma_start(out=W1[:], in_=w1.rearrange("(k p) c -> p k c", p=128)), writes=[W1_t])
    for kc in range(16):
        S.op("dve" if kc % 2 else "pool", lambda e, kc=kc: e.tensor_scalar(out=W1[:, kc, :], in0=W1[:, kc, :], scalar1=g1c[:, kc:kc + 1], scalar2=0.0,
                                                                            op0=ALU.mult, op1=ALU.add),
             reads=[W1_t, g1c_t], writes=[W1_t])
    S.op("dve", lambda e: e.tensor_copy(out=identb[:], in_=cst[:, 0, :]), reads=[cst_t], writes=[identb_t])
    S.op("dve", lambda e: e.tensor_copy(out=trib[:], in_=cst[:, 4, :]), reads=[cst_t], writes=[trib_t])
    dve_ts(omm[:, 0:8], cv[:, 0:8], -1.0, 1.0, ALU.mult, ALU.add, [cv_t], [omm_t])
    dve_ts(cv[:, 14:15], cv[:, 10:11], -1.0, 1.0, ALU.mult, ALU.add, [cv_t], [cv_t])
    dve_ts(sgv[:], sgv[:], 0.8, None, ALU.mult, None, [sgv_t], [sgv_t])
    dve_tt(lamp[:, 0, :], lam_s[:, 0, :], lam_s[:, 1, :], ALU.mult, [lam_t], [lamp_t])
    dve_tt(lamp[:, 1, :], lam_s[:, 2, :], lam_s[:, 3, :], ALU.mult, [lam_t], [lamp_t])
    S.op("dve", lambda e: e.tensor_reduce(out=lacc[:, 0:2], in_=lamp[:, 0:2, :], axis=AX.X, op=ALU.add), reads=[lamp_t], writes=[lacc_t])
    act(lacc[:, 2:4], lacc[:, 0:2], AF.Exp, [lacc_t], [lacc_t])
    dve_tt(neglam[:], lacc[:, 3:4], lacc[:, 2:3], ALU.subtract, [lacc_t], [neglam_t])
    dve_ts(neglam[:], neglam[:], -0.2, None, ALU.add, None, [neglam_t], [neglam_t])
    for i in range(7):
        S.op("pool", lambda e, i=i: e.memset(PB[i][0][:, 0:1], 0.0), writes=[PB[i][1]])
    for i in range(2):
        S.op("pool", lambda e, i=i: e.memset(Sst[i][0][:], 0.0), writes=[Sst[i][1]])
        S.op("pool", lambda e, i=i: e.memset(kmax2[i][0][:], 0.0), writes=[kmax2[i][1]])
        S.op("pool", lambda e, i=i: e.memset(qmax2[i][0][:], 0.0), writes=[qmax2[i][1]])
    S.op("pool", lambda e: e.memset(VA[:, :, 128:130], 1.0), writes=[VA_t])
    S.op("pool", lambda e: e.memset(ones128[:], 1.0), writes=[ones_t])

    scur = [0]

    def head_chain(h, cs):
        hb = HB[h]
        XT, XX, Z = hb["XT"], hb["XX"], hb["Z"]
        LakT, LakT_t = hb["LakT"]; MrbT, MrbT_t = hb["MrbT"]; MrkT, MrkT_t = hb["MrkT"]
        zf, zf_t = hb["ZF"]
        MZ, MZ_t = hb["MZ"]; MB, MB_t = hb["MB"]; MK, MK_t = hb["MK"]
        pb, pb_t = ps[h], ps_t[h]
        hs = slice(64 * h, 64 * h + 64)
        Ah, Bh, Kh, Rh = At[hs, cs], Bt[hs, cs], Kt[hs, cs], Rt[hs, cs]
        S.op("pe", mm(pb[:, 0:128], Bh, Ah), reads=[Bt_t, At_t], writes=[pb_t])
        dve_tt(XT[0][0][:], pb[:, 0:128], MU, ALU.mult, [pb_t, cst_t], [XT[0][1]])
        yield
        S.op("pe", mm(pb[:, 0:128], Ah, Bh), reads=[Bt_t, At_t], writes=[pb_t])
        dve_tt(XX[0][0][:], pb[:, 0:128], ML, ALU.mult, [pb_t, cst_t], [XX[0][1]])
        yield
        S.op("pe", mm(pb[:, 0:128], Kh, Ah), reads=[Kt_t, At_t], writes=[pb_t])
        dve_tt(LakT[:], pb[:, 0:128], MU, ALU.mult, [pb_t, cst_t], [LakT_t])
        yield
        S.op("pe", mm(pb[:, 0:128], Bh, Rh), reads=[Bt_t, Rt_t], writes=[pb_t])
        dve_tt(MrbT[:], pb[:, 0:128], MUI, ALU.mult, [pb_t, cst_t], [MrbT_t])
        yield
        S.op("pe", mm(pb[:, 0:128], Kh, Rh), reads=[Kt_t, Rt_t], writes=[pb_t])
        dve_tt(MrkT[:], pb[:, 0:128], MUI, ALU.mult, [pb_t, cst_t], [MrkT_t])
        yield
        S.op("pe", mm(pb[:, 0:64], LakT[:], TOK[:, 3, hs]), reads=[LakT_t, TOK_t], writes=[pb_t])
        zc, zc_t = Z[0]
        copy_out(zc[:, 64:128], pb[:, 0:64], [pb_t], [zc_t], eng="dve")
        S.op("pool", lambda e: e.tensor_copy(out=zc[:, 0:64], in_=TOK[:, 0, hs]), reads=[TOK_t], writes=[zc_t])
        yield
        for i in range(4):
            S.op("pe", mm(pb[:, 0:128], XX[i][0][:], XT[i][0][:]), reads=[XX[i][1], XT[i][1]], writes=[pb_t])
            copy_out(XT[i + 1][0][:], pb[:, 0:128], [pb_t], [XT[i + 1][1]], eng="dve")
            yield
            if i < 3:
                S.op("pe", mm(pb[:, 0:128], XT[i][0][:], XX[i][0][:]), reads=[XX[i][1], XT[i][1]], writes=[pb_t])
                copy_out(XX[i + 1][0][:], pb[:, 0:128], [pb_t], [XX[i + 1][1]], eng="act")
                yield
            zc, zc_t = Z[i % 2]
            zn, zn_t = Z[(i + 1) % 2]
            S.op("pe", mm(pb[:, 0:128], XT[i][0][:], zc[:]), reads=[XT[i][1], zc_t], writes=[pb_t])
            dve_tt(zn[:], pb[:, 0:128], zc[:], ALU.add, [pb_t, zc_t], [zn_t])
            yield
        zc, zc_t = Z[0]
        S.op("pe", mm(pb[:, 0:128], XT[4][0][:], zc[:]), reads=[XT[4][1], zc_t], writes=[pb_t])
        dve_tt(zf[:], pb[:, 0:128], zc[:], ALU.add, [pb_t, zc_t], [zf_t])
        yield
        S.op("pe", mm(pb[hs, 0:128], zf[:, 0:64], MrbT[:]), reads=[zf_t, MrbT_t], writes=[pb_t])
        dve_tt(RhT[hs, :], pb[hs, 0:128], Rh, ALU.add, [pb_t, Rt_t], [RhT_t])
        yield
        S.op("pe", mm(pb[hs, 0:128], zf[:, 64:128], MrbT[:], True, False), reads=[zf_t, MrbT_t], writes=[pb_t])
        S.op("pe", mm(pb[hs, 0:128], TOK[:, 3, hs], MrkT[:], False, True), reads=[TOK_t, MrkT_t], writes=[pb_t])
        copy_out(YhT[hs, :], pb[hs, 0:128], [pb_t], [YhT_t], eng="act")
        cmb = cv[:, 16:20].unsqueeze(2).broadcast_to([128, 4, 64])
        dve_tt(MZ[:], zf[:, 0:64].unsqueeze(1).broadcast_to([128, 4, 64]), cmb, ALU.mult, [zf_t, cv_t], [MZ_t])
        dve_tt(MB[:], TOK[:, 1, hs].unsqueeze(1).broadcast_to([128, 4, 64]), cmb, ALU.mult, [TOK_t, cv_t], [MB_t], eng="pool")
        dve_tt(MK[:], TOK[:, 2, hs].unsqueeze(1).broadcast_to([128, 4, 64]), cmb, ALU.mult, [TOK_t, cv_t], [MK_t], eng="pool")
        yield
        for c in range(4):
            S.op("pe", mm(ps[2][hs, c * 64:(c + 1) * 64], MZ[:, c, :], TOK[:, 1, hs]), reads=[MZ_t, TOK_t], writes=[ps_t[2]])
        for c in range(4):
            S.op("pe", mm(ps[3][hs, c * 64:(c + 1) * 64], MB[:, c, :], zf[:, 64:128], True, False), reads=[zf_t, MB_t], writes=[ps_t[3]])
            S.op("pe", mm(ps[3][hs, c * 64:(c + 1) * 64], MK[:, c, :], TOK[:, 3, hs], False, True), reads=[TOK_t, MK_t], writes=[ps_t[3]])
        yield

    def rwkv_group(g):
        t0 = g * G1
        r_, k_, v_, xw_, xa_, xg0_, xg1_ = range(7)
        rows = [128, 128, 128, 96, 96, 128, 128]
        for b in range(7):
            n = rows[b]
            pbuf, pbt = PB[b]
            sh, sht = SH[b]
            dve_ts(tmpA[0:n, :], pbuf[0:n, 0:G1], cv[0:n, b:b + 1], None, ALU.mult, None, [pbt, cv_t], [tmpA_t])
            dve_stt(sh[0:n, :], pbuf[0:n, 1:G1 + 1], omm[0:n, b:b + 1], tmpA[0:n, :], ALU.mult, ALU.add, [pbt, omm_t, tmpA_t], [sht])
            S.op("pool", lambda e, pbuf=pbuf, n=n: e.tensor_copy(out=pbuf[0:n, 0:1], in_=pbuf[0:n, G1:G1 + 1]), reads=[pbt], writes=[pbt])
            if b % 2:
                yield
        shr, shr_t = SH[r_]; shk, shk_t = SH[k_]; shv, shv_t = SH[v_]
        act(tmpB[0:96, :], SH[xw_][0][0:96, :], AF.Tanh, [SH[xw_][1]], [tmpB_t])
        pb, pb_t = scr()
        S.op("pe", mm(pb[:, 0:G1], wdu[:, :], tmpB[0:96, :]), reads=[wdu_t, tmpB_t], writes=[pb_t])
        act(logw[:], pb[:, 0:G1], AF.Sigmoid, [pb_t, cv_t], [logw_t], bias=cv[:, 7:8])
        dve_ts(logw[:], logw[:], -0.6065306597126334, None, ALU.mult, None, [logw_t], [logw_t])
        pb, pb_t = scr()
        S.op("pe", mm(pb[:, 0:G1], wiu[:, :], SH[xa_][0][0:96, :]), reads=[wiu_t, SH[xa_][1]], writes=[pb_t])
        act(av[:], pb[:, 0:G1], AF.Sigmoid, [pb_t, cv_t], [av_t], bias=cv[:, 8:9])
        act(SH[xg0_][0][:], SH[xg0_][0][:], AF.Sigmoid, [SH[xg0_][1]], [SH[xg0_][1]])
        act(SH[xg1_][0][:], SH[xg1_][0][:], AF.Sigmoid, [SH[xg1_][1]], [SH[xg1_][1]])
        yield
        pb, pb_t = scr()
        S.op("pe", mm(pb[:, 0:G1], wgu[:, 0, :], SH[xg0_][0][:], True, False), reads=[wgu_t, SH[xg0_][1]], writes=[pb_t])
        S.op("pe", mm(pb[:, 0:G1], wgu[:, 1, :], SH[xg1_][0][:], False, True), reads=[wgu_t, SH[xg1_][1]], writes=[pb_t])
        copy_out(gg[:], pb[:, 0:G1], [pb_t], [gg_t], eng="dve")
        dve_ts(kkn[:], shk[:], cv[:, 9:10], None, ALU.mult, None, [shk_t, cv_t], [kkn_t])
        dve_tt(tmpA[:], kkn[:], kkn[:], ALU.mult, [kkn_t], [tmpA_t], eng="pool")
        pb, pb_t = scr()
        S.op("pe", mm(pb[:, 0:G1], onesblk, tmpA[:]), reads=[cst_t, tmpA_t], writes=[pb_t])
        act(tmpB[:], pb[:, 0:G1], AF.Sqrt, [pb_t], [tmpB_t])
        dve_ts(tmpB[:], tmpB[:], 1e-12, None, ALU.max, None, [tmpB_t], [tmpB_t])
        S.op("dve", lambda e: e.reciprocal(out=tmpB[:], in_=tmpB[:]), reads=[tmpB_t], writes=[tmpB_t])
        dve_tt(kkn[:], kkn[:], tmpB[:], ALU.mult, [kkn_t, tmpB_t], [kkn_t])
        yield
        dve_ts(tmpA[:], av[:], cv[:, 10:11], cv[:, 14:15], ALU.mult, ALU.add, [av_t, cv_t], [tmpA_t])
        dve_tt(k2[:], shk[:], tmpA[:], ALU.mult, [shk_t, tmpA_t], [k2_t])
        dve_tt(tmpA[:], shr[:], k2[:], ALU.mult, [shr_t, k2_t], [tmpA_t], eng="pool")
        dve_ts(tmpA[:], tmpA[:], cv[:, 11:12], None, ALU.mult, None, [tmpA_t, cv_t], [tmpA_t])
        pb, pb_t = scr()
        S.op("pe", mm(pb[:, 0:G1], onesblk, tmpA[:]), reads=[cst_t, tmpA_t], writes=[pb_t])
        dve_tt(bonus[:], pb[:, 0:G1], shv[:], ALU.mult, [pb_t, shv_t], [bonus_t])
        S.op("dve", lambda e: e.tensor_tensor_scan(out=cum[:], data0=rmask[:], data1=logw[:], initial=0.0, op0=ALU.mult, op1=ALU.add),
             reads=[rmask_t, logw_t], writes=[cum_t])
        yield
        act(Pm[:], cum[:], AF.Exp, [cum_t], [Pm_t])
        act(Pinv[:], cum[:], AF.Exp, [cum_t], [Pinv_t], scale=-1.0)
        dve_tt(tmpA[:], cum[:], logw[:], ALU.subtract, [cum_t, logw_t], [tmpA_t], eng="pool")
        act(Pprev[:], tmpA[:], AF.Exp, [tmpA_t], [Pprev_t])
        dve_stt(At[:], kkn[:], -1.0, Pprev[:], ALU.mult, ALU.mult, [kkn_t, Pprev_t], [At_t])
        dve_tt(tmpB[:], kkn[:], av[:], ALU.mult, [kkn_t, av_t], [tmpB_t], eng="pool")
        dve_tt(Bt[:], tmpB[:], Pinv[:], ALU.mult, [tmpB_t, Pinv_t], [Bt_t])
        dve_tt(Kt[:], k2[:], Pinv[:], ALU.mult, [k2_t, Pinv_t], [Kt_t], eng="pool")
        dve_tt(Rt[:], shr[:], Pm[:], ALU.mult, [shr_t, Pm_t], [Rt_t])
        yield
        for tl in range(G1 // 128):
            cs = slice(tl * 128, (tl + 1) * 128)
            for j, (src, srct) in enumerate([(At, At_t), (Bt, Bt_t), (Kt, Kt_t), (shv, shv_t)]):
                S.op("pe", lambda e, j=j, src=src: e.transpose(out=ps[2][:, j * 128:(j + 1) * 128], in_=src[:, cs], identity=ident),
                     reads=[srct, cst_t], writes=[ps_t[2]])
            copy_out(TOK[:].rearrange("p a b -> p (a b)"), ps[2][:, :], [ps_t[2]], [TOK_t], eng="act")
            yield
            chains = [head_chain(0, cs), head_chain(1, cs)]
            while chains:
                for ch in list(chains):
                    try:
                        next(ch)
                    except StopIteration:
                        chains.remove(ch)
                yield
            dve_tt(MT[:], ps[2][:, 0:256].rearrange("p (c k) -> p c k", c=4), I64.unsqueeze(1).broadcast_to([128, 4, 64]), ALU.add,
                   [ps_t[2], cst_t], [MT_t])
            for c in range(4):
                col = tl * 128 + 32 * c + 31
                dve_ts(HP[:, c, :], ps[3][:, c * 64:(c + 1) * 64], Pm[:, col:col + 1], None, ALU.mult, None, [ps_t[3], Pm_t], [HP_t])
            yield
            for c in range(4):
                col = tl * 128 + 32 * c + 31
                sc_, sc_t = Sst[scur[0]]
                sn_, sn_t = Sst[1 - scur[0]]
                for h in range(2):
                    hs = slice(64 * h, 64 * h + 64)
                    S.op("pe", mm(ps[0][hs, 0:64], MT[hs, c, :], sc_[hs, :]), reads=[MT_t, sc_t], writes=[ps_t[0]])
                for h in range(2):
                    hs = slice(64 * h, 64 * h + 64)
                    S.op("pe", mm(ps[1][hs, 32 * c:32 * c + 32], sc_[hs, :], RhT[hs, 32 * c:32 * c + 32]), reads=[sc_t, RhT_t], writes=[ps_t[1]])
                dve_stt(sn_[:], ps[0][:, 0:64], Pm[:, col:col + 1], HP[:, c, :], ALU.mult, ALU.add, [ps_t[0], Pm_t, HP_t], [sn_t])
                scur[0] = 1 - scur[0]
                yield
            dve_tt(yT[:, cs], ps[1][:, 0:128], YhT[:], ALU.add, [ps_t[1], YhT_t], [yT_t])
            yield
        pb, pb_t = scr()
        S.op("pe", mm(pb[:, 0:G1], onesblk, yT[:]), reads=[cst_t, yT_t], writes=[pb_t])
        act(tmpA[:], pb[:, 0:G1], AF.Copy, [pb_t], [tmpA_t], scale=1.0 / 64)
        act(tmpB[:], yT[:], AF.Square, [yT_t], [tmpB_t])
        pb, pb_t = scr()
        S.op("pe", mm(pb[:, 0:G1], onesblk, tmpB[:]), reads=[cst_t, tmpB_t], writes=[pb_t])
        dve_tt(tmpB[:], tmpA[:], tmpA[:], ALU.mult, [tmpA_t], [tmpB_t], eng="pool")
        dve_stt(tmpB[:], pb[:, 0:G1], 1.0 / 64, tmpB[:], ALU.mult, ALU.subtract, [pb_t, tmpB_t], [tmpB_t])
        yield
        act(tmpB[:], tmpB[:], AF.Sqrt, [tmpB_t], [tmpB_t], bias=64e-5)
        S.op("dve", lambda e: e.reciprocal(out=tmpB[:], in_=tmpB[:]), reads=[tmpB_t], writes=[tmpB_t])
        dve_tt(yo[:], yT[:], tmpA[:], ALU.subtract, [yT_t, tmpA_t], [yo_t])
        dve_tt(yo[:], yo[:], tmpB[:], ALU.mult, [yo_t, tmpB_t], [yo_t])
        dve_ts(yo[:], yo[:], cv[:, 12:13], cv[:, 13:14], ALU.mult, ALU.add, [yo_t, cv_t], [yo_t])
        dve_tt(yo[:], yo[:], bonus[:], ALU.add, [yo_t, bonus_t], [yo_t], eng="pool")
        dve_tt(yo[:], yo[:], gg[:], ALU.mult, [yo_t, gg_t], [yo_t])
        S.dma("sp", "o", lambda e: e.dma_start(out=yaT[:, t0:t0 + G1], in_=yo[:]), reads=[yo_t], writes=[])
        yield

    def attn_group(g):
        for m in range(2):
            ms = slice(64 * m, 64 * m + 64)
            nsh, nsh_t = nshift[m]
            act(sqq[:], qmax2[m][0][:], AF.Sqrt, [qmax2[m][1], kmax2[m][1]], [sqq_t], scale=kmax2[m][0][:, 0:1])
            dve_ts(nsh[:], sqq[:], -0.125, None, ALU.mult, None, [sqq_t], [nsh_t])

            def stage_a(j):
                P_, P_t = Pb[j % 2]
                if j < g:
                    for a in range(2):
                        kt = 2 * j + a
                        S.op("pe", mm(ps[5][:, a * 256:(a + 1) * 256], KT[ms, kt * 128:(kt + 1) * 128], QT[ms, :]), reads=[QT_t, KT_t], writes=[ps_t[5]])
                    act(P_[:], ps[5][:, :], AF.Exp, [ps_t[5], nsh_t], [P_t], scale=0.125, bias=nsh[:, 0:1])
                else:
                    kt = 2 * g
                    S.op("pe", mm(ps[5][:, 0:256], KT[ms, kt * 128:(kt + 1) * 128], QT[ms, :]), reads=[QT_t, KT_t], writes=[ps_t[5]])
                    S.op("pe", mm(ps[5][:, 384:512], KT[ms, (kt + 1) * 128:(kt + 2) * 128], QT[ms, 128:256]), reads=[QT_t, KT_t], writes=[ps_t[5]])
                    act(P_[:, 0:256], ps[5][:, 0:256], AF.Exp, [ps_t[5], nsh_t], [P_t], scale=0.125, bias=nsh[:, 0:1])
                    act(P_[:, 384:512], ps[5][:, 384:512], AF.Exp, [ps_t[5], nsh_t], [P_t], scale=0.125, bias=nsh[:, 0:1])
                    dve_tt(P_[:, 0:128], P_[:, 0:128], trib[:], ALU.mult, [P_t, trib_t], [P_t], eng="pool")
                    dve_tt(P_[:, 384:512], P_[:, 384:512], trib[:], ALU.mult, [P_t, trib_t], [P_t], eng="pool")

            def stage_b(j):
                P_, P_t = Pb[j % 2]
                if j < g:
                    for a in range(2):
                        kt = 2 * j + a
                        for tl in range(2):
                            ob = OB[tl]
                            S.op("pe", mm(ps[ob][:, 0:129], P_[:, a * 256 + tl * 128:a * 256 + (tl + 1) * 128], VA[:, kt, 0:129], kt == 0, False),
                                 reads=[P_t, VA_t], writes=[ps_t[ob]])
                else:
                    kt = 2 * g
                    S.op("pe", mm(ps[OB[0]][:, 0:129], P_[:, 0:128], VA[:, kt, 0:129], kt == 0, True), reads=[P_t, VA_t], writes=[ps_t[OB[0]]])
                    S.op("pe", mm(ps[OB[1]][:, 0:129], P_[:, 128:256], VA[:, kt, 0:129], kt == 0, False), reads=[P_t, VA_t], writes=[ps_t[OB[1]]])
                    S.op("pe", mm(ps[OB[1]][:, 0:129], P_[:, 384:512], VA[:, kt + 1, 0:129], False, True), reads=[P_t, VA_t], writes=[ps_t[OB[1]]])

            stage_a(0)
            for j in range(g + 1):
                if j + 1 <= g:
                    stage_a(j + 1)
                stage_b(j)
                yield
            for tl in range(2):
                ob = OB[tl]
                S.op("dve", lambda e, ob=ob: e.reciprocal(out=rs_[:], in_=ps[ob][:, 128:129]), reads=[ps_t[ob]], writes=[rs_t])
                dve_ts(om[tl][m][0][:], ps[ob][:, 0:128], rs_[:, 0:1], None, ALU.mult, None, [ps_t[ob], rs_t], [om[tl][m][1]])
            yield
        for tl in range(2):
            qt = 2 * g + tl
            dve_stt(attn[:], om[tl][1][0][:], neglam[:, 0:1], om[tl][0][0][:], ALU.mult, ALU.add, [om[tl][0][1], om[tl][1][1], neglam_t], [attn_t])
            S.op("act", lambda e: e.activation(out=junk2[:], in_=attn[:], func=AF.Square, accum_out=ss2[:]), reads=[attn_t], writes=[junk2_t, ss2_t])
            S.op("act", lambda e: e.activation(out=ss2[:], in_=ss2[:], func=AF.Sqrt, scale=1.0 / 128, bias=1e-5), reads=[ss2_t], writes=[ss2_t])
            S.op("dve", lambda e: e.reciprocal(out=rstd2[:], in_=ss2[:]), reads=[ss2_t], writes=[rstd2_t])
            dve_stt(ybo[:], attn[:], rstd2[:, 0:1], sgv[:], ALU.mult, ALU.mult, [attn_t, rstd2_t, sgv_t], [ybo_t])
            S.dma("sp", "o", lambda e, qt=qt: e.dma_start(out=yb[qt * 128:(qt + 1) * 128, :], in_=ybo[:]), reads=[ybo_t], writes=[])
            yield

    for g in range(ngroups):
        t0 = g * G1
        for tl in range(2):
            xb, xb_t = xt[tl]
            S.dma("sp", "x", lambda e, tl=tl, xb=xb: e.dma_start(out=xb[:], in_=x[t0 + tl * 128:t0 + (tl + 1) * 128, :]), writes=[xb_t])
            _rms_rstd(S, "act", xb[:], xb_t, junk[:], junk_t, ss[:], ss_t, rstd[:], rstd_t, 1e-6)
            dve_ts(xnb[:], xb[:], rstd[:, 0:1], None, ALU.mult, None, [xb_t, rstd_t], [xnb_t])
            for half in range(2):
                for j in range(8):
                    kc = half * 8 + j
                    S.op("pe", lambda e, kc=kc, j=j: e.transpose(out=psb[0][:, j * 128:(j + 1) * 128], in_=xnb[:, kc * 128:(kc + 1) * 128], identity=identb[:]),
                         reads=[xnb_t, identb_t], writes=[psb_t[0]])
                copy_out(xnT[:, half * 8:(half + 1) * 8, tl * 128:(tl + 1) * 128], psb[0][:].rearrange("p (k t) -> p k t", k=8), [psb_t[0]], [xnT_t])
        blocks = [(C_R, 128), (C_K, 128), (C_V, 128), (C_XW, 96), (C_XA, 96), (C_XG, 128), (C_XG + 128, 128)]
        for bi, (c0, w) in enumerate(blocks):
            pb, pb_t = scr()
            for kc in range(16):
                S.op("pe", mm(pb[0:w, 0:G1], W1[:, kc, c0:c0 + w], xnT[:, kc, :], kc == 0, kc == 15), reads=[W1_t, xnT_t], writes=[pb_t])
            copy_out(PB[bi][0][0:w, 1:G1 + 1], pb[0:w, 0:G1], [pb_t], [PB[bi][1]])
        pb, pb_t = scr()
        for kc in range(16):
            S.op("pe", mm(pb[:, 0:G1], W1[:, kc, C_QD:C_QD + 128], xnT[:, kc, :], kc == 0, kc == 15), reads=[W1_t, xnT_t], writes=[pb_t])
        act(QT[:], pb[:, 0:G1], AF.Copy, [pb_t], [QT_t])
        act(QSQ[:], pb[:, 0:G1], AF.Square, [pb_t], [QSQ_t])
        pb, pb_t = scr()
        for kc in range(16):
            S.op("pe", mm(pb[:, 0:G1], W1[:, kc, C_KD:C_KD + 128], xnT[:, kc, :], kc == 0, kc == 15), reads=[W1_t, xnT_t], writes=[pb_t])
        act(KT[:, t0:t0 + G1], pb[:, 0:G1], AF.Copy, [pb_t], [KT_t])
        act(KSQ[:], pb[:, 0:G1], AF.Square, [pb_t], [KSQ_t])
        for m in range(2):
            ms = slice(64 * m, 64 * m + 64)
            pb, pb_t = scr()
            S.op("pe", mm(pb[:, 0:G1], ones128[ms, :], KSQ[ms, :]), reads=[ones_t, KSQ_t], writes=[pb_t])
            S.op("dve", lambda e, pb=pb: e.tensor_reduce(out=kred[:], in_=pb[:, 0:G1], axis=AX.X, op=ALU.max), reads=[pb_t], writes=[kred_t])
            dve_tt(kmax2[m][0][:], kmax2[m][0][:], kred[:], ALU.max, [kred_t, kmax2[m][1]], [kmax2[m][1]])
            pb, pb_t = scr()
            S.op("pe", mm(pb[:, 0:G1], ones128[ms, :], QSQ[ms, :]), reads=[ones_t, QSQ_t], writes=[pb_t])
            S.op("dve", lambda e, pb=pb: e.tensor_reduce(out=kred[:], in_=pb[:, 0:G1], axis=AX.X, op=ALU.max), reads=[pb_t], writes=[kred_t])
            dve_tt(qmax2[m][0][:], qmax2[m][0][:], kred[:], ALU.max, [kred_t, qmax2[m][1]], [qmax2[m][1]])
        for tl in range(2):
            pb, pb_t = scr()
            for kc in range(16):
                S.op("pe", mm(pb[:, 0:128], xnT[:, kc, tl * 128:(tl + 1) * 128], W1[:, kc, C_VD:C_VD + 128], kc == 0, kc == 15),
                     reads=[W1_t, xnT_t], writes=[pb_t])
            copy_out(VA[:, 2 * g + tl, 0:128], pb[:, 0:128], [pb_t], [VA_t])
        gens = []
        if do_rwkv:
            gr = rwkv_group(g)
            if rw_stage < 9000:
                def lim(gr=gr):
                    for _ in range(rw_stage):
                        next(gr)
                        yield
                gr = lim()
            gens.append(gr)
        if do_attn:
            gens.append(attn_group(g))
        _roundrobin(gens)
    k = "dma:o"
    if k not in S.dsem:
        S.dma("sp", "o", lambda e: e.dma_start(out=yaT[:, 0:G1], in_=yo[:]), reads=[yo_t], writes=[])
    nc.sync.wait_ge(S.dsem[k][0], S.dsem[k][1])
    return nc, S


def _consts():
    t = np.arange(128)
    same = (t[:, None] // CHK) == (t[None, :] // CHK)
    MU = (same & (t[:, None] < t[None, :])).astype(np.float32)
    ML = MU.T.copy()
    MUI = (same & (t[:, None] <= t[None, :])).astype(np.float32)
    TRI = (t[:, None] <= t[None, :]).astype(np.float32)
    ob = ((t[:, None] // 64) == (t[None, :] // 64)).astype(np.float32)
    i64 = np.zeros((128, 128), np.float32)
    i64[t, t % 64] = 1.0
    return np.stack([np.eye(128, dtype=np.float32), MU, ML, MUI, TRI, ob, i64])


def prep_l1(inp, c, ntok=SEQ):
    w_in = inp["w_in"][0]
    hs = slice(128 * c, 128 * c + 128)
    o_d = 3520
    cols = np.concatenate([np.arange(128 * c, 128 * c + 128), 1024 + np.arange(128 * c, 128 * c + 128),
                           2048 + np.arange(128 * c, 128 * c + 128), np.arange(3072, 3520),
                           o_d + np.arange(128 * c, 128 * c + 128), o_d + 1024 + np.arange(128 * c, 128 * c + 128),
                           o_d + 2048 + np.arange(128 * c, 128 * c + 128)])
    mu = inp["shift_mu"][0]
    cvec = np.zeros((128, 20), np.float32)
    cvec[:, 0] = mu[0:1024][hs]; cvec[:, 1] = mu[1024:2048][hs]; cvec[:, 2] = mu[2048:3072][hs]
    cvec[:96, 3] = mu[3072:3168]; cvec[:96, 4] = mu[3168:3264]; cvec[:, 5] = mu[3264:3392]; cvec[:, 6] = mu[3392:3520]
    cvec[:, 7] = inp["rwkv_w0"][0][hs]; cvec[:, 8] = inp["rwkv_a0"][0][hs]; cvec[:, 9] = inp["k_k"][0][hs]
    cvec[:, 10] = inp["k_a"][0][hs]; cvec[:, 11] = inp["r_k"][0].reshape(-1)[hs]
    cvec[:, 12] = inp["lnx_g"][0][hs]; cvec[:, 13] = inp["lnx_b"][0][hs]
    for c_ in range(4):
        cvec[32 * c_:32 * c_ + 32, 16 + c_] = 1.0
    rm = np.ones((1, G1), np.float32); rm[0, ::CHK] = 0.0
    return dict(
        x=np.ascontiguousarray(inp["x"][0, :ntok]), w1=np.ascontiguousarray(w_in[:, cols]), cvec=cvec,
        wdu=np.ascontiguousarray(inp["w_decay_up"][0][:, hs]), wiu=np.ascontiguousarray(inp["w_iclr_up"][0][:, hs]),
        wgu=np.ascontiguousarray(inp["w_gate_up"][0][:, hs]),
        lamv=np.concatenate([inp["lam_q1"][0], inp["lam_k1"][0], inp["lam_q2"][0], inp["lam_k2"][0]])[None, :].astype(np.float32),
        sublng=inp["subln_g"][0][None, :].astype(np.float32), g1=inp["norm1_g"][0][None, :].astype(np.float32),
        consts=_consts(), rmask=rm)


def _blk(w, nb):
    K_ = w.shape[0]
    return np.ascontiguousarray(w.reshape(K_ // 128, 128, nb, 512).transpose(2, 1, 0, 3).reshape(nb, 128, (K_ // 128) * 512))


W_BLOCKS = (("s_wg", 8), ("s_wab", 4), ("s_wo", 4), ("s_wq", 4), ("s_u", 32), ("s_v", 32))


def l2_weight_blocks(inp):
    w_in = inp["w_in"][0]
    wgb = _blk(w_in[:, 6592:], 8)
    wabb = np.concatenate([_blk(inp["w_proj_a"][0], 4), _blk(inp["w_proj_b"][0], 4)], axis=2)
    uTb = _blk(np.ascontiguousarray(inp["peer_u"][0].T), 32)
    vtb = inp["peer_v"][0].reshape(32, 4, 128, D).transpose(0, 2, 1, 3).reshape(32, 128, 4 * D)
    allb = np.zeros((NCAST * NCORES, 128, 8192), np.float32)
    o = 0
    for a in (wgb, wabb, _blk(inp["w_out"][0], 4), _blk(inp["peer_wq"][0], 4), uTb, vtb):
        allb[o:o + a.shape[0]] = a
        o += a.shape[0]
    return allb


def l2_shared(inp, cast_all):
    m = dict(
        skT=np.ascontiguousarray(inp["peer_sub_keys"][0].reshape(16, 128, 128).transpose(0, 2, 1)),
        gv=np.stack([inp["norm1_g"][0], inp["norm2_g"][0], inp["final_g"]]).astype(np.float32),
        ident=np.eye(128, dtype=np.float32))
    o = 0
    for name, nb in W_BLOCKS:
        m[name] = np.ascontiguousarray(cast_all[o:o + nb])
        o += nb
    return m


def prep_l2(inp, c, yaT_full, ybT_full, shared):
    ts = slice(TOK * c, TOK * (c + 1))
    m = dict(shared)
    m["x"] = np.ascontiguousarray(inp["x"][0, ts])
    m["yaT"] = np.ascontiguousarray(yaT_full[:, ts])
    m["ybT"] = np.ascontiguousarray(ybT_full[:, ts])
    return m


def kernel(**inputs):
    inp = {k: np.asarray(v) for k, v in inputs.items()}
    nc1, _ = build_l1()
    allb = l2_weight_blocks(inp)
    maps1 = [prep_l1(inp, c) for c in range(NCORES)]
    for c in range(NCORES):
        maps1[c]["castin"] = allb[NCAST * c:NCAST * (c + 1)]
    r1 = run_bass_kernel_spmd(nc1, maps1, core_ids=list(range(NCORES))).results
    del maps1, allb
    cast_all = np.concatenate([r1[c]["castout"] for c in range(NCORES)], axis=0)
    yaT_full = np.concatenate([r1[c]["yaT"] for c in range(NCORES)], axis=0)
    ybT_full = np.concatenate([r1[c]["yb"].T for c in range(NCORES)], axis=0)
    w_in = inp["w_in"][0]
    shared = l2_shared(inp, cast_all)
    nc2, _ = build_l2()
    maps2 = [prep_l2(inp, c, yaT_full, ybT_full, shared) for c in range(NCORES)]
    r2 = run_bass_kernel_spmd(nc2, maps2, core_ids=list(range(NCORES))).results
    out = np.concatenate([r2[c]["y"] for c in range(NCORES)], axis=0)
    return out.reshape(1, SEQ, D).astype(np.float32)
```

```python
import numpy as np
import concourse.bass as bass
import concourse.mybir as mybir
from concourse.bass_utils import run_bass_kernel_spmd

F32 = mybir.dt.float32
BF16 = mybir.dt.bfloat16
AF = mybir.ActivationFunctionType
ALU = mybir.AluOpType
AX = mybir.AxisListType

NCORES = 8
D = 2048
SEQ = 16384
TOK = SEQ // NCORES
NEG = -1.0e30


class Tok:
    __slots__ = ("w", "r")

    def __init__(self):
        self.w = None
        self.r = []


class Sync:
    def __init__(self, nc):
        self.nc = nc
        self.engs = {"pe": nc.tensor, "act": nc.scalar, "dve": nc.vector, "pool": nc.gpsimd, "sp": nc.sync}
        self.sem = {k: nc.alloc_semaphore("s_" + k) for k in ("pe", "act", "dve", "pool")}
        self.cnt = {k: 0 for k in self.sem}
        self.waited = {e: {} for e in self.engs}
        self.dsem = {}
        self.ninst = 0

    def _deps(self, reads, writes):
        deps = {}
        for b in reads:
            if b.w is not None:
                k, v = b.w
                deps[k] = max(deps.get(k, 0), v)
        for b in writes:
            if b.w is not None:
                k, v = b.w
                deps[k] = max(deps.get(k, 0), v)
            for (k, v) in b.r:
                deps[k] = max(deps.get(k, 0), v)
        return deps

    def _wait(self, ek, deps):
        eng = self.engs[ek]
        wd = self.waited[ek]
        for k, v in deps.items():
            if k == ek and ek == "pe":
                continue
            if k.startswith("dma:"):
                v = self.dsem[k][1]
                s = self.dsem[k][0]
            else:
                s = self.sem[k]
            if wd.get(k, 0) >= v:
                continue
            eng.wait_ge(s, v)
            wd[k] = v
            self.ninst += 1

    def _mark(self, ev, reads, writes):
        for b in reads:
            b.r.append(ev)
        for b in writes:
            b.w = ev
            b.r = []

    def op(self, ek, fn, reads=(), writes=()):
        self._wait(ek, self._deps(reads, writes))
        inst = fn(self.engs[ek])
        self.cnt[ek] += 1
        inst.then_inc(self.sem[ek], 1)
        self.ninst += 1
        self._mark((ek, self.cnt[ek]), reads, writes)

    def dma(self, qk, stream, fn, reads=(), writes=()):
        self._wait(qk, self._deps(reads, writes))
        inst = fn(self.engs[qk])
        k = "dma:" + stream
        if k not in self.dsem:
            self.dsem[k] = [self.nc.alloc_semaphore("d_" + stream), 0]
        self.dsem[k][1] += 16
        inst.then_inc(self.dsem[k][0], 16)
        self.ninst += 1
        self._mark((k, self.dsem[k][1]), reads, writes)

    def finish(self, toks):
        deps = self._deps(toks, ())
        self._wait("sp", deps)


def _rms_rstd(S, ek_sq, src_ap, src_tok, junk, junk_tok, ss, ss_tok, rstd, rstd_tok, eps):
    S.op("act", lambda e: e.activation(out=junk, in_=src_ap, func=AF.Square, accum_out=ss),
         reads=[src_tok], writes=[junk_tok, ss_tok])
    S.op("act", lambda e: e.activation(out=ss, in_=ss, func=AF.Sqrt, scale=1.0 / D, bias=float(eps)),
         reads=[ss_tok], writes=[ss_tok])
    S.op("dve", lambda e: e.reciprocal(out=rstd, in_=ss), reads=[ss_tok], writes=[rstd_tok])


CH = 256
NT = CH // 128


def build_l2(nchunks=TOK // CH, peer_blocks=32, stage=99):
    nc = bass.Bass("TRN2", target_bir_lowering=False)
    ntok = nchunks * CH
    dr = lambda n, s, k="ExternalInput": nc.dram_tensor(n, s, F32, kind=k).ap()
    x = dr("x", [ntok, D])
    yaT = dr("yaT", [1024, ntok])
    ybT = dr("ybT", [1024, ntok])
    bfi = lambda n, nb: nc.dram_tensor(n, [nb, 128, 8192], BF16, kind="ExternalInput").ap()
    s_wg, s_wab, s_wo, s_wq, s_u, s_v = bfi("s_wg", 8), bfi("s_wab", 4), bfi("s_wo", 4), bfi("s_wq", 4), bfi("s_u", 32), bfi("s_v", 32)
    skT = dr("skT", [16, 128, 128])
    gv = dr("gv", [3, D])
    ident_d = dr("ident", [128, 128])
    y = dr("y", [ntok, D], "ExternalOutput")

    S = Sync(nc)
    sb = lambda n, s, dt=F32: nc.alloc_sbuf_tensor(n, s, dt)
    ps = [nc.alloc_psum_tensor("ps%d" % i, [128, 512], F32) for i in range(6)]
    psb = [nc.alloc_psum_tensor("psb%d" % i, [128, 1024], BF16) for i in range(2)]
    ps_t = [Tok() for _ in range(6)]
    psb_t = [Tok() for _ in range(2)]

    gvec = sb("gvec", [128, D]); gvec_t = Tok()
    xh = sb("xh", [128, NT, D]); xh_t = [Tok() for _ in range(NT)]
    R1 = sb("R1", [128, 16, CH], BF16); R1_t = Tok()
    R2 = sb("R2", [128, 16, CH], BF16); R2_t = Tok()
    R3 = sb("R3", [128, 16, CH], BF16); R3_t = Tok()
    WS = [sb("WS%d" % i, [128, 16, 512], BF16) for i in range(3)]; WS_t = [Tok() for _ in range(3)]
    VS = [sb("VS%d" % i, [128, 4, D], BF16) for i in range(2)]; VS_t = [Tok() for _ in range(2)]
    acc = sb("acc", [128, NT, D]); acc_t = [Tok() for _ in range(NT)]
    sc = sb("sc", [128, NT, 16, 128]); sc_t = [Tok() for _ in range(NT)]
    xnb = sb("xnb", [128, D], BF16); xnb_t = Tok()
    junk, junk_t = xnb, xnb_t
    ident = sb("identb", [128, 128], BF16); ident_t = Tok()
    identf = sb("identf", [128, 128]); identf_t = Tok()
    skb = sb("skb", [128, 16, 128], BF16); skb_t = Tok()
    ss = sb("ss", [128, 1]); ss_t = Tok()
    rstd = sb("rstd", [128, 1]); rstd_t = Tok()
    sg = [sb("sg%d" % i, [128, CH], BF16) for i in range(2)]; sg_t = [Tok() for _ in range(2)]
    mm = [sb("mm%d" % i, [128, CH], BF16) for i in range(2)]; mm_t = [Tok() for _ in range(2)]
    top = sb("top", [128, 1, 16, 16]); top_t = [Tok()] * NT
    tmpk = sb("tmpk", [128, 256]); tmpk_t = Tok()
    cand = sb("cand", [128, 256]); cand_t = Tok()
    best = sb("best", [128, NT, 8, 16]); best_t = [Tok() for _ in range(NT)]
    negmx = sb("negmx", [128, NT, 8]); negmx_t = [Tok() for _ in range(NT)]
    zz = sb("zz", [128, NT, 8]); zz_t = [Tok() for _ in range(NT)]
    nbias = sb("nbias", [128, NT, 8]); nbias_t = [Tok() for _ in range(NT)]
    ebuf = sb("ebuf", [128, 16]); ebuf_t = Tok()
    Ab = [sb("Ab%d" % i, [128, 512], BF16) for i in range(2)]; Ab_t = [Tok() for _ in range(2)]
    Wc = [sb("Wc%d" % i, [128, 512], BF16) for i in range(2)]; Wc_t = [Tok() for _ in range(2)]
    Tb = [sb("Tb%d" % i, [128, 512]) for i in range(3)]; Tb_t = [Tok() for _ in range(3)]
    Eb = [sb("Eb%d" % i, [128, 512]) for i in range(3)]; Eb_t = [Tok() for _ in range(3)]
    Wh = [sb("Wh%d" % i, [128, 512], BF16) for i in range(2)]; Wh_t = [Tok() for _ in range(2)]
    Wb = [sb("Wb%d" % i, [128, 512], BF16) for i in range(2)]; Wb_t = [Tok() for _ in range(2)]
    AW = [sb("AW%d" % i, [128, 512], BF16) for i in range(2)]; AW_t = [Tok() for _ in range(2)]
    AWT = [sb("AWT%d" % i, [128, 4, 128], BF16) for i in range(2)]; AWT_t = [Tok() for _ in range(2)]

    S.dma("sp", "c", lambda e: e.dma_start(out=identf[:], in_=ident_d[:, :]), writes=[identf_t])
    S.op("dve", lambda e: e.tensor_copy(out=ident[:], in_=identf[:]), reads=[identf_t], writes=[ident_t])
    S.dma("pool", "w", lambda e: e.dma_start(out=skb[:], in_=skT.rearrange("b d n -> d b n")), writes=[skb_t])

    def load_blk(dst_tile, dst_tok, name, b, scr):
        S.dma("sp", "ws", lambda e: e.dma_start(out=dst_tile[:].rearrange("p a b -> p (a b)"), in_=scr[b]),
              writes=[dst_tok])

    def norm_transpose(t, g_row, dstT, dstT_t, first):
        src = xh[:, t, :]
        _rms_rstd(S, "act", src, xh_t[t], junk[:], junk_t, ss[:], ss_t, rstd[:], rstd_t, 1e-6)
        S.op("dve", lambda e: e.scalar_tensor_tensor(out=xnb[:], in0=src, scalar=rstd[:, 0:1], in1=gvec[:],
                                                     op0=ALU.mult, op1=ALU.mult),
             reads=[xh_t[t], rstd_t, gvec_t], writes=[xnb_t])
        for half in range(2):
            pb = psb[half]
            for j in range(8):
                kc = half * 8 + j
                S.op("pe", lambda e, kc=kc, j=j, pb=pb: e.transpose(out=pb[:, j * 128:(j + 1) * 128],
                                                                    in_=xnb[:, kc * 128:(kc + 1) * 128], identity=ident[:]),
                     reads=[xnb_t, ident_t], writes=[psb_t[half]])
            S.op("act" if half == 0 else "dve",
                 (lambda e, half=half, pb=pb: e.activation(out=dstT[:, half * 8:(half + 1) * 8, t * 128:(t + 1) * 128],
                                                           in_=pb[:].rearrange("p (k t) -> p k t", k=8), func=AF.Copy))
                 if half == 0 else
                 (lambda e, half=half, pb=pb: e.tensor_copy(out=dstT[:, half * 8:(half + 1) * 8, t * 128:(t + 1) * 128],
                                                            in_=pb[:].rearrange("p (k t) -> p k t", k=8))),
                 reads=[psb_t[half]], writes=[dstT_t])

    for c in range(nchunks):
        t0 = c * CH
        for t in range(NT):
            S.dma("sp", "x", lambda e, t=t: e.dma_start(out=xh[:, t, :], in_=x[t0 + t * 128:t0 + (t + 1) * 128, :]),
                  writes=[xh_t[t]])
        S.dma("sp", "g", lambda e: e.dma_start(out=gvec[:], in_=gv[0:1, :].partition_broadcast(128)), writes=[gvec_t])
        S.dma("pool", "w", lambda e: e.dma_start(out=R2[:, 0:8, :], in_=yaT[:, t0:t0 + CH].rearrange("(k p) t -> p k t", p=128)),
              writes=[R2_t])
        S.dma("pool", "w", lambda e: e.dma_start(out=R2[:, 8:16, :], in_=ybT[:, t0:t0 + CH].rearrange("(k p) t -> p k t", p=128)),
              writes=[R2_t])
        for t in range(NT):
            norm_transpose(t, 0, R1, R1_t, True)
        for cg in range(4):
            cs = slice(cg * 512, (cg + 1) * 512)
            load_blk(WS[0], WS_t[0], "wg", cg, s_wg)
            load_blk(WS[1], WS_t[1], "wg", 4 + cg, s_wg)
            load_blk(WS[2], WS_t[2], "wab", cg, s_wab)
            for cb in range(4):
                cc = slice(cb * 128, (cb + 1) * 128)
                cidx = cg * 4 + cb
                for kc in range(16):
                    S.op("pe", lambda e, kc=kc, cc=cc: e.matmul(out=ps[0][:, 0:CH], lhsT=WS[0][:, kc, cc], rhs=R1[:, kc, :],
                                                                start=(kc == 0), stop=(kc == 15)),
                         reads=[WS_t[0], R1_t], writes=[ps_t[0]])
                for kc in range(16):
                    S.op("pe", lambda e, kc=kc, cc=cc: e.matmul(out=ps[1][:, 0:CH], lhsT=WS[1][:, kc, cc], rhs=R1[:, kc, :],
                                                                start=(kc == 0), stop=(kc == 15)),
                         reads=[WS_t[1], R1_t], writes=[ps_t[1]])
                for kc in range(8):
                    S.op("pe", lambda e, kc=kc, cc=cc: e.matmul(out=ps[2][:, 0:CH], lhsT=WS[2][:, kc, cc], rhs=R2[:, kc, :],
                                                                start=(kc == 0), stop=(kc == 7)),
                         reads=[WS_t[2], R2_t], writes=[ps_t[2]])
                for kc in range(8):
                    S.op("pe", lambda e, kc=kc, cc=cc: e.matmul(out=ps[3][:, 0:CH], lhsT=WS[2][:, 8 + kc, cc], rhs=R2[:, 8 + kc, :],
                                                                start=(kc == 0), stop=(kc == 7)),
                         reads=[WS_t[2], R2_t], writes=[ps_t[3]])
                for i in range(2):
                    S.op("act", lambda e, i=i: e.activation(out=sg[i][:], in_=ps[i][:, 0:CH], func=AF.Sigmoid),
                         reads=[ps_t[i]], writes=[sg_t[i]])
                for i in range(2):
                    S.op("dve", lambda e, i=i: e.tensor_tensor(out=mm[i][:], in0=sg[i][:], in1=ps[2 + i][:, 0:CH], op=ALU.mult),
                         reads=[sg_t[i], ps_t[2 + i]], writes=[mm_t[i]])
                S.op("pool", lambda e, cidx=cidx: e.tensor_tensor(out=R3[:, cidx, :], in0=mm[0][:], in1=mm[1][:], op=ALU.add),
                     reads=[mm_t[0], mm_t[1]], writes=[R3_t])
        for db in range(4):
            slot = db % 2
            load_blk(WS[slot], WS_t[slot], "wo", db, s_wo)
            for t in range(NT):
                pbk = 4 + (t % 2)
                for kc in range(16):
                    S.op("pe", lambda e, kc=kc, t=t, pbk=pbk, slot=slot: e.matmul(out=ps[pbk][:, :], lhsT=R3[:, kc, t * 128:(t + 1) * 128],
                                                                                 rhs=WS[slot][:, kc, :], start=(kc == 0), stop=(kc == 15)),
                         reads=[R3_t, WS_t[slot]], writes=[ps_t[pbk]])
                S.op("dve", lambda e, t=t, db=db, pbk=pbk: e.tensor_tensor(out=xh[:, t, db * 512:(db + 1) * 512],
                                                                         in0=xh[:, t, db * 512:(db + 1) * 512], in1=ps[pbk][:, :], op=ALU.add),
                     reads=[ps_t[pbk], xh_t[t]], writes=[xh_t[t]])
        if stage >= 2:
            S.dma("sp", "g", lambda e: e.dma_start(out=gvec[:], in_=gv[1:2, :].partition_broadcast(128)), writes=[gvec_t])
            for t in range(NT):
                norm_transpose(t, 1, R1, R1_t, False)
            for qg in range(4):
                slot = qg % 2
                load_blk(WS[slot], WS_t[slot], "wq", qg, s_wq)
                for qb in range(4):
                    blk = qg * 4 + qb
                    pbk = blk % 2
                    for kc in range(16):
                        S.op("pe", lambda e, kc=kc, qb=qb, pbk=pbk, slot=slot: e.matmul(out=ps[pbk][:, 0:CH], lhsT=WS[slot][:, kc, qb * 128:(qb + 1) * 128],
                                                                                       rhs=R1[:, kc, :], start=(kc == 0), stop=(kc == 15)),
                             reads=[WS_t[slot], R1_t], writes=[ps_t[pbk]])
                    S.op("act", lambda e, blk=blk, pbk=pbk: e.activation(out=R2[:, blk, :], in_=ps[pbk][:, 0:CH], func=AF.Copy),
                         reads=[ps_t[pbk]], writes=[R2_t])
            for t in range(NT):
                for g4 in range(4):
                    pbk = 2 + (g4 % 2)
                    for j in range(4):
                        blk = g4 * 4 + j
                        S.op("pe", lambda e, blk=blk, j=j, pbk=pbk, t=t: e.matmul(out=ps[pbk][:, j * 128:(j + 1) * 128],
                                                                                 lhsT=R2[:, blk, t * 128:(t + 1) * 128], rhs=skb[:, blk, :],
                                                                                 start=True, stop=True),
                             reads=[R2_t, skb_t], writes=[ps_t[pbk]])
                    S.op("act", lambda e, g4=g4, pbk=pbk, t=t: e.activation(out=sc[:, t, g4 * 4:(g4 + 1) * 4, :],
                                                                           in_=ps[pbk][:].rearrange("p (b n) -> p b n", b=4), func=AF.Copy),
                         reads=[ps_t[pbk]], writes=[sc_t[t]])
                for blk in range(16):
                    S.op("dve", lambda e, blk=blk, t=t: e.max(out=top[:, 0, blk, 0:8], in_=sc[:, t, blk, :]),
                         reads=[sc_t[t]], writes=[top_t[t]])
                    S.op("dve", lambda e, blk=blk, t=t: e.match_replace(out=tmpk[:, 0:128], in_to_replace=top[:, 0, blk, 0:8],
                                                                        in_values=sc[:, t, blk, :], imm_value=NEG),
                         reads=[sc_t[t], top_t[t]], writes=[tmpk_t])
                    S.op("dve", lambda e, blk=blk, t=t: e.max(out=top[:, 0, blk, 8:16], in_=tmpk[:, 0:128]),
                         reads=[tmpk_t], writes=[top_t[t]])
                for h in range(8):
                    S.op("dve", lambda e, h=h, t=t: e.tensor_tensor(
                        out=cand[:].rearrange("p (a b) -> p a b", a=16),
                        in0=top[:, 0, 2 * h, :].unsqueeze(2).broadcast_to([128, 16, 16]),
                        in1=top[:, 0, 2 * h + 1, :].unsqueeze(1).broadcast_to([128, 16, 16]), op=ALU.add),
                         reads=[top_t[t]], writes=[cand_t])
                    S.op("dve", lambda e, h=h, t=t: e.max(out=best[:, t, h, 0:8], in_=cand[:]),
                         reads=[cand_t], writes=[best_t[t]])
                    S.op("dve", lambda e, h=h, t=t: e.match_replace(out=tmpk[:], in_to_replace=best[:, t, h, 0:8],
                                                                    in_values=cand[:], imm_value=NEG),
                         reads=[cand_t, best_t[t]], writes=[tmpk_t])
                    S.op("dve", lambda e, h=h, t=t: e.max(out=best[:, t, h, 8:16], in_=tmpk[:]),
                         reads=[tmpk_t], writes=[best_t[t]])
                S.op("dve", lambda e, t=t: e.tensor_scalar(out=negmx[:, t, :], in0=best[:, t, :, 0], scalar1=-1.0, scalar2=None, op0=ALU.mult),
                     reads=[best_t[t]], writes=[negmx_t[t]])
                for h in range(8):
                    S.op("act", lambda e, h=h, t=t: e.activation(out=ebuf[:], in_=best[:, t, h, :], func=AF.Exp,
                                                                 bias=negmx[:, t, h:h + 1], accum_out=zz[:, t, h:h + 1]),
                         reads=[best_t[t], negmx_t[t]], writes=[ebuf_t, zz_t[t]])
                S.op("act", lambda e, t=t: e.activation(out=zz[:, t, :], in_=zz[:, t, :], func=AF.Ln),
                     reads=[zz_t[t]], writes=[zz_t[t]])
                S.op("dve", lambda e, t=t: e.tensor_tensor(out=nbias[:, t, :], in0=negmx[:, t, :], in1=zz[:, t, :], op=ALU.subtract),
                     reads=[negmx_t[t], zz_t[t]], writes=[nbias_t[t]])
            its = [(eb, t) for eb in range(peer_blocks) for t in range(NT)]
            nit = len(its)

            def st1(k):
                eb, t = its[k]
                slot = eb % 2
                if t == 0:
                    load_blk(WS[slot], WS_t[slot], "u", eb, s_u)
                    load_blk(VS[slot], VS_t[slot], "v", eb, s_v)
                pa = k % 2
                for kc in range(16):
                    S.op("pe", lambda e, kc=kc, t=t, slot=slot, pa=pa: e.matmul(out=ps[pa][:, :], lhsT=R1[:, kc, t * 128:(t + 1) * 128],
                                                                               rhs=WS[slot][:, kc, :], start=(kc == 0), stop=(kc == 15)),
                         reads=[R1_t, WS_t[slot]], writes=[ps_t[pa]])

            def st1g(k):
                pa = k % 2
                S.op("act", lambda e, pa=pa: e.activation(out=Ab[pa][:], in_=ps[pa][:, :], func=AF.Gelu),
                     reads=[ps_t[pa]], writes=[Ab_t[pa]])

            def st2(k):
                eb, t = its[k]
                pa = k % 2

                def emit_T(h):
                    i = h % 3
                    S.op("dve", lambda e, h=h, i=i: e.tensor_tensor(
                        out=Tb[i][:].rearrange("p (a b) -> p a b", a=4),
                        in0=sc[:, t, 2 * h, eb * 4:(eb + 1) * 4].unsqueeze(2).broadcast_to([128, 4, 128]),
                        in1=sc[:, t, 2 * h + 1, :].unsqueeze(1).broadcast_to([128, 4, 128]), op=ALU.add),
                         reads=[sc_t[t]], writes=[Tb_t[i]])
                    S.op("act", lambda e, h=h, i=i: e.activation(out=Eb[i][:], in_=Tb[i][:], func=AF.Exp, bias=nbias[:, t, h:h + 1]),
                         reads=[Tb_t[i], nbias_t[t]], writes=[Eb_t[i]])
                emit_T(0)
                emit_T(1)
                for h in range(8):
                    i = h % 3
                    if h + 2 < 8:
                        emit_T(h + 2)
                    accb, accb_t = (Wb[pa], Wb_t[pa]) if h % 2 == 0 else (Wc[pa], Wc_t[pa])
                    dst, dst_t = (accb, accb_t) if h < 2 else (Wh[h % 2], Wh_t[h % 2])
                    S.op("dve", lambda e, h=h, i=i, dst=dst: e.scalar_tensor_tensor(
                        out=dst[:], in0=Tb[i][:], scalar=best[:, t, h, 15:16], in1=Eb[i][:], op0=ALU.is_ge, op1=ALU.mult),
                         reads=[Tb_t[i], Eb_t[i], best_t[t]], writes=[dst_t])
                    if h >= 2:
                        S.op("pool" if h % 2 == 0 else "dve", lambda e, h=h, accb=accb: e.tensor_tensor(out=accb[:], in0=accb[:], in1=Wh[h % 2][:], op=ALU.add),
                             reads=[Wh_t[h % 2], accb_t], writes=[accb_t])
                S.op("dve", lambda e, pa=pa: e.tensor_tensor(out=Wb[pa][:], in0=Wb[pa][:], in1=Wc[pa][:], op=ALU.add),
                     reads=[Wb_t[pa], Wc_t[pa]], writes=[Wb_t[pa]])
                S.op("dve", lambda e, pa=pa: e.tensor_tensor(out=AW[pa][:], in0=Ab[pa][:], in1=Wb[pa][:], op=ALU.mult),
                     reads=[Ab_t[pa], Wb_t[pa]], writes=[AW_t[pa]])

            def st3a(k):
                pa = k % 2
                for es in range(4):
                    S.op("pe", lambda e, es=es, pa=pa: e.transpose(out=psb[pa][:, es * 128:(es + 1) * 128], in_=AW[pa][:, es * 128:(es + 1) * 128],
                                                                   identity=ident[:]),
                         reads=[AW_t[pa], ident_t], writes=[psb_t[pa]])
                S.op("act", lambda e, pa=pa: e.activation(out=AWT[pa][:], in_=psb[pa][:, 0:512].rearrange("p (s t) -> p s t", s=4), func=AF.Copy),
                     reads=[psb_t[pa]], writes=[AWT_t[pa]])

            def st3b(k):
                eb, t = its[k]
                slot = eb % 2
                pa = k % 2
                for db in range(4):
                    pbk = 2 + db
                    for es in range(4):
                        S.op("pe", lambda e, es=es, db=db, pbk=pbk, slot=slot, pa=pa: e.matmul(
                            out=ps[pbk][:, :], lhsT=AWT[pa][:, es, :], rhs=VS[slot][:, es, db * 512:(db + 1) * 512],
                            start=(es == 0), stop=(es == 3)),
                             reads=[AWT_t[pa], VS_t[slot]], writes=[ps_t[pbk]])

            def st3c(k):
                eb, t = its[k]
                for db in range(4):
                    pbk = 2 + db
                    if eb == 0:
                        S.op("dve", lambda e, db=db, t=t, pbk=pbk: e.tensor_copy(out=acc[:, t, db * 512:(db + 1) * 512], in_=ps[pbk][:, :]),
                             reads=[ps_t[pbk]], writes=[acc_t[t]])
                    else:
                        S.op("dve", lambda e, db=db, t=t, pbk=pbk: e.tensor_tensor(out=acc[:, t, db * 512:(db + 1) * 512],
                                                                                 in0=acc[:, t, db * 512:(db + 1) * 512], in1=ps[pbk][:, :], op=ALU.add),
                             reads=[ps_t[pbk], acc_t[t]], writes=[acc_t[t]])

            for k in range(nit + 2):
                if 0 <= k - 2 < nit:
                    st3a(k - 2)
                if k < nit:
                    st1(k)
                if 0 <= k - 2 < nit:
                    st3b(k - 2)
                if 0 <= k - 1 < nit:
                    st2(k - 1)
                if k < nit:
                    st1g(k)
                if 0 <= k - 2 < nit:
                    st3c(k - 2)
            for t in range(NT):
                S.op("pool", lambda e, t=t: e.tensor_tensor(out=xh[:, t, :], in0=xh[:, t, :], in1=acc[:, t, :], op=ALU.add),
                     reads=[acc_t[t], xh_t[t]], writes=[xh_t[t]])
        if stage >= 3:
            S.dma("sp", "g", lambda e: e.dma_start(out=gvec[:], in_=gv[2:3, :].partition_broadcast(128)), writes=[gvec_t])
        for t in range(NT):
            if stage >= 3:
                _rms_rstd(S, "act", xh[:, t, :], xh_t[t], junk[:], junk_t, ss[:], ss_t, rstd[:], rstd_t, 1e-6)
                S.op("dve", lambda e, t=t: e.scalar_tensor_tensor(out=acc[:, t, :], in0=xh[:, t, :], scalar=rstd[:, 0:1], in1=gvec[:],
                                                                 op0=ALU.mult, op1=ALU.mult),
                     reads=[xh_t[t], rstd_t, gvec_t], writes=[acc_t[t]])
            else:
                S.op("dve", lambda e, t=t: e.tensor_copy(out=acc[:, t, :], in_=xh[:, t, :]), reads=[xh_t[t]], writes=[acc_t[t]])
            S.dma("sp", "o", lambda e, t=t: e.dma_start(out=y[t0 + t * 128:t0 + (t + 1) * 128, :], in_=acc[:, t, :]),
                  reads=[acc_t[t]], writes=[])
    k = "dma:o"
    nc.sync.wait_ge(S.dsem[k][0], S.dsem[k][1])
    return nc, S


G1 = 256
NC1 = 1216
C_R, C_K, C_V, C_XW, C_XA, C_XG, C_QD, C_KD, C_VD = 0, 128, 256, 384, 480, 576, 832, 960, 1088
CHK = 32
NCAST = 11


def _roundrobin(gens):
    gens = list(gens)
    while gens:
        for g_ in list(gens):
            try:
                next(g_)
            except StopIteration:
                gens.remove(g_)


def build_l1(ngroups=SEQ // G1, do_rwkv=True, do_attn=True, rw_stage=9999):
    nc = bass.Bass("TRN2", target_bir_lowering=False)
    ntok = ngroups * G1
    ntile = ntok // 128
    dr = lambda n, s, k="ExternalInput": nc.dram_tensor(n, s, F32, kind=k).ap()
    x = dr("x", [ntok, D])
    w1 = dr("w1", [D, NC1])
    cvec = dr("cvec", [128, 20])
    wdu_d = dr("wdu", [96, 128]); wiu_d = dr("wiu", [96, 128]); wgu_d = dr("wgu", [256, 128])
    lamv = dr("lamv", [1, 256]); sublng = dr("sublng", [1, 128]); g1d = dr("g1", [1, D])
    consts = dr("consts", [7, 128, 128])
    rmask_d = dr("rmask", [1, G1])
    yaT = dr("yaT", [128, ntok], "ExternalOutput")
    yb = dr("yb", [ntok, 128], "ExternalOutput")
    castin = dr("castin", [NCAST, 128, 8192])
    castout = nc.dram_tensor("castout", [NCAST, 128, 8192], BF16, kind="ExternalOutput").ap()

    S = Sync(nc)
    for b in range(NCAST):
        S.dma("pool", "o", lambda e, b=b: e.dma_start(out=castout[b].rearrange("p (s e) -> p s e", e=2048),
                                                      in_=castin[b].rearrange("p (s e) -> p s e", e=2048)))
    sb = lambda n, s, dt=F32: nc.alloc_sbuf_tensor(n, s, dt)
    ps = [nc.alloc_psum_tensor("ps%d" % i, [128, 512], F32) for i in range(7)]
    psb = [nc.alloc_psum_tensor("psb%d" % i, [128, 1024], BF16) for i in range(1)]
    ps_t = [Tok() for _ in range(7)]
    psb_t = [Tok() for _ in range(1)]
    OB = [4, 6]
    scr_i = [0]

    def scr():
        scr_i[0] ^= 1
        return ps[scr_i[0]], ps_t[scr_i[0]]

    def T_(name, shape, dt=F32):
        return sb(name, shape, dt), Tok()

    W1, W1_t = T_("W1", [128, 16, NC1], BF16)
    KT, KT_t = T_("KT", [128, ntok], BF16)
    VA, VA_t = T_("VA", [128, ntile, 130], BF16)
    g1c, g1c_t = T_("g1c", [128, 16])
    xt = [T_("xt%d" % i, [128, D]) for i in range(2)]
    xnb, xnb_t = T_("xnb", [128, D], BF16)
    junk, junk_t = xnb, xnb_t
    junk2, junk2_t = T_("junk2", [128, 128], BF16)
    xnT, xnT_t = T_("xnT", [128, 16, G1], BF16)
    cst, cst_t = T_("cst", [128, 7, 128])
    identb, identb_t = T_("identb", [128, 128], BF16)
    trib, trib_t = T_("trib", [128, 128], BF16)
    rmask, rmask_t = T_("rmask_s", [128, G1])
    cv, cv_t = T_("cv_s", [128, 20])
    omm, omm_t = T_("omm", [128, 8])
    wdu, wdu_t = T_("wdus", [96, 128]); wiu, wiu_t = T_("wius", [96, 128]); wgu, wgu_t = T_("wgus", [128, 2, 128])
    lacc, lacc_t = T_("lacc", [128, 4])
    neglam, neglam_t = T_("neglam", [128, 1])
    sgv, sgv_t = T_("sgv", [128, 128])
    ss, ss_t = T_("ss", [128, 1]); rstd, rstd_t = T_("rstd", [128, 1])
    ss2, ss2_t = T_("ss2", [128, 1]); rstd2, rstd2_t = T_("rstd2", [128, 1])
    ident = cst[:, 0, :]; MU = cst[:, 1, :]; ML = cst[:, 2, :]; MUI = cst[:, 3, :]; onesblk = cst[:, 5, :]; I64 = cst[:, 6, 0:64]
    PB = [T_("PB%d" % i, [128, G1 + 1]) for i in range(7)]
    SH = [T_("SH%d" % i, [128, G1]) for i in range(7)]
    tmpA, tmpA_t = T_("tmpA", [128, G1]); tmpB, tmpB_t = T_("tmpB", [128, G1])
    logw, logw_t = T_("logw", [128, G1]); av, av_t = T_("av", [128, G1]); gg, gg_t = T_("gg", [128, G1])
    kkn, kkn_t = T_("kkn", [128, G1]); k2, k2_t = T_("k2", [128, G1]); bonus, bonus_t = T_("bonus", [128, G1])
    cum, cum_t = T_("cum", [128, G1]); Pm, Pm_t = T_("Pm", [128, G1]); Pinv, Pinv_t = T_("Pinv", [128, G1]); Pprev, Pprev_t = T_("Pprev", [128, G1])
    At, At_t = T_("At", [128, G1], BF16); Bt, Bt_t = T_("Bt", [128, G1], BF16); Kt, Kt_t = T_("Kt", [128, G1], BF16); Rt, Rt_t = T_("Rt", [128, G1], BF16)
    vb16, vb16_t = T_("vb16", [128, G1], BF16)
    yT, yT_t = T_("yT", [128, G1]); yo, yo_t = T_("yo", [128, G1])
    TOK, TOK_t = T_("TOK", [128, 4, 128], BF16)
    lam_s = yT[:, 0:256].rearrange("p (a c) -> p a c", c=64); lam_t = yT_t
    lamp = yo[:, 0:128].rearrange("p (b c) -> p b c", c=64); lamp_t = yo_t
    HB = []
    for h in range(2):
        HB.append(dict(
            XT=[T_("XT%d_%d" % (h, i), [128, 128], BF16) for i in range(5)], XX=[T_("XX%d_%d" % (h, i), [128, 128], BF16) for i in range(4)],
            LakT=T_("LakT%d" % h, [128, 128], BF16), MrbT=T_("MrbT%d" % h, [128, 128], BF16), MrkT=T_("MrkT%d" % h, [128, 128], BF16),
            Z=[T_("Z%d_%d" % (h, i), [128, 128], BF16) for i in range(2)], ZF=T_("ZF%d" % h, [128, 128], BF16),
            MZ=T_("MZ%d" % h, [128, 4, 64], BF16), MB=T_("MB%d" % h, [128, 4, 64], BF16), MK=T_("MK%d" % h, [128, 4, 64], BF16)))
    RhT, RhT_t = T_("RhT", [128, 128]); YhT, YhT_t = T_("YhT", [128, 128])
    MT, MT_t = T_("MT", [128, 4, 64]); HP, HP_t = T_("HP", [128, 4, 64])
    Sst = [T_("Sst%d" % i, [128, 64]) for i in range(2)]
    QT, QT_t = T_("QT", [128, G1], BF16); QSQ, QSQ_t = T_("QSQ", [128, G1]); KSQ, KSQ_t = T_("KSQ", [128, G1])
    kmax2 = [T_("kmax2_%d" % i, [128, 1]) for i in range(2)]
    kred, kred_t = T_("kred", [128, 1]); sqq, sqq_t = T_("sqq", [128, 1])
    nshift = [T_("nshift%d" % i, [128, 1]) for i in range(2)]
    Pb = [T_("Pb%d" % i, [128, 512], BF16) for i in range(2)]
    om = [[T_("om%d_%d" % (i, j), [128, 128]) for j in range(2)] for i in range(2)]
    qmax2 = [T_("qmax2_%d" % i, [128, 1]) for i in range(2)]
    rs_, rs_t = T_("rs_", [128, 1]); attn, attn_t = T_("attn", [128, 128]); ybo, ybo_t = T_("ybo", [128, 128])
    ones128, ones_t = T_("ones128", [128, 128])

    def mm(out, lhsT, rhs, start=True, stop=True):
        return lambda e: e.matmul(out=out, lhsT=lhsT, rhs=rhs, start=start, stop=stop)

    cpy_i = [0]

    def copy_out(out, in_, reads, writes, eng=None):
        if eng is None:
            cpy_i[0] ^= 1
            eng = "act" if cpy_i[0] else "dve"
        if eng == "act":
            S.op("act", lambda e: e.activation(out=out, in_=in_, func=AF.Copy), reads=reads, writes=writes)
        else:
            S.op("dve", lambda e: e.tensor_copy(out=out, in_=in_), reads=reads, writes=writes)

    def dve_tt(out, in0, in1, op, reads, writes, eng="dve"):
        S.op(eng, lambda e: e.tensor_tensor(out=out, in0=in0, in1=in1, op=op), reads=reads, writes=writes)

    def dve_ts(out, in0, s1, s2, op0, op1, reads, writes, eng="dve"):
        if op1 is None:
            S.op(eng, lambda e: e.tensor_scalar(out=out, in0=in0, scalar1=s1, scalar2=None, op0=op0), reads=reads, writes=writes)
        else:
            S.op(eng, lambda e: e.tensor_scalar(out=out, in0=in0, scalar1=s1, scalar2=s2, op0=op0, op1=op1), reads=reads, writes=writes)

    def dve_stt(out, in0, scalar, in1, op0, op1, reads, writes):
        S.op("dve", lambda e: e.scalar_tensor_tensor(out=out, in0=in0, scalar=scalar, in1=in1, op0=op0, op1=op1), reads=reads, writes=writes)

    def act(out, in_, func, reads, writes, **kw):
        S.op("act", lambda e: e.activation(out=out, in_=in_, func=func, **kw), reads=reads, writes=writes)

    S.dma("sp", "c", lambda e: e.dma_start(out=cst[:], in_=consts.rearrange("c p n -> p c n")), writes=[cst_t])
    S.dma("sp", "c", lambda e: e.dma_start(out=cv[:], in_=cvec[:, :]), writes=[cv_t])
    S.dma("sp", "c", lambda e: e.dma_start(out=wdu[:], in_=wdu_d[:, :]), writes=[wdu_t])
    S.dma("sp", "c", lambda e: e.dma_start(out=wiu[:], in_=wiu_d[:, :]), writes=[wiu_t])
    S.dma("sp", "c", lambda e: e.dma_start(out=wgu[:], in_=wgu_d.rearrange("(k p) c -> p k c", p=128)), writes=[wgu_t])
    S.dma("sp", "c", lambda e: e.dma_start(out=yT[:, 0:256], in_=lamv[0:1, :].partition_broadcast(128)), writes=[lam_t])
    S.dma("sp", "c", lambda e: e.dma_start(out=sgv[:], in_=sublng[0:1, :].partition_broadcast(128)), writes=[sgv_t])
    S.dma("sp", "c", lambda e: e.dma_start(out=rmask[:], in_=rmask_d[0:1, :].partition_broadcast(128)), writes=[rmask_t])
    with nc.allow_non_contiguous_dma(reason="tiny gain vector"):
        S.dma("sp", "c", lambda e: e.dma_start(out=g1c[:], in_=g1d.rearrange("o (k p) -> p (o k)", p=128)), writes=[g1c_t])
    S.dma("pool", "w", lambda e: e.dma_start(out=W1[:], in_=w1.rearrange("(k p) c -> p k c", p=128)), writes=[W1_t])
    for kc in range(16):
        S.op("dve" if kc % 2 else "pool", lambda e, kc=kc: e.tensor_scalar(out=W1[:, kc, :], in0=W1[:, kc, :], scalar1=g1c[:, kc:kc + 1], scalar2=0.0,
                                                                            op0=ALU.mult, op1=ALU.add),
             reads=[W1_t, g1c_t], writes=[W1_t])
    S.op("dve", lambda e: e.tensor_copy(out=identb[:], in_=cst[:, 0, :]), reads=[cst_t], writes=[identb_t])
    S.op("dve", lambda e: e.tensor_copy(out=trib[:], in_=cst[:, 4, :]), reads=[cst_t], writes=[trib_t])
    dve_ts(omm[:, 0:8], cv[:, 0:8], -1.0, 1.0, ALU.mult, ALU.add, [cv_t], [omm_t])
    dve_ts(cv[:, 14:15], cv[:, 10:11], -1.0, 1.0, ALU.mult, ALU.add, [cv_t], [cv_t])
    dve_ts(sgv[:], sgv[:], 0.8, None, ALU.mult, None, [sgv_t], [sgv_t])
    dve_tt(lamp[:, 0, :], lam_s[:, 0, :], lam_s[:, 1, :], ALU.mult, [lam_t], [lamp_t])
    dve_tt(lamp[:, 1, :], lam_s[:, 2, :], lam_s[:, 3, :], ALU.mult, [lam_t], [lamp_t])
    S.op("dve", lambda e: e.tensor_reduce(out=lacc[:, 0:2], in_=lamp[:, 0:2, :], axis=AX.X, op=ALU.add), reads=[lamp_t], writes=[lacc_t])
    act(lacc[:, 2:4], lacc[:, 0:2], AF.Exp, [lacc_t], [lacc_t])
    dve_tt(neglam[:], lacc[:, 3:4], lacc[:, 2:3], ALU.subtract, [lacc_t], [neglam_t])
    dve_ts(neglam[:], neglam[:], -0.2, None, ALU.add, None, [neglam_t], [neglam_t])
    for i in range(7):
        S.op("pool", lambda e, i=i: e.memset(PB[i][0][:, 0:1], 0.0), writes=[PB[i][1]])
    for i in range(2):
        S.op("pool", lambda e, i=i: e.memset(Sst[i][0][:], 0.0), writes=[Sst[i][1]])
        S.op("pool", lambda e, i=i: e.memset(kmax2[i][0][:], 0.0), writes=[kmax2[i][1]])
        S.op("pool", lambda e, i=i: e.memset(qmax2[i][0][:], 0.0), writes=[qmax2[i][1]])
    S.op("pool", lambda e: e.memset(VA[:, :, 128:130], 1.0), writes=[VA_t])
    S.op("pool", lambda e: e.memset(ones128[:], 1.0), writes=[ones_t])

    scur = [0]

    def head_chain(h, cs):
        hb = HB[h]
        XT, XX, Z = hb["XT"], hb["XX"], hb["Z"]
        LakT, LakT_t = hb["LakT"]; MrbT, MrbT_t = hb["MrbT"]; MrkT, MrkT_t = hb["MrkT"]
        zf, zf_t = hb["ZF"]
        MZ, MZ_t = hb["MZ"]; MB, MB_t = hb["MB"]; MK, MK_t = hb["MK"]
        pb, pb_t = ps[h], ps_t[h]
        hs = slice(64 * h, 64 * h + 64)
        Ah, Bh, Kh, Rh = At[hs, cs], Bt[hs, cs], Kt[hs, cs], Rt[hs, cs]
        S.op("pe", mm(pb[:, 0:128], Bh, Ah), reads=[Bt_t, At_t], writes=[pb_t])
        dve_tt(XT[0][0][:], pb[:, 0:128], MU, ALU.mult, [pb_t, cst_t], [XT[0][1]])
        yield
        S.op("pe", mm(pb[:, 0:128], Ah, Bh), reads=[Bt_t, At_t], writes=[pb_t])
        dve_tt(XX[0][0][:], pb[:, 0:128], ML, ALU.mult, [pb_t, cst_t], [XX[0][1]])
        yield
        S.op("pe", mm(pb[:, 0:128], Kh, Ah), reads=[Kt_t, At_t], writes=[pb_t])
        dve_tt(LakT[:], pb[:, 0:128], MU, ALU.mult, [pb_t, cst_t], [LakT_t])
        yield
        S.op("pe", mm(pb[:, 0:128], Bh, Rh), reads=[Bt_t, Rt_t], writes=[pb_t])
        dve_tt(MrbT[:], pb[:, 0:128], MUI, ALU.mult, [pb_t, cst_t], [MrbT_t])
        yield
        S.op("pe", mm(pb[:, 0:128], Kh, Rh), reads=[Kt_t, Rt_t], writes=[pb_t])
        dve_tt(MrkT[:], pb[:, 0:128], MUI, ALU.mult, [pb_t, cst_t], [MrkT_t])
        yield
        S.op("pe", mm(pb[:, 0:64], LakT[:], TOK[:, 3, hs]), reads=[LakT_t, TOK_t], writes=[pb_t])
        zc, zc_t = Z[0]
        copy_out(zc[:, 64:128], pb[:, 0:64], [pb_t], [zc_t], eng="dve")
        S.op("pool", lambda e: e.tensor_copy(out=zc[:, 0:64], in_=TOK[:, 0, hs]), reads=[TOK_t], writes=[zc_t])
        yield
        for i in range(4):
            S.op("pe", mm(pb[:, 0:128], XX[i][0][:], XT[i][0][:]), reads=[XX[i][1], XT[i][1]], writes=[pb_t])
            copy_out(XT[i + 1][0][:], pb[:, 0:128], [pb_t], [XT[i + 1][1]], eng="dve")
            yield
            if i < 3:
                S.op("pe", mm(pb[:, 0:128], XT[i][0][:], XX[i][0][:]), reads=[XX[i][1], XT[i][1]], writes=[pb_t])
                copy_out(XX[i + 1][0][:], pb[:, 0:128], [pb_t], [XX[i + 1][1]], eng="act")
                yield
            zc, zc_t = Z[i % 2]
            zn, zn_t = Z[(i + 1) % 2]
            S.op("pe", mm(pb[:, 0:128], XT[i][0][:], zc[:]), reads=[XT[i][1], zc_t], writes=[pb_t])
            dve_tt(zn[:], pb[:, 0:128], zc[:], ALU.add, [pb_t, zc_t], [zn_t])
            yield
        zc, zc_t = Z[0]
        S.op("pe", mm(pb[:, 0:128], XT[4][0][:], zc[:]), reads=[XT[4][1], zc_t], writes=[pb_t])
        dve_tt(zf[:], pb[:, 0:128], zc[:], ALU.add, [pb_t, zc_t], [zf_t])
        yield
        S.op("pe", mm(pb[hs, 0:128], zf[:, 0:64], MrbT[:]), reads=[zf_t, MrbT_t], writes=[pb_t])
        dve_tt(RhT[hs, :], pb[hs, 0:128], Rh, ALU.add, [pb_t, Rt_t], [RhT_t])
        yield
        S.op("pe", mm(pb[hs, 0:128], zf[:, 64:128], MrbT[:], True, False), reads=[zf_t, MrbT_t], writes=[pb_t])
        S.op("pe", mm(pb[hs, 0:128], TOK[:, 3, hs], MrkT[:], False, True), reads=[TOK_t, MrkT_t], writes=[pb_t])
        copy_out(YhT[hs, :], pb[hs, 0:128], [pb_t], [YhT_t], eng="act")
        cmb = cv[:, 16:20].unsqueeze(2).broadcast_to([128, 4, 64])
        dve_tt(MZ[:], zf[:, 0:64].unsqueeze(1).broadcast_to([128, 4, 64]), cmb, ALU.mult, [zf_t, cv_t], [MZ_t])
        dve_tt(MB[:], TOK[:, 1, hs].unsqueeze(1).broadcast_to([128, 4, 64]), cmb, ALU.mult, [TOK_t, cv_t], [MB_t], eng="pool")
        dve_tt(MK[:], TOK[:, 2, hs].unsqueeze(1).broadcast_to([128, 4, 64]), cmb, ALU.mult, [TOK_t, cv_t], [MK_t], eng="pool")
        yield
        for c in range(4):
            S.op("pe", mm(ps[2][hs, c * 64:(c + 1) * 64], MZ[:, c, :], TOK[:, 1, hs]), reads=[MZ_t, TOK_t], writes=[ps_t[2]])
        for c in range(4):
            S.op("pe", mm(ps[3][hs, c * 64:(c + 1) * 64], MB[:, c, :], zf[:, 64:128], True, False), reads=[zf_t, MB_t], writes=[ps_t[3]])
            S.op("pe", mm(ps[3][hs, c * 64:(c + 1) * 64], MK[:, c, :], TOK[:, 3, hs], False, True), reads=[TOK_t, MK_t], writes=[ps_t[3]])
        yield

    def rwkv_group(g):
        t0 = g * G1
        r_, k_, v_, xw_, xa_, xg0_, xg1_ = range(7)
        rows = [128, 128, 128, 96, 96, 128, 128]
        for b in range(7):
            n = rows[b]
            pbuf, pbt = PB[b]
            sh, sht = SH[b]
            dve_ts(tmpA[0:n, :], pbuf[0:n, 0:G1], cv[0:n, b:b + 1], None, ALU.mult, None, [pbt, cv_t], [tmpA_t])
            dve_stt(sh[0:n, :], pbuf[0:n, 1:G1 + 1], omm[0:n, b:b + 1], tmpA[0:n, :], ALU.mult, ALU.add, [pbt, omm_t, tmpA_t], [sht])
            S.op("pool", lambda e, pbuf=pbuf, n=n: e.tensor_copy(out=pbuf[0:n, 0:1], in_=pbuf[0:n, G1:G1 + 1]), reads=[pbt], writes=[pbt])
            if b % 2:
                yield
        shr, shr_t = SH[r_]; shk, shk_t = SH[k_]; shv, shv_t = SH[v_]
        act(tmpB[0:96, :], SH[xw_][0][0:96, :], AF.Tanh, [SH[xw_][1]], [tmpB_t])
        pb, pb_t = scr()
        S.op("pe", mm(pb[:, 0:G1], wdu[:, :], tmpB[0:96, :]), reads=[wdu_t, tmpB_t], writes=[pb_t])
        act(logw[:], pb[:, 0:G1], AF.Sigmoid, [pb_t, cv_t], [logw_t], bias=cv[:, 7:8])
        dve_ts(logw[:], logw[:], -0.6065306597126334, None, ALU.mult, None, [logw_t], [logw_t])
        pb, pb_t = scr()
        S.op("pe", mm(pb[:, 0:G1], wiu[:, :], SH[xa_][0][0:96, :]), reads=[wiu_t, SH[xa_][1]], writes=[pb_t])
        act(av[:], pb[:, 0:G1], AF.Sigmoid, [pb_t, cv_t], [av_t], bias=cv[:, 8:9])
        act(SH[xg0_][0][:], SH[xg0_][0][:], AF.Sigmoid, [SH[xg0_][1]], [SH[xg0_][1]])
        act(SH[xg1_][0][:], SH[xg1_][0][:], AF.Sigmoid, [SH[xg1_][1]], [SH[xg1_][1]])
        yield
        pb, pb_t = scr()
        S.op("pe", mm(pb[:, 0:G1], wgu[:, 0, :], SH[xg0_][0][:], True, False), reads=[wgu_t, SH[xg0_][1]], writes=[pb_t])
        S.op("pe", mm(pb[:, 0:G1], wgu[:, 1, :], SH[xg1_][0][:], False, True), reads=[wgu_t, SH[xg1_][1]], writes=[pb_t])
        copy_out(gg[:], pb[:, 0:G1], [pb_t], [gg_t], eng="dve")
        dve_ts(kkn[:], shk[:], cv[:, 9:10], None, ALU.mult, None, [shk_t, cv_t], [kkn_t])
        dve_tt(tmpA[:], kkn[:], kkn[:], ALU.mult, [kkn_t], [tmpA_t], eng="pool")
        pb, pb_t = scr()
        S.op("pe", mm(pb[:, 0:G1], onesblk, tmpA[:]), reads=[cst_t, tmpA_t], writes=[pb_t])
        act(tmpB[:], pb[:, 0:G1], AF.Sqrt, [pb_t], [tmpB_t])
        dve_ts(tmpB[:], tmpB[:], 1e-12, None, ALU.max, None, [tmpB_t], [tmpB_t])
        S.op("dve", lambda e: e.reciprocal(out=tmpB[:], in_=tmpB[:]), reads=[tmpB_t], writes=[tmpB_t])
        dve_tt(kkn[:], kkn[:], tmpB[:], ALU.mult, [kkn_t, tmpB_t], [kkn_t])
        yield
        dve_ts(tmpA[:], av[:], cv[:, 10:11], cv[:, 14:15], ALU.mult, ALU.add, [av_t, cv_t], [tmpA_t])
        dve_tt(k2[:], shk[:], tmpA[:], ALU.mult, [shk_t, tmpA_t], [k2_t])
        dve_tt(tmpA[:], shr[:], k2[:], ALU.mult, [shr_t, k2_t], [tmpA_t], eng="pool")
        dve_ts(tmpA[:], tmpA[:], cv[:, 11:12], None, ALU.mult, None, [tmpA_t, cv_t], [tmpA_t])
        pb, pb_t = scr()
        S.op("pe", mm(pb[:, 0:G1], onesblk, tmpA[:]), reads=[cst_t, tmpA_t], writes=[pb_t])
        dve_tt(bonus[:], pb[:, 0:G1], shv[:], ALU.mult, [pb_t, shv_t], [bonus_t])
        S.op("dve", lambda e: e.tensor_tensor_scan(out=cum[:], data0=rmask[:], data1=logw[:], initial=0.0, op0=ALU.mult, op1=ALU.add),
             reads=[rmask_t, logw_t], writes=[cum_t])
        yield
        act(Pm[:], cum[:], AF.Exp, [cum_t], [Pm_t])
        act(Pinv[:], cum[:], AF.Exp, [cum_t], [Pinv_t], scale=-1.0)
        dve_tt(tmpA[:], cum[:], logw[:], ALU.subtract, [cum_t, logw_t], [tmpA_t], eng="pool")
        act(Pprev[:], tmpA[:], AF.Exp, [tmpA_t], [Pprev_t])
        dve_stt(At[:], kkn[:], -1.0, Pprev[:], ALU.mult, ALU.mult, [kkn_t, Pprev_t], [At_t])
        dve_tt(tmpB[:], kkn[:], av[:], ALU.mult, [kkn_t, av_t], [tmpB_t], eng="pool")
        dve_tt(Bt[:], tmpB[:], Pinv[:], ALU.mult, [tmpB_t, Pinv_t], [Bt_t])
        dve_tt(Kt[:], k2[:], Pinv[:], ALU.mult, [k2_t, Pinv_t], [Kt_t], eng="pool")
        dve_tt(Rt[:], shr[:], Pm[:], ALU.mult, [shr_t, Pm_t], [Rt_t])
        S.op("pool", lambda e: e.tensor_copy(out=vb16[:], in_=shv[:]), reads=[shv_t], writes=[vb16_t])
        yield
        for tl in range(G1 // 128):
            cs = slice(tl * 128, (tl + 1) * 128)
            for j, (src, srct) in enumerate([(At, At_t), (Bt, Bt_t), (Kt, Kt_t), (vb16, vb16_t)]):
                S.op("pe", lambda e, j=j, src=src: e.transpose(out=psb[0][:, j * 128:(j + 1) * 128], in_=src[:, cs], identity=identb[:]),
                     reads=[srct, identb_t], writes=[psb_t[0]])
            copy_out(TOK[:].rearrange("p a b -> p (a b)"), psb[0][:, 0:512], [psb_t[0]], [TOK_t], eng="act")
            yield
            chains = [head_chain(0, cs), head_chain(1, cs)]
            while chains:
                for ch in list(chains):
                    try:
                        next(ch)
                    except StopIteration:
                        chains.remove(ch)
                yield
            dve_tt(MT[:], ps[2][:, 0:256].rearrange("p (c k) -> p c k", c=4), I64.unsqueeze(1).broadcast_to([128, 4, 64]), ALU.add,
                   [ps_t[2], cst_t], [MT_t])
            for c in range(4):
                col = tl * 128 + 32 * c + 31
                dve_ts(HP[:, c, :], ps[3][:, c * 64:(c + 1) * 64], Pm[:, col:col + 1], None, ALU.mult, None, [ps_t[3], Pm_t], [HP_t])
            yield
            for c in range(4):
                col = tl * 128 + 32 * c + 31
                sc_, sc_t = Sst[scur[0]]
                sn_, sn_t = Sst[1 - scur[0]]
                for h in range(2):
                    hs = slice(64 * h, 64 * h + 64)
                    S.op("pe", mm(ps[0][hs, 0:64], MT[hs, c, :], sc_[hs, :]), reads=[MT_t, sc_t], writes=[ps_t[0]])
                for h in range(2):
                    hs = slice(64 * h, 64 * h + 64)
                    S.op("pe", mm(ps[1][hs, 32 * c:32 * c + 32], sc_[hs, :], RhT[hs, 32 * c:32 * c + 32]), reads=[sc_t, RhT_t], writes=[ps_t[1]])
                dve_stt(sn_[:], ps[0][:, 0:64], Pm[:, col:col + 1], HP[:, c, :], ALU.mult, ALU.add, [ps_t[0], Pm_t, HP_t], [sn_t])
                scur[0] = 1 - scur[0]
                yield
            dve_tt(yT[:, cs], ps[1][:, 0:128], YhT[:], ALU.add, [ps_t[1], YhT_t], [yT_t])
            yield
        pb, pb_t = scr()
        S.op("pe", mm(pb[:, 0:G1], onesblk, yT[:]), reads=[cst_t, yT_t], writes=[pb_t])
        act(tmpA[:], pb[:, 0:G1], AF.Copy, [pb_t], [tmpA_t], scale=1.0 / 64)
        act(tmpB[:], yT[:], AF.Square, [yT_t], [tmpB_t])
        pb, pb_t = scr()
        S.op("pe", mm(pb[:, 0:G1], onesblk, tmpB[:]), reads=[cst_t, tmpB_t], writes=[pb_t])
        dve_tt(tmpB[:], tmpA[:], tmpA[:], ALU.mult, [tmpA_t], [tmpB_t], eng="pool")
        dve_stt(tmpB[:], pb[:, 0:G1], 1.0 / 64, tmpB[:], ALU.mult, ALU.subtract, [pb_t, tmpB_t], [tmpB_t])
        yield
        act(tmpB[:], tmpB[:], AF.Sqrt, [tmpB_t], [tmpB_t], bias=64e-5)
        S.op("dve", lambda e: e.reciprocal(out=tmpB[:], in_=tmpB[:]), reads=[tmpB_t], writes=[tmpB_t])
        dve_tt(yo[:], yT[:], tmpA[:], ALU.subtract, [yT_t, tmpA_t], [yo_t])
        dve_tt(yo[:], yo[:], tmpB[:], ALU.mult, [yo_t, tmpB_t], [yo_t])
        dve_ts(yo[:], yo[:], cv[:, 12:13], cv[:, 13:14], ALU.mult, ALU.add, [yo_t, cv_t], [yo_t])
        dve_tt(yo[:], yo[:], bonus[:], ALU.add, [yo_t, bonus_t], [yo_t], eng="pool")
        dve_tt(yo[:], yo[:], gg[:], ALU.mult, [yo_t, gg_t], [yo_t])
        S.dma("sp", "o", lambda e: e.dma_start(out=yaT[:, t0:t0 + G1], in_=yo[:]), reads=[yo_t], writes=[])
        yield

    def attn_group(g):
        for m in range(2):
            ms = slice(64 * m, 64 * m + 64)
            nsh, nsh_t = nshift[m]
            act(sqq[:], qmax2[m][0][:], AF.Sqrt, [qmax2[m][1], kmax2[m][1]], [sqq_t], scale=kmax2[m][0][:, 0:1])
            dve_ts(nsh[:], sqq[:], -0.125, None, ALU.mult, None, [sqq_t], [nsh_t])

            def stage_a(j):
                P_, P_t = Pb[j % 2]
                if j < g:
                    for a in range(2):
                        kt = 2 * j + a
                        S.op("pe", mm(ps[5][:, a * 256:(a + 1) * 256], KT[ms, kt * 128:(kt + 1) * 128], QT[ms, :]), reads=[QT_t, KT_t], writes=[ps_t[5]])
                    act(P_[:], ps[5][:, :], AF.Exp, [ps_t[5], nsh_t], [P_t], scale=0.125, bias=nsh[:, 0:1])
                else:
                    kt = 2 * g
                    S.op("pe", mm(ps[5][:, 0:256], KT[ms, kt * 128:(kt + 1) * 128], QT[ms, :]), reads=[QT_t, KT_t], writes=[ps_t[5]])
                    S.op("pe", mm(ps[5][:, 384:512], KT[ms, (kt + 1) * 128:(kt + 2) * 128], QT[ms, 128:256]), reads=[QT_t, KT_t], writes=[ps_t[5]])
                    act(P_[:, 0:256], ps[5][:, 0:256], AF.Exp, [ps_t[5], nsh_t], [P_t], scale=0.125, bias=nsh[:, 0:1])
                    act(P_[:, 384:512], ps[5][:, 384:512], AF.Exp, [ps_t[5], nsh_t], [P_t], scale=0.125, bias=nsh[:, 0:1])
                    dve_tt(P_[:, 0:128], P_[:, 0:128], trib[:], ALU.mult, [P_t, trib_t], [P_t], eng="pool")
                    dve_tt(P_[:, 384:512], P_[:, 384:512], trib[:], ALU.mult, [P_t, trib_t], [P_t], eng="pool")

            def stage_b(j):
                P_, P_t = Pb[j % 2]
                if j < g:
                    for a in range(2):
                        kt = 2 * j + a
                        for tl in range(2):
                            ob = OB[tl]
                            S.op("pe", mm(ps[ob][:, 0:129], P_[:, a * 256 + tl * 128:a * 256 + (tl + 1) * 128], VA[:, kt, 0:129], kt == 0, False),
                                 reads=[P_t, VA_t], writes=[ps_t[ob]])
                else:
                    kt = 2 * g
                    S.op("pe", mm(ps[OB[0]][:, 0:129], P_[:, 0:128], VA[:, kt, 0:129], kt == 0, True), reads=[P_t, VA_t], writes=[ps_t[OB[0]]])
                    S.op("pe", mm(ps[OB[1]][:, 0:129], P_[:, 128:256], VA[:, kt, 0:129], kt == 0, False), reads=[P_t, VA_t], writes=[ps_t[OB[1]]])
                    S.op("pe", mm(ps[OB[1]][:, 0:129], P_[:, 384:512], VA[:, kt + 1, 0:129], False, True), reads=[P_t, VA_t], writes=[ps_t[OB[1]]])

            stage_a(0)
            for j in range(g + 1):
                if j + 1 <= g:
                    stage_a(j + 1)
                stage_b(j)
                yield
            for tl in range(2):
                ob = OB[tl]
                S.op("dve", lambda e, ob=ob: e.reciprocal(out=rs_[:], in_=ps[ob][:, 128:129]), reads=[ps_t[ob]], writes=[rs_t])
                dve_ts(om[tl][m][0][:], ps[ob][:, 0:128], rs_[:, 0:1], None, ALU.mult, None, [ps_t[ob], rs_t], [om[tl][m][1]])
            yield
        for tl in range(2):
            qt = 2 * g + tl
            dve_stt(attn[:], om[tl][1][0][:], neglam[:, 0:1], om[tl][0][0][:], ALU.mult, ALU.add, [om[tl][0][1], om[tl][1][1], neglam_t], [attn_t])
            S.op("act", lambda e: e.activation(out=junk2[:], in_=attn[:], func=AF.Square, accum_out=ss2[:]), reads=[attn_t], writes=[junk2_t, ss2_t])
            S.op("act", lambda e: e.activation(out=ss2[:], in_=ss2[:], func=AF.Sqrt, scale=1.0 / 128, bias=1e-5), reads=[ss2_t], writes=[ss2_t])
            S.op("dve", lambda e: e.reciprocal(out=rstd2[:], in_=ss2[:]), reads=[ss2_t], writes=[rstd2_t])
            dve_stt(ybo[:], attn[:], rstd2[:, 0:1], sgv[:], ALU.mult, ALU.mult, [attn_t, rstd2_t, sgv_t], [ybo_t])
            S.dma("sp", "o", lambda e, qt=qt: e.dma_start(out=yb[qt * 128:(qt + 1) * 128, :], in_=ybo[:]), reads=[ybo_t], writes=[])
            yield

    for g in range(ngroups):
        t0 = g * G1
        for tl in range(2):
            xb, xb_t = xt[tl]
            S.dma("sp", "x", lambda e, tl=tl, xb=xb: e.dma_start(out=xb[:], in_=x[t0 + tl * 128:t0 + (tl + 1) * 128, :]), writes=[xb_t])
            _rms_rstd(S, "act", xb[:], xb_t, junk[:], junk_t, ss[:], ss_t, rstd[:], rstd_t, 1e-6)
            dve_ts(xnb[:], xb[:], rstd[:, 0:1], None, ALU.mult, None, [xb_t, rstd_t], [xnb_t])
            for half in range(2):
                for j in range(8):
                    kc = half * 8 + j
                    S.op("pe", lambda e, kc=kc, j=j: e.transpose(out=psb[0][:, j * 128:(j + 1) * 128], in_=xnb[:, kc * 128:(kc + 1) * 128], identity=identb[:]),
                         reads=[xnb_t, identb_t], writes=[psb_t[0]])
                copy_out(xnT[:, half * 8:(half + 1) * 8, tl * 128:(tl + 1) * 128], psb[0][:].rearrange("p (k t) -> p k t", k=8), [psb_t[0]], [xnT_t])
        blocks = [(C_R, 128), (C_K, 128), (C_V, 128), (C_XW, 96), (C_XA, 96), (C_XG, 128), (C_XG + 128, 128)]
        for bi, (c0, w) in enumerate(blocks):
            pb, pb_t = scr()
            for kc in range(16):
                S.op("pe", mm(pb[0:w, 0:G1], W1[:, kc, c0:c0 + w], xnT[:, kc, :], kc == 0, kc == 15), reads=[W1_t, xnT_t], writes=[pb_t])
            copy_out(PB[bi][0][0:w, 1:G1 + 1], pb[0:w, 0:G1], [pb_t], [PB[bi][1]])
        pb, pb_t = scr()
        for kc in range(16):
            S.op("pe", mm(pb[:, 0:G1], W1[:, kc, C_QD:C_QD + 128], xnT[:, kc, :], kc == 0, kc == 15), reads=[W1_t, xnT_t], writes=[pb_t])
        act(QT[:], pb[:, 0:G1], AF.Copy, [pb_t], [QT_t])
        act(QSQ[:], pb[:, 0:G1], AF.Square, [pb_t], [QSQ_t])
        pb, pb_t = scr()
        for kc in range(16):
            S.op("pe", mm(pb[:, 0:G1], W1[:, kc, C_KD:C_KD + 128], xnT[:, kc, :], kc == 0, kc == 15), reads=[W1_t, xnT_t], writes=[pb_t])
        act(KT[:, t0:t0 + G1], pb[:, 0:G1], AF.Copy, [pb_t], [KT_t])
        act(KSQ[:], pb[:, 0:G1], AF.Square, [pb_t], [KSQ_t])
        for m in range(2):
            ms = slice(64 * m, 64 * m + 64)
            pb, pb_t = scr()
            S.op("pe", mm(pb[:, 0:G1], ones128[ms, :], KSQ[ms, :]), reads=[ones_t, KSQ_t], writes=[pb_t])
            S.op("dve", lambda e, pb=pb: e.tensor_reduce(out=kred[:], in_=pb[:, 0:G1], axis=AX.X, op=ALU.max), reads=[pb_t], writes=[kred_t])
            dve_tt(kmax2[m][0][:], kmax2[m][0][:], kred[:], ALU.max, [kred_t, kmax2[m][1]], [kmax2[m][1]])
            pb, pb_t = scr()
            S.op("pe", mm(pb[:, 0:G1], ones128[ms, :], QSQ[ms, :]), reads=[ones_t, QSQ_t], writes=[pb_t])
            S.op("dve", lambda e, pb=pb: e.tensor_reduce(out=kred[:], in_=pb[:, 0:G1], axis=AX.X, op=ALU.max), reads=[pb_t], writes=[kred_t])
            dve_tt(qmax2[m][0][:], qmax2[m][0][:], kred[:], ALU.max, [kred_t, qmax2[m][1]], [qmax2[m][1]])
        for tl in range(2):
            pb, pb_t = scr()
            for kc in range(16):
                S.op("pe", mm(pb[:, 0:128], xnT[:, kc, tl * 128:(tl + 1) * 128], W1[:, kc, C_VD:C_VD + 128], kc == 0, kc == 15),
                     reads=[W1_t, xnT_t], writes=[pb_t])
            copy_out(VA[:, 2 * g + tl, 0:128], pb[:, 0:128], [pb_t], [VA_t])
        gens = []
        if do_rwkv:
            gr = rwkv_group(g)
            if rw_stage < 9000:
                def lim(gr=gr):
                    for _ in range(rw_stage):
                        next(gr)
                        yield
                gr = lim()
            gens.append(gr)
        if do_attn:
            gens.append(attn_group(g))
        _roundrobin(gens)
    k = "dma:o"
    if k not in S.dsem:
        S.dma("sp", "o", lambda e: e.dma_start(out=yaT[:, 0:G1], in_=yo[:]), reads=[yo_t], writes=[])
    nc.sync.wait_ge(S.dsem[k][0], S.dsem[k][1])
    return nc, S


def _consts():
    t = np.arange(128)
    same = (t[:, None] // CHK) == (t[None, :] // CHK)
    MU = (same & (t[:, None] < t[None, :])).astype(np.float32)
    ML = MU.T.copy()
    MUI = (same & (t[:, None] <= t[None, :])).astype(np.float32)
    TRI = (t[:, None] <= t[None, :]).astype(np.float32)
    ob = ((t[:, None] // 64) == (t[None, :] // 64)).astype(np.float32)
    i64 = np.zeros((128, 128), np.float32)
    i64[t, t % 64] = 1.0
    return np.stack([np.eye(128, dtype=np.float32), MU, ML, MUI, TRI, ob, i64])


def prep_l1(inp, c, ntok=SEQ):
    w_in = inp["w_in"][0]
    hs = slice(128 * c, 128 * c + 128)
    o_d = 3520
    cols = np.concatenate([np.arange(128 * c, 128 * c + 128), 1024 + np.arange(128 * c, 128 * c + 128),
                           2048 + np.arange(128 * c, 128 * c + 128), np.arange(3072, 3520),
                           o_d + np.arange(128 * c, 128 * c + 128), o_d + 1024 + np.arange(128 * c, 128 * c + 128),
                           o_d + 2048 + np.arange(128 * c, 128 * c + 128)])
    mu = inp["shift_mu"][0]
    cvec = np.zeros((128, 20), np.float32)
    cvec[:, 0] = mu[0:1024][hs]; cvec[:, 1] = mu[1024:2048][hs]; cvec[:, 2] = mu[2048:3072][hs]
    cvec[:96, 3] = mu[3072:3168]; cvec[:96, 4] = mu[3168:3264]; cvec[:, 5] = mu[3264:3392]; cvec[:, 6] = mu[3392:3520]
    cvec[:, 7] = inp["rwkv_w0"][0][hs]; cvec[:, 8] = inp["rwkv_a0"][0][hs]; cvec[:, 9] = inp["k_k"][0][hs]
    cvec[:, 10] = inp["k_a"][0][hs]; cvec[:, 11] = inp["r_k"][0].reshape(-1)[hs]
    cvec[:, 12] = inp["lnx_g"][0][hs]; cvec[:, 13] = inp["lnx_b"][0][hs]
    for c_ in range(4):
        cvec[32 * c_:32 * c_ + 32, 16 + c_] = 1.0
    rm = np.ones((1, G1), np.float32); rm[0, ::CHK] = 0.0
    return dict(
        x=np.ascontiguousarray(inp["x"][0, :ntok]), w1=np.ascontiguousarray(w_in[:, cols]), cvec=cvec,
        wdu=np.ascontiguousarray(inp["w_decay_up"][0][:, hs]), wiu=np.ascontiguousarray(inp["w_iclr_up"][0][:, hs]),
        wgu=np.ascontiguousarray(inp["w_gate_up"][0][:, hs]),
        lamv=np.concatenate([inp["lam_q1"][0], inp["lam_k1"][0], inp["lam_q2"][0], inp["lam_k2"][0]])[None, :].astype(np.float32),
        sublng=inp["subln_g"][0][None, :].astype(np.float32), g1=inp["norm1_g"][0][None, :].astype(np.float32),
        consts=_consts(), rmask=rm)


def _blk(w, nb):
    K_ = w.shape[0]
    return np.ascontiguousarray(w.reshape(K_ // 128, 128, nb, 512).transpose(2, 1, 0, 3).reshape(nb, 128, (K_ // 128) * 512))


W_BLOCKS = (("s_wg", 8), ("s_wab", 4), ("s_wo", 4), ("s_wq", 4), ("s_u", 32), ("s_v", 32))


def l2_weight_blocks(inp):
    w_in = inp["w_in"][0]
    wgb = _blk(w_in[:, 6592:], 8)
    wabb = np.concatenate([_blk(inp["w_proj_a"][0], 4), _blk(inp["w_proj_b"][0], 4)], axis=2)
    uTb = _blk(np.ascontiguousarray(inp["peer_u"][0].T), 32)
    vtb = inp["peer_v"][0].reshape(32, 4, 128, D).transpose(0, 2, 1, 3).reshape(32, 128, 4 * D)
    allb = np.zeros((NCAST * NCORES, 128, 8192), np.float32)
    o = 0
    for a in (wgb, wabb, _blk(inp["w_out"][0], 4), _blk(inp["peer_wq"][0], 4), uTb, vtb):
        allb[o:o + a.shape[0]] = a
        o += a.shape[0]
    return allb


def l2_shared(inp, cast_all):
    m = dict(
        skT=np.ascontiguousarray(inp["peer_sub_keys"][0].reshape(16, 128, 128).transpose(0, 2, 1)),
        gv=np.stack([inp["norm1_g"][0], inp["norm2_g"][0], inp["final_g"]]).astype(np.float32),
        ident=np.eye(128, dtype=np.float32))
    o = 0
    for name, nb in W_BLOCKS:
        m[name] = np.ascontiguousarray(cast_all[o:o + nb])
        o += nb
    return m


def prep_l2(inp, c, yaT_full, ybT_full, shared):
    ts = slice(TOK * c, TOK * (c + 1))
    m = dict(shared)
    m["x"] = np.ascontiguousarray(inp["x"][0, ts])
    m["yaT"] = np.ascontiguousarray(yaT_full[:, ts])
    m["ybT"] = np.ascontiguousarray(ybT_full[:, ts])
    return m


def kernel(**inputs):
    inp = {k: np.asarray(v) for k, v in inputs.items()}
    nc1, _ = build_l1()
    allb = l2_weight_blocks(inp)
    maps1 = [prep_l1(inp, c) for c in range(NCORES)]
    for c in range(NCORES):
        maps1[c]["castin"] = allb[NCAST * c:NCAST * (c + 1)]
    r1 = run_bass_kernel_spmd(nc1, maps1, core_ids=list(range(NCORES))).results
    del maps1, allb
    cast_all = np.concatenate([r1[c]["castout"] for c in range(NCORES)], axis=0)
    yaT_full = np.concatenate([r1[c]["yaT"] for c in range(NCORES)], axis=0)
    ybT_full = np.concatenate([r1[c]["yb"].T for c in range(NCORES)], axis=0)
    w_in = inp["w_in"][0]
    shared = l2_shared(inp, cast_all)
    nc2, _ = build_l2()
    maps2 = [prep_l2(inp, c, yaT_full, ybT_full, shared) for c in range(NCORES)]
    r2 = run_bass_kernel_spmd(nc2, maps2, core_ids=list(range(NCORES))).results
    out = np.concatenate([r2[c]["y"] for c in range(NCORES)], axis=0)
    return out.reshape(1, SEQ, D).astype(np.float32)
```

```python
import numpy as np
import concourse.bass as bass
import concourse.mybir as mybir
from concourse.bass_utils import run_bass_kernel_spmd

F32 = mybir.dt.float32
BF16 = mybir.dt.bfloat16
AF = mybir.ActivationFunctionType
ALU = mybir.AluOpType
AX = mybir.AxisListType

NCORES = 8
D = 2048
SEQ = 16384
TOK = SEQ // NCORES
NEG = -1.0e30


class Tok:
    __slots__ = ("w", "r")

    def __init__(self):
        self.w = None
        self.r = []


class Sync:
    def __init__(self, nc):
        self.nc = nc
        self.engs = {"pe": nc.tensor, "act": nc.scalar, "dve": nc.vector, "pool": nc.gpsimd, "sp": nc.sync}
        self.sem = {k: nc.alloc_semaphore("s_" + k) for k in ("pe", "act", "dve", "pool")}
        self.cnt = {k: 0 for k in self.sem}
        self.waited = {e: {} for e in self.engs}
        self.dsem = {}
        self.ninst = 0

    def _deps(self, reads, writes):
        deps = {}
        for b in reads:
            if b.w is not None:
                k, v = b.w
                deps[k] = max(deps.get(k, 0), v)
        for b in writes:
            if b.w is not None:
                k, v = b.w
                deps[k] = max(deps.get(k, 0), v)
            for (k, v) in b.r:
                deps[k] = max(deps.get(k, 0), v)
        return deps

    def _wait(self, ek, deps):
        eng = self.engs[ek]
        wd = self.waited[ek]
        for k, v in deps.items():
            if k == ek and ek == "pe":
                continue
            if k.startswith("dma:"):
                v = self.dsem[k][1]
                s = self.dsem[k][0]
            else:
                s = self.sem[k]
            if wd.get(k, 0) >= v:
                continue
            eng.wait_ge(s, v)
            wd[k] = v
            self.ninst += 1

    def _mark(self, ev, reads, writes):
        for b in reads:
            b.r.append(ev)
        for b in writes:
            b.w = ev
            b.r = []

    def op(self, ek, fn, reads=(), writes=()):
        self._wait(ek, self._deps(reads, writes))
        inst = fn(self.engs[ek])
        self.cnt[ek] += 1
        inst.then_inc(self.sem[ek], 1)
        self.ninst += 1
        self._mark((ek, self.cnt[ek]), reads, writes)

    def dma(self, qk, stream, fn, reads=(), writes=()):
        self._wait(qk, self._deps(reads, writes))
        inst = fn(self.engs[qk])
        k = "dma:" + stream
        if k not in self.dsem:
            self.dsem[k] = [self.nc.alloc_semaphore("d_" + stream), 0]
        self.dsem[k][1] += 16
        inst.then_inc(self.dsem[k][0], 16)
        self.ninst += 1
        self._mark((k, self.dsem[k][1]), reads, writes)

    def finish(self, toks):
        deps = self._deps(toks, ())
        self._wait("sp", deps)


def _rms_rstd(S, ek_sq, src_ap, src_tok, junk, junk_tok, ss, ss_tok, rstd, rstd_tok, eps):
    S.op("act", lambda e: e.activation(out=junk, in_=src_ap, func=AF.Square, accum_out=ss),
         reads=[src_tok], writes=[junk_tok, ss_tok])
    S.op("act", lambda e: e.activation(out=ss, in_=ss, func=AF.Sqrt, scale=1.0 / D, bias=float(eps)),
         reads=[ss_tok], writes=[ss_tok])
    S.op("dve", lambda e: e.reciprocal(out=rstd, in_=ss), reads=[ss_tok], writes=[rstd_tok])


CH = 256
NT = CH // 128


def build_l2(nchunks=TOK // CH, peer_blocks=32, stage=99):
    nc = bass.Bass("TRN2", target_bir_lowering=False)
    ntok = nchunks * CH
    dr = lambda n, s, k="ExternalInput": nc.dram_tensor(n, s, F32, kind=k).ap()
    x = dr("x", [ntok, D])
    yaT = dr("yaT", [1024, ntok])
    ybT = dr("ybT", [1024, ntok])
    bfi = lambda n, nb: nc.dram_tensor(n, [nb, 128, 8192], BF16, kind="ExternalInput").ap()
    s_wg, s_wab, s_wo, s_wq, s_u, s_v = bfi("s_wg", 8), bfi("s_wab", 4), bfi("s_wo", 4), bfi("s_wq", 4), bfi("s_u", 32), bfi("s_v", 32)
    skT = dr("skT", [16, 128, 128])
    gv = dr("gv", [3, D])
    ident_d = dr("ident", [128, 128])
    y = dr("y", [ntok, D], "ExternalOutput")

    S = Sync(nc)
    sb = lambda n, s, dt=F32: nc.alloc_sbuf_tensor(n, s, dt)
    ps = [nc.alloc_psum_tensor("ps%d" % i, [128, 512], F32) for i in range(6)]
    psb = [nc.alloc_psum_tensor("psb%d" % i, [128, 1024], BF16) for i in range(2)]
    ps_t = [Tok() for _ in range(6)]
    psb_t = [Tok() for _ in range(2)]

    gvec = sb("gvec", [128, D]); gvec_t = Tok()
    xh = sb("xh", [128, NT, D]); xh_t = [Tok() for _ in range(NT)]
    R1 = sb("R1", [128, 16, CH], BF16); R1_t = Tok()
    R2 = sb("R2", [128, 16, CH], BF16); R2_t = Tok()
    R3 = sb("R3", [128, 16, CH], BF16); R3_t = Tok()
    WS = [sb("WS%d" % i, [128, 16, 512], BF16) for i in range(3)]; WS_t = [Tok() for _ in range(3)]
    VS = [sb("VS%d" % i, [128, 4, D], BF16) for i in range(2)]; VS_t = [Tok() for _ in range(2)]
    acc = sb("acc", [128, NT, D]); acc_t = [Tok() for _ in range(NT)]
    sc = sb("sc", [128, NT, 16, 128]); sc_t = [Tok() for _ in range(NT)]
    xnb = sb("xnb", [128, D], BF16); xnb_t = Tok()
    junk, junk_t = xnb, xnb_t
    ident = sb("identb", [128, 128], BF16); ident_t = Tok()
    identf = sb("identf", [128, 128]); identf_t = Tok()
    skb = sb("skb", [128, 16, 128], BF16); skb_t = Tok()
    ss = sb("ss", [128, 1]); ss_t = Tok()
    rstd = sb("rstd", [128, 1]); rstd_t = Tok()
    sg = [sb("sg%d" % i, [128, CH], BF16) for i in range(2)]; sg_t = [Tok() for _ in range(2)]
    mm = [sb("mm%d" % i, [128, CH], BF16) for i in range(2)]; mm_t = [Tok() for _ in range(2)]
    top = sb("top", [128, 1, 16, 16]); top_t = [Tok()] * NT
    tmpk = sb("tmpk", [128, 256]); tmpk_t = Tok()
    cand = sb("cand", [128, 256]); cand_t = Tok()
    best = sb("best", [128, NT, 8, 16]); best_t = [Tok() for _ in range(NT)]
    negmx = sb("negmx", [128, NT, 8]); negmx_t = [Tok() for _ in range(NT)]
    zz = sb("zz", [128, NT, 8]); zz_t = [Tok() for _ in range(NT)]
    nbias = sb("nbias", [128, NT, 8]); nbias_t = [Tok() for _ in range(NT)]
    ebuf = sb("ebuf", [128, 16]); ebuf_t = Tok()
    Ab = [sb("Ab%d" % i, [128, 512], BF16) for i in range(2)]; Ab_t = [Tok() for _ in range(2)]
    Wc = [sb("Wc%d" % i, [128, 512], BF16) for i in range(2)]; Wc_t = [Tok() for _ in range(2)]
    Tb = [sb("Tb%d" % i, [128, 512]) for i in range(3)]; Tb_t = [Tok() for _ in range(3)]
    Eb = [sb("Eb%d" % i, [128, 512]) for i in range(3)]; Eb_t = [Tok() for _ in range(3)]
    stg = [(sb("stg%d" % i, [128, 512]), Tok()) for i in range(2)]
    Wh = [sb("Wh%d" % i, [128, 512], BF16) for i in range(2)]; Wh_t = [Tok() for _ in range(2)]
    Wb = [sb("Wb%d" % i, [128, 512], BF16) for i in range(2)]; Wb_t = [Tok() for _ in range(2)]
    AW = [sb("AW%d" % i, [128, 512], BF16) for i in range(2)]; AW_t = [Tok() for _ in range(2)]
    AWT = [sb("AWT%d" % i, [128, 4, 128], BF16) for i in range(2)]; AWT_t = [Tok() for _ in range(2)]

    S.dma("sp", "c", lambda e: e.dma_start(out=identf[:], in_=ident_d[:, :]), writes=[identf_t])
    S.op("dve", lambda e: e.tensor_copy(out=ident[:], in_=identf[:]), reads=[identf_t], writes=[ident_t])
    S.dma("pool", "w", lambda e: e.dma_start(out=skb[:], in_=skT.rearrange("b d n -> d b n")), writes=[skb_t])

    def load_blk(dst_tile, dst_tok, name, b, scr):
        S.dma("sp", "ws", lambda e: e.dma_start(out=dst_tile[:].rearrange("p a b -> p (a b)"), in_=scr[b]),
              writes=[dst_tok])

    def norm_transpose(t, g_row, dstT, dstT_t, first):
        src = xh[:, t, :]
        _rms_rstd(S, "act", src, xh_t[t], junk[:], junk_t, ss[:], ss_t, rstd[:], rstd_t, 1e-6)
        S.op("dve", lambda e: e.scalar_tensor_tensor(out=xnb[:], in0=src, scalar=rstd[:, 0:1], in1=gvec[:],
                                                     op0=ALU.mult, op1=ALU.mult),
             reads=[xh_t[t], rstd_t, gvec_t], writes=[xnb_t])
        for half in range(2):
            pb = psb[half]
            for j in range(8):
                kc = half * 8 + j
                S.op("pe", lambda e, kc=kc, j=j, pb=pb: e.transpose(out=pb[:, j * 128:(j + 1) * 128],
                                                                    in_=xnb[:, kc * 128:(kc + 1) * 128], identity=ident[:]),
                     reads=[xnb_t, ident_t], writes=[psb_t[half]])
            S.op("act" if half == 0 else "dve",
                 (lambda e, half=half, pb=pb: e.activation(out=dstT[:, half * 8:(half + 1) * 8, t * 128:(t + 1) * 128],
                                                           in_=pb[:].rearrange("p (k t) -> p k t", k=8), func=AF.Copy))
                 if half == 0 else
                 (lambda e, half=half, pb=pb: e.tensor_copy(out=dstT[:, half * 8:(half + 1) * 8, t * 128:(t + 1) * 128],
                                                            in_=pb[:].rearrange("p (k t) -> p k t", k=8))),
                 reads=[psb_t[half]], writes=[dstT_t])

    for c in range(nchunks):
        t0 = c * CH
        for t in range(NT):
            S.dma("sp", "x", lambda e, t=t: e.dma_start(out=xh[:, t, :], in_=x[t0 + t * 128:t0 + (t + 1) * 128, :]),
                  writes=[xh_t[t]])
        S.dma("sp", "g", lambda e: e.dma_start(out=gvec[:], in_=gv[0:1, :].partition_broadcast(128)), writes=[gvec_t])
        S.dma("pool", "w", lambda e: e.dma_start(out=R2[:, 0:8, :], in_=yaT[:, t0:t0 + CH].rearrange("(k p) t -> p k t", p=128)),
              writes=[R2_t])
        S.dma("pool", "w", lambda e: e.dma_start(out=R2[:, 8:16, :], in_=ybT[:, t0:t0 + CH].rearrange("(k p) t -> p k t", p=128)),
              writes=[R2_t])
        for t in range(NT):
            norm_transpose(t, 0, R1, R1_t, True)
        for cg in range(4):
            cs = slice(cg * 512, (cg + 1) * 512)
            load_blk(WS[0], WS_t[0], "wg", cg, s_wg)
            load_blk(WS[1], WS_t[1], "wg", 4 + cg, s_wg)
            load_blk(WS[2], WS_t[2], "wab", cg, s_wab)
            for cb in range(4):
                cc = slice(cb * 128, (cb + 1) * 128)
                cidx = cg * 4 + cb
                for kc in range(16):
                    S.op("pe", lambda e, kc=kc, cc=cc: e.matmul(out=ps[0][:, 0:CH], lhsT=WS[0][:, kc, cc], rhs=R1[:, kc, :],
                                                                start=(kc == 0), stop=(kc == 15)),
                         reads=[WS_t[0], R1_t], writes=[ps_t[0]])
                for kc in range(16):
                    S.op("pe", lambda e, kc=kc, cc=cc: e.matmul(out=ps[1][:, 0:CH], lhsT=WS[1][:, kc, cc], rhs=R1[:, kc, :],
                                                                start=(kc == 0), stop=(kc == 15)),
                         reads=[WS_t[1], R1_t], writes=[ps_t[1]])
                for kc in range(8):
                    S.op("pe", lambda e, kc=kc, cc=cc: e.matmul(out=ps[2][:, 0:CH], lhsT=WS[2][:, kc, cc], rhs=R2[:, kc, :],
                                                                start=(kc == 0), stop=(kc == 7)),
                         reads=[WS_t[2], R2_t], writes=[ps_t[2]])
                for kc in range(8):
                    S.op("pe", lambda e, kc=kc, cc=cc: e.matmul(out=ps[3][:, 0:CH], lhsT=WS[2][:, 8 + kc, cc], rhs=R2[:, 8 + kc, :],
                                                                start=(kc == 0), stop=(kc == 7)),
                         reads=[WS_t[2], R2_t], writes=[ps_t[3]])
                for i in range(2):
                    S.op("act", lambda e, i=i: e.activation(out=sg[i][:], in_=ps[i][:, 0:CH], func=AF.Sigmoid),
                         reads=[ps_t[i]], writes=[sg_t[i]])
                for i in range(2):
                    S.op("dve", lambda e, i=i: e.tensor_tensor(out=mm[i][:], in0=sg[i][:], in1=ps[2 + i][:, 0:CH], op=ALU.mult),
                         reads=[sg_t[i], ps_t[2 + i]], writes=[mm_t[i]])
                S.op("pool", lambda e, cidx=cidx: e.tensor_tensor(out=R3[:, cidx, :], in0=mm[0][:], in1=mm[1][:], op=ALU.add),
                     reads=[mm_t[0], mm_t[1]], writes=[R3_t])
        for db in range(4):
            slot = db % 2
            load_blk(WS[slot], WS_t[slot], "wo", db, s_wo)
            for t in range(NT):
                pbk = 4 + (t % 2)
                for kc in range(16):
                    S.op("pe", lambda e, kc=kc, t=t, pbk=pbk, slot=slot: e.matmul(out=ps[pbk][:, :], lhsT=R3[:, kc, t * 128:(t + 1) * 128],
                                                                                 rhs=WS[slot][:, kc, :], start=(kc == 0), stop=(kc == 15)),
                         reads=[R3_t, WS_t[slot]], writes=[ps_t[pbk]])
                S.op("dve", lambda e, t=t, db=db, pbk=pbk: e.tensor_tensor(out=xh[:, t, db * 512:(db + 1) * 512],
                                                                         in0=xh[:, t, db * 512:(db + 1) * 512], in1=ps[pbk][:, :], op=ALU.add),
                     reads=[ps_t[pbk], xh_t[t]], writes=[xh_t[t]])
        if stage >= 2:
            S.dma("sp", "g", lambda e: e.dma_start(out=gvec[:], in_=gv[1:2, :].partition_broadcast(128)), writes=[gvec_t])
            for t in range(NT):
                norm_transpose(t, 1, R1, R1_t, False)
            for qg in range(4):
                slot = qg % 2
                load_blk(WS[slot], WS_t[slot], "wq", qg, s_wq)
                for qb in range(4):
                    blk = qg * 4 + qb
                    pbk = blk % 2
                    for kc in range(16):
                        S.op("pe", lambda e, kc=kc, qb=qb, pbk=pbk, slot=slot: e.matmul(out=ps[pbk][:, 0:CH], lhsT=WS[slot][:, kc, qb * 128:(qb + 1) * 128],
                                                                                       rhs=R1[:, kc, :], start=(kc == 0), stop=(kc == 15)),
                             reads=[WS_t[slot], R1_t], writes=[ps_t[pbk]])
                    S.op("act", lambda e, blk=blk, pbk=pbk: e.activation(out=R2[:, blk, :], in_=ps[pbk][:, 0:CH], func=AF.Copy),
                         reads=[ps_t[pbk]], writes=[R2_t])
            for t in range(NT):
                for g4 in range(4):
                    pbk = 2 + (g4 % 2)
                    for j in range(4):
                        blk = g4 * 4 + j
                        S.op("pe", lambda e, blk=blk, j=j, pbk=pbk, t=t: e.matmul(out=ps[pbk][:, j * 128:(j + 1) * 128],
                                                                                 lhsT=R2[:, blk, t * 128:(t + 1) * 128], rhs=skb[:, blk, :],
                                                                                 start=True, stop=True),
                             reads=[R2_t, skb_t], writes=[ps_t[pbk]])
                    S.op("act", lambda e, g4=g4, pbk=pbk, t=t: e.activation(out=sc[:, t, g4 * 4:(g4 + 1) * 4, :],
                                                                           in_=ps[pbk][:].rearrange("p (b n) -> p b n", b=4), func=AF.Copy),
                         reads=[ps_t[pbk]], writes=[sc_t[t]])
                for blk in range(16):
                    S.op("dve", lambda e, blk=blk, t=t: e.max(out=top[:, 0, blk, 0:8], in_=sc[:, t, blk, :]),
                         reads=[sc_t[t]], writes=[top_t[t]])
                    S.op("dve", lambda e, blk=blk, t=t: e.match_replace(out=tmpk[:, 0:128], in_to_replace=top[:, 0, blk, 0:8],
                                                                        in_values=sc[:, t, blk, :], imm_value=NEG),
                         reads=[sc_t[t], top_t[t]], writes=[tmpk_t])
                    S.op("dve", lambda e, blk=blk, t=t: e.max(out=top[:, 0, blk, 8:16], in_=tmpk[:, 0:128]),
                         reads=[tmpk_t], writes=[top_t[t]])
                for h in range(8):
                    S.op("dve", lambda e, h=h, t=t: e.tensor_tensor(
                        out=cand[:].rearrange("p (a b) -> p a b", a=16),
                        in0=top[:, 0, 2 * h, :].unsqueeze(2).broadcast_to([128, 16, 16]),
                        in1=top[:, 0, 2 * h + 1, :].unsqueeze(1).broadcast_to([128, 16, 16]), op=ALU.add),
                         reads=[top_t[t]], writes=[cand_t])
                    S.op("dve", lambda e, h=h, t=t: e.max(out=best[:, t, h, 0:8], in_=cand[:]),
                         reads=[cand_t], writes=[best_t[t]])
                    S.op("dve", lambda e, h=h, t=t: e.match_replace(out=tmpk[:], in_to_replace=best[:, t, h, 0:8],
                                                                    in_values=cand[:], imm_value=NEG),
                         reads=[cand_t, best_t[t]], writes=[tmpk_t])
                    S.op("dve", lambda e, h=h, t=t: e.max(out=best[:, t, h, 8:16], in_=tmpk[:]),
                         reads=[tmpk_t], writes=[best_t[t]])
                S.op("dve", lambda e, t=t: e.tensor_scalar(out=negmx[:, t, :], in0=best[:, t, :, 0], scalar1=-1.0, scalar2=None, op0=ALU.mult),
                     reads=[best_t[t]], writes=[negmx_t[t]])
                for h in range(8):
                    S.op("act", lambda e, h=h, t=t: e.activation(out=ebuf[:], in_=best[:, t, h, :], func=AF.Exp,
                                                                 bias=negmx[:, t, h:h + 1], accum_out=zz[:, t, h:h + 1]),
                         reads=[best_t[t], negmx_t[t]], writes=[ebuf_t, zz_t[t]])
                S.op("act", lambda e, t=t: e.activation(out=zz[:, t, :], in_=zz[:, t, :], func=AF.Ln),
                     reads=[zz_t[t]], writes=[zz_t[t]])
                S.op("dve", lambda e, t=t: e.tensor_tensor(out=nbias[:, t, :], in0=negmx[:, t, :], in1=zz[:, t, :], op=ALU.subtract),
                     reads=[negmx_t[t], zz_t[t]], writes=[nbias_t[t]])
            its = [(eb, t) for eb in range(peer_blocks) for t in range(NT)]
            nit = len(its)

            def st1(k):
                eb, t = its[k]
                slot = eb % 2
                if t == 0:
                    load_blk(WS[slot], WS_t[slot], "u", eb, s_u)
                    load_blk(VS[slot], VS_t[slot], "v", eb, s_v)
                pa = k % 2
                for kc in range(16):
                    S.op("pe", lambda e, kc=kc, t=t, slot=slot, pa=pa: e.matmul(out=ps[pa][:, :], lhsT=R1[:, kc, t * 128:(t + 1) * 128],
                                                                               rhs=WS[slot][:, kc, :], start=(kc == 0), stop=(kc == 15)),
                         reads=[R1_t, WS_t[slot]], writes=[ps_t[pa]])

            def st1g(k):
                pa = k % 2
                S.op("act", lambda e, pa=pa: e.activation(out=Ab[pa][:], in_=ps[pa][:, :], func=AF.Gelu),
                     reads=[ps_t[pa]], writes=[Ab_t[pa]])

            def st2(k):
                eb, t = its[k]
                pa = k % 2

                def emit_T(h):
                    i = h % 3
                    S.op("dve", lambda e, h=h, i=i: e.tensor_tensor(
                        out=Tb[i][:].rearrange("p (a b) -> p a b", a=4),
                        in0=sc[:, t, 2 * h, eb * 4:(eb + 1) * 4].unsqueeze(2).broadcast_to([128, 4, 128]),
                        in1=sc[:, t, 2 * h + 1, :].unsqueeze(1).broadcast_to([128, 4, 128]), op=ALU.add),
                         reads=[sc_t[t]], writes=[Tb_t[i]])
                    S.op("act", lambda e, h=h, i=i: e.activation(out=Eb[i][:], in_=Tb[i][:], func=AF.Exp, bias=nbias[:, t, h:h + 1]),
                         reads=[Tb_t[i], nbias_t[t]], writes=[Eb_t[i]])
                emit_T(0)
                emit_T(1)
                for h in range(8):
                    i = h % 3
                    if h + 2 < 8:
                        emit_T(h + 2)
                    accb, accb_t = (Wb[pa], Wb_t[pa]) if h % 2 == 0 else (Wc[pa], Wc_t[pa])
                    dst, dst_t = (accb, accb_t) if h < 2 else (Wh[h % 2], Wh_t[h % 2])
                    S.op("dve", lambda e, h=h, i=i, dst=dst: e.scalar_tensor_tensor(
                        out=dst[:], in0=Tb[i][:], scalar=best[:, t, h, 15:16], in1=Eb[i][:], op0=ALU.is_ge, op1=ALU.mult),
                         reads=[Tb_t[i], Eb_t[i], best_t[t]], writes=[dst_t])
                    if h >= 2:
                        S.op("pool" if h % 2 == 0 else "dve", lambda e, h=h, accb=accb: e.tensor_tensor(out=accb[:], in0=accb[:], in1=Wh[h % 2][:], op=ALU.add),
                             reads=[Wh_t[h % 2], accb_t], writes=[accb_t])
                S.op("dve", lambda e, pa=pa: e.tensor_tensor(out=Wb[pa][:], in0=Wb[pa][:], in1=Wc[pa][:], op=ALU.add),
                     reads=[Wb_t[pa], Wc_t[pa]], writes=[Wb_t[pa]])
                S.op("dve", lambda e, pa=pa: e.tensor_tensor(out=AW[pa][:], in0=Ab[pa][:], in1=Wb[pa][:], op=ALU.mult),
                     reads=[Ab_t[pa], Wb_t[pa]], writes=[AW_t[pa]])

            def st3a(k):
                pa = k % 2
                for es in range(4):
                    S.op("pe", lambda e, es=es, pa=pa: e.transpose(out=psb[pa][:, es * 128:(es + 1) * 128], in_=AW[pa][:, es * 128:(es + 1) * 128],
                                                                   identity=ident[:]),
                         reads=[AW_t[pa], ident_t], writes=[psb_t[pa]])
                S.op("act", lambda e, pa=pa: e.activation(out=AWT[pa][:], in_=psb[pa][:, 0:512].rearrange("p (s t) -> p s t", s=4), func=AF.Copy),
                     reads=[psb_t[pa]], writes=[AWT_t[pa]])

            def st3b(k):
                eb, t = its[k]
                slot = eb % 2
                pa = k % 2
                for db in range(4):
                    pbk = 2 + db
                    for es in range(4):
                        S.op("pe", lambda e, es=es, db=db, pbk=pbk, slot=slot, pa=pa: e.matmul(
                            out=ps[pbk][:, :], lhsT=AWT[pa][:, es, :], rhs=VS[slot][:, es, db * 512:(db + 1) * 512],
                            start=(es == 0), stop=(es == 3)),
                             reads=[AWT_t[pa], VS_t[slot]], writes=[ps_t[pbk]])

            def st3c(k):
                eb, t = its[k]
                for db in range(4):
                    pbk = 2 + db
                    if eb == 0:
                        S.op("act", lambda e, db=db, t=t, pbk=pbk: e.activation(out=acc[:, t, db * 512:(db + 1) * 512], in_=ps[pbk][:, :], func=AF.Copy),
                             reads=[ps_t[pbk]], writes=[acc_t[t]])
                    elif db % 2 == 0:
                        S.op("dve", lambda e, db=db, t=t, pbk=pbk: e.tensor_tensor(out=acc[:, t, db * 512:(db + 1) * 512],
                                                                                 in0=acc[:, t, db * 512:(db + 1) * 512], in1=ps[pbk][:, :], op=ALU.add),
                             reads=[ps_t[pbk], acc_t[t]], writes=[acc_t[t]])
                    else:
                        sg_, sg_t = stg[db // 2]
                        S.op("act", lambda e, pbk=pbk, sg_=sg_: e.activation(out=sg_[:], in_=ps[pbk][:, :], func=AF.Copy),
                             reads=[ps_t[pbk]], writes=[sg_t])
                        S.op("pool", lambda e, db=db, t=t, sg_=sg_: e.tensor_tensor(out=acc[:, t, db * 512:(db + 1) * 512],
                                                                                   in0=acc[:, t, db * 512:(db + 1) * 512], in1=sg_[:], op=ALU.add),
                             reads=[sg_t, acc_t[t]], writes=[acc_t[t]])

            for k in range(nit + 2):
                if 0 <= k - 2 < nit:
                    st3a(k - 2)
                if k < nit:
                    st1(k)
                if 0 <= k - 2 < nit:
                    st3b(k - 2)
                if 0 <= k - 1 < nit:
                    st2(k - 1)
                if k < nit:
                    st1g(k)
                if 0 <= k - 2 < nit:
                    st3c(k - 2)
            for t in range(NT):
                S.op("pool", lambda e, t=t: e.tensor_tensor(out=xh[:, t, :], in0=xh[:, t, :], in1=acc[:, t, :], op=ALU.add),
                     reads=[acc_t[t], xh_t[t]], writes=[xh_t[t]])
        if stage >= 3:
            S.dma("sp", "g", lambda e: e.dma_start(out=gvec[:], in_=gv[2:3, :].partition_broadcast(128)), writes=[gvec_t])
        for t in range(NT):
            if stage >= 3:
                _rms_rstd(S, "act", xh[:, t, :], xh_t[t], junk[:], junk_t, ss[:], ss_t, rstd[:], rstd_t, 1e-6)
                S.op("dve", lambda e, t=t: e.scalar_tensor_tensor(out=acc[:, t, :], in0=xh[:, t, :], scalar=rstd[:, 0:1], in1=gvec[:],
                                                                 op0=ALU.mult, op1=ALU.mult),
                     reads=[xh_t[t], rstd_t, gvec_t], writes=[acc_t[t]])
            else:
                S.op("dve", lambda e, t=t: e.tensor_copy(out=acc[:, t, :], in_=xh[:, t, :]), reads=[xh_t[t]], writes=[acc_t[t]])
            S.dma("sp", "o", lambda e, t=t: e.dma_start(out=y[t0 + t * 128:t0 + (t + 1) * 128, :], in_=acc[:, t, :]),
                  reads=[acc_t[t]], writes=[])
    k = "dma:o"
    nc.sync.wait_ge(S.dsem[k][0], S.dsem[k][1])
    return nc, S


G1 = 256
NC1 = 1216
C_R, C_K, C_V, C_XW, C_XA, C_XG, C_QD, C_KD, C_VD = 0, 128, 256, 384, 480, 576, 832, 960, 1088
CHK = 32
NCAST = 11


def _roundrobin(gens):
    gens = list(gens)
    while gens:
        for g_ in list(gens):
            try:
                next(g_)
            except StopIteration:
                gens.remove(g_)


def build_l1(ngroups=SEQ // G1, do_rwkv=True, do_attn=True, rw_stage=9999):
    nc = bass.Bass("TRN2", target_bir_lowering=False)
    ntok = ngroups * G1
    ntile = ntok // 128
    dr = lambda n, s, k="ExternalInput": nc.dram_tensor(n, s, F32, kind=k).ap()
    x = dr("x", [ntok, D])
    w1 = dr("w1", [D, NC1])
    cvec = dr("cvec", [128, 20])
    wdu_d = dr("wdu", [96, 128]); wiu_d = dr("wiu", [96, 128]); wgu_d = dr("wgu", [256, 128])
    lamv = dr("lamv", [1, 256]); sublng = dr("sublng", [1, 128]); g1d = dr("g1", [1, D])
    consts = dr("consts", [7, 128, 128])
    rmask_d = dr("rmask", [1, G1])
    yaT = dr("yaT", [128, ntok], "ExternalOutput")
    yb = dr("yb", [ntok, 128], "ExternalOutput")
    castin = dr("castin", [NCAST, 128, 8192])
    castout = nc.dram_tensor("castout", [NCAST, 128, 8192], BF16, kind="ExternalOutput").ap()

    S = Sync(nc)
    for b in range(NCAST):
        S.dma("pool", "o", lambda e, b=b: e.dma_start(out=castout[b].rearrange("p (s e) -> p s e", e=2048),
                                                      in_=castin[b].rearrange("p (s e) -> p s e", e=2048)))
    sb = lambda n, s, dt=F32: nc.alloc_sbuf_tensor(n, s, dt)
    ps = [nc.alloc_psum_tensor("ps%d" % i, [128, 512], F32) for i in range(7)]
    psb = [nc.alloc_psum_tensor("psb%d" % i, [128, 1024], BF16) for i in range(1)]
    ps_t = [Tok() for _ in range(7)]
    psb_t = [Tok() for _ in range(1)]
    OB = [4, 6]
    scr_i = [0]

    def scr():
        scr_i[0] ^= 1
        return ps[scr_i[0]], ps_t[scr_i[0]]

    def T_(name, shape, dt=F32):
        return sb(name, shape, dt), Tok()

    W1, W1_t = T_("W1", [128, 16, NC1], BF16)
    KT, KT_t = T_("KT", [128, ntok], BF16)
    VA, VA_t = T_("VA", [128, ntile, 130], BF16)
    g1c, g1c_t = T_("g1c", [128, 16])
    xt = [T_("xt%d" % i, [128, D]) for i in range(2)]
    xnb, xnb_t = T_("xnb", [128, D], BF16)
    junk, junk_t = xnb, xnb_t
    junk2, junk2_t = T_("junk2", [128, 128], BF16)
    xnT, xnT_t = T_("xnT", [128, 16, G1], BF16)
    cst, cst_t = T_("cst", [128, 7, 128])
    identb, identb_t = T_("identb", [128, 128], BF16)
    trib, trib_t = T_("trib", [128, 128], BF16)
    rmask, rmask_t = T_("rmask_s", [128, G1])
    cv, cv_t = T_("cv_s", [128, 20])
    omm, omm_t = T_("omm", [128, 8])
    wdu, wdu_t = T_("wdus", [96, 128]); wiu, wiu_t = T_("wius", [96, 128]); wgu, wgu_t = T_("wgus", [128, 2, 128])
    lacc, lacc_t = T_("lacc", [128, 4])
    neglam, neglam_t = T_("neglam", [128, 1])
    sgv, sgv_t = T_("sgv", [128, 128])
    ss, ss_t = T_("ss", [128, 1]); rstd, rstd_t = T_("rstd", [128, 1])
    ss2, ss2_t = T_("ss2", [128, 1]); rstd2, rstd2_t = T_("rstd2", [128, 1])
    ident = cst[:, 0, :]; MU = cst[:, 1, :]; ML = cst[:, 2, :]; MUI = cst[:, 3, :]; onesblk = cst[:, 5, :]; I64 = cst[:, 6, 0:64]
    PB = [T_("PB%d" % i, [128, G1 + 1]) for i in range(7)]
    SH = [T_("SH%d" % i, [128, G1]) for i in range(7)]
    tmpA, tmpA_t = T_("tmpA", [128, G1]); tmpB, tmpB_t = T_("tmpB", [128, G1])
    logw, logw_t = T_("logw", [128, G1]); av, av_t = T_("av", [128, G1]); gg, gg_t = T_("gg", [128, G1])
    kkn, kkn_t = T_("kkn", [128, G1]); k2, k2_t = T_("k2", [128, G1]); bonus, bonus_t = T_("bonus", [128, G1])
    cum, cum_t = T_("cum", [128, G1]); Pm, Pm_t = T_("Pm", [128, G1]); Pinv, Pinv_t = T_("Pinv", [128, G1]); Pprev, Pprev_t = T_("Pprev", [128, G1])
    At, At_t = T_("At", [128, G1], BF16); Bt, Bt_t = T_("Bt", [128, G1], BF16); Kt, Kt_t = T_("Kt", [128, G1], BF16); Rt, Rt_t = T_("Rt", [128, G1], BF16)
    vb16, vb16_t = T_("vb16", [128, G1], BF16)
    yT, yT_t = T_("yT", [128, G1]); yo, yo_t = T_("yo", [128, G1])
    TOK, TOK_t = T_("TOK", [128, 4, 128], BF16)
    lam_s = yT[:, 0:256].rearrange("p (a c) -> p a c", c=64); lam_t = yT_t
    lamp = yo[:, 0:128].rearrange("p (b c) -> p b c", c=64); lamp_t = yo_t
    HB = []
    for h in range(2):
        HB.append(dict(
            XT=[T_("XT%d_%d" % (h, i), [128, 128], BF16) for i in range(5)], XX=[T_("XX%d_%d" % (h, i), [128, 128], BF16) for i in range(4)],
            LakT=T_("LakT%d" % h, [128, 128], BF16), MrbT=T_("MrbT%d" % h, [128, 128], BF16), MrkT=T_("MrkT%d" % h, [128, 128], BF16),
            Z=[T_("Z%d_%d" % (h, i), [128, 128], BF16) for i in range(2)], ZF=T_("ZF%d" % h, [128, 128], BF16),
            MZ=T_("MZ%d" % h, [128, 4, 64], BF16), MB=T_("MB%d" % h, [128, 4, 64], BF16), MK=T_("MK%d" % h, [128, 4, 64], BF16)))
    RhT, RhT_t = T_("RhT", [128, 128]); YhT, YhT_t = T_("YhT", [128, 128])
    MT, MT_t = T_("MT", [128, 4, 64]); HP, HP_t = T_("HP", [128, 4, 64])
    Sst = [T_("Sst%d" % i, [128, 64]) for i in range(2)]
    QT, QT_t = T_("QT", [128, G1], BF16); QSQ, QSQ_t = T_("QSQ", [128, G1]); KSQ, KSQ_t = T_("KSQ", [128, G1])
    kmax2 = [T_("kmax2_%d" % i, [128, 1]) for i in range(2)]
    kred, kred_t = T_("kred", [128, 1]); sqq, sqq_t = T_("sqq", [128, 1])
    nshift = [T_("nshift%d" % i, [128, 1]) for i in range(2)]
    Pb = [T_("Pb%d" % i, [128, 512], BF16) for i in range(2)]
    om = [[T_("om%d_%d" % (i, j), [128, 128]) for j in range(2)] for i in range(2)]
    qmax2 = [T_("qmax2_%d" % i, [128, 1]) for i in range(2)]
    rs_, rs_t = T_("rs_", [128, 1]); attn, attn_t = T_("attn", [128, 128]); ybo, ybo_t = T_("ybo", [128, 128])
    ones128, ones_t = T_("ones128", [128, 128])

    def mm(out, lhsT, rhs, start=True, stop=True):
        return lambda e: e.matmul(out=out, lhsT=lhsT, rhs=rhs, start=start, stop=stop)

    cpy_i = [0]

    def copy_out(out, in_, reads, writes, eng=None):
        if eng is None:
            cpy_i[0] ^= 1
            eng = "act" if cpy_i[0] else "dve"
        if eng == "act":
            S.op("act", lambda e: e.activation(out=out, in_=in_, func=AF.Copy), reads=reads, writes=writes)
        else:
            S.op("dve", lambda e: e.tensor_copy(out=out, in_=in_), reads=reads, writes=writes)

    def dve_tt(out, in0, in1, op, reads, writes, eng="dve"):
        S.op(eng, lambda e: e.tensor_tensor(out=out, in0=in0, in1=in1, op=op), reads=reads, writes=writes)

    def dve_ts(out, in0, s1, s2, op0, op1, reads, writes, eng="dve"):
        if op1 is None:
            S.op(eng, lambda e: e.tensor_scalar(out=out, in0=in0, scalar1=s1, scalar2=None, op0=op0), reads=reads, writes=writes)
        else:
            S.op(eng, lambda e: e.tensor_scalar(out=out, in0=in0, scalar1=s1, scalar2=s2, op0=op0, op1=op1), reads=reads, writes=writes)

    def dve_stt(out, in0, scalar, in1, op0, op1, reads, writes):
        S.op("dve", lambda e: e.scalar_tensor_tensor(out=out, in0=in0, scalar=scalar, in1=in1, op0=op0, op1=op1), reads=reads, writes=writes)

    def act(out, in_, func, reads, writes, **kw):
        S.op("act", lambda e: e.activation(out=out, in_=in_, func=func, **kw), reads=reads, writes=writes)

    S.dma("sp", "c", lambda e: e.dma_start(out=cst[:], in_=consts.rearrange("c p n -> p c n")), writes=[cst_t])
    S.dma("sp", "c", lambda e: e.dma_start(out=cv[:], in_=cvec[:, :]), writes=[cv_t])
    S.dma("sp", "c", lambda e: e.dma_start(out=wdu[:], in_=wdu_d[:, :]), writes=[wdu_t])
    S.dma("sp", "c", lambda e: e.dma_start(out=wiu[:], in_=wiu_d[:, :]), writes=[wiu_t])
    S.dma("sp", "c", lambda e: e.dma_start(out=wgu[:], in_=wgu_d.rearrange("(k p) c -> p k c", p=128)), writes=[wgu_t])
    S.dma("sp", "c", lambda e: e.dma_start(out=yT[:, 0:256], in_=lamv[0:1, :].partition_broadcast(128)), writes=[lam_t])
    S.dma("sp", "c", lambda e: e.dma_start(out=sgv[:], in_=sublng[0:1, :].partition_broadcast(128)), writes=[sgv_t])
    S.dma("sp", "c", lambda e: e.dma_start(out=rmask[:], in_=rmask_d[0:1, :].partition_broadcast(128)), writes=[rmask_t])
    with nc.allow_non_contiguous_dma(reason="tiny gain vector"):
        S.dma("sp", "c", lambda e: e.dma_start(out=g1c[:], in_=g1d.rearrange("o (k p) -> p (o k)", p=128)), writes=[g1c_t])
    S.dma("pool", "w", lambda e: e.dma_start(out=W1[:], in_=w1.rearrange("(k p) c -> p k c", p=128)), writes=[W1_t])
    for kc in range(16):
        S.op("dve" if kc % 2 else "pool", lambda e, kc=kc: e.tensor_scalar(out=W1[:, kc, :], in0=W1[:, kc, :], scalar1=g1c[:, kc:kc + 1], scalar2=0.0,
                                                                            op0=ALU.mult, op1=ALU.add),
             reads=[W1_t, g1c_t], writes=[W1_t])
    S.op("dve", lambda e: e.tensor_copy(out=identb[:], in_=cst[:, 0, :]), reads=[cst_t], writes=[identb_t])
    S.op("dve", lambda e: e.tensor_copy(out=trib[:], in_=cst[:, 4, :]), reads=[cst_t], writes=[trib_t])
    dve_ts(omm[:, 0:8], cv[:, 0:8], -1.0, 1.0, ALU.mult, ALU.add, [cv_t], [omm_t])
    dve_ts(cv[:, 14:15], cv[:, 10:11], -1.0, 1.0, ALU.mult, ALU.add, [cv_t], [cv_t])
    dve_ts(sgv[:], sgv[:], 0.8, None, ALU.mult, None, [sgv_t], [sgv_t])
    dve_tt(lamp[:, 0, :], lam_s[:, 0, :], lam_s[:, 1, :], ALU.mult, [lam_t], [lamp_t])
    dve_tt(lamp[:, 1, :], lam_s[:, 2, :], lam_s[:, 3, :], ALU.mult, [lam_t], [lamp_t])
    S.op("dve", lambda e: e.tensor_reduce(out=lacc[:, 0:2], in_=lamp[:, 0:2, :], axis=AX.X, op=ALU.add), reads=[lamp_t], writes=[lacc_t])
    act(lacc[:, 2:4], lacc[:, 0:2], AF.Exp, [lacc_t], [lacc_t])
    dve_tt(neglam[:], lacc[:, 3:4], lacc[:, 2:3], ALU.subtract, [lacc_t], [neglam_t])
    dve_ts(neglam[:], neglam[:], -0.2, None, ALU.add, None, [neglam_t], [neglam_t])
    for i in range(7):
        S.op("pool", lambda e, i=i: e.memset(PB[i][0][:, 0:1], 0.0), writes=[PB[i][1]])
    for i in range(2):
        S.op("pool", lambda e, i=i: e.memset(Sst[i][0][:], 0.0), writes=[Sst[i][1]])
        S.op("pool", lambda e, i=i: e.memset(kmax2[i][0][:], 0.0), writes=[kmax2[i][1]])
        S.op("pool", lambda e, i=i: e.memset(qmax2[i][0][:], 0.0), writes=[qmax2[i][1]])
    S.op("pool", lambda e: e.memset(VA[:, :, 128:130], 1.0), writes=[VA_t])
    S.op("pool", lambda e: e.memset(ones128[:], 1.0), writes=[ones_t])

    scur = [0]

    def head_chain(h, cs):
        hb = HB[h]
        XT, XX, Z = hb["XT"], hb["XX"], hb["Z"]
        LakT, LakT_t = hb["LakT"]; MrbT, MrbT_t = hb["MrbT"]; MrkT, MrkT_t = hb["MrkT"]
        zf, zf_t = hb["ZF"]
        MZ, MZ_t = hb["MZ"]; MB, MB_t = hb["MB"]; MK, MK_t = hb["MK"]
        pb, pb_t = ps[h], ps_t[h]
        hs = slice(64 * h, 64 * h + 64)
        Ah, Bh, Kh, Rh = At[hs, cs], Bt[hs, cs], Kt[hs, cs], Rt[hs, cs]
        S.op("pe", mm(pb[:, 0:128], Bh, Ah), reads=[Bt_t, At_t], writes=[pb_t])
        dve_tt(XT[0][0][:], pb[:, 0:128], MU, ALU.mult, [pb_t, cst_t], [XT[0][1]])
        yield
        S.op("pe", mm(pb[:, 0:128], Ah, Bh), reads=[Bt_t, At_t], writes=[pb_t])
        dve_tt(XX[0][0][:], pb[:, 0:128], ML, ALU.mult, [pb_t, cst_t], [XX[0][1]])
        yield
        S.op("pe", mm(pb[:, 0:128], Kh, Ah), reads=[Kt_t, At_t], writes=[pb_t])
        dve_tt(LakT[:], pb[:, 0:128], MU, ALU.mult, [pb_t, cst_t], [LakT_t])
        yield
        S.op("pe", mm(pb[:, 0:128], Bh, Rh), reads=[Bt_t, Rt_t], writes=[pb_t])
        dve_tt(MrbT[:], pb[:, 0:128], MUI, ALU.mult, [pb_t, cst_t], [MrbT_t])
        yield
        S.op("pe", mm(pb[:, 0:128], Kh, Rh), reads=[Kt_t, Rt_t], writes=[pb_t])
        dve_tt(MrkT[:], pb[:, 0:128], MUI, ALU.mult, [pb_t, cst_t], [MrkT_t])
        yield
        S.op("pe", mm(pb[:, 0:64], LakT[:], TOK[:, 3, hs]), reads=[LakT_t, TOK_t], writes=[pb_t])
        zc, zc_t = Z[0]
        copy_out(zc[:, 64:128], pb[:, 0:64], [pb_t], [zc_t], eng="dve")
        S.op("pool", lambda e: e.tensor_copy(out=zc[:, 0:64], in_=TOK[:, 0, hs]), reads=[TOK_t], writes=[zc_t])
        yield
        for i in range(4):
            S.op("pe", mm(pb[:, 0:128], XX[i][0][:], XT[i][0][:]), reads=[XX[i][1], XT[i][1]], writes=[pb_t])
            copy_out(XT[i + 1][0][:], pb[:, 0:128], [pb_t], [XT[i + 1][1]], eng="dve")
            yield
            if i < 3:
                S.op("pe", mm(pb[:, 0:128], XT[i][0][:], XX[i][0][:]), reads=[XX[i][1], XT[i][1]], writes=[pb_t])
                copy_out(XX[i + 1][0][:], pb[:, 0:128], [pb_t], [XX[i + 1][1]], eng="act")
                yield
            zc, zc_t = Z[i % 2]
            zn, zn_t = Z[(i + 1) % 2]
            S.op("pe", mm(pb[:, 0:128], XT[i][0][:], zc[:]), reads=[XT[i][1], zc_t], writes=[pb_t])
            dve_tt(zn[:], pb[:, 0:128], zc[:], ALU.add, [pb_t, zc_t], [zn_t])
            yield
        zc, zc_t = Z[0]
        S.op("pe", mm(pb[:, 0:128], XT[4][0][:], zc[:]), reads=[XT[4][1], zc_t], writes=[pb_t])
        dve_tt(zf[:], pb[:, 0:128], zc[:], ALU.add, [pb_t, zc_t], [zf_t])
        yield
        S.op("pe", mm(pb[hs, 0:128], zf[:, 0:64], MrbT[:]), reads=[zf_t, MrbT_t], writes=[pb_t])
        dve_tt(RhT[hs, :], pb[hs, 0:128], Rh, ALU.add, [pb_t, Rt_t], [RhT_t])
        yield
        S.op("pe", mm(pb[hs, 0:128], zf[:, 64:128], MrbT[:], True, False), reads=[zf_t, MrbT_t], writes=[pb_t])
        S.op("pe", mm(pb[hs, 0:128], TOK[:, 3, hs], MrkT[:], False, True), reads=[TOK_t, MrkT_t], writes=[pb_t])
        copy_out(YhT[hs, :], pb[hs, 0:128], [pb_t], [YhT_t], eng="act")
        cmb = cv[:, 16:20].unsqueeze(2).broadcast_to([128, 4, 64])
        dve_tt(MZ[:], zf[:, 0:64].unsqueeze(1).broadcast_to([128, 4, 64]), cmb, ALU.mult, [zf_t, cv_t], [MZ_t])
        dve_tt(MB[:], TOK[:, 1, hs].unsqueeze(1).broadcast_to([128, 4, 64]), cmb, ALU.mult, [TOK_t, cv_t], [MB_t], eng="pool")
        dve_tt(MK[:], TOK[:, 2, hs].unsqueeze(1).broadcast_to([128, 4, 64]), cmb, ALU.mult, [TOK_t, cv_t], [MK_t], eng="pool")
        yield
        for c in range(4):
            S.op("pe", mm(ps[2][hs, c * 64:(c + 1) * 64], MZ[:, c, :], TOK[:, 1, hs]), reads=[MZ_t, TOK_t], writes=[ps_t[2]])
        for c in range(4):
            S.op("pe", mm(ps[3][hs, c * 64:(c + 1) * 64], MB[:, c, :], zf[:, 64:128], True, False), reads=[zf_t, MB_t], writes=[ps_t[3]])
            S.op("pe", mm(ps[3][hs, c * 64:(c + 1) * 64], MK[:, c, :], TOK[:, 3, hs], False, True), reads=[TOK_t, MK_t], writes=[ps_t[3]])
        yield

    def rwkv_group(g):
        t0 = g * G1
        r_, k_, v_, xw_, xa_, xg0_, xg1_ = range(7)
        rows = [128, 128, 128, 96, 96, 128, 128]
        for b in range(7):
            n = rows[b]
            pbuf, pbt = PB[b]
            sh, sht = SH[b]
            dve_ts(tmpA[0:n, :], pbuf[0:n, 0:G1], cv[0:n, b:b + 1], None, ALU.mult, None, [pbt, cv_t], [tmpA_t])
            dve_stt(sh[0:n, :], pbuf[0:n, 1:G1 + 1], omm[0:n, b:b + 1], tmpA[0:n, :], ALU.mult, ALU.add, [pbt, omm_t, tmpA_t], [sht])
            S.op("pool", lambda e, pbuf=pbuf, n=n: e.tensor_copy(out=pbuf[0:n, 0:1], in_=pbuf[0:n, G1:G1 + 1]), reads=[pbt], writes=[pbt])
            if b % 2:
                yield
        shr, shr_t = SH[r_]; shk, shk_t = SH[k_]; shv, shv_t = SH[v_]
        act(tmpB[0:96, :], SH[xw_][0][0:96, :], AF.Tanh, [SH[xw_][1]], [tmpB_t])
        pb, pb_t = scr()
        S.op("pe", mm(pb[:, 0:G1], wdu[:, :], tmpB[0:96, :]), reads=[wdu_t, tmpB_t], writes=[pb_t])
        act(logw[:], pb[:, 0:G1], AF.Sigmoid, [pb_t, cv_t], [logw_t], bias=cv[:, 7:8])
        dve_ts(logw[:], logw[:], -0.6065306597126334, None, ALU.mult, None, [logw_t], [logw_t])
        pb, pb_t = scr()
        S.op("pe", mm(pb[:, 0:G1], wiu[:, :], SH[xa_][0][0:96, :]), reads=[wiu_t, SH[xa_][1]], writes=[pb_t])
        act(av[:], pb[:, 0:G1], AF.Sigmoid, [pb_t, cv_t], [av_t], bias=cv[:, 8:9])
        act(SH[xg0_][0][:], SH[xg0_][0][:], AF.Sigmoid, [SH[xg0_][1]], [SH[xg0_][1]])
        act(SH[xg1_][0][:], SH[xg1_][0][:], AF.Sigmoid, [SH[xg1_][1]], [SH[xg1_][1]])
        yield
        pb, pb_t = scr()
        S.op("pe", mm(pb[:, 0:G1], wgu[:, 0, :], SH[xg0_][0][:], True, False), reads=[wgu_t, SH[xg0_][1]], writes=[pb_t])
        S.op("pe", mm(pb[:, 0:G1], wgu[:, 1, :], SH[xg1_][0][:], False, True), reads=[wgu_t, SH[xg1_][1]], writes=[pb_t])
        copy_out(gg[:], pb[:, 0:G1], [pb_t], [gg_t], eng="dve")
        dve_ts(kkn[:], shk[:], cv[:, 9:10], None, ALU.mult, None, [shk_t, cv_t], [kkn_t])
        dve_tt(tmpA[:], kkn[:], kkn[:], ALU.mult, [kkn_t], [tmpA_t], eng="pool")
        pb, pb_t = scr()
        S.op("pe", mm(pb[:, 0:G1], onesblk, tmpA[:]), reads=[cst_t, tmpA_t], writes=[pb_t])
        act(tmpB[:], pb[:, 0:G1], AF.Sqrt, [pb_t], [tmpB_t])
        dve_ts(tmpB[:], tmpB[:], 1e-12, None, ALU.max, None, [tmpB_t], [tmpB_t])
        S.op("dve", lambda e: e.reciprocal(out=tmpB[:], in_=tmpB[:]), reads=[tmpB_t], writes=[tmpB_t])
        dve_tt(kkn[:], kkn[:], tmpB[:], ALU.mult, [kkn_t, tmpB_t], [kkn_t])
        yield
        dve_ts(tmpA[:], av[:], cv[:, 10:11], cv[:, 14:15], ALU.mult, ALU.add, [av_t, cv_t], [tmpA_t])
        dve_tt(k2[:], shk[:], tmpA[:], ALU.mult, [shk_t, tmpA_t], [k2_t])
        dve_tt(tmpA[:], shr[:], k2[:], ALU.mult, [shr_t, k2_t], [tmpA_t], eng="pool")
        dve_ts(tmpA[:], tmpA[:], cv[:, 11:12], None, ALU.mult, None, [tmpA_t, cv_t], [tmpA_t])
        pb, pb_t = scr()
        S.op("pe", mm(pb[:, 0:G1], onesblk, tmpA[:]), reads=[cst_t, tmpA_t], writes=[pb_t])
        dve_tt(bonus[:], pb[:, 0:G1], shv[:], ALU.mult, [pb_t, shv_t], [bonus_t])
        S.op("dve", lambda e: e.tensor_tensor_scan(out=cum[:], data0=rmask[:], data1=logw[:], initial=0.0, op0=ALU.mult, op1=ALU.add),
             reads=[rmask_t, logw_t], writes=[cum_t])
        yield
        act(Pm[:], cum[:], AF.Exp, [cum_t], [Pm_t])
        act(Pinv[:], cum[:], AF.Exp, [cum_t], [Pinv_t], scale=-1.0)
        dve_tt(tmpA[:], cum[:], logw[:], ALU.subtract, [cum_t, logw_t], [tmpA_t], eng="pool")
        act(Pprev[:], tmpA[:], AF.Exp, [tmpA_t], [Pprev_t])
        dve_stt(At[:], kkn[:], -1.0, Pprev[:], ALU.mult, ALU.mult, [kkn_t, Pprev_t], [At_t])
        dve_tt(tmpB[:], kkn[:], av[:], ALU.mult, [kkn_t, av_t], [tmpB_t], eng="pool")
        dve_tt(Bt[:], tmpB[:], Pinv[:], ALU.mult, [tmpB_t, Pinv_t], [Bt_t])
        dve_tt(Kt[:], k2[:], Pinv[:], ALU.mult, [k2_t, Pinv_t], [Kt_t], eng="pool")
        dve_tt(Rt[:], shr[:], Pm[:], ALU.mult, [shr_t, Pm_t], [Rt_t])
        S.op("pool", lambda e: e.tensor_copy(out=vb16[:], in_=shv[:]), reads=[shv_t], writes=[vb16_t])
        yield
        for tl in range(G1 // 128):
            cs = slice(tl * 128, (tl + 1) * 128)
            for j, (src, srct) in enumerate([(At, At_t), (Bt, Bt_t), (Kt, Kt_t), (vb16, vb16_t)]):
                S.op("pe", lambda e, j=j, src=src: e.transpose(out=psb[0][:, j * 128:(j + 1) * 128], in_=src[:, cs], identity=identb[:]),
                     reads=[srct, identb_t], writes=[psb_t[0]])
            copy_out(TOK[:].rearrange("p a b -> p (a b)"), psb[0][:, 0:512], [psb_t[0]], [TOK_t], eng="act")
            yield
            chains = [head_chain(0, cs), head_chain(1, cs)]
            while chains:
                for ch in list(chains):
                    try:
                        next(ch)
                    except StopIteration:
                        chains.remove(ch)
                yield
            dve_tt(MT[:], ps[2][:, 0:256].rearrange("p (c k) -> p c k", c=4), I64.unsqueeze(1).broadcast_to([128, 4, 64]), ALU.add,
                   [ps_t[2], cst_t], [MT_t])
            for c in range(4):
                col = tl * 128 + 32 * c + 31
                dve_ts(HP[:, c, :], ps[3][:, c * 64:(c + 1) * 64], Pm[:, col:col + 1], None, ALU.mult, None, [ps_t[3], Pm_t], [HP_t])
            yield
            for c in range(4):
                col = tl * 128 + 32 * c + 31
                sc_, sc_t = Sst[scur[0]]
                sn_, sn_t = Sst[1 - scur[0]]
                for h in range(2):
                    hs = slice(64 * h, 64 * h + 64)
                    S.op("pe", mm(ps[0][hs, 0:64], MT[hs, c, :], sc_[hs, :]), reads=[MT_t, sc_t], writes=[ps_t[0]])
                for h in range(2):
                    hs = slice(64 * h, 64 * h + 64)
                    S.op("pe", mm(ps[1][hs, 32 * c:32 * c + 32], sc_[hs, :], RhT[hs, 32 * c:32 * c + 32]), reads=[sc_t, RhT_t], writes=[ps_t[1]])
                dve_stt(sn_[:], ps[0][:, 0:64], Pm[:, col:col + 1], HP[:, c, :], ALU.mult, ALU.add, [ps_t[0], Pm_t, HP_t], [sn_t])
                scur[0] = 1 - scur[0]
                yield
            dve_tt(yT[:, cs], ps[1][:, 0:128], YhT[:], ALU.add, [ps_t[1], YhT_t], [yT_t])
            yield
        pb, pb_t = scr()
        S.op("pe", mm(pb[:, 0:G1], onesblk, yT[:]), reads=[cst_t, yT_t], writes=[pb_t])
        act(tmpA[:], pb[:, 0:G1], AF.Copy, [pb_t], [tmpA_t], scale=1.0 / 64)
        act(tmpB[:], yT[:], AF.Square, [yT_t], [tmpB_t])
        pb, pb_t = scr()
        S.op("pe", mm(pb[:, 0:G1], onesblk, tmpB[:]), reads=[cst_t, tmpB_t], writes=[pb_t])
        dve_tt(tmpB[:], tmpA[:], tmpA[:], ALU.mult, [tmpA_t], [tmpB_t], eng="pool")
        dve_stt(tmpB[:], pb[:, 0:G1], 1.0 / 64, tmpB[:], ALU.mult, ALU.subtract, [pb_t, tmpB_t], [tmpB_t])
        yield
        act(tmpB[:], tmpB[:], AF.Sqrt, [tmpB_t], [tmpB_t], bias=64e-5)
        S.op("dve", lambda e: e.reciprocal(out=tmpB[:], in_=tmpB[:]), reads=[tmpB_t], writes=[tmpB_t])
        dve_tt(yo[:], yT[:], tmpA[:], ALU.subtract, [yT_t, tmpA_t], [yo_t])
        dve_tt(yo[:], yo[:], tmpB[:], ALU.mult, [yo_t, tmpB_t], [yo_t])
        dve_ts(yo[:], yo[:], cv[:, 12:13], cv[:, 13:14], ALU.mult, ALU.add, [yo_t, cv_t], [yo_t])
        dve_tt(yo[:], yo[:], bonus[:], ALU.add, [yo_t, bonus_t], [yo_t], eng="pool")
        dve_tt(yo[:], yo[:], gg[:], ALU.mult, [yo_t, gg_t], [yo_t])
        S.dma("sp", "o", lambda e: e.dma_start(out=yaT[:, t0:t0 + G1], in_=yo[:]), reads=[yo_t], writes=[])
        yield

    def attn_group(g):
        for m in range(2):
            ms = slice(64 * m, 64 * m + 64)
            nsh, nsh_t = nshift[m]
            act(sqq[:], qmax2[m][0][:], AF.Sqrt, [qmax2[m][1], kmax2[m][1]], [sqq_t], scale=kmax2[m][0][:, 0:1])
            dve_ts(nsh[:], sqq[:], -0.125, None, ALU.mult, None, [sqq_t], [nsh_t])

            def stage_a(j):
                P_, P_t = Pb[j % 2]
                if j < g:
                    for a in range(2):
                        kt = 2 * j + a
                        S.op("pe", mm(ps[5][:, a * 256:(a + 1) * 256], KT[ms, kt * 128:(kt + 1) * 128], QT[ms, :]), reads=[QT_t, KT_t], writes=[ps_t[5]])
                    act(P_[:], ps[5][:, :], AF.Exp, [ps_t[5], nsh_t], [P_t], scale=0.125, bias=nsh[:, 0:1])
                else:
                    kt = 2 * g
                    S.op("pe", mm(ps[5][:, 0:256], KT[ms, kt * 128:(kt + 1) * 128], QT[ms, :]), reads=[QT_t, KT_t], writes=[ps_t[5]])
                    S.op("pe", mm(ps[5][:, 384:512], KT[ms, (kt + 1) * 128:(kt + 2) * 128], QT[ms, 128:256]), reads=[QT_t, KT_t], writes=[ps_t[5]])
                    act(P_[:, 0:256], ps[5][:, 0:256], AF.Exp, [ps_t[5], nsh_t], [P_t], scale=0.125, bias=nsh[:, 0:1])
                    act(P_[:, 384:512], ps[5][:, 384:512], AF.Exp, [ps_t[5], nsh_t], [P_t], scale=0.125, bias=nsh[:, 0:1])
                    dve_tt(P_[:, 0:128], P_[:, 0:128], trib[:], ALU.mult, [P_t, trib_t], [P_t], eng="pool")
                    dve_tt(P_[:, 384:512], P_[:, 384:512], trib[:], ALU.mult, [P_t, trib_t], [P_t], eng="pool")

            def stage_b(j):
                P_, P_t = Pb[j % 2]
                if j < g:
                    for a in range(2):
                        kt = 2 * j + a
                        for tl in range(2):
                            ob = OB[tl]
                            S.op("pe", mm(ps[ob][:, 0:129], P_[:, a * 256 + tl * 128:a * 256 + (tl + 1) * 128], VA[:, kt, 0:129], kt == 0, False),
                                 reads=[P_t, VA_t], writes=[ps_t[ob]])
                else:
                    kt = 2 * g
                    S.op("pe", mm(ps[OB[0]][:, 0:129], P_[:, 0:128], VA[:, kt, 0:129], kt == 0, True), reads=[P_t, VA_t], writes=[ps_t[OB[0]]])
                    S.op("pe", mm(ps[OB[1]][:, 0:129], P_[:, 128:256], VA[:, kt, 0:129], kt == 0, False), reads=[P_t, VA_t], writes=[ps_t[OB[1]]])
                    S.op("pe", mm(ps[OB[1]][:, 0:129], P_[:, 384:512], VA[:, kt + 1, 0:129], False, True), reads=[P_t, VA_t], writes=[ps_t[OB[1]]])

            stage_a(0)
            for j in range(g + 1):
                if j + 1 <= g:
                    stage_a(j + 1)
                stage_b(j)
                yield
            for tl in range(2):
                ob = OB[tl]
                S.op("dve", lambda e, ob=ob: e.reciprocal(out=rs_[:], in_=ps[ob][:, 128:129]), reads=[ps_t[ob]], writes=[rs_t])
                dve_ts(om[tl][m][0][:], ps[ob][:, 0:128], rs_[:, 0:1], None, ALU.mult, None, [ps_t[ob], rs_t], [om[tl][m][1]])
            yield
        for tl in range(2):
            qt = 2 * g + tl
            dve_stt(attn[:], om[tl][1][0][:], neglam[:, 0:1], om[tl][0][0][:], ALU.mult, ALU.add, [om[tl][0][1], om[tl][1][1], neglam_t], [attn_t])
            S.op("act", lambda e: e.activation(out=junk2[:], in_=attn[:], func=AF.Square, accum_out=ss2[:]), reads=[attn_t], writes=[junk2_t, ss2_t])
            S.op("act", lambda e: e.activation(out=ss2[:], in_=ss2[:], func=AF.Sqrt, scale=1.0 / 128, bias=1e-5), reads=[ss2_t], writes=[ss2_t])
            S.op("dve", lambda e: e.reciprocal(out=rstd2[:], in_=ss2[:]), reads=[ss2_t], writes=[rstd2_t])
            dve_stt(ybo[:], attn[:], rstd2[:, 0:1], sgv[:], ALU.mult, ALU.mult, [attn_t, rstd2_t, sgv_t], [ybo_t])
            S.dma("sp", "o", lambda e, qt=qt: e.dma_start(out=yb[qt * 128:(qt + 1) * 128, :], in_=ybo[:]), reads=[ybo_t], writes=[])
            yield

    for g in range(ngroups):
        t0 = g * G1
        for tl in range(2):
            xb, xb_t = xt[tl]
            S.dma("sp", "x", lambda e, tl=tl, xb=xb: e.dma_start(out=xb[:], in_=x[t0 + tl * 128:t0 + (tl + 1) * 128, :]), writes=[xb_t])
            _rms_rstd(S, "act", xb[:], xb_t, junk[:], junk_t, ss[:], ss_t, rstd[:], rstd_t, 1e-6)
            dve_ts(xnb[:], xb[:], rstd[:, 0:1], None, ALU.mult, None, [xb_t, rstd_t], [xnb_t])
            for half in range(2):
                for j in range(8):
                    kc = half * 8 + j
                    S.op("pe", lambda e, kc=kc, j=j: e.transpose(out=psb[0][:, j * 128:(j + 1) * 128], in_=xnb[:, kc * 128:(kc + 1) * 128], identity=identb[:]),
                         reads=[xnb_t, identb_t], writes=[psb_t[0]])
                copy_out(xnT[:, half * 8:(half + 1) * 8, tl * 128:(tl + 1) * 128], psb[0][:].rearrange("p (k t) -> p k t", k=8), [psb_t[0]], [xnT_t])
        blocks = [(C_R, 128), (C_K, 128), (C_V, 128), (C_XW, 96), (C_XA, 96), (C_XG, 128), (C_XG + 128, 128)]
        for bi, (c0, w) in enumerate(blocks):
            pb, pb_t = scr()
            for kc in range(16):
                S.op("pe", mm(pb[0:w, 0:G1], W1[:, kc, c0:c0 + w], xnT[:, kc, :], kc == 0, kc == 15), reads=[W1_t, xnT_t], writes=[pb_t])
            copy_out(PB[bi][0][0:w, 1:G1 + 1], pb[0:w, 0:G1], [pb_t], [PB[bi][1]])
        pb, pb_t = scr()
        for kc in range(16):
            S.op("pe", mm(pb[:, 0:G1], W1[:, kc, C_QD:C_QD + 128], xnT[:, kc, :], kc == 0, kc == 15), reads=[W1_t, xnT_t], writes=[pb_t])
        act(QT[:], pb[:, 0:G1], AF.Copy, [pb_t], [QT_t])
        act(QSQ[:], pb[:, 0:G1], AF.Square, [pb_t], [QSQ_t])
        pb, pb_t = scr()
        for kc in range(16):
            S.op("pe", mm(pb[:, 0:G1], W1[:, kc, C_KD:C_KD + 128], xnT[:, kc, :], kc == 0, kc == 15), reads=[W1_t, xnT_t], writes=[pb_t])
        act(KT[:, t0:t0 + G1], pb[:, 0:G1], AF.Copy, [pb_t], [KT_t])
        act(KSQ[:], pb[:, 0:G1], AF.Square, [pb_t], [KSQ_t])
        for m in range(2):
            ms = slice(64 * m, 64 * m + 64)
            pb, pb_t = scr()
            S.op("pe", mm(pb[:, 0:G1], ones128[ms, :], KSQ[ms, :]), reads=[ones_t, KSQ_t], writes=[pb_t])
            S.op("dve", lambda e, pb=pb: e.tensor_reduce(out=kred[:], in_=pb[:, 0:G1], axis=AX.X, op=ALU.max), reads=[pb_t], writes=[kred_t])
            dve_tt(kmax2[m][0][:], kmax2[m][0][:], kred[:], ALU.max, [kred_t, kmax2[m][1]], [kmax2[m][1]])
            pb, pb_t = scr()
            S.op("pe", mm(pb[:, 0:G1], ones128[ms, :], QSQ[ms, :]), reads=[ones_t, QSQ_t], writes=[pb_t])
            S.op("dve", lambda e, pb=pb: e.tensor_reduce(out=kred[:], in_=pb[:, 0:G1], axis=AX.X, op=ALU.max), reads=[pb_t], writes=[kred_t])
            dve_tt(qmax2[m][0][:], qmax2[m][0][:], kred[:], ALU.max, [kred_t, qmax2[m][1]], [qmax2[m][1]])
        for tl in range(2):
            pb, pb_t = scr()
            for kc in range(16):
                S.op("pe", mm(pb[:, 0:128], xnT[:, kc, tl * 128:(tl + 1) * 128], W1[:, kc, C_VD:C_VD + 128], kc == 0, kc == 15),
                     reads=[W1_t, xnT_t], writes=[pb_t])
            copy_out(VA[:, 2 * g + tl, 0:128], pb[:, 0:128], [pb_t], [VA_t])
        gens = []
        if do_rwkv:
            gr = rwkv_group(g)
            if rw_stage < 9000:
                def lim(gr=gr):
                    for _ in range(rw_stage):
                        next(gr)
                        yield
                gr = lim()
            gens.append(gr)
        if do_attn:
            gens.append(attn_group(g))
        _roundrobin(gens)
    k = "dma:o"
    if k not in S.dsem:
        S.dma("sp", "o", lambda e: e.dma_start(out=yaT[:, 0:G1], in_=yo[:]), reads=[yo_t], writes=[])
    nc.sync.wait_ge(S.dsem[k][0], S.dsem[k][1])
    return nc, S


def _consts():
    t = np.arange(128)
    same = (t[:, None] // CHK) == (t[None, :] // CHK)
    MU = (same & (t[:, None] < t[None, :])).astype(np.float32)
    ML = MU.T.copy()
    MUI = (same & (t[:, None] <= t[None, :])).astype(np.float32)
    TRI = (t[:, None] <= t[None, :]).astype(np.float32)
    ob = ((t[:, None] // 64) == (t[None, :] // 64)).astype(np.float32)
    i64 = np.zeros((128, 128), np.float32)
    i64[t, t % 64] = 1.0
    return np.stack([np.eye(128, dtype=np.float32), MU, ML, MUI, TRI, ob, i64])


def prep_l1(inp, c, ntok=SEQ):
    w_in = inp["w_in"][0]
    hs = slice(128 * c, 128 * c + 128)
    o_d = 3520
    cols = np.concatenate([np.arange(128 * c, 128 * c + 128), 1024 + np.arange(128 * c, 128 * c + 128),
                           2048 + np.arange(128 * c, 128 * c + 128), np.arange(3072, 3520),
                           o_d + np.arange(128 * c, 128 * c + 128), o_d + 1024 + np.arange(128 * c, 128 * c + 128),
                           o_d + 2048 + np.arange(128 * c, 128 * c + 128)])
    mu = inp["shift_mu"][0]
    cvec = np.zeros((128, 20), np.float32)
    cvec[:, 0] = mu[0:1024][hs]; cvec[:, 1] = mu[1024:2048][hs]; cvec[:, 2] = mu[2048:3072][hs]
    cvec[:96, 3] = mu[3072:3168]; cvec[:96, 4] = mu[3168:3264]; cvec[:, 5] = mu[3264:3392]; cvec[:, 6] = mu[3392:3520]
    cvec[:, 7] = inp["rwkv_w0"][0][hs]; cvec[:, 8] = inp["rwkv_a0"][0][hs]; cvec[:, 9] = inp["k_k"][0][hs]
    cvec[:, 10] = inp["k_a"][0][hs]; cvec[:, 11] = inp["r_k"][0].reshape(-1)[hs]
    cvec[:, 12] = inp["lnx_g"][0][hs]; cvec[:, 13] = inp["lnx_b"][0][hs]
    for c_ in range(4):
        cvec[32 * c_:32 * c_ + 32, 16 + c_] = 1.0
    rm = np.ones((1, G1), np.float32); rm[0, ::CHK] = 0.0
    return dict(
        x=np.ascontiguousarray(inp["x"][0, :ntok]), w1=np.ascontiguousarray(w_in[:, cols]), cvec=cvec,
        wdu=np.ascontiguousarray(inp["w_decay_up"][0][:, hs]), wiu=np.ascontiguousarray(inp["w_iclr_up"][0][:, hs]),
        wgu=np.ascontiguousarray(inp["w_gate_up"][0][:, hs]),
        lamv=np.concatenate([inp["lam_q1"][0], inp["lam_k1"][0], inp["lam_q2"][0], inp["lam_k2"][0]])[None, :].astype(np.float32),
        sublng=inp["subln_g"][0][None, :].astype(np.float32), g1=inp["norm1_g"][0][None, :].astype(np.float32),
        consts=_consts(), rmask=rm)


def _blk(w, nb):
    K_ = w.shape[0]
    return np.ascontiguousarray(w.reshape(K_ // 128, 128, nb, 512).transpose(2, 1, 0, 3).reshape(nb, 128, (K_ // 128) * 512))


W_BLOCKS = (("s_wg", 8), ("s_wab", 4), ("s_wo", 4), ("s_wq", 4), ("s_u", 32), ("s_v", 32))


def l2_weight_blocks(inp):
    w_in = inp["w_in"][0]
    wgb = _blk(w_in[:, 6592:], 8)
    wabb = np.concatenate([_blk(inp["w_proj_a"][0], 4), _blk(inp["w_proj_b"][0], 4)], axis=2)
    uTb = _blk(np.ascontiguousarray(inp["peer_u"][0].T), 32)
    vtb = inp["peer_v"][0].reshape(32, 4, 128, D).transpose(0, 2, 1, 3).reshape(32, 128, 4 * D)
    allb = np.zeros((NCAST * NCORES, 128, 8192), np.float32)
    o = 0
    for a in (wgb, wabb, _blk(inp["w_out"][0], 4), _blk(inp["peer_wq"][0], 4), uTb, vtb):
        allb[o:o + a.shape[0]] = a
        o += a.shape[0]
    return allb


def l2_shared(inp, cast_all):
    m = dict(
        skT=np.ascontiguousarray(inp["peer_sub_keys"][0].reshape(16, 128, 128).transpose(0, 2, 1)),
        gv=np.stack([inp["norm1_g"][0], inp["norm2_g"][0], inp["final_g"]]).astype(np.float32),
        ident=np.eye(128, dtype=np.float32))
    o = 0
    for name, nb in W_BLOCKS:
        m[name] = np.ascontiguousarray(cast_all[o:o + nb])
        o += nb
    return m


def prep_l2(inp, c, yaT_full, ybT_full, shared):
    ts = slice(TOK * c, TOK * (c + 1))
    m = dict(shared)
    m["x"] = np.ascontiguousarray(inp["x"][0, ts])
    m["yaT"] = np.ascontiguousarray(yaT_full[:, ts])
    m["ybT"] = np.ascontiguousarray(ybT_full[:, ts])
    return m


def kernel(**inputs):
    inp = {k: np.asarray(v) for k, v in inputs.items()}
    nc1, _ = build_l1()
    allb = l2_weight_blocks(inp)
    maps1 = [prep_l1(inp, c) for c in range(NCORES)]
    for c in range(NCORES):
        maps1[c]["castin"] = allb[NCAST * c:NCAST * (c + 1)]
    r1 = run_bass_kernel_spmd(nc1, maps1, core_ids=list(range(NCORES))).results
    del maps1, allb
    cast_all = np.concatenate([r1[c]["castout"] for c in range(NCORES)], axis=0)
    yaT_full = np.concatenate([r1[c]["yaT"] for c in range(NCORES)], axis=0)
    ybT_full = np.concatenate([r1[c]["yb"].T for c in range(NCORES)], axis=0)
    w_in = inp["w_in"][0]
    shared = l2_shared(inp, cast_all)
    nc2, _ = build_l2()
    maps2 = [prep_l2(inp, c, yaT_full, ybT_full, shared) for c in range(NCORES)]
    r2 = run_bass_kernel_spmd(nc2, maps2, core_ids=list(range(NCORES))).results
    out = np.concatenate([r2[c]["y"] for c in range(NCORES)], axis=0)
    return out.reshape(1, SEQ, D).astype(np.float32)
```

```python
import numpy as np
import concourse.bass as bass
import concourse.mybir as mybir
from concourse.bass_utils import run_bass_kernel_spmd

F32 = mybir.dt.float32
BF16 = mybir.dt.bfloat16
AF = mybir.ActivationFunctionType
ALU = mybir.AluOpType
AX = mybir.AxisListType

NCORES = 8
D = 2048
SEQ = 16384
TOK = SEQ // NCORES
NEG = -1.0e30


class Tok:
    __slots__ = ("w", "r")

    def __init__(self):
        self.w = None
        self.r = []


class Sync:
    def __init__(self, nc):
        self.nc = nc
        self.engs = {"pe": nc.tensor, "act": nc.scalar, "dve": nc.vector, "pool": nc.gpsimd, "sp": nc.sync}
        self.sem = {k: nc.alloc_semaphore("s_" + k) for k in ("pe", "act", "dve", "pool")}
        self.cnt = {k: 0 for k in self.sem}
        self.waited = {e: {} for e in self.engs}
        self.dsem = {}
        self.ninst = 0

    def _deps(self, reads, writes):
        deps = {}
        for b in reads:
            if b.w is not None:
                k, v = b.w
                deps[k] = max(deps.get(k, 0), v)
        for b in writes:
            if b.w is not None:
                k, v = b.w
                deps[k] = max(deps.get(k, 0), v)
            for (k, v) in b.r:
                deps[k] = max(deps.get(k, 0), v)
        return deps

    def _wait(self, ek, deps):
        eng = self.engs[ek]
        wd = self.waited[ek]
        for k, v in deps.items():
            if k == ek and ek == "pe":
                continue
            if k.startswith("dma:"):
                v = self.dsem[k][1]
                s = self.dsem[k][0]
            else:
                s = self.sem[k]
            if wd.get(k, 0) >= v:
                continue
            eng.wait_ge(s, v)
            wd[k] = v
            self.ninst += 1

    def _mark(self, ev, reads, writes):
        for b in reads:
            b.r.append(ev)
        for b in writes:
            b.w = ev
            b.r = []

    def op(self, ek, fn, reads=(), writes=()):
        self._wait(ek, self._deps(reads, writes))
        inst = fn(self.engs[ek])
        self.cnt[ek] += 1
        inst.then_inc(self.sem[ek], 1)
        self.ninst += 1
        self._mark((ek, self.cnt[ek]), reads, writes)

    def dma(self, qk, stream, fn, reads=(), writes=()):
        self._wait(qk, self._deps(reads, writes))
        inst = fn(self.engs[qk])
        k = "dma:" + stream
        if k not in self.dsem:
            self.dsem[k] = [self.nc.alloc_semaphore("d_" + stream), 0]
        self.dsem[k][1] += 16
        inst.then_inc(self.dsem[k][0], 16)
        self.ninst += 1
        self._mark((k, self.dsem[k][1]), reads, writes)

    def finish(self, toks):
        deps = self._deps(toks, ())
        self._wait("sp", deps)


def _rms_rstd(S, ek_sq, src_ap, src_tok, junk, junk_tok, ss, ss_tok, rstd, rstd_tok, eps):
    S.op("act", lambda e: e.activation(out=junk, in_=src_ap, func=AF.Square, accum_out=ss),
         reads=[src_tok], writes=[junk_tok, ss_tok])
    S.op("act", lambda e: e.activation(out=ss, in_=ss, func=AF.Sqrt, scale=1.0 / D, bias=float(eps)),
         reads=[ss_tok], writes=[ss_tok])
    S.op("dve", lambda e: e.reciprocal(out=rstd, in_=ss), reads=[ss_tok], writes=[rstd_tok])


CH = 256
NT = CH // 128


def build_l2(nchunks=TOK // CH, peer_blocks=32, stage=99):
    nc = bass.Bass("TRN2", target_bir_lowering=False)
    ntok = nchunks * CH
    dr = lambda n, s, k="ExternalInput": nc.dram_tensor(n, s, F32, kind=k).ap()
    x = dr("x", [ntok, D])
    yaT = dr("yaT", [1024, ntok])
    ybT = dr("ybT", [1024, ntok])
    bfi = lambda n, nb: nc.dram_tensor(n, [nb, 128, 8192], BF16, kind="ExternalInput").ap()
    s_wg, s_wab, s_wo, s_wq, s_u, s_v = bfi("s_wg", 8), bfi("s_wab", 4), bfi("s_wo", 4), bfi("s_wq", 4), bfi("s_u", 32), bfi("s_v", 32)
    skT = dr("skT", [16, 128, 128])
    gv = dr("gv", [3, D])
    ident_d = dr("ident", [128, 128])
    y = dr("y", [ntok, D], "ExternalOutput")

    S = Sync(nc)
    sb = lambda n, s, dt=F32: nc.alloc_sbuf_tensor(n, s, dt)
    ps = [nc.alloc_psum_tensor("ps%d" % i, [128, 512], F32) for i in range(6)]
    psb = [nc.alloc_psum_tensor("psb%d" % i, [128, 1024], BF16) for i in range(2)]
    ps_t = [Tok() for _ in range(6)]
    psb_t = [Tok() for _ in range(2)]

    gvec = sb("gvec", [128, D]); gvec_t = Tok()
    xh = sb("xh", [128, NT, D]); xh_t = [Tok() for _ in range(NT)]
    R1 = sb("R1", [128, 16, CH], BF16); R1_t = Tok()
    R2 = sb("R2", [128, 16, CH], BF16); R2_t = Tok()
    R3 = sb("R3", [128, 16, CH], BF16); R3_t = Tok()
    WS = [sb("WS%d" % i, [128, 16, 512], BF16) for i in range(3)]; WS_t = [Tok() for _ in range(3)]
    VS = [sb("VS%d" % i, [128, 4, D], BF16) for i in range(2)]; VS_t = [Tok() for _ in range(2)]
    acc = sb("acc", [128, NT, D]); acc_t = [Tok() for _ in range(NT)]
    sc = sb("sc", [128, NT, 16, 128]); sc_t = [Tok() for _ in range(NT)]
    xnb = sb("xnb", [128, D], BF16); xnb_t = Tok()
    junk, junk_t = xnb, xnb_t
    ident = sb("identb", [128, 128], BF16); ident_t = Tok()
    identf = sb("identf", [128, 128]); identf_t = Tok()
    skb = sb("skb", [128, 16, 128], BF16); skb_t = Tok()
    ss = sb("ss", [128, 1]); ss_t = Tok()
    rstd = sb("rstd", [128, 1]); rstd_t = Tok()
    sg = [sb("sg%d" % i, [128, CH], BF16) for i in range(2)]; sg_t = [Tok() for _ in range(2)]
    mm = [sb("mm%d" % i, [128, CH], BF16) for i in range(2)]; mm_t = [Tok() for _ in range(2)]
    top = sb("top", [128, 1, 16, 16]); top_t = [Tok()] * NT
    tmpk = sb("tmpk", [128, 256]); tmpk_t = Tok()
    cand = sb("cand", [128, 256]); cand_t = Tok()
    best = sb("best", [128, NT, 8, 16]); best_t = [Tok() for _ in range(NT)]
    negmx = sb("negmx", [128, NT, 8]); negmx_t = [Tok() for _ in range(NT)]
    zz = sb("zz", [128, NT, 8]); zz_t = [Tok() for _ in range(NT)]
    nbias = sb("nbias", [128, NT, 8]); nbias_t = [Tok() for _ in range(NT)]
    ebuf = sb("ebuf", [128, 16]); ebuf_t = Tok()
    Ab = [sb("Ab%d" % i, [128, 512], BF16) for i in range(2)]; Ab_t = [Tok() for _ in range(2)]
    Wc = [sb("Wc%d" % i, [128, 512], BF16) for i in range(2)]; Wc_t = [Tok() for _ in range(2)]
    Tb = [sb("Tb%d" % i, [128, 512]) for i in range(3)]; Tb_t = [Tok() for _ in range(3)]
    Eb = [sb("Eb%d" % i, [128, 512]) for i in range(3)]; Eb_t = [Tok() for _ in range(3)]
    stg = [(sb("stg%d" % i, [128, 512]), Tok()) for i in range(2)]
    Wh = [sb("Wh%d" % i, [128, 512], BF16) for i in range(2)]; Wh_t = [Tok() for _ in range(2)]
    Wb = [sb("Wb%d" % i, [128, 512], BF16) for i in range(2)]; Wb_t = [Tok() for _ in range(2)]
    AW = [sb("AW%d" % i, [128, 512], BF16) for i in range(2)]; AW_t = [Tok() for _ in range(2)]
    AWT = [sb("AWT%d" % i, [128, 4, 128], BF16) for i in range(2)]; AWT_t = [Tok() for _ in range(2)]

    S.dma("sp", "c", lambda e: e.dma_start(out=identf[:], in_=ident_d[:, :]), writes=[identf_t])
    S.op("dve", lambda e: e.tensor_copy(out=ident[:], in_=identf[:]), reads=[identf_t], writes=[ident_t])
    S.dma("pool", "w", lambda e: e.dma_start(out=skb[:], in_=skT.rearrange("b d n -> d b n")), writes=[skb_t])

    def load_blk(dst_tile, dst_tok, name, b, scr):
        S.dma("sp", "ws_" + dst_tile.name, lambda e: e.dma_start(out=dst_tile[:].rearrange("p a b -> p (a b)"), in_=scr[b]),
              writes=[dst_tok])

    def norm_transpose(t, g_row, dstT, dstT_t, first):
        src = xh[:, t, :]
        _rms_rstd(S, "act", src, xh_t[t], junk[:], junk_t, ss[:], ss_t, rstd[:], rstd_t, 1e-6)
        S.op("dve", lambda e: e.scalar_tensor_tensor(out=xnb[:], in0=src, scalar=rstd[:, 0:1], in1=gvec[:],
                                                     op0=ALU.mult, op1=ALU.mult),
             reads=[xh_t[t], rstd_t, gvec_t], writes=[xnb_t])
        for half in range(2):
            pb = psb[half]
            for j in range(8):
                kc = half * 8 + j
                S.op("pe", lambda e, kc=kc, j=j, pb=pb: e.transpose(out=pb[:, j * 128:(j + 1) * 128],
                                                                    in_=xnb[:, kc * 128:(kc + 1) * 128], identity=ident[:]),
                     reads=[xnb_t, ident_t], writes=[psb_t[half]])
            S.op("act" if half == 0 else "dve",
                 (lambda e, half=half, pb=pb: e.activation(out=dstT[:, half * 8:(half + 1) * 8, t * 128:(t + 1) * 128],
                                                           in_=pb[:].rearrange("p (k t) -> p k t", k=8), func=AF.Copy))
                 if half == 0 else
                 (lambda e, half=half, pb=pb: e.tensor_copy(out=dstT[:, half * 8:(half + 1) * 8, t * 128:(t + 1) * 128],
                                                            in_=pb[:].rearrange("p (k t) -> p k t", k=8))),
                 reads=[psb_t[half]], writes=[dstT_t])

    for c in range(nchunks):
        t0 = c * CH
        for t in range(NT):
            S.dma("sp", "x%d" % t, lambda e, t=t: e.dma_start(out=xh[:, t, :], in_=x[t0 + t * 128:t0 + (t + 1) * 128, :]),
                  writes=[xh_t[t]])
        S.dma("sp", "g", lambda e: e.dma_start(out=gvec[:], in_=gv[0:1, :].partition_broadcast(128)), writes=[gvec_t])
        S.dma("pool", "w", lambda e: e.dma_start(out=R2[:, 0:8, :], in_=yaT[:, t0:t0 + CH].rearrange("(k p) t -> p k t", p=128)),
              writes=[R2_t])
        S.dma("pool", "w", lambda e: e.dma_start(out=R2[:, 8:16, :], in_=ybT[:, t0:t0 + CH].rearrange("(k p) t -> p k t", p=128)),
              writes=[R2_t])
        for t in range(NT):
            norm_transpose(t, 0, R1, R1_t, True)
        for cg in range(4):
            cs = slice(cg * 512, (cg + 1) * 512)
            load_blk(WS[0], WS_t[0], "wg", cg, s_wg)
            load_blk(WS[1], WS_t[1], "wg", 4 + cg, s_wg)
            load_blk(WS[2], WS_t[2], "wab", cg, s_wab)
            for cb in range(4):
                cc = slice(cb * 128, (cb + 1) * 128)
                cidx = cg * 4 + cb
                for kc in range(16):
                    S.op("pe", lambda e, kc=kc, cc=cc: e.matmul(out=ps[0][:, 0:CH], lhsT=WS[0][:, kc, cc], rhs=R1[:, kc, :],
                                                                start=(kc == 0), stop=(kc == 15)),
                         reads=[WS_t[0], R1_t], writes=[ps_t[0]])
                for kc in range(16):
                    S.op("pe", lambda e, kc=kc, cc=cc: e.matmul(out=ps[1][:, 0:CH], lhsT=WS[1][:, kc, cc], rhs=R1[:, kc, :],
                                                                start=(kc == 0), stop=(kc == 15)),
                         reads=[WS_t[1], R1_t], writes=[ps_t[1]])
                for kc in range(8):
                    S.op("pe", lambda e, kc=kc, cc=cc: e.matmul(out=ps[2][:, 0:CH], lhsT=WS[2][:, kc, cc], rhs=R2[:, kc, :],
                                                                start=(kc == 0), stop=(kc == 7)),
                         reads=[WS_t[2], R2_t], writes=[ps_t[2]])
                for kc in range(8):
                    S.op("pe", lambda e, kc=kc, cc=cc: e.matmul(out=ps[3][:, 0:CH], lhsT=WS[2][:, 8 + kc, cc], rhs=R2[:, 8 + kc, :],
                                                                start=(kc == 0), stop=(kc == 7)),
                         reads=[WS_t[2], R2_t], writes=[ps_t[3]])
                for i in range(2):
                    S.op("act", lambda e, i=i: e.activation(out=sg[i][:], in_=ps[i][:, 0:CH], func=AF.Sigmoid),
                         reads=[ps_t[i]], writes=[sg_t[i]])
                for i in range(2):
                    S.op("dve", lambda e, i=i: e.tensor_tensor(out=mm[i][:], in0=sg[i][:], in1=ps[2 + i][:, 0:CH], op=ALU.mult),
                         reads=[sg_t[i], ps_t[2 + i]], writes=[mm_t[i]])
                S.op("pool", lambda e, cidx=cidx: e.tensor_tensor(out=R3[:, cidx, :], in0=mm[0][:], in1=mm[1][:], op=ALU.add),
                     reads=[mm_t[0], mm_t[1]], writes=[R3_t])
        for db in range(4):
            slot = db % 2
            load_blk(WS[slot], WS_t[slot], "wo", db, s_wo)
            for t in range(NT):
                pbk = 4 + (t % 2)
                for kc in range(16):
                    S.op("pe", lambda e, kc=kc, t=t, pbk=pbk, slot=slot: e.matmul(out=ps[pbk][:, :], lhsT=R3[:, kc, t * 128:(t + 1) * 128],
                                                                                 rhs=WS[slot][:, kc, :], start=(kc == 0), stop=(kc == 15)),
                         reads=[R3_t, WS_t[slot]], writes=[ps_t[pbk]])
                S.op("dve", lambda e, t=t, db=db, pbk=pbk: e.tensor_tensor(out=xh[:, t, db * 512:(db + 1) * 512],
                                                                         in0=xh[:, t, db * 512:(db + 1) * 512], in1=ps[pbk][:, :], op=ALU.add),
                     reads=[ps_t[pbk], xh_t[t]], writes=[xh_t[t]])
        if stage >= 2:
            S.dma("sp", "g", lambda e: e.dma_start(out=gvec[:], in_=gv[1:2, :].partition_broadcast(128)), writes=[gvec_t])
            for t in range(NT):
                norm_transpose(t, 1, R1, R1_t, False)
            for qg in range(4):
                slot = qg % 2
                load_blk(WS[slot], WS_t[slot], "wq", qg, s_wq)
                for qb in range(4):
                    blk = qg * 4 + qb
                    pbk = blk % 2
                    for kc in range(16):
                        S.op("pe", lambda e, kc=kc, qb=qb, pbk=pbk, slot=slot: e.matmul(out=ps[pbk][:, 0:CH], lhsT=WS[slot][:, kc, qb * 128:(qb + 1) * 128],
                                                                                       rhs=R1[:, kc, :], start=(kc == 0), stop=(kc == 15)),
                             reads=[WS_t[slot], R1_t], writes=[ps_t[pbk]])
                    S.op("act", lambda e, blk=blk, pbk=pbk: e.activation(out=R2[:, blk, :], in_=ps[pbk][:, 0:CH], func=AF.Copy),
                         reads=[ps_t[pbk]], writes=[R2_t])
            for t in range(NT):
                for g4 in range(4):
                    pbk = 2 + (g4 % 2)
                    for j in range(4):
                        blk = g4 * 4 + j
                        S.op("pe", lambda e, blk=blk, j=j, pbk=pbk, t=t: e.matmul(out=ps[pbk][:, j * 128:(j + 1) * 128],
                                                                                 lhsT=R2[:, blk, t * 128:(t + 1) * 128], rhs=skb[:, blk, :],
                                                                                 start=True, stop=True),
                             reads=[R2_t, skb_t], writes=[ps_t[pbk]])
                    S.op("act", lambda e, g4=g4, pbk=pbk, t=t: e.activation(out=sc[:, t, g4 * 4:(g4 + 1) * 4, :],
                                                                           in_=ps[pbk][:].rearrange("p (b n) -> p b n", b=4), func=AF.Copy),
                         reads=[ps_t[pbk]], writes=[sc_t[t]])
                for blk in range(16):
                    S.op("dve", lambda e, blk=blk, t=t: e.max(out=top[:, 0, blk, 0:8], in_=sc[:, t, blk, :]),
                         reads=[sc_t[t]], writes=[top_t[t]])
                    S.op("dve", lambda e, blk=blk, t=t: e.match_replace(out=tmpk[:, 0:128], in_to_replace=top[:, 0, blk, 0:8],
                                                                        in_values=sc[:, t, blk, :], imm_value=NEG),
                         reads=[sc_t[t], top_t[t]], writes=[tmpk_t])
                    S.op("dve", lambda e, blk=blk, t=t: e.max(out=top[:, 0, blk, 8:16], in_=tmpk[:, 0:128]),
                         reads=[tmpk_t], writes=[top_t[t]])
                for h in range(8):
                    S.op("dve", lambda e, h=h, t=t: e.tensor_tensor(
                        out=cand[:].rearrange("p (a b) -> p a b", a=16),
                        in0=top[:, 0, 2 * h, :].unsqueeze(2).broadcast_to([128, 16, 16]),
                        in1=top[:, 0, 2 * h + 1, :].unsqueeze(1).broadcast_to([128, 16, 16]), op=ALU.add),
                         reads=[top_t[t]], writes=[cand_t])
                    S.op("dve", lambda e, h=h, t=t: e.max(out=best[:, t, h, 0:8], in_=cand[:]),
                         reads=[cand_t], writes=[best_t[t]])
                    S.op("dve", lambda e, h=h, t=t: e.match_replace(out=tmpk[:], in_to_replace=best[:, t, h, 0:8],
                                                                    in_values=cand[:], imm_value=NEG),
                         reads=[cand_t, best_t[t]], writes=[tmpk_t])
                    S.op("dve", lambda e, h=h, t=t: e.max(out=best[:, t, h, 8:16], in_=tmpk[:]),
                         reads=[tmpk_t], writes=[best_t[t]])
                S.op("dve", lambda e, t=t: e.tensor_scalar(out=negmx[:, t, :], in0=best[:, t, :, 0], scalar1=-1.0, scalar2=None, op0=ALU.mult),
                     reads=[best_t[t]], writes=[negmx_t[t]])
                for h in range(8):
                    S.op("act", lambda e, h=h, t=t: e.activation(out=ebuf[:], in_=best[:, t, h, :], func=AF.Exp,
                                                                 bias=negmx[:, t, h:h + 1], accum_out=zz[:, t, h:h + 1]),
                         reads=[best_t[t], negmx_t[t]], writes=[ebuf_t, zz_t[t]])
                S.op("act", lambda e, t=t: e.activation(out=zz[:, t, :], in_=zz[:, t, :], func=AF.Ln),
                     reads=[zz_t[t]], writes=[zz_t[t]])
                S.op("dve", lambda e, t=t: e.tensor_tensor(out=nbias[:, t, :], in0=negmx[:, t, :], in1=zz[:, t, :], op=ALU.subtract),
                     reads=[negmx_t[t], zz_t[t]], writes=[nbias_t[t]])
            its = [(eb, t) for eb in range(peer_blocks) for t in range(NT)]
            nit = len(its)

            def st1(k):
                eb, t = its[k]
                slot = eb % 2
                if t == 0:
                    load_blk(WS[slot], WS_t[slot], "u", eb, s_u)
                    load_blk(VS[slot], VS_t[slot], "v", eb, s_v)
                pa = k % 2
                for kc in range(16):
                    S.op("pe", lambda e, kc=kc, t=t, slot=slot, pa=pa: e.matmul(out=ps[pa][:, :], lhsT=R1[:, kc, t * 128:(t + 1) * 128],
                                                                               rhs=WS[slot][:, kc, :], start=(kc == 0), stop=(kc == 15)),
                         reads=[R1_t, WS_t[slot]], writes=[ps_t[pa]])

            def st1g(k):
                pa = k % 2
                S.op("act", lambda e, pa=pa: e.activation(out=Ab[pa][:], in_=ps[pa][:, :], func=AF.Gelu),
                     reads=[ps_t[pa]], writes=[Ab_t[pa]])

            def st2(k):
                eb, t = its[k]
                pa = k % 2

                def emit_T(h):
                    i = h % 3
                    S.op("dve", lambda e, h=h, i=i: e.tensor_tensor(
                        out=Tb[i][:].rearrange("p (a b) -> p a b", a=4),
                        in0=sc[:, t, 2 * h, eb * 4:(eb + 1) * 4].unsqueeze(2).broadcast_to([128, 4, 128]),
                        in1=sc[:, t, 2 * h + 1, :].unsqueeze(1).broadcast_to([128, 4, 128]), op=ALU.add),
                         reads=[sc_t[t]], writes=[Tb_t[i]])
                    S.op("act", lambda e, h=h, i=i: e.activation(out=Eb[i][:], in_=Tb[i][:], func=AF.Exp, bias=nbias[:, t, h:h + 1]),
                         reads=[Tb_t[i], nbias_t[t]], writes=[Eb_t[i]])
                emit_T(0)
                emit_T(1)
                for h in range(8):
                    i = h % 3
                    if h + 2 < 8:
                        emit_T(h + 2)
                    accb, accb_t = (Wb[pa], Wb_t[pa]) if h % 2 == 0 else (Wc[pa], Wc_t[pa])
                    dst, dst_t = (accb, accb_t) if h < 2 else (Wh[h % 2], Wh_t[h % 2])
                    S.op("dve", lambda e, h=h, i=i, dst=dst: e.scalar_tensor_tensor(
                        out=dst[:], in0=Tb[i][:], scalar=best[:, t, h, 15:16], in1=Eb[i][:], op0=ALU.is_ge, op1=ALU.mult),
                         reads=[Tb_t[i], Eb_t[i], best_t[t]], writes=[dst_t])
                    if h >= 2:
                        S.op("pool" if h % 2 == 0 else "dve", lambda e, h=h, accb=accb: e.tensor_tensor(out=accb[:], in0=accb[:], in1=Wh[h % 2][:], op=ALU.add),
                             reads=[Wh_t[h % 2], accb_t], writes=[accb_t])
                S.op("dve", lambda e, pa=pa: e.tensor_tensor(out=Wb[pa][:], in0=Wb[pa][:], in1=Wc[pa][:], op=ALU.add),
                     reads=[Wb_t[pa], Wc_t[pa]], writes=[Wb_t[pa]])
                S.op("dve", lambda e, pa=pa: e.tensor_tensor(out=AW[pa][:], in0=Ab[pa][:], in1=Wb[pa][:], op=ALU.mult),
                     reads=[Ab_t[pa], Wb_t[pa]], writes=[AW_t[pa]])

            def st3a(k):
                pa = k % 2
                for es in range(4):
                    S.op("pe", lambda e, es=es, pa=pa: e.transpose(out=psb[pa][:, es * 128:(es + 1) * 128], in_=AW[pa][:, es * 128:(es + 1) * 128],
                                                                   identity=ident[:]),
                         reads=[AW_t[pa], ident_t], writes=[psb_t[pa]])
                S.op("act", lambda e, pa=pa: e.activation(out=AWT[pa][:], in_=psb[pa][:, 0:512].rearrange("p (s t) -> p s t", s=4), func=AF.Copy),
                     reads=[psb_t[pa]], writes=[AWT_t[pa]])

            def st3b(k):
                eb, t = its[k]
                slot = eb % 2
                pa = k % 2
                for db in range(4):
                    pbk = 2 + db
                    for es in range(4):
                        S.op("pe", lambda e, es=es, db=db, pbk=pbk, slot=slot, pa=pa: e.matmul(
                            out=ps[pbk][:, :], lhsT=AWT[pa][:, es, :], rhs=VS[slot][:, es, db * 512:(db + 1) * 512],
                            start=(es == 0), stop=(es == 3)),
                             reads=[AWT_t[pa], VS_t[slot]], writes=[ps_t[pbk]])

            def st3c(k):
                eb, t = its[k]
                for db in range(4):
                    pbk = 2 + db
                    if eb == 0:
                        S.op("act", lambda e, db=db, t=t, pbk=pbk: e.activation(out=acc[:, t, db * 512:(db + 1) * 512], in_=ps[pbk][:, :], func=AF.Copy),
                             reads=[ps_t[pbk]], writes=[acc_t[t]])
                    elif db % 2 == 0:
                        S.op("dve", lambda e, db=db, t=t, pbk=pbk: e.tensor_tensor(out=acc[:, t, db * 512:(db + 1) * 512],
                                                                                 in0=acc[:, t, db * 512:(db + 1) * 512], in1=ps[pbk][:, :], op=ALU.add),
                             reads=[ps_t[pbk], acc_t[t]], writes=[acc_t[t]])
                    else:
                        sg_, sg_t = stg[db // 2]
                        S.op("act", lambda e, pbk=pbk, sg_=sg_: e.activation(out=sg_[:], in_=ps[pbk][:, :], func=AF.Copy),
                             reads=[ps_t[pbk]], writes=[sg_t])
                        S.op("pool", lambda e, db=db, t=t, sg_=sg_: e.tensor_tensor(out=acc[:, t, db * 512:(db + 1) * 512],
                                                                                   in0=acc[:, t, db * 512:(db + 1) * 512], in1=sg_[:], op=ALU.add),
                             reads=[sg_t, acc_t[t]], writes=[acc_t[t]])

            for k in range(nit + 2):
                if 0 <= k - 2 < nit:
                    st3a(k - 2)
                if k < nit:
                    st1(k)
                if 0 <= k - 2 < nit:
                    st3b(k - 2)
                if 0 <= k - 1 < nit:
                    st2(k - 1)
                if k < nit:
                    st1g(k)
                if 0 <= k - 2 < nit:
                    st3c(k - 2)
            for t in range(NT):
                S.op("pool", lambda e, t=t: e.tensor_tensor(out=xh[:, t, :], in0=xh[:, t, :], in1=acc[:, t, :], op=ALU.add),
                     reads=[acc_t[t], xh_t[t]], writes=[xh_t[t]])
        if stage >= 3:
            S.dma("sp", "g", lambda e: e.dma_start(out=gvec[:], in_=gv[2:3, :].partition_broadcast(128)), writes=[gvec_t])
        for t in range(NT):
            if stage >= 3:
                _rms_rstd(S, "act", xh[:, t, :], xh_t[t], junk[:], junk_t, ss[:], ss_t, rstd[:], rstd_t, 1e-6)
                S.op("dve", lambda e, t=t: e.scalar_tensor_tensor(out=acc[:, t, :], in0=xh[:, t, :], scalar=rstd[:, 0:1], in1=gvec[:],
                                                                 op0=ALU.mult, op1=ALU.mult),
                     reads=[xh_t[t], rstd_t, gvec_t], writes=[acc_t[t]])
            else:
                S.op("dve", lambda e, t=t: e.tensor_copy(out=acc[:, t, :], in_=xh[:, t, :]), reads=[xh_t[t]], writes=[acc_t[t]])
            S.dma("sp", "o%d" % t, lambda e, t=t: e.dma_start(out=y[t0 + t * 128:t0 + (t + 1) * 128, :], in_=acc[:, t, :]),
                  reads=[acc_t[t]], writes=[])
    for k in S.dsem:
        if k.startswith("dma:o"):
            nc.sync.wait_ge(S.dsem[k][0], S.dsem[k][1])
    return nc, S


G1 = 256
NC1 = 1216
C_R, C_K, C_V, C_XW, C_XA, C_XG, C_QD, C_KD, C_VD = 0, 128, 256, 384, 480, 576, 832, 960, 1088
CHK = 32
NCAST = 11


def _roundrobin(gens):
    gens = list(gens)
    while gens:
        for g_ in list(gens):
            try:
                next(g_)
            except StopIteration:
                gens.remove(g_)


def build_l1(ngroups=SEQ // G1, do_rwkv=True, do_attn=True, rw_stage=9999):
    nc = bass.Bass("TRN2", target_bir_lowering=False)
    ntok = ngroups * G1
    ntile = ntok // 128
    dr = lambda n, s, k="ExternalInput": nc.dram_tensor(n, s, F32, kind=k).ap()
    x = dr("x", [ntok, D])
    w1 = dr("w1", [D, NC1])
    cvec = dr("cvec", [128, 20])
    wdu_d = dr("wdu", [96, 128]); wiu_d = dr("wiu", [96, 128]); wgu_d = dr("wgu", [256, 128])
    lamv = dr("lamv", [1, 256]); sublng = dr("sublng", [1, 128]); g1d = dr("g1", [1, D])
    consts = dr("consts", [7, 128, 128])
    rmask_d = dr("rmask", [1, G1])
    yaT = dr("yaT", [128, ntok], "ExternalOutput")
    yb = dr("yb", [ntok, 128], "ExternalOutput")
    castin = dr("castin", [NCAST, 128, 8192])
    castout = nc.dram_tensor("castout", [NCAST, 128, 8192], BF16, kind="ExternalOutput").ap()

    S = Sync(nc)
    for b in range(NCAST):
        S.dma("pool", "ocast", lambda e, b=b: e.dma_start(out=castout[b].rearrange("p (s e) -> p s e", e=2048),
                                                      in_=castin[b].rearrange("p (s e) -> p s e", e=2048)))
    sb = lambda n, s, dt=F32: nc.alloc_sbuf_tensor(n, s, dt)
    ps = [nc.alloc_psum_tensor("ps%d" % i, [128, 512], F32) for i in range(7)]
    psb = [nc.alloc_psum_tensor("psb%d" % i, [128, 1024], BF16) for i in range(1)]
    ps_t = [Tok() for _ in range(7)]
    psb_t = [Tok() for _ in range(1)]
    OB = [4, 6]
    scr_i = [0]

    def scr():
        scr_i[0] ^= 1
        return ps[scr_i[0]], ps_t[scr_i[0]]

    def T_(name, shape, dt=F32):
        return sb(name, shape, dt), Tok()

    W1, W1_t = T_("W1", [128, 16, NC1], BF16)
    KT, KT_t = T_("KT", [128, ntok], BF16)
    VA, VA_t = T_("VA", [128, ntile, 130], BF16)
    g1c, g1c_t = T_("g1c", [128, 16])
    xt = [T_("xt%d" % i, [128, D]) for i in range(2)]
    xnb, xnb_t = T_("xnb", [128, D], BF16)
    junk, junk_t = xnb, xnb_t
    junk2, junk2_t = T_("junk2", [128, 128], BF16)
    xnT, xnT_t = T_("xnT", [128, 16, G1], BF16)
    cst, cst_t = T_("cst", [128, 7, 128])
    identb, identb_t = T_("identb", [128, 128], BF16)
    trib, trib_t = T_("trib", [128, 128], BF16)
    rmask, rmask_t = T_("rmask_s", [128, G1])
    cv, cv_t = T_("cv_s", [128, 20])
    omm, omm_t = T_("omm", [128, 8])
    wdu, wdu_t = T_("wdus", [96, 128]); wiu, wiu_t = T_("wius", [96, 128]); wgu, wgu_t = T_("wgus", [128, 2, 128])
    lacc, lacc_t = T_("lacc", [128, 4])
    neglam, neglam_t = T_("neglam", [128, 1])
    sgv, sgv_t = T_("sgv", [128, 128])
    ss, ss_t = T_("ss", [128, 1]); rstd, rstd_t = T_("rstd", [128, 1])
    ss2, ss2_t = T_("ss2", [128, 1]); rstd2, rstd2_t = T_("rstd2", [128, 1])
    ident = cst[:, 0, :]; MU = cst[:, 1, :]; ML = cst[:, 2, :]; MUI = cst[:, 3, :]; onesblk = cst[:, 5, :]; I64 = cst[:, 6, 0:64]
    PB = [T_("PB%d" % i, [128, G1 + 1]) for i in range(7)]
    SH = [T_("SH%d" % i, [128, G1]) for i in range(7)]
    tmpA, tmpA_t = T_("tmpA", [128, G1]); tmpB, tmpB_t = T_("tmpB", [128, G1])
    logw, logw_t = T_("logw", [128, G1]); av, av_t = T_("av", [128, G1]); gg, gg_t = T_("gg", [128, G1])
    kkn, kkn_t = T_("kkn", [128, G1]); k2, k2_t = T_("k2", [128, G1]); bonus, bonus_t = T_("bonus", [128, G1])
    cum, cum_t = T_("cum", [128, G1]); Pm, Pm_t = T_("Pm", [128, G1]); Pinv, Pinv_t = T_("Pinv", [128, G1]); Pprev, Pprev_t = T_("Pprev", [128, G1])
    At, At_t = T_("At", [128, G1], BF16); Bt, Bt_t = T_("Bt", [128, G1], BF16); Kt, Kt_t = T_("Kt", [128, G1], BF16); Rt, Rt_t = T_("Rt", [128, G1], BF16)
    vb16, vb16_t = T_("vb16", [128, G1], BF16)
    yT, yT_t = T_("yT", [128, G1]); yo, yo_t = T_("yo", [128, G1])
    TOK, TOK_t = T_("TOK", [128, 4, 128], BF16)
    lam_s = yT[:, 0:256].rearrange("p (a c) -> p a c", c=64); lam_t = yT_t
    lamp = yo[:, 0:128].rearrange("p (b c) -> p b c", c=64); lamp_t = yo_t
    HB = []
    for h in range(2):
        HB.append(dict(
            XT=[T_("XT%d_%d" % (h, i), [128, 128], BF16) for i in range(5)], XX=[T_("XX%d_%d" % (h, i), [128, 128], BF16) for i in range(4)],
            LakT=T_("LakT%d" % h, [128, 128], BF16), MrbT=T_("MrbT%d" % h, [128, 128], BF16), MrkT=T_("MrkT%d" % h, [128, 128], BF16),
            Z=[T_("Z%d_%d" % (h, i), [128, 128], BF16) for i in range(2)], ZF=T_("ZF%d" % h, [128, 128], BF16),
            MZ=T_("MZ%d" % h, [128, 4, 64], BF16), MB=T_("MB%d" % h, [128, 4, 64], BF16), MK=T_("MK%d" % h, [128, 4, 64], BF16)))
    RhT, RhT_t = T_("RhT", [128, 128]); YhT, YhT_t = T_("YhT", [128, 128])
    MT, MT_t = T_("MT", [128, 4, 64]); HP, HP_t = T_("HP", [128, 4, 64])
    Sst = [T_("Sst%d" % i, [128, 64]) for i in range(2)]
    QT, QT_t = T_("QT", [128, G1], BF16); QSQ, QSQ_t = T_("QSQ", [128, G1]); KSQ, KSQ_t = T_("KSQ", [128, G1])
    kmax2 = [T_("kmax2_%d" % i, [128, 1]) for i in range(2)]
    kred, kred_t = T_("kred", [128, 1]); sqq, sqq_t = T_("sqq", [128, 1])
    nshift = [T_("nshift%d" % i, [128, 1]) for i in range(2)]
    Pb = [T_("Pb%d" % i, [128, 512], BF16) for i in range(2)]
    om = [[T_("om%d_%d" % (i, j), [128, 128]) for j in range(2)] for i in range(2)]
    qmax2 = [T_("qmax2_%d" % i, [128, 1]) for i in range(2)]
    rs_, rs_t = T_("rs_", [128, 1]); attn, attn_t = T_("attn", [128, 128]); ybo, ybo_t = T_("ybo", [128, 128])
    ones128, ones_t = T_("ones128", [128, 128])

    def mm(out, lhsT, rhs, start=True, stop=True):
        return lambda e: e.matmul(out=out, lhsT=lhsT, rhs=rhs, start=start, stop=stop)

    cpy_i = [0]

    def copy_out(out, in_, reads, writes, eng=None):
        if eng is None:
            cpy_i[0] ^= 1
            eng = "act" if cpy_i[0] else "dve"
        if eng == "act":
            S.op("act", lambda e: e.activation(out=out, in_=in_, func=AF.Copy), reads=reads, writes=writes)
        else:
            S.op("dve", lambda e: e.tensor_copy(out=out, in_=in_), reads=reads, writes=writes)

    def dve_tt(out, in0, in1, op, reads, writes, eng="dve"):
        S.op(eng, lambda e: e.tensor_tensor(out=out, in0=in0, in1=in1, op=op), reads=reads, writes=writes)

    def dve_ts(out, in0, s1, s2, op0, op1, reads, writes, eng="dve"):
        if op1 is None:
            S.op(eng, lambda e: e.tensor_scalar(out=out, in0=in0, scalar1=s1, scalar2=None, op0=op0), reads=reads, writes=writes)
        else:
            S.op(eng, lambda e: e.tensor_scalar(out=out, in0=in0, scalar1=s1, scalar2=s2, op0=op0, op1=op1), reads=reads, writes=writes)

    def dve_stt(out, in0, scalar, in1, op0, op1, reads, writes):
        S.op("dve", lambda e: e.scalar_tensor_tensor(out=out, in0=in0, scalar=scalar, in1=in1, op0=op0, op1=op1), reads=reads, writes=writes)

    def act(out, in_, func, reads, writes, **kw):
        S.op("act", lambda e: e.activation(out=out, in_=in_, func=func, **kw), reads=reads, writes=writes)

    S.dma("sp", "c", lambda e: e.dma_start(out=cst[:], in_=consts.rearrange("c p n -> p c n")), writes=[cst_t])
    S.dma("sp", "c", lambda e: e.dma_start(out=cv[:], in_=cvec[:, :]), writes=[cv_t])
    S.dma("sp", "c", lambda e: e.dma_start(out=wdu[:], in_=wdu_d[:, :]), writes=[wdu_t])
    S.dma("sp", "c", lambda e: e.dma_start(out=wiu[:], in_=wiu_d[:, :]), writes=[wiu_t])
    S.dma("sp", "c", lambda e: e.dma_start(out=wgu[:], in_=wgu_d.rearrange("(k p) c -> p k c", p=128)), writes=[wgu_t])
    S.dma("sp", "c", lambda e: e.dma_start(out=yT[:, 0:256], in_=lamv[0:1, :].partition_broadcast(128)), writes=[lam_t])
    S.dma("sp", "c", lambda e: e.dma_start(out=sgv[:], in_=sublng[0:1, :].partition_broadcast(128)), writes=[sgv_t])
    S.dma("sp", "c", lambda e: e.dma_start(out=rmask[:], in_=rmask_d[0:1, :].partition_broadcast(128)), writes=[rmask_t])
    with nc.allow_non_contiguous_dma(reason="tiny gain vector"):
        S.dma("sp", "c", lambda e: e.dma_start(out=g1c[:], in_=g1d.rearrange("o (k p) -> p (o k)", p=128)), writes=[g1c_t])
    S.dma("pool", "w", lambda e: e.dma_start(out=W1[:], in_=w1.rearrange("(k p) c -> p k c", p=128)), writes=[W1_t])
    for kc in range(16):
        S.op("dve" if kc % 2 else "pool", lambda e, kc=kc: e.tensor_scalar(out=W1[:, kc, :], in0=W1[:, kc, :], scalar1=g1c[:, kc:kc + 1], scalar2=0.0,
                                                                            op0=ALU.mult, op1=ALU.add),
             reads=[W1_t, g1c_t], writes=[W1_t])
    S.op("dve", lambda e: e.tensor_copy(out=identb[:], in_=cst[:, 0, :]), reads=[cst_t], writes=[identb_t])
    S.op("dve", lambda e: e.tensor_copy(out=trib[:], in_=cst[:, 4, :]), reads=[cst_t], writes=[trib_t])
    dve_ts(omm[:, 0:8], cv[:, 0:8], -1.0, 1.0, ALU.mult, ALU.add, [cv_t], [omm_t])
    dve_ts(cv[:, 14:15], cv[:, 10:11], -1.0, 1.0, ALU.mult, ALU.add, [cv_t], [cv_t])
    dve_ts(sgv[:], sgv[:], 0.8, None, ALU.mult, None, [sgv_t], [sgv_t])
    dve_tt(lamp[:, 0, :], lam_s[:, 0, :], lam_s[:, 1, :], ALU.mult, [lam_t], [lamp_t])
    dve_tt(lamp[:, 1, :], lam_s[:, 2, :], lam_s[:, 3, :], ALU.mult, [lam_t], [lamp_t])
    S.op("dve", lambda e: e.tensor_reduce(out=lacc[:, 0:2], in_=lamp[:, 0:2, :], axis=AX.X, op=ALU.add), reads=[lamp_t], writes=[lacc_t])
    act(lacc[:, 2:4], lacc[:, 0:2], AF.Exp, [lacc_t], [lacc_t])
    dve_tt(neglam[:], lacc[:, 3:4], lacc[:, 2:3], ALU.subtract, [lacc_t], [neglam_t])
    dve_ts(neglam[:], neglam[:], -0.2, None, ALU.add, None, [neglam_t], [neglam_t])
    for i in range(7):
        S.op("pool", lambda e, i=i: e.memset(PB[i][0][:, 0:1], 0.0), writes=[PB[i][1]])
    for i in range(2):
        S.op("pool", lambda e, i=i: e.memset(Sst[i][0][:], 0.0), writes=[Sst[i][1]])
        S.op("pool", lambda e, i=i: e.memset(kmax2[i][0][:], 0.0), writes=[kmax2[i][1]])
        S.op("pool", lambda e, i=i: e.memset(qmax2[i][0][:], 0.0), writes=[qmax2[i][1]])
    S.op("pool", lambda e: e.memset(VA[:, :, 128:130], 1.0), writes=[VA_t])
    S.op("pool", lambda e: e.memset(ones128[:], 1.0), writes=[ones_t])

    scur = [0]

    def head_chain(h, cs):
        hb = HB[h]
        XT, XX, Z = hb["XT"], hb["XX"], hb["Z"]
        LakT, LakT_t = hb["LakT"]; MrbT, MrbT_t = hb["MrbT"]; MrkT, MrkT_t = hb["MrkT"]
        zf, zf_t = hb["ZF"]
        MZ, MZ_t = hb["MZ"]; MB, MB_t = hb["MB"]; MK, MK_t = hb["MK"]
        pb, pb_t = ps[h], ps_t[h]
        hs = slice(64 * h, 64 * h + 64)
        Ah, Bh, Kh, Rh = At[hs, cs], Bt[hs, cs], Kt[hs, cs], Rt[hs, cs]
        S.op("pe", mm(pb[:, 0:128], Bh, Ah), reads=[Bt_t, At_t], writes=[pb_t])
        dve_tt(XT[0][0][:], pb[:, 0:128], MU, ALU.mult, [pb_t, cst_t], [XT[0][1]])
        yield
        S.op("pe", mm(pb[:, 0:128], Ah, Bh), reads=[Bt_t, At_t], writes=[pb_t])
        dve_tt(XX[0][0][:], pb[:, 0:128], ML, ALU.mult, [pb_t, cst_t], [XX[0][1]])
        yield
        S.op("pe", mm(pb[:, 0:128], Kh, Ah), reads=[Kt_t, At_t], writes=[pb_t])
        dve_tt(LakT[:], pb[:, 0:128], MU, ALU.mult, [pb_t, cst_t], [LakT_t])
        yield
        S.op("pe", mm(pb[:, 0:128], Bh, Rh), reads=[Bt_t, Rt_t], writes=[pb_t])
        dve_tt(MrbT[:], pb[:, 0:128], MUI, ALU.mult, [pb_t, cst_t], [MrbT_t])
        yield
        S.op("pe", mm(pb[:, 0:128], Kh, Rh), reads=[Kt_t, Rt_t], writes=[pb_t])
        dve_tt(MrkT[:], pb[:, 0:128], MUI, ALU.mult, [pb_t, cst_t], [MrkT_t])
        yield
        S.op("pe", mm(pb[:, 0:64], LakT[:], TOK[:, 3, hs]), reads=[LakT_t, TOK_t], writes=[pb_t])
        zc, zc_t = Z[0]
        copy_out(zc[:, 64:128], pb[:, 0:64], [pb_t], [zc_t], eng="dve")
        S.op("pool", lambda e: e.tensor_copy(out=zc[:, 0:64], in_=TOK[:, 0, hs]), reads=[TOK_t], writes=[zc_t])
        yield
        for i in range(4):
            S.op("pe", mm(pb[:, 0:128], XX[i][0][:], XT[i][0][:]), reads=[XX[i][1], XT[i][1]], writes=[pb_t])
            copy_out(XT[i + 1][0][:], pb[:, 0:128], [pb_t], [XT[i + 1][1]], eng="dve")
            yield
            if i < 3:
                S.op("pe", mm(pb[:, 0:128], XT[i][0][:], XX[i][0][:]), reads=[XX[i][1], XT[i][1]], writes=[pb_t])
                copy_out(XX[i + 1][0][:], pb[:, 0:128], [pb_t], [XX[i + 1][1]], eng="act")
                yield
            zc, zc_t = Z[i % 2]
            zn, zn_t = Z[(i + 1) % 2]
            S.op("pe", mm(pb[:, 0:128], XT[i][0][:], zc[:]), reads=[XT[i][1], zc_t], writes=[pb_t])
            dve_tt(zn[:], pb[:, 0:128], zc[:], ALU.add, [pb_t, zc_t], [zn_t])
            yield
        zc, zc_t = Z[0]
        S.op("pe", mm(pb[:, 0:128], XT[4][0][:], zc[:]), reads=[XT[4][1], zc_t], writes=[pb_t])
        dve_tt(zf[:], pb[:, 0:128], zc[:], ALU.add, [pb_t, zc_t], [zf_t])
        yield
        S.op("pe", mm(pb[hs, 0:128], zf[:, 0:64], MrbT[:]), reads=[zf_t, MrbT_t], writes=[pb_t])
        dve_tt(RhT[hs, :], pb[hs, 0:128], Rh, ALU.add, [pb_t, Rt_t], [RhT_t])
        yield
        S.op("pe", mm(pb[hs, 0:128], zf[:, 64:128], MrbT[:], True, False), reads=[zf_t, MrbT_t], writes=[pb_t])
        S.op("pe", mm(pb[hs, 0:128], TOK[:, 3, hs], MrkT[:], False, True), reads=[TOK_t, MrkT_t], writes=[pb_t])
        copy_out(YhT[hs, :], pb[hs, 0:128], [pb_t], [YhT_t], eng="act")
        cmb = cv[:, 16:20].unsqueeze(2).broadcast_to([128, 4, 64])
        dve_tt(MZ[:], zf[:, 0:64].unsqueeze(1).broadcast_to([128, 4, 64]), cmb, ALU.mult, [zf_t, cv_t], [MZ_t])
        dve_tt(MB[:], TOK[:, 1, hs].unsqueeze(1).broadcast_to([128, 4, 64]), cmb, ALU.mult, [TOK_t, cv_t], [MB_t], eng="pool")
        dve_tt(MK[:], TOK[:, 2, hs].unsqueeze(1).broadcast_to([128, 4, 64]), cmb, ALU.mult, [TOK_t, cv_t], [MK_t], eng="pool")
        yield
        for c in range(4):
            S.op("pe", mm(ps[2][hs, c * 64:(c + 1) * 64], MZ[:, c, :], TOK[:, 1, hs]), reads=[MZ_t, TOK_t], writes=[ps_t[2]])
        for c in range(4):
            S.op("pe", mm(ps[3][hs, c * 64:(c + 1) * 64], MB[:, c, :], zf[:, 64:128], True, False), reads=[zf_t, MB_t], writes=[ps_t[3]])
            S.op("pe", mm(ps[3][hs, c * 64:(c + 1) * 64], MK[:, c, :], TOK[:, 3, hs], False, True), reads=[TOK_t, MK_t], writes=[ps_t[3]])
        yield

    def rwkv_group(g):
        t0 = g * G1
        r_, k_, v_, xw_, xa_, xg0_, xg1_ = range(7)
        rows = [128, 128, 128, 96, 96, 128, 128]
        for b in range(7):
            n = rows[b]
            pbuf, pbt = PB[b]
            sh, sht = SH[b]
            dve_ts(tmpA[0:n, :], pbuf[0:n, 0:G1], cv[0:n, b:b + 1], None, ALU.mult, None, [pbt, cv_t], [tmpA_t])
            dve_stt(sh[0:n, :], pbuf[0:n, 1:G1 + 1], omm[0:n, b:b + 1], tmpA[0:n, :], ALU.mult, ALU.add, [pbt, omm_t, tmpA_t], [sht])
            S.op("pool", lambda e, pbuf=pbuf, n=n: e.tensor_copy(out=pbuf[0:n, 0:1], in_=pbuf[0:n, G1:G1 + 1]), reads=[pbt], writes=[pbt])
            if b % 2:
                yield
        shr, shr_t = SH[r_]; shk, shk_t = SH[k_]; shv, shv_t = SH[v_]
        act(tmpB[0:96, :], SH[xw_][0][0:96, :], AF.Tanh, [SH[xw_][1]], [tmpB_t])
        pb, pb_t = scr()
        S.op("pe", mm(pb[:, 0:G1], wdu[:, :], tmpB[0:96, :]), reads=[wdu_t, tmpB_t], writes=[pb_t])
        act(logw[:], pb[:, 0:G1], AF.Sigmoid, [pb_t, cv_t], [logw_t], bias=cv[:, 7:8])
        dve_ts(logw[:], logw[:], -0.6065306597126334, None, ALU.mult, None, [logw_t], [logw_t])
        pb, pb_t = scr()
        S.op("pe", mm(pb[:, 0:G1], wiu[:, :], SH[xa_][0][0:96, :]), reads=[wiu_t, SH[xa_][1]], writes=[pb_t])
        act(av[:], pb[:, 0:G1], AF.Sigmoid, [pb_t, cv_t], [av_t], bias=cv[:, 8:9])
        act(SH[xg0_][0][:], SH[xg0_][0][:], AF.Sigmoid, [SH[xg0_][1]], [SH[xg0_][1]])
        act(SH[xg1_][0][:], SH[xg1_][0][:], AF.Sigmoid, [SH[xg1_][1]], [SH[xg1_][1]])
        yield
        pb, pb_t = scr()
        S.op("pe", mm(pb[:, 0:G1], wgu[:, 0, :], SH[xg0_][0][:], True, False), reads=[wgu_t, SH[xg0_][1]], writes=[pb_t])
        S.op("pe", mm(pb[:, 0:G1], wgu[:, 1, :], SH[xg1_][0][:], False, True), reads=[wgu_t, SH[xg1_][1]], writes=[pb_t])
        copy_out(gg[:], pb[:, 0:G1], [pb_t], [gg_t], eng="dve")
        dve_ts(kkn[:], shk[:], cv[:, 9:10], None, ALU.mult, None, [shk_t, cv_t], [kkn_t])
        dve_tt(tmpA[:], kkn[:], kkn[:], ALU.mult, [kkn_t], [tmpA_t], eng="pool")
        pb, pb_t = scr()
        S.op("pe", mm(pb[:, 0:G1], onesblk, tmpA[:]), reads=[cst_t, tmpA_t], writes=[pb_t])
        act(tmpB[:], pb[:, 0:G1], AF.Sqrt, [pb_t], [tmpB_t])
        dve_ts(tmpB[:], tmpB[:], 1e-12, None, ALU.max, None, [tmpB_t], [tmpB_t])
        S.op("dve", lambda e: e.reciprocal(out=tmpB[:], in_=tmpB[:]), reads=[tmpB_t], writes=[tmpB_t])
        dve_tt(kkn[:], kkn[:], tmpB[:], ALU.mult, [kkn_t, tmpB_t], [kkn_t])
        yield
        dve_ts(tmpA[:], av[:], cv[:, 10:11], cv[:, 14:15], ALU.mult, ALU.add, [av_t, cv_t], [tmpA_t])
        dve_tt(k2[:], shk[:], tmpA[:], ALU.mult, [shk_t, tmpA_t], [k2_t])
        dve_tt(tmpA[:], shr[:], k2[:], ALU.mult, [shr_t, k2_t], [tmpA_t], eng="pool")
        dve_ts(tmpA[:], tmpA[:], cv[:, 11:12], None, ALU.mult, None, [tmpA_t, cv_t], [tmpA_t])
        pb, pb_t = scr()
        S.op("pe", mm(pb[:, 0:G1], onesblk, tmpA[:]), reads=[cst_t, tmpA_t], writes=[pb_t])
        dve_tt(bonus[:], pb[:, 0:G1], shv[:], ALU.mult, [pb_t, shv_t], [bonus_t])
        S.op("dve", lambda e: e.tensor_tensor_scan(out=cum[:], data0=rmask[:], data1=logw[:], initial=0.0, op0=ALU.mult, op1=ALU.add),
             reads=[rmask_t, logw_t], writes=[cum_t])
        yield
        act(Pm[:], cum[:], AF.Exp, [cum_t], [Pm_t])
        act(Pinv[:], cum[:], AF.Exp, [cum_t], [Pinv_t], scale=-1.0)
        dve_tt(tmpA[:], cum[:], logw[:], ALU.subtract, [cum_t, logw_t], [tmpA_t], eng="pool")
        act(Pprev[:], tmpA[:], AF.Exp, [tmpA_t], [Pprev_t])
        dve_stt(At[:], kkn[:], -1.0, Pprev[:], ALU.mult, ALU.mult, [kkn_t, Pprev_t], [At_t])
        dve_tt(tmpB[:], kkn[:], av[:], ALU.mult, [kkn_t, av_t], [tmpB_t], eng="pool")
        dve_tt(Bt[:], tmpB[:], Pinv[:], ALU.mult, [tmpB_t, Pinv_t], [Bt_t])
        dve_tt(Kt[:], k2[:], Pinv[:], ALU.mult, [k2_t, Pinv_t], [Kt_t], eng="pool")
        dve_tt(Rt[:], shr[:], Pm[:], ALU.mult, [shr_t, Pm_t], [Rt_t])
        S.op("pool", lambda e: e.tensor_copy(out=vb16[:], in_=shv[:]), reads=[shv_t], writes=[vb16_t])
        yield
        for tl in range(G1 // 128):
            cs = slice(tl * 128, (tl + 1) * 128)
            for j, (src, srct) in enumerate([(At, At_t), (Bt, Bt_t), (Kt, Kt_t), (vb16, vb16_t)]):
                S.op("pe", lambda e, j=j, src=src: e.transpose(out=psb[0][:, j * 128:(j + 1) * 128], in_=src[:, cs], identity=identb[:]),
                     reads=[srct, identb_t], writes=[psb_t[0]])
            copy_out(TOK[:].rearrange("p a b -> p (a b)"), psb[0][:, 0:512], [psb_t[0]], [TOK_t], eng="act")
            yield
            chains = [head_chain(0, cs), head_chain(1, cs)]
            while chains:
                for ch in list(chains):
                    try:
                        next(ch)
                    except StopIteration:
                        chains.remove(ch)
                yield
            dve_tt(MT[:], ps[2][:, 0:256].rearrange("p (c k) -> p c k", c=4), I64.unsqueeze(1).broadcast_to([128, 4, 64]), ALU.add,
                   [ps_t[2], cst_t], [MT_t])
            for c in range(4):
                col = tl * 128 + 32 * c + 31
                dve_ts(HP[:, c, :], ps[3][:, c * 64:(c + 1) * 64], Pm[:, col:col + 1], None, ALU.mult, None, [ps_t[3], Pm_t], [HP_t])
            yield
            for c in range(4):
                col = tl * 128 + 32 * c + 31
                sc_, sc_t = Sst[scur[0]]
                sn_, sn_t = Sst[1 - scur[0]]
                for h in range(2):
                    hs = slice(64 * h, 64 * h + 64)
                    S.op("pe", mm(ps[0][hs, 0:64], MT[hs, c, :], sc_[hs, :]), reads=[MT_t, sc_t], writes=[ps_t[0]])
                for h in range(2):
                    hs = slice(64 * h, 64 * h + 64)
                    S.op("pe", mm(ps[1][hs, 32 * c:32 * c + 32], sc_[hs, :], RhT[hs, 32 * c:32 * c + 32]), reads=[sc_t, RhT_t], writes=[ps_t[1]])
                dve_stt(sn_[:], ps[0][:, 0:64], Pm[:, col:col + 1], HP[:, c, :], ALU.mult, ALU.add, [ps_t[0], Pm_t, HP_t], [sn_t])
                scur[0] = 1 - scur[0]
                yield
            dve_tt(yT[:, cs], ps[1][:, 0:128], YhT[:], ALU.add, [ps_t[1], YhT_t], [yT_t])
            yield
        pb, pb_t = scr()
        S.op("pe", mm(pb[:, 0:G1], onesblk, yT[:]), reads=[cst_t, yT_t], writes=[pb_t])
        act(tmpA[:], pb[:, 0:G1], AF.Copy, [pb_t], [tmpA_t], scale=1.0 / 64)
        act(tmpB[:], yT[:], AF.Square, [yT_t], [tmpB_t])
        pb, pb_t = scr()
        S.op("pe", mm(pb[:, 0:G1], onesblk, tmpB[:]), reads=[cst_t, tmpB_t], writes=[pb_t])
        dve_tt(tmpB[:], tmpA[:], tmpA[:], ALU.mult, [tmpA_t], [tmpB_t], eng="pool")
        dve_stt(tmpB[:], pb[:, 0:G1], 1.0 / 64, tmpB[:], ALU.mult, ALU.subtract, [pb_t, tmpB_t], [tmpB_t])
        yield
        act(tmpB[:], tmpB[:], AF.Sqrt, [tmpB_t], [tmpB_t], bias=64e-5)
        S.op("dve", lambda e: e.reciprocal(out=tmpB[:], in_=tmpB[:]), reads=[tmpB_t], writes=[tmpB_t])
        dve_tt(yo[:], yT[:], tmpA[:], ALU.subtract, [yT_t, tmpA_t], [yo_t])
        dve_tt(yo[:], yo[:], tmpB[:], ALU.mult, [yo_t, tmpB_t], [yo_t])
        dve_ts(yo[:], yo[:], cv[:, 12:13], cv[:, 13:14], ALU.mult, ALU.add, [yo_t, cv_t], [yo_t])
        dve_tt(yo[:], yo[:], bonus[:], ALU.add, [yo_t, bonus_t], [yo_t], eng="pool")
        dve_tt(yo[:], yo[:], gg[:], ALU.mult, [yo_t, gg_t], [yo_t])
        S.dma("sp", "oya", lambda e: e.dma_start(out=yaT[:, t0:t0 + G1], in_=yo[:]), reads=[yo_t], writes=[])
        yield

    def attn_group(g):
        for m in range(2):
            ms = slice(64 * m, 64 * m + 64)
            nsh, nsh_t = nshift[m]
            act(sqq[:], qmax2[m][0][:], AF.Sqrt, [qmax2[m][1], kmax2[m][1]], [sqq_t], scale=kmax2[m][0][:, 0:1])
            dve_ts(nsh[:], sqq[:], -0.125, None, ALU.mult, None, [sqq_t], [nsh_t])

            def stage_a(j):
                P_, P_t = Pb[j % 2]
                if j < g:
                    for a in range(2):
                        kt = 2 * j + a
                        S.op("pe", mm(ps[5][:, a * 256:(a + 1) * 256], KT[ms, kt * 128:(kt + 1) * 128], QT[ms, :]), reads=[QT_t, KT_t], writes=[ps_t[5]])
                    act(P_[:], ps[5][:, :], AF.Exp, [ps_t[5], nsh_t], [P_t], scale=0.125, bias=nsh[:, 0:1])
                else:
                    kt = 2 * g
                    S.op("pe", mm(ps[5][:, 0:256], KT[ms, kt * 128:(kt + 1) * 128], QT[ms, :]), reads=[QT_t, KT_t], writes=[ps_t[5]])
                    S.op("pe", mm(ps[5][:, 384:512], KT[ms, (kt + 1) * 128:(kt + 2) * 128], QT[ms, 128:256]), reads=[QT_t, KT_t], writes=[ps_t[5]])
                    act(P_[:, 0:256], ps[5][:, 0:256], AF.Exp, [ps_t[5], nsh_t], [P_t], scale=0.125, bias=nsh[:, 0:1])
                    act(P_[:, 384:512], ps[5][:, 384:512], AF.Exp, [ps_t[5], nsh_t], [P_t], scale=0.125, bias=nsh[:, 0:1])
                    dve_tt(P_[:, 0:128], P_[:, 0:128], trib[:], ALU.mult, [P_t, trib_t], [P_t], eng="pool")
                    dve_tt(P_[:, 384:512], P_[:, 384:512], trib[:], ALU.mult, [P_t, trib_t], [P_t], eng="pool")

            def stage_b(j):
                P_, P_t = Pb[j % 2]
                if j < g:
                    for a in range(2):
                        kt = 2 * j + a
                        for tl in range(2):
                            ob = OB[tl]
                            S.op("pe", mm(ps[ob][:, 0:129], P_[:, a * 256 + tl * 128:a * 256 + (tl + 1) * 128], VA[:, kt, 0:129], kt == 0, False),
                                 reads=[P_t, VA_t], writes=[ps_t[ob]])
                else:
                    kt = 2 * g
                    S.op("pe", mm(ps[OB[0]][:, 0:129], P_[:, 0:128], VA[:, kt, 0:129], kt == 0, True), reads=[P_t, VA_t], writes=[ps_t[OB[0]]])
                    S.op("pe", mm(ps[OB[1]][:, 0:129], P_[:, 128:256], VA[:, kt, 0:129], kt == 0, False), reads=[P_t, VA_t], writes=[ps_t[OB[1]]])
                    S.op("pe", mm(ps[OB[1]][:, 0:129], P_[:, 384:512], VA[:, kt + 1, 0:129], False, True), reads=[P_t, VA_t], writes=[ps_t[OB[1]]])

            stage_a(0)
            for j in range(g + 1):
                if j + 1 <= g:
                    stage_a(j + 1)
                stage_b(j)
                yield
            for tl in range(2):
                ob = OB[tl]
                S.op("dve", lambda e, ob=ob: e.reciprocal(out=rs_[:], in_=ps[ob][:, 128:129]), reads=[ps_t[ob]], writes=[rs_t])
                dve_ts(om[tl][m][0][:], ps[ob][:, 0:128], rs_[:, 0:1], None, ALU.mult, None, [ps_t[ob], rs_t], [om[tl][m][1]])
            yield
        for tl in range(2):
            qt = 2 * g + tl
            dve_stt(attn[:], om[tl][1][0][:], neglam[:, 0:1], om[tl][0][0][:], ALU.mult, ALU.add, [om[tl][0][1], om[tl][1][1], neglam_t], [attn_t])
            S.op("act", lambda e: e.activation(out=junk2[:], in_=attn[:], func=AF.Square, accum_out=ss2[:]), reads=[attn_t], writes=[junk2_t, ss2_t])
            S.op("act", lambda e: e.activation(out=ss2[:], in_=ss2[:], func=AF.Sqrt, scale=1.0 / 128, bias=1e-5), reads=[ss2_t], writes=[ss2_t])
            S.op("dve", lambda e: e.reciprocal(out=rstd2[:], in_=ss2[:]), reads=[ss2_t], writes=[rstd2_t])
            dve_stt(ybo[:], attn[:], rstd2[:, 0:1], sgv[:], ALU.mult, ALU.mult, [attn_t, rstd2_t, sgv_t], [ybo_t])
            S.dma("sp", "oyb", lambda e, qt=qt: e.dma_start(out=yb[qt * 128:(qt + 1) * 128, :], in_=ybo[:]), reads=[ybo_t], writes=[])
            yield

    for g in range(ngroups):
        t0 = g * G1
        for tl in range(2):
            xb, xb_t = xt[tl]
            S.dma("sp", "x%d" % tl, lambda e, tl=tl, xb=xb: e.dma_start(out=xb[:], in_=x[t0 + tl * 128:t0 + (tl + 1) * 128, :]), writes=[xb_t])
            _rms_rstd(S, "act", xb[:], xb_t, junk[:], junk_t, ss[:], ss_t, rstd[:], rstd_t, 1e-6)
            dve_ts(xnb[:], xb[:], rstd[:, 0:1], None, ALU.mult, None, [xb_t, rstd_t], [xnb_t])
            for half in range(2):
                for j in range(8):
                    kc = half * 8 + j
                    S.op("pe", lambda e, kc=kc, j=j: e.transpose(out=psb[0][:, j * 128:(j + 1) * 128], in_=xnb[:, kc * 128:(kc + 1) * 128], identity=identb[:]),
                         reads=[xnb_t, identb_t], writes=[psb_t[0]])
                copy_out(xnT[:, half * 8:(half + 1) * 8, tl * 128:(tl + 1) * 128], psb[0][:].rearrange("p (k t) -> p k t", k=8), [psb_t[0]], [xnT_t])
        blocks = [(C_R, 128), (C_K, 128), (C_V, 128), (C_XW, 96), (C_XA, 96), (C_XG, 128), (C_XG + 128, 128)]
        for bi, (c0, w) in enumerate(blocks):
            pb, pb_t = scr()
            for kc in range(16):
                S.op("pe", mm(pb[0:w, 0:G1], W1[:, kc, c0:c0 + w], xnT[:, kc, :], kc == 0, kc == 15), reads=[W1_t, xnT_t], writes=[pb_t])
            copy_out(PB[bi][0][0:w, 1:G1 + 1], pb[0:w, 0:G1], [pb_t], [PB[bi][1]])
        pb, pb_t = scr()
        for kc in range(16):
            S.op("pe", mm(pb[:, 0:G1], W1[:, kc, C_QD:C_QD + 128], xnT[:, kc, :], kc == 0, kc == 15), reads=[W1_t, xnT_t], writes=[pb_t])
        act(QT[:], pb[:, 0:G1], AF.Copy, [pb_t], [QT_t])
        act(QSQ[:], pb[:, 0:G1], AF.Square, [pb_t], [QSQ_t])
        pb, pb_t = scr()
        for kc in range(16):
            S.op("pe", mm(pb[:, 0:G1], W1[:, kc, C_KD:C_KD + 128], xnT[:, kc, :], kc == 0, kc == 15), reads=[W1_t, xnT_t], writes=[pb_t])
        act(KT[:, t0:t0 + G1], pb[:, 0:G1], AF.Copy, [pb_t], [KT_t])
        act(KSQ[:], pb[:, 0:G1], AF.Square, [pb_t], [KSQ_t])
        for m in range(2):
            ms = slice(64 * m, 64 * m + 64)
            pb, pb_t = scr()
            S.op("pe", mm(pb[:, 0:G1], ones128[ms, :], KSQ[ms, :]), reads=[ones_t, KSQ_t], writes=[pb_t])
            S.op("dve", lambda e, pb=pb: e.tensor_reduce(out=kred[:], in_=pb[:, 0:G1], axis=AX.X, op=ALU.max), reads=[pb_t], writes=[kred_t])
            dve_tt(kmax2[m][0][:], kmax2[m][0][:], kred[:], ALU.max, [kred_t, kmax2[m][1]], [kmax2[m][1]])
            pb, pb_t = scr()
            S.op("pe", mm(pb[:, 0:G1], ones128[ms, :], QSQ[ms, :]), reads=[ones_t, QSQ_t], writes=[pb_t])
            S.op("dve", lambda e, pb=pb: e.tensor_reduce(out=kred[:], in_=pb[:, 0:G1], axis=AX.X, op=ALU.max), reads=[pb_t], writes=[kred_t])
            dve_tt(qmax2[m][0][:], qmax2[m][0][:], kred[:], ALU.max, [kred_t, qmax2[m][1]], [qmax2[m][1]])
        for tl in range(2):
            pb, pb_t = scr()
            for kc in range(16):
                S.op("pe", mm(pb[:, 0:128], xnT[:, kc, tl * 128:(tl + 1) * 128], W1[:, kc, C_VD:C_VD + 128], kc == 0, kc == 15),
                     reads=[W1_t, xnT_t], writes=[pb_t])
            copy_out(VA[:, 2 * g + tl, 0:128], pb[:, 0:128], [pb_t], [VA_t])
        gens = []
        if do_rwkv:
            gr = rwkv_group(g)
            if rw_stage < 9000:
                def lim(gr=gr):
                    for _ in range(rw_stage):
                        next(gr)
                        yield
                gr = lim()
            gens.append(gr)
        if do_attn:
            gens.append(attn_group(g))
        _roundrobin(gens)
    for k in S.dsem:
        if k.startswith("dma:o"):
            nc.sync.wait_ge(S.dsem[k][0], S.dsem[k][1])
    return nc, S


def _consts():
    t = np.arange(128)
    same = (t[:, None] // CHK) == (t[None, :] // CHK)
    MU = (same & (t[:, None] < t[None, :])).astype(np.float32)
    ML = MU.T.copy()
    MUI = (same & (t[:, None] <= t[None, :])).astype(np.float32)
    TRI = (t[:, None] <= t[None, :]).astype(np.float32)
    ob = ((t[:, None] // 64) == (t[None, :] // 64)).astype(np.float32)
    i64 = np.zeros((128, 128), np.float32)
    i64[t, t % 64] = 1.0
    return np.stack([np.eye(128, dtype=np.float32), MU, ML, MUI, TRI, ob, i64])


def prep_l1(inp, c, ntok=SEQ):
    w_in = inp["w_in"][0]
    hs = slice(128 * c, 128 * c + 128)
    o_d = 3520
    cols = np.concatenate([np.arange(128 * c, 128 * c + 128), 1024 + np.arange(128 * c, 128 * c + 128),
                           2048 + np.arange(128 * c, 128 * c + 128), np.arange(3072, 3520),
                           o_d + np.arange(128 * c, 128 * c + 128), o_d + 1024 + np.arange(128 * c, 128 * c + 128),
                           o_d + 2048 + np.arange(128 * c, 128 * c + 128)])
    mu = inp["shift_mu"][0]
    cvec = np.zeros((128, 20), np.float32)
    cvec[:, 0] = mu[0:1024][hs]; cvec[:, 1] = mu[1024:2048][hs]; cvec[:, 2] = mu[2048:3072][hs]
    cvec[:96, 3] = mu[3072:3168]; cvec[:96, 4] = mu[3168:3264]; cvec[:, 5] = mu[3264:3392]; cvec[:, 6] = mu[3392:3520]
    cvec[:, 7] = inp["rwkv_w0"][0][hs]; cvec[:, 8] = inp["rwkv_a0"][0][hs]; cvec[:, 9] = inp["k_k"][0][hs]
    cvec[:, 10] = inp["k_a"][0][hs]; cvec[:, 11] = inp["r_k"][0].reshape(-1)[hs]
    cvec[:, 12] = inp["lnx_g"][0][hs]; cvec[:, 13] = inp["lnx_b"][0][hs]
    for c_ in range(4):
        cvec[32 * c_:32 * c_ + 32, 16 + c_] = 1.0
    rm = np.ones((1, G1), np.float32); rm[0, ::CHK] = 0.0
    return dict(
        x=np.ascontiguousarray(inp["x"][0, :ntok]), w1=np.ascontiguousarray(w_in[:, cols]), cvec=cvec,
        wdu=np.ascontiguousarray(inp["w_decay_up"][0][:, hs]), wiu=np.ascontiguousarray(inp["w_iclr_up"][0][:, hs]),
        wgu=np.ascontiguousarray(inp["w_gate_up"][0][:, hs]),
        lamv=np.concatenate([inp["lam_q1"][0], inp["lam_k1"][0], inp["lam_q2"][0], inp["lam_k2"][0]])[None, :].astype(np.float32),
        sublng=inp["subln_g"][0][None, :].astype(np.float32), g1=inp["norm1_g"][0][None, :].astype(np.float32),
        consts=_consts(), rmask=rm)


def _blk(w, nb):
    K_ = w.shape[0]
    return np.ascontiguousarray(w.reshape(K_ // 128, 128, nb, 512).transpose(2, 1, 0, 3).reshape(nb, 128, (K_ // 128) * 512))


W_BLOCKS = (("s_wg", 8), ("s_wab", 4), ("s_wo", 4), ("s_wq", 4), ("s_u", 32), ("s_v", 32))


def l2_weight_blocks(inp):
    w_in = inp["w_in"][0]
    wgb = _blk(w_in[:, 6592:], 8)
    wabb = np.concatenate([_blk(inp["w_proj_a"][0], 4), _blk(inp["w_proj_b"][0], 4)], axis=2)
    uTb = _blk(np.ascontiguousarray(inp["peer_u"][0].T), 32)
    vtb = inp["peer_v"][0].reshape(32, 4, 128, D).transpose(0, 2, 1, 3).reshape(32, 128, 4 * D)
    allb = np.zeros((NCAST * NCORES, 128, 8192), np.float32)
    o = 0
    for a in (wgb, wabb, _blk(inp["w_out"][0], 4), _blk(inp["peer_wq"][0], 4), uTb, vtb):
        allb[o:o + a.shape[0]] = a
        o += a.shape[0]
    return allb


def l2_shared(inp, cast_all):
    m = dict(
        skT=np.ascontiguousarray(inp["peer_sub_keys"][0].reshape(16, 128, 128).transpose(0, 2, 1)),
        gv=np.stack([inp["norm1_g"][0], inp["norm2_g"][0], inp["final_g"]]).astype(np.float32),
        ident=np.eye(128, dtype=np.float32))
    o = 0
    for name, nb in W_BLOCKS:
        m[name] = np.ascontiguousarray(cast_all[o:o + nb])
        o += nb
    return m


def prep_l2(inp, c, yaT_full, ybT_full, shared):
    ts = slice(TOK * c, TOK * (c + 1))
    m = dict(shared)
    m["x"] = np.ascontiguousarray(inp["x"][0, ts])
    m["yaT"] = np.ascontiguousarray(yaT_full[:, ts])
    m["ybT"] = np.ascontiguousarray(ybT_full[:, ts])
    return m


def kernel(**inputs):
    inp = {k: np.asarray(v) for k, v in inputs.items()}
    nc1, _ = build_l1()
    allb = l2_weight_blocks(inp)
    maps1 = [prep_l1(inp, c) for c in range(NCORES)]
    for c in range(NCORES):
        maps1[c]["castin"] = allb[NCAST * c:NCAST * (c + 1)]
    r1 = run_bass_kernel_spmd(nc1, maps1, core_ids=list(range(NCORES))).results
    del maps1, allb
    cast_all = np.concatenate([r1[c]["castout"] for c in range(NCORES)], axis=0)
    yaT_full = np.concatenate([r1[c]["yaT"] for c in range(NCORES)], axis=0)
    ybT_full = np.concatenate([r1[c]["yb"].T for c in range(NCORES)], axis=0)
    w_in = inp["w_in"][0]
    shared = l2_shared(inp, cast_all)
    nc2, _ = build_l2()
    maps2 = [prep_l2(inp, c, yaT_full, ybT_full, shared) for c in range(NCORES)]
    r2 = run_bass_kernel_spmd(nc2, maps2, core_ids=list(range(NCORES))).results
    out = np.concatenate([r2[c]["y"] for c in range(NCORES)], axis=0)
    return out.reshape(1, SEQ, D).astype(np.float32)
```

```python
import numpy as np
import concourse.bass as bass
import concourse.mybir as mybir
from concourse.bass_utils import run_bass_kernel_spmd

F32 = mybir.dt.float32
BF16 = mybir.dt.bfloat16
AF = mybir.ActivationFunctionType
ALU = mybir.AluOpType
AX = mybir.AxisListType

NCORES = 8
D = 2048
SEQ = 16384
TOK = SEQ // NCORES
NEG = -1.0e30


class Tok:
    __slots__ = ("w", "r")

    def __init__(self):
        self.w = None
        self.r = []


class Sync:
    def __init__(self, nc):
        self.nc = nc
        self.engs = {"pe": nc.tensor, "act": nc.scalar, "dve": nc.vector, "pool": nc.gpsimd, "sp": nc.sync}
        self.sem = {k: nc.alloc_semaphore("s_" + k) for k in ("pe", "act", "dve", "pool")}
        self.cnt = {k: 0 for k in self.sem}
        self.waited = {e: {} for e in self.engs}
        self.dsem = {}
        self.ninst = 0

    def _deps(self, reads, writes):
        deps = {}
        for b in reads:
            if b.w is not None:
                k, v = b.w
                deps[k] = max(deps.get(k, 0), v)
        for b in writes:
            if b.w is not None:
                k, v = b.w
                deps[k] = max(deps.get(k, 0), v)
            for (k, v) in b.r:
                deps[k] = max(deps.get(k, 0), v)
        return deps

    def _wait(self, ek, deps):
        eng = self.engs[ek]
        wd = self.waited[ek]
        for k, v in deps.items():
            if k == ek and ek == "pe":
                continue
            if k.startswith("dma:"):
                v = self.dsem[k][1]
                s = self.dsem[k][0]
            else:
                s = self.sem[k]
            if wd.get(k, 0) >= v:
                continue
            eng.wait_ge(s, v)
            wd[k] = v
            self.ninst += 1

    def _mark(self, ev, reads, writes):
        for b in reads:
            b.r.append(ev)
        for b in writes:
            b.w = ev
            b.r = []

    def op(self, ek, fn, reads=(), writes=()):
        self._wait(ek, self._deps(reads, writes))
        inst = fn(self.engs[ek])
        self.cnt[ek] += 1
        inst.then_inc(self.sem[ek], 1)
        self.ninst += 1
        self._mark((ek, self.cnt[ek]), reads, writes)

    def dma(self, qk, stream, fn, reads=(), writes=()):
        self._wait(qk, self._deps(reads, writes))
        inst = fn(self.engs[qk])
        k = "dma:" + stream
        if k not in self.dsem:
            self.dsem[k] = [self.nc.alloc_semaphore("d_" + stream), 0]
        self.dsem[k][1] += 16
        inst.then_inc(self.dsem[k][0], 16)
        self.ninst += 1
        self._mark((k, self.dsem[k][1]), reads, writes)

    def finish(self, toks):
        deps = self._deps(toks, ())
        self._wait("sp", deps)


def _rms_rstd(S, ek_sq, src_ap, src_tok, junk, junk_tok, ss, ss_tok, rstd, rstd_tok, eps):
    S.op("act", lambda e: e.activation(out=junk, in_=src_ap, func=AF.Square, accum_out=ss),
         reads=[src_tok], writes=[junk_tok, ss_tok])
    S.op("act", lambda e: e.activation(out=ss, in_=ss, func=AF.Sqrt, scale=1.0 / D, bias=float(eps)),
         reads=[ss_tok], writes=[ss_tok])
    S.op("dve", lambda e: e.reciprocal(out=rstd, in_=ss), reads=[ss_tok], writes=[rstd_tok])


CH = 256
NT = CH // 128


def build_l2(nchunks=TOK // CH, peer_blocks=32, stage=99):
    nc = bass.Bass("TRN2", target_bir_lowering=False)
    ntok = nchunks * CH
    dr = lambda n, s, k="ExternalInput": nc.dram_tensor(n, s, F32, kind=k).ap()
    x = dr("x", [ntok, D])
    yaT = dr("yaT", [1024, ntok])
    ybT = dr("ybT", [1024, ntok])
    bfi = lambda n, nb: nc.dram_tensor(n, [nb, 128, 8192], BF16, kind="ExternalInput").ap()
    s_wg, s_wab, s_wo, s_wq, s_u, s_v = bfi("s_wg", 8), bfi("s_wab", 4), bfi("s_wo", 4), bfi("s_wq", 4), bfi("s_u", 32), bfi("s_v", 32)
    skT = dr("skT", [16, 128, 128])
    gv = dr("gv", [3, D])
    ident_d = dr("ident", [128, 128])
    y = dr("y", [ntok, D], "ExternalOutput")

    S = Sync(nc)
    sb = lambda n, s, dt=F32: nc.alloc_sbuf_tensor(n, s, dt)
    ps = [nc.alloc_psum_tensor("ps%d" % i, [128, 512], F32) for i in range(6)]
    psb = [nc.alloc_psum_tensor("psb%d" % i, [128, 1024], BF16) for i in range(2)]
    ps_t = [Tok() for _ in range(6)]
    psb_t = [Tok() for _ in range(2)]

    gvec = sb("gvec", [128, D]); gvec_t = Tok()
    xh = sb("xh", [128, NT, D]); xh_t = [Tok() for _ in range(NT)]
    R1 = sb("R1", [128, 16, CH], BF16); R1_t = Tok()
    R2 = sb("R2", [128, 16, CH], BF16); R2_t = Tok()
    R3 = sb("R3", [128, 16, CH], BF16); R3_t = Tok()
    WS = [sb("WS%d" % i, [128, 16, 512], BF16) for i in range(3)]; WS_t = [Tok() for _ in range(3)]
    VS = [sb("VS%d" % i, [128, 4, D], BF16) for i in range(2)]; VS_t = [Tok() for _ in range(2)]
    acc = sb("acc", [128, NT, D]); acc_t = [Tok() for _ in range(NT)]
    sc = sb("sc", [128, NT, 16, 128]); sc_t = [Tok() for _ in range(NT)]
    xnb = sb("xnb", [128, D], BF16); xnb_t = Tok()
    junk, junk_t = xnb, xnb_t
    ident = sb("identb", [128, 128], BF16); ident_t = Tok()
    identf = sb("identf", [128, 128]); identf_t = Tok()
    skb = sb("skb", [128, 16, 128], BF16); skb_t = Tok()
    ss = sb("ss", [128, 1]); ss_t = Tok()
    rstd = sb("rstd", [128, 1]); rstd_t = Tok()
    sg = [sb("sg%d" % i, [128, CH], BF16) for i in range(2)]; sg_t = [Tok() for _ in range(2)]
    mm = [sb("mm%d" % i, [128, CH], BF16) for i in range(2)]; mm_t = [Tok() for _ in range(2)]
    top = sb("top", [128, 1, 16, 16]); top_t = [Tok()] * NT
    tmpk = sb("tmpk", [128, 256]); tmpk_t = Tok()
    cand = sb("cand", [128, 256]); cand_t = Tok()
    best = sb("best", [128, NT, 8, 16]); best_t = [Tok() for _ in range(NT)]
    negmx = sb("negmx", [128, NT, 8]); negmx_t = [Tok() for _ in range(NT)]
    zz = sb("zz", [128, NT, 8]); zz_t = [Tok() for _ in range(NT)]
    nbias = sb("nbias", [128, NT, 8]); nbias_t = [Tok() for _ in range(NT)]
    ebuf = sb("ebuf", [128, 16]); ebuf_t = Tok()
    Ab = [sb("Ab%d" % i, [128, 512], BF16) for i in range(2)]; Ab_t = [Tok() for _ in range(2)]
    Wc = [sb("Wc%d" % i, [128, 512], BF16) for i in range(2)]; Wc_t = [Tok() for _ in range(2)]
    Tb = [sb("Tb%d" % i, [128, 512]) for i in range(3)]; Tb_t = [Tok() for _ in range(3)]
    Eb = [sb("Eb%d" % i, [128, 512]) for i in range(3)]; Eb_t = [Tok() for _ in range(3)]
    stg = [(sb("stg%d" % i, [128, 512]), Tok()) for i in range(2)]
    Wh = [sb("Wh%d" % i, [128, 512], BF16) for i in range(2)]; Wh_t = [Tok() for _ in range(2)]
    Wb = [sb("Wb%d" % i, [128, 512], BF16) for i in range(2)]; Wb_t = [Tok() for _ in range(2)]
    AW = [sb("AW%d" % i, [128, 512], BF16) for i in range(2)]; AW_t = [Tok() for _ in range(2)]
    AWT = [sb("AWT%d" % i, [128, 4, 128], BF16) for i in range(2)]; AWT_t = [Tok() for _ in range(2)]

    S.dma("sp", "c", lambda e: e.dma_start(out=identf[:], in_=ident_d[:, :]), writes=[identf_t])
    S.op("dve", lambda e: e.tensor_copy(out=ident[:], in_=identf[:]), reads=[identf_t], writes=[ident_t])
    S.dma("pool", "w", lambda e: e.dma_start(out=skb[:], in_=skT.rearrange("b d n -> d b n")), writes=[skb_t])

    def load_blk(dst_tile, dst_tok, name, b, scr):
        S.dma("sp", "ws_" + dst_tile.name, lambda e: e.dma_start(out=dst_tile[:].rearrange("p a b -> p (a b)"), in_=scr[b]),
              writes=[dst_tok])

    def norm_transpose(t, g_row, dstT, dstT_t, first):
        src = xh[:, t, :]
        _rms_rstd(S, "act", src, xh_t[t], junk[:], junk_t, ss[:], ss_t, rstd[:], rstd_t, 1e-6)
        S.op("dve", lambda e: e.scalar_tensor_tensor(out=xnb[:], in0=src, scalar=rstd[:, 0:1], in1=gvec[:],
                                                     op0=ALU.mult, op1=ALU.mult),
             reads=[xh_t[t], rstd_t, gvec_t], writes=[xnb_t])
        for half in range(2):
            pb = psb[half]
            for j in range(8):
                kc = half * 8 + j
                S.op("pe", lambda e, kc=kc, j=j, pb=pb: e.transpose(out=pb[:, j * 128:(j + 1) * 128],
                                                                    in_=xnb[:, kc * 128:(kc + 1) * 128], identity=ident[:]),
                     reads=[xnb_t, ident_t], writes=[psb_t[half]])
            S.op("act" if half == 0 else "dve",
                 (lambda e, half=half, pb=pb: e.activation(out=dstT[:, half * 8:(half + 1) * 8, t * 128:(t + 1) * 128],
                                                           in_=pb[:].rearrange("p (k t) -> p k t", k=8), func=AF.Copy))
                 if half == 0 else
                 (lambda e, half=half, pb=pb: e.tensor_copy(out=dstT[:, half * 8:(half + 1) * 8, t * 128:(t + 1) * 128],
                                                            in_=pb[:].rearrange("p (k t) -> p k t", k=8))),
                 reads=[psb_t[half]], writes=[dstT_t])

    for c in range(nchunks):
        t0 = c * CH
        for t in range(NT):
            S.dma("sp", "x%d" % t, lambda e, t=t: e.dma_start(out=xh[:, t, :], in_=x[t0 + t * 128:t0 + (t + 1) * 128, :]),
                  writes=[xh_t[t]])
        S.dma("sp", "g", lambda e: e.dma_start(out=gvec[:], in_=gv[0:1, :].partition_broadcast(128)), writes=[gvec_t])
        S.dma("pool", "w", lambda e: e.dma_start(out=R2[:, 0:8, :], in_=yaT[:, t0:t0 + CH].rearrange("(k p) t -> p k t", p=128)),
              writes=[R2_t])
        S.dma("pool", "w", lambda e: e.dma_start(out=R2[:, 8:16, :], in_=ybT[:, t0:t0 + CH].rearrange("(k p) t -> p k t", p=128)),
              writes=[R2_t])
        for t in range(NT):
            norm_transpose(t, 0, R1, R1_t, True)
        for cg in range(4):
            cs = slice(cg * 512, (cg + 1) * 512)
            load_blk(WS[0], WS_t[0], "wg", cg, s_wg)
            load_blk(WS[1], WS_t[1], "wg", 4 + cg, s_wg)
            load_blk(WS[2], WS_t[2], "wab", cg, s_wab)
            for cb in range(4):
                cc = slice(cb * 128, (cb + 1) * 128)
                cidx = cg * 4 + cb
                for kc in range(16):
                    S.op("pe", lambda e, kc=kc, cc=cc: e.matmul(out=ps[0][:, 0:CH], lhsT=WS[0][:, kc, cc], rhs=R1[:, kc, :],
                                                                start=(kc == 0), stop=(kc == 15)),
                         reads=[WS_t[0], R1_t], writes=[ps_t[0]])
                for kc in range(16):
                    S.op("pe", lambda e, kc=kc, cc=cc: e.matmul(out=ps[1][:, 0:CH], lhsT=WS[1][:, kc, cc], rhs=R1[:, kc, :],
                                                                start=(kc == 0), stop=(kc == 15)),
                         reads=[WS_t[1], R1_t], writes=[ps_t[1]])
                for kc in range(8):
                    S.op("pe", lambda e, kc=kc, cc=cc: e.matmul(out=ps[2][:, 0:CH], lhsT=WS[2][:, kc, cc], rhs=R2[:, kc, :],
                                                                start=(kc == 0), stop=(kc == 7)),
                         reads=[WS_t[2], R2_t], writes=[ps_t[2]])
                for kc in range(8):
                    S.op("pe", lambda e, kc=kc, cc=cc: e.matmul(out=ps[3][:, 0:CH], lhsT=WS[2][:, 8 + kc, cc], rhs=R2[:, 8 + kc, :],
                                                                start=(kc == 0), stop=(kc == 7)),
                         reads=[WS_t[2], R2_t], writes=[ps_t[3]])
                for i in range(2):
                    S.op("act", lambda e, i=i: e.activation(out=sg[i][:], in_=ps[i][:, 0:CH], func=AF.Sigmoid),
                         reads=[ps_t[i]], writes=[sg_t[i]])
                for i in range(2):
                    S.op("dve", lambda e, i=i: e.tensor_tensor(out=mm[i][:], in0=sg[i][:], in1=ps[2 + i][:, 0:CH], op=ALU.mult),
                         reads=[sg_t[i], ps_t[2 + i]], writes=[mm_t[i]])
                S.op("pool", lambda e, cidx=cidx: e.tensor_tensor(out=R3[:, cidx, :], in0=mm[0][:], in1=mm[1][:], op=ALU.add),
                     reads=[mm_t[0], mm_t[1]], writes=[R3_t])
        for db in range(4):
            slot = db % 2
            load_blk(WS[slot], WS_t[slot], "wo", db, s_wo)
            for t in range(NT):
                pbk = 4 + (t % 2)
                for kc in range(16):
                    S.op("pe", lambda e, kc=kc, t=t, pbk=pbk, slot=slot: e.matmul(out=ps[pbk][:, :], lhsT=R3[:, kc, t * 128:(t + 1) * 128],
                                                                                 rhs=WS[slot][:, kc, :], start=(kc == 0), stop=(kc == 15)),
                         reads=[R3_t, WS_t[slot]], writes=[ps_t[pbk]])
                S.op("dve", lambda e, t=t, db=db, pbk=pbk: e.tensor_tensor(out=xh[:, t, db * 512:(db + 1) * 512],
                                                                         in0=xh[:, t, db * 512:(db + 1) * 512], in1=ps[pbk][:, :], op=ALU.add),
                     reads=[ps_t[pbk], xh_t[t]], writes=[xh_t[t]])
        if stage >= 2:
            S.dma("sp", "g", lambda e: e.dma_start(out=gvec[:], in_=gv[1:2, :].partition_broadcast(128)), writes=[gvec_t])
            for t in range(NT):
                norm_transpose(t, 1, R1, R1_t, False)
            for qg in range(4):
                slot = qg % 2
                load_blk(WS[slot], WS_t[slot], "wq", qg, s_wq)
                for qb in range(4):
                    blk = qg * 4 + qb
                    pbk = blk % 2
                    for kc in range(16):
                        S.op("pe", lambda e, kc=kc, qb=qb, pbk=pbk, slot=slot: e.matmul(out=ps[pbk][:, 0:CH], lhsT=WS[slot][:, kc, qb * 128:(qb + 1) * 128],
                                                                                       rhs=R1[:, kc, :], start=(kc == 0), stop=(kc == 15)),
                             reads=[WS_t[slot], R1_t], writes=[ps_t[pbk]])
                    S.op("act", lambda e, blk=blk, pbk=pbk: e.activation(out=R2[:, blk, :], in_=ps[pbk][:, 0:CH], func=AF.Copy),
                         reads=[ps_t[pbk]], writes=[R2_t])
            for t in range(NT):
                for g4 in range(4):
                    pbk = 2 + (g4 % 2)
                    for j in range(4):
                        blk = g4 * 4 + j
                        S.op("pe", lambda e, blk=blk, j=j, pbk=pbk, t=t: e.matmul(out=ps[pbk][:, j * 128:(j + 1) * 128],
                                                                                 lhsT=R2[:, blk, t * 128:(t + 1) * 128], rhs=skb[:, blk, :],
                                                                                 start=True, stop=True),
                             reads=[R2_t, skb_t], writes=[ps_t[pbk]])
                    S.op("act", lambda e, g4=g4, pbk=pbk, t=t: e.activation(out=sc[:, t, g4 * 4:(g4 + 1) * 4, :],
                                                                           in_=ps[pbk][:].rearrange("p (b n) -> p b n", b=4), func=AF.Copy),
                         reads=[ps_t[pbk]], writes=[sc_t[t]])
                for blk in range(16):
                    S.op("dve", lambda e, blk=blk, t=t: e.max(out=top[:, 0, blk, 0:8], in_=sc[:, t, blk, :]),
                         reads=[sc_t[t]], writes=[top_t[t]])
                    S.op("dve", lambda e, blk=blk, t=t: e.match_replace(out=tmpk[:, 0:128], in_to_replace=top[:, 0, blk, 0:8],
                                                                        in_values=sc[:, t, blk, :], imm_value=NEG),
                         reads=[sc_t[t], top_t[t]], writes=[tmpk_t])
                    S.op("dve", lambda e, blk=blk, t=t: e.max(out=top[:, 0, blk, 8:16], in_=tmpk[:, 0:128]),
                         reads=[tmpk_t], writes=[top_t[t]])
                for h in range(8):
                    S.op("dve", lambda e, h=h, t=t: e.tensor_tensor(
                        out=cand[:].rearrange("p (a b) -> p a b", a=16),
                        in0=top[:, 0, 2 * h, :].unsqueeze(2).broadcast_to([128, 16, 16]),
                        in1=top[:, 0, 2 * h + 1, :].unsqueeze(1).broadcast_to([128, 16, 16]), op=ALU.add),
                         reads=[top_t[t]], writes=[cand_t])
                    S.op("dve", lambda e, h=h, t=t: e.max(out=best[:, t, h, 0:8], in_=cand[:]),
                         reads=[cand_t], writes=[best_t[t]])
                    S.op("dve", lambda e, h=h, t=t: e.match_replace(out=tmpk[:], in_to_replace=best[:, t, h, 0:8],
                                                                    in_values=cand[:], imm_value=NEG),
                         reads=[cand_t, best_t[t]], writes=[tmpk_t])
                    S.op("dve", lambda e, h=h, t=t: e.max(out=best[:, t, h, 8:16], in_=tmpk[:]),
                         reads=[tmpk_t], writes=[best_t[t]])
                S.op("dve", lambda e, t=t: e.tensor_scalar(out=negmx[:, t, :], in0=best[:, t, :, 0], scalar1=-1.0, scalar2=None, op0=ALU.mult),
                     reads=[best_t[t]], writes=[negmx_t[t]])
                for h in range(8):
                    S.op("act", lambda e, h=h, t=t: e.activation(out=ebuf[:], in_=best[:, t, h, :], func=AF.Exp,
                                                                 bias=negmx[:, t, h:h + 1], accum_out=zz[:, t, h:h + 1]),
                         reads=[best_t[t], negmx_t[t]], writes=[ebuf_t, zz_t[t]])
                S.op("act", lambda e, t=t: e.activation(out=zz[:, t, :], in_=zz[:, t, :], func=AF.Ln),
                     reads=[zz_t[t]], writes=[zz_t[t]])
                S.op("dve", lambda e, t=t: e.tensor_tensor(out=nbias[:, t, :], in0=negmx[:, t, :], in1=zz[:, t, :], op=ALU.subtract),
                     reads=[negmx_t[t], zz_t[t]], writes=[nbias_t[t]])
            its = [(eb, t) for eb in range(peer_blocks) for t in range(NT)]
            nit = len(its)

            def st1(k):
                eb, t = its[k]
                slot = eb % 2
                if t == 0:
                    load_blk(WS[slot], WS_t[slot], "u", eb, s_u)
                    load_blk(VS[slot], VS_t[slot], "v", eb, s_v)
                pa = k % 2
                for kc in range(16):
                    S.op("pe", lambda e, kc=kc, t=t, slot=slot, pa=pa: e.matmul(out=ps[pa][:, :], lhsT=R1[:, kc, t * 128:(t + 1) * 128],
                                                                               rhs=WS[slot][:, kc, :], start=(kc == 0), stop=(kc == 15)),
                         reads=[R1_t, WS_t[slot]], writes=[ps_t[pa]])

            def st1g(k):
                pa = k % 2
                S.op("act", lambda e, pa=pa: e.activation(out=Ab[pa][:], in_=ps[pa][:, :], func=AF.Gelu),
                     reads=[ps_t[pa]], writes=[Ab_t[pa]])

            def st2(k):
                eb, t = its[k]
                pa = k % 2

                def emit_T(h):
                    i = h % 3
                    S.op("dve", lambda e, h=h, i=i: e.tensor_tensor(
                        out=Tb[i][:].rearrange("p (a b) -> p a b", a=4),
                        in0=sc[:, t, 2 * h, eb * 4:(eb + 1) * 4].unsqueeze(2).broadcast_to([128, 4, 128]),
                        in1=sc[:, t, 2 * h + 1, :].unsqueeze(1).broadcast_to([128, 4, 128]), op=ALU.add),
                         reads=[sc_t[t]], writes=[Tb_t[i]])
                    S.op("act", lambda e, h=h, i=i: e.activation(out=Eb[i][:], in_=Tb[i][:], func=AF.Exp, bias=nbias[:, t, h:h + 1]),
                         reads=[Tb_t[i], nbias_t[t]], writes=[Eb_t[i]])
                emit_T(0)
                emit_T(1)
                for h in range(8):
                    i = h % 3
                    if h + 2 < 8:
                        emit_T(h + 2)
                    accb, accb_t = (Wb[pa], Wb_t[pa]) if h % 2 == 0 else (Wc[pa], Wc_t[pa])
                    dst, dst_t = (accb, accb_t) if h < 2 else (Wh[h % 2], Wh_t[h % 2])
                    S.op("dve", lambda e, h=h, i=i, dst=dst: e.scalar_tensor_tensor(
                        out=dst[:], in0=Tb[i][:], scalar=best[:, t, h, 15:16], in1=Eb[i][:], op0=ALU.is_ge, op1=ALU.mult),
                         reads=[Tb_t[i], Eb_t[i], best_t[t]], writes=[dst_t])
                    if h >= 2:
                        S.op("pool" if h % 2 == 0 else "dve", lambda e, h=h, accb=accb: e.tensor_tensor(out=accb[:], in0=accb[:], in1=Wh[h % 2][:], op=ALU.add),
                             reads=[Wh_t[h % 2], accb_t], writes=[accb_t])
                S.op("dve", lambda e, pa=pa: e.tensor_tensor(out=Wb[pa][:], in0=Wb[pa][:], in1=Wc[pa][:], op=ALU.add),
                     reads=[Wb_t[pa], Wc_t[pa]], writes=[Wb_t[pa]])
                S.op("dve", lambda e, pa=pa: e.tensor_tensor(out=AW[pa][:], in0=Ab[pa][:], in1=Wb[pa][:], op=ALU.mult),
                     reads=[Ab_t[pa], Wb_t[pa]], writes=[AW_t[pa]])

            def st3a(k):
                pa = k % 2
                for es in range(4):
                    S.op("pe", lambda e, es=es, pa=pa: e.transpose(out=psb[pa][:, es * 128:(es + 1) * 128], in_=AW[pa][:, es * 128:(es + 1) * 128],
                                                                   identity=ident[:]),
                         reads=[AW_t[pa], ident_t], writes=[psb_t[pa]])
                S.op("act", lambda e, pa=pa: e.activation(out=AWT[pa][:], in_=psb[pa][:, 0:512].rearrange("p (s t) -> p s t", s=4), func=AF.Copy),
                     reads=[psb_t[pa]], writes=[AWT_t[pa]])

            def st3b(k):
                eb, t = its[k]
                slot = eb % 2
                pa = k % 2
                for db in range(4):
                    pbk = 2 + db
                    for es in range(4):
                        S.op("pe", lambda e, es=es, db=db, pbk=pbk, slot=slot, pa=pa: e.matmul(
                            out=ps[pbk][:, :], lhsT=AWT[pa][:, es, :], rhs=VS[slot][:, es, db * 512:(db + 1) * 512],
                            start=(es == 0), stop=(es == 3)),
                             reads=[AWT_t[pa], VS_t[slot]], writes=[ps_t[pbk]])

            def st3c(k):
                eb, t = its[k]
                for db in range(4):
                    pbk = 2 + db
                    if eb == 0:
                        S.op("act", lambda e, db=db, t=t, pbk=pbk: e.activation(out=acc[:, t, db * 512:(db + 1) * 512], in_=ps[pbk][:, :], func=AF.Copy),
                             reads=[ps_t[pbk]], writes=[acc_t[t]])
                    elif db % 2 == 0:
                        S.op("dve", lambda e, db=db, t=t, pbk=pbk: e.tensor_tensor(out=acc[:, t, db * 512:(db + 1) * 512],
                                                                                 in0=acc[:, t, db * 512:(db + 1) * 512], in1=ps[pbk][:, :], op=ALU.add),
                             reads=[ps_t[pbk], acc_t[t]], writes=[acc_t[t]])
                    else:
                        sg_, sg_t = stg[db // 2]
                        S.op("act", lambda e, pbk=pbk, sg_=sg_: e.activation(out=sg_[:], in_=ps[pbk][:, :], func=AF.Copy),
                             reads=[ps_t[pbk]], writes=[sg_t])
                        S.op("pool", lambda e, db=db, t=t, sg_=sg_: e.tensor_tensor(out=acc[:, t, db * 512:(db + 1) * 512],
                                                                                   in0=acc[:, t, db * 512:(db + 1) * 512], in1=sg_[:], op=ALU.add),
                             reads=[sg_t, acc_t[t]], writes=[acc_t[t]])

            for k in range(nit + 2):
                if 0 <= k - 2 < nit:
                    st3a(k - 2)
                if k < nit:
                    st1(k)
                if 0 <= k - 2 < nit:
                    st3b(k - 2)
                if 0 <= k - 1 < nit:
                    st2(k - 1)
                if k < nit:
                    st1g(k)
                if 0 <= k - 2 < nit:
                    st3c(k - 2)
            for t in range(NT):
                S.op("pool", lambda e, t=t: e.tensor_tensor(out=xh[:, t, :], in0=xh[:, t, :], in1=acc[:, t, :], op=ALU.add),
                     reads=[acc_t[t], xh_t[t]], writes=[xh_t[t]])
        if stage >= 3:
            S.dma("sp", "g", lambda e: e.dma_start(out=gvec[:], in_=gv[2:3, :].partition_broadcast(128)), writes=[gvec_t])
        for t in range(NT):
            if stage >= 3:
                _rms_rstd(S, "act", xh[:, t, :], xh_t[t], junk[:], junk_t, ss[:], ss_t, rstd[:], rstd_t, 1e-6)
                S.op("dve", lambda e, t=t: e.scalar_tensor_tensor(out=acc[:, t, :], in0=xh[:, t, :], scalar=rstd[:, 0:1], in1=gvec[:],
                                                                 op0=ALU.mult, op1=ALU.mult),
                     reads=[xh_t[t], rstd_t, gvec_t], writes=[acc_t[t]])
            else:
                S.op("dve", lambda e, t=t: e.tensor_copy(out=acc[:, t, :], in_=xh[:, t, :]), reads=[xh_t[t]], writes=[acc_t[t]])
            S.dma("sp", "o%d" % t, lambda e, t=t: e.dma_start(out=y[t0 + t * 128:t0 + (t + 1) * 128, :], in_=acc[:, t, :]),
                  reads=[acc_t[t]], writes=[])
    for k in S.dsem:
        if k.startswith("dma:o"):
            nc.sync.wait_ge(S.dsem[k][0], S.dsem[k][1])
    return nc, S


G1 = 256
NC1 = 1216
C_R, C_K, C_V, C_XW, C_XA, C_XG, C_QD, C_KD, C_VD = 0, 128, 256, 384, 480, 576, 832, 960, 1088
CHK = 32
NCAST = 11


def _roundrobin(gens):
    gens = list(gens)
    while gens:
        for g_ in list(gens):
            try:
                next(g_)
            except StopIteration:
                gens.remove(g_)


def build_l1(ngroups=SEQ // G1, do_rwkv=True, do_attn=True, rw_stage=9999):
    nc = bass.Bass("TRN2", target_bir_lowering=False)
    ntok = ngroups * G1
    ntile = ntok // 128
    dr = lambda n, s, k="ExternalInput": nc.dram_tensor(n, s, F32, kind=k).ap()
    x = dr("x", [ntok, D])
    w1 = dr("w1", [D, NC1])
    cvec = dr("cvec", [128, 20])
    wdu_d = dr("wdu", [96, 128]); wiu_d = dr("wiu", [96, 128]); wgu_d = dr("wgu", [256, 128])
    lamv = dr("lamv", [1, 256]); sublng = dr("sublng", [1, 128]); g1d = dr("g1", [1, D])
    consts = dr("consts", [7, 128, 128])
    rmask_d = dr("rmask", [1, G1])
    yaT = dr("yaT", [128, ntok], "ExternalOutput")
    yb = dr("yb", [ntok, 128], "ExternalOutput")
    castin = dr("castin", [NCAST, 128, 8192])
    castout = nc.dram_tensor("castout", [NCAST, 128, 8192], BF16, kind="ExternalOutput").ap()

    S = Sync(nc)
    sb = lambda n, s, dt=F32: nc.alloc_sbuf_tensor(n, s, dt)
    ps = [nc.alloc_psum_tensor("ps%d" % i, [128, 512], F32) for i in range(7)]
    psb = [nc.alloc_psum_tensor("psb%d" % i, [128, 1024], BF16) for i in range(1)]
    ps_t = [Tok() for _ in range(7)]
    psb_t = [Tok() for _ in range(1)]
    OB = [4, 6]
    scr_i = [0]

    def scr():
        scr_i[0] ^= 1
        return ps[scr_i[0]], ps_t[scr_i[0]]

    def T_(name, shape, dt=F32):
        return sb(name, shape, dt), Tok()

    W1, W1_t = T_("W1", [128, 16, NC1], BF16)
    KT, KT_t = T_("KT", [128, ntok], BF16)
    VA, VA_t = T_("VA", [128, ntile, 130], BF16)
    g1c, g1c_t = T_("g1c", [128, 16])
    xt = [T_("xt%d" % i, [128, D]) for i in range(2)]
    xnb, xnb_t = T_("xnb", [128, D], BF16)
    junk, junk_t = xnb, xnb_t
    junk2, junk2_t = T_("junk2", [128, 128], BF16)
    xnT, xnT_t = T_("xnT", [128, 16, G1], BF16)
    cst, cst_t = T_("cst", [128, 7, 128])
    identb, identb_t = T_("identb", [128, 128], BF16)
    trib, trib_t = T_("trib", [128, 128], BF16)
    rmask, rmask_t = T_("rmask_s", [128, G1])
    cv, cv_t = T_("cv_s", [128, 20])
    omm, omm_t = T_("omm", [128, 8])
    wdu, wdu_t = T_("wdus", [96, 128]); wiu, wiu_t = T_("wius", [96, 128]); wgu, wgu_t = T_("wgus", [128, 2, 128])
    lacc, lacc_t = T_("lacc", [128, 4])
    neglam, neglam_t = T_("neglam", [128, 1])
    sgv, sgv_t = T_("sgv", [128, 128])
    ss, ss_t = T_("ss", [128, 1]); rstd, rstd_t = T_("rstd", [128, 1])
    ss2, ss2_t = T_("ss2", [128, 1]); rstd2, rstd2_t = T_("rstd2", [128, 1])
    ident = cst[:, 0, :]; MU = cst[:, 1, :]; ML = cst[:, 2, :]; MUI = cst[:, 3, :]; onesblk = cst[:, 5, :]; I64 = cst[:, 6, 0:64]
    PB = [T_("PB%d" % i, [128, G1 + 1]) for i in range(7)]
    SH = [T_("SH%d" % i, [128, G1]) for i in range(7)]
    tmpA, tmpA_t = T_("tmpA", [128, G1]); tmpB, tmpB_t = T_("tmpB", [128, G1])
    logw, logw_t = T_("logw", [128, G1]); av, av_t = T_("av", [128, G1]); gg, gg_t = T_("gg", [128, G1])
    kkn, kkn_t = T_("kkn", [128, G1]); k2, k2_t = T_("k2", [128, G1]); bonus, bonus_t = T_("bonus", [128, G1])
    cum, cum_t = T_("cum", [128, G1]); Pm, Pm_t = T_("Pm", [128, G1]); Pinv, Pinv_t = T_("Pinv", [128, G1]); Pprev, Pprev_t = T_("Pprev", [128, G1])
    At, At_t = T_("At", [128, G1], BF16); Bt, Bt_t = T_("Bt", [128, G1], BF16); Kt, Kt_t = T_("Kt", [128, G1], BF16); Rt, Rt_t = T_("Rt", [128, G1], BF16)
    vb16, vb16_t = T_("vb16", [128, G1], BF16)
    yT, yT_t = T_("yT", [128, G1]); yo, yo_t = T_("yo", [128, G1])
    TOK, TOK_t = T_("TOK", [128, 4, 128], BF16)
    lam_s = yT[:, 0:256].rearrange("p (a c) -> p a c", c=64); lam_t = yT_t
    lamp = yo[:, 0:128].rearrange("p (b c) -> p b c", c=64); lamp_t = yo_t
    HB = []
    for h in range(2):
        HB.append(dict(
            XT=[T_("XT%d_%d" % (h, i), [128, 128], BF16) for i in range(5)], XX=[T_("XX%d_%d" % (h, i), [128, 128], BF16) for i in range(4)],
            LakT=T_("LakT%d" % h, [128, 128], BF16), MrbT=T_("MrbT%d" % h, [128, 128], BF16), MrkT=T_("MrkT%d" % h, [128, 128], BF16),
            Z=[T_("Z%d_%d" % (h, i), [128, 128], BF16) for i in range(2)], ZF=T_("ZF%d" % h, [128, 128], BF16),
            MZ=T_("MZ%d" % h, [128, 4, 64], BF16), MB=T_("MB%d" % h, [128, 4, 64], BF16), MK=T_("MK%d" % h, [128, 4, 64], BF16)))
    RhT, RhT_t = T_("RhT", [128, 128]); YhT, YhT_t = T_("YhT", [128, 128])
    MT, MT_t = T_("MT", [128, 4, 64]); HP, HP_t = T_("HP", [128, 4, 64])
    Sst = [T_("Sst%d" % i, [128, 64]) for i in range(2)]
    QT, QT_t = T_("QT", [128, G1], BF16); QSQ, QSQ_t = T_("QSQ", [128, G1]); KSQ, KSQ_t = T_("KSQ", [128, G1])
    kmax2 = [T_("kmax2_%d" % i, [128, 1]) for i in range(2)]
    kred, kred_t = T_("kred", [128, 1]); sqq, sqq_t = T_("sqq", [128, 1])
    nshift = [T_("nshift%d" % i, [128, 1]) for i in range(2)]
    Pb = [T_("Pb%d" % i, [128, 512], BF16) for i in range(2)]
    om = [[T_("om%d_%d" % (i, j), [128, 128]) for j in range(2)] for i in range(2)]
    qmax2 = [T_("qmax2_%d" % i, [128, 1]) for i in range(2)]
    rs_, rs_t = T_("rs_", [128, 1]); attn, attn_t = T_("attn", [128, 128]); ybo, ybo_t = T_("ybo", [128, 128])
    ones128, ones_t = T_("ones128", [128, 128])

    def mm(out, lhsT, rhs, start=True, stop=True):
        return lambda e: e.matmul(out=out, lhsT=lhsT, rhs=rhs, start=start, stop=stop)

    cpy_i = [0]

    def copy_out(out, in_, reads, writes, eng=None):
        if eng is None:
            cpy_i[0] ^= 1
            eng = "act" if cpy_i[0] else "dve"
        if eng == "act":
            S.op("act", lambda e: e.activation(out=out, in_=in_, func=AF.Copy), reads=reads, writes=writes)
        else:
            S.op("dve", lambda e: e.tensor_copy(out=out, in_=in_), reads=reads, writes=writes)

    def dve_tt(out, in0, in1, op, reads, writes, eng="dve"):
        S.op(eng, lambda e: e.tensor_tensor(out=out, in0=in0, in1=in1, op=op), reads=reads, writes=writes)

    def dve_ts(out, in0, s1, s2, op0, op1, reads, writes, eng="dve"):
        if op1 is None:
            S.op(eng, lambda e: e.tensor_scalar(out=out, in0=in0, scalar1=s1, scalar2=None, op0=op0), reads=reads, writes=writes)
        else:
            S.op(eng, lambda e: e.tensor_scalar(out=out, in0=in0, scalar1=s1, scalar2=s2, op0=op0, op1=op1), reads=reads, writes=writes)

    def dve_stt(out, in0, scalar, in1, op0, op1, reads, writes):
        S.op("dve", lambda e: e.scalar_tensor_tensor(out=out, in0=in0, scalar=scalar, in1=in1, op0=op0, op1=op1), reads=reads, writes=writes)

    def act(out, in_, func, reads, writes, **kw):
        S.op("act", lambda e: e.activation(out=out, in_=in_, func=func, **kw), reads=reads, writes=writes)

    S.dma("sp", "c", lambda e: e.dma_start(out=cst[:], in_=consts.rearrange("c p n -> p c n")), writes=[cst_t])
    S.dma("sp", "c", lambda e: e.dma_start(out=cv[:], in_=cvec[:, :]), writes=[cv_t])
    S.dma("sp", "c", lambda e: e.dma_start(out=wdu[:], in_=wdu_d[:, :]), writes=[wdu_t])
    S.dma("sp", "c", lambda e: e.dma_start(out=wiu[:], in_=wiu_d[:, :]), writes=[wiu_t])
    S.dma("sp", "c", lambda e: e.dma_start(out=wgu[:], in_=wgu_d.rearrange("(k p) c -> p k c", p=128)), writes=[wgu_t])
    S.dma("sp", "c", lambda e: e.dma_start(out=yT[:, 0:256], in_=lamv[0:1, :].partition_broadcast(128)), writes=[lam_t])
    S.dma("sp", "c", lambda e: e.dma_start(out=sgv[:], in_=sublng[0:1, :].partition_broadcast(128)), writes=[sgv_t])
    S.dma("sp", "c", lambda e: e.dma_start(out=rmask[:], in_=rmask_d[0:1, :].partition_broadcast(128)), writes=[rmask_t])
    with nc.allow_non_contiguous_dma(reason="tiny gain vector"):
        S.dma("sp", "c", lambda e: e.dma_start(out=g1c[:], in_=g1d.rearrange("o (k p) -> p (o k)", p=128)), writes=[g1c_t])
    S.dma("pool", "w", lambda e: e.dma_start(out=W1[:], in_=w1.rearrange("(k p) c -> p k c", p=128)), writes=[W1_t])
    for b in range(NCAST):
        S.dma("pool", "ocast", lambda e, b=b: e.dma_start(out=castout[b].rearrange("p (s e) -> p s e", e=2048),
                                                      in_=castin[b].rearrange("p (s e) -> p s e", e=2048)))
    for kc in range(16):
        S.op("dve" if kc % 2 else "pool", lambda e, kc=kc: e.tensor_scalar(out=W1[:, kc, :], in0=W1[:, kc, :], scalar1=g1c[:, kc:kc + 1], scalar2=0.0,
                                                                            op0=ALU.mult, op1=ALU.add),
             reads=[W1_t, g1c_t], writes=[W1_t])
    S.op("dve", lambda e: e.tensor_copy(out=identb[:], in_=cst[:, 0, :]), reads=[cst_t], writes=[identb_t])
    S.op("dve", lambda e: e.tensor_copy(out=trib[:], in_=cst[:, 4, :]), reads=[cst_t], writes=[trib_t])
    dve_ts(omm[:, 0:8], cv[:, 0:8], -1.0, 1.0, ALU.mult, ALU.add, [cv_t], [omm_t])
    dve_ts(cv[:, 14:15], cv[:, 10:11], -1.0, 1.0, ALU.mult, ALU.add, [cv_t], [cv_t])
    dve_ts(sgv[:], sgv[:], 0.8, None, ALU.mult, None, [sgv_t], [sgv_t])
    dve_tt(lamp[:, 0, :], lam_s[:, 0, :], lam_s[:, 1, :], ALU.mult, [lam_t], [lamp_t])
    dve_tt(lamp[:, 1, :], lam_s[:, 2, :], lam_s[:, 3, :], ALU.mult, [lam_t], [lamp_t])
    S.op("dve", lambda e: e.tensor_reduce(out=lacc[:, 0:2], in_=lamp[:, 0:2, :], axis=AX.X, op=ALU.add), reads=[lamp_t], writes=[lacc_t])
    act(lacc[:, 2:4], lacc[:, 0:2], AF.Exp, [lacc_t], [lacc_t])
    dve_tt(neglam[:], lacc[:, 3:4], lacc[:, 2:3], ALU.subtract, [lacc_t], [neglam_t])
    dve_ts(neglam[:], neglam[:], -0.2, None, ALU.add, None, [neglam_t], [neglam_t])
    for i in range(7):
        S.op("pool", lambda e, i=i: e.memset(PB[i][0][:, 0:1], 0.0), writes=[PB[i][1]])
    for i in range(2):
        S.op("pool", lambda e, i=i: e.memset(Sst[i][0][:], 0.0), writes=[Sst[i][1]])
        S.op("pool", lambda e, i=i: e.memset(kmax2[i][0][:], 0.0), writes=[kmax2[i][1]])
        S.op("pool", lambda e, i=i: e.memset(qmax2[i][0][:], 0.0), writes=[qmax2[i][1]])
    S.op("pool", lambda e: e.memset(VA[:, :, 128:130], 1.0), writes=[VA_t])
    S.op("pool", lambda e: e.memset(ones128[:], 1.0), writes=[ones_t])

    scur = [0]

    def head_chain(h, cs):
        hb = HB[h]
        XT, XX, Z = hb["XT"], hb["XX"], hb["Z"]
        LakT, LakT_t = hb["LakT"]; MrbT, MrbT_t = hb["MrbT"]; MrkT, MrkT_t = hb["MrkT"]
        zf, zf_t = hb["ZF"]
        MZ, MZ_t = hb["MZ"]; MB, MB_t = hb["MB"]; MK, MK_t = hb["MK"]
        pb, pb_t = ps[h], ps_t[h]
        hs = slice(64 * h, 64 * h + 64)
        Ah, Bh, Kh, Rh = At[hs, cs], Bt[hs, cs], Kt[hs, cs], Rt[hs, cs]
        S.op("pe", mm(pb[:, 0:128], Bh, Ah), reads=[Bt_t, At_t], writes=[pb_t])
        dve_tt(XT[0][0][:], pb[:, 0:128], MU, ALU.mult, [pb_t, cst_t], [XT[0][1]])
        yield
        S.op("pe", mm(pb[:, 0:128], Ah, Bh), reads=[Bt_t, At_t], writes=[pb_t])
        dve_tt(XX[0][0][:], pb[:, 0:128], ML, ALU.mult, [pb_t, cst_t], [XX[0][1]])
        yield
        S.op("pe", mm(pb[:, 0:128], Kh, Ah), reads=[Kt_t, At_t], writes=[pb_t])
        dve_tt(LakT[:], pb[:, 0:128], MU, ALU.mult, [pb_t, cst_t], [LakT_t])
        yield
        S.op("pe", mm(pb[:, 0:128], Bh, Rh), reads=[Bt_t, Rt_t], writes=[pb_t])
        dve_tt(MrbT[:], pb[:, 0:128], MUI, ALU.mult, [pb_t, cst_t], [MrbT_t])
        yield
        S.op("pe", mm(pb[:, 0:128], Kh, Rh), reads=[Kt_t, Rt_t], writes=[pb_t])
        dve_tt(MrkT[:], pb[:, 0:128], MUI, ALU.mult, [pb_t, cst_t], [MrkT_t])
        yield
        S.op("pe", mm(pb[:, 0:64], LakT[:], TOK[:, 3, hs]), reads=[LakT_t, TOK_t], writes=[pb_t])
        zc, zc_t = Z[0]
        copy_out(zc[:, 64:128], pb[:, 0:64], [pb_t], [zc_t], eng="dve")
        S.op("pool", lambda e: e.tensor_copy(out=zc[:, 0:64], in_=TOK[:, 0, hs]), reads=[TOK_t], writes=[zc_t])
        yield
        for i in range(4):
            S.op("pe", mm(pb[:, 0:128], XX[i][0][:], XT[i][0][:]), reads=[XX[i][1], XT[i][1]], writes=[pb_t])
            copy_out(XT[i + 1][0][:], pb[:, 0:128], [pb_t], [XT[i + 1][1]], eng="dve")
            yield
            if i < 3:
                S.op("pe", mm(pb[:, 0:128], XT[i][0][:], XX[i][0][:]), reads=[XX[i][1], XT[i][1]], writes=[pb_t])
                copy_out(XX[i + 1][0][:], pb[:, 0:128], [pb_t], [XX[i + 1][1]], eng="act")
                yield
            zc, zc_t = Z[i % 2]
            zn, zn_t = Z[(i + 1) % 2]
            S.op("pe", mm(pb[:, 0:128], XT[i][0][:], zc[:]), reads=[XT[i][1], zc_t], writes=[pb_t])
            dve_tt(zn[:], pb[:, 0:128], zc[:], ALU.add, [pb_t, zc_t], [zn_t])
            yield
        zc, zc_t = Z[0]
        S.op("pe", mm(pb[:, 0:128], XT[4][0][:], zc[:]), reads=[XT[4][1], zc_t], writes=[pb_t])
        dve_tt(zf[:], pb[:, 0:128], zc[:], ALU.add, [pb_t, zc_t], [zf_t])
        yield
        S.op("pe", mm(pb[hs, 0:128], zf[:, 0:64], MrbT[:]), reads=[zf_t, MrbT_t], writes=[pb_t])
        dve_tt(RhT[hs, :], pb[hs, 0:128], Rh, ALU.add, [pb_t, Rt_t], [RhT_t])
        yield
        S.op("pe", mm(pb[hs, 0:128], zf[:, 64:128], MrbT[:], True, False), reads=[zf_t, MrbT_t], writes=[pb_t])
        S.op("pe", mm(pb[hs, 0:128], TOK[:, 3, hs], MrkT[:], False, True), reads=[TOK_t, MrkT_t], writes=[pb_t])
        copy_out(YhT[hs, :], pb[hs, 0:128], [pb_t], [YhT_t], eng="act")
        cmb = cv[:, 16:20].unsqueeze(2).broadcast_to([128, 4, 64])
        dve_tt(MZ[:], zf[:, 0:64].unsqueeze(1).broadcast_to([128, 4, 64]), cmb, ALU.mult, [zf_t, cv_t], [MZ_t])
        dve_tt(MB[:], TOK[:, 1, hs].unsqueeze(1).broadcast_to([128, 4, 64]), cmb, ALU.mult, [TOK_t, cv_t], [MB_t], eng="pool")
        dve_tt(MK[:], TOK[:, 2, hs].unsqueeze(1).broadcast_to([128, 4, 64]), cmb, ALU.mult, [TOK_t, cv_t], [MK_t], eng="pool")
        yield
        for c in range(4):
            S.op("pe", mm(ps[2][hs, c * 64:(c + 1) * 64], MZ[:, c, :], TOK[:, 1, hs]), reads=[MZ_t, TOK_t], writes=[ps_t[2]])
        for c in range(4):
            S.op("pe", mm(ps[3][hs, c * 64:(c + 1) * 64], MB[:, c, :], zf[:, 64:128], True, False), reads=[zf_t, MB_t], writes=[ps_t[3]])
            S.op("pe", mm(ps[3][hs, c * 64:(c + 1) * 64], MK[:, c, :], TOK[:, 3, hs], False, True), reads=[TOK_t, MK_t], writes=[ps_t[3]])
        yield

    def rwkv_group(g):
        t0 = g * G1
        r_, k_, v_, xw_, xa_, xg0_, xg1_ = range(7)
        rows = [128, 128, 128, 96, 96, 128, 128]
        for b in range(7):
            n = rows[b]
            pbuf, pbt = PB[b]
            sh, sht = SH[b]
            dve_ts(tmpA[0:n, :], pbuf[0:n, 0:G1], cv[0:n, b:b + 1], None, ALU.mult, None, [pbt, cv_t], [tmpA_t])
            dve_stt(sh[0:n, :], pbuf[0:n, 1:G1 + 1], omm[0:n, b:b + 1], tmpA[0:n, :], ALU.mult, ALU.add, [pbt, omm_t, tmpA_t], [sht])
            S.op("pool", lambda e, pbuf=pbuf, n=n: e.tensor_copy(out=pbuf[0:n, 0:1], in_=pbuf[0:n, G1:G1 + 1]), reads=[pbt], writes=[pbt])
            if b % 2:
                yield
        shr, shr_t = SH[r_]; shk, shk_t = SH[k_]; shv, shv_t = SH[v_]
        act(tmpB[0:96, :], SH[xw_][0][0:96, :], AF.Tanh, [SH[xw_][1]], [tmpB_t])
        pb, pb_t = scr()
        S.op("pe", mm(pb[:, 0:G1], wdu[:, :], tmpB[0:96, :]), reads=[wdu_t, tmpB_t], writes=[pb_t])
        act(logw[:], pb[:, 0:G1], AF.Sigmoid, [pb_t, cv_t], [logw_t], bias=cv[:, 7:8])
        dve_ts(logw[:], logw[:], -0.6065306597126334, None, ALU.mult, None, [logw_t], [logw_t])
        pb, pb_t = scr()
        S.op("pe", mm(pb[:, 0:G1], wiu[:, :], SH[xa_][0][0:96, :]), reads=[wiu_t, SH[xa_][1]], writes=[pb_t])
        act(av[:], pb[:, 0:G1], AF.Sigmoid, [pb_t, cv_t], [av_t], bias=cv[:, 8:9])
        act(SH[xg0_][0][:], SH[xg0_][0][:], AF.Sigmoid, [SH[xg0_][1]], [SH[xg0_][1]])
        act(SH[xg1_][0][:], SH[xg1_][0][:], AF.Sigmoid, [SH[xg1_][1]], [SH[xg1_][1]])
        yield
        pb, pb_t = scr()
        S.op("pe", mm(pb[:, 0:G1], wgu[:, 0, :], SH[xg0_][0][:], True, False), reads=[wgu_t, SH[xg0_][1]], writes=[pb_t])
        S.op("pe", mm(pb[:, 0:G1], wgu[:, 1, :], SH[xg1_][0][:], False, True), reads=[wgu_t, SH[xg1_][1]], writes=[pb_t])
        copy_out(gg[:], pb[:, 0:G1], [pb_t], [gg_t], eng="dve")
        dve_ts(kkn[:], shk[:], cv[:, 9:10], None, ALU.mult, None, [shk_t, cv_t], [kkn_t])
        dve_tt(tmpA[:], kkn[:], kkn[:], ALU.mult, [kkn_t], [tmpA_t], eng="pool")
        pb, pb_t = scr()
        S.op("pe", mm(pb[:, 0:G1], onesblk, tmpA[:]), reads=[cst_t, tmpA_t], writes=[pb_t])
        act(tmpB[:], pb[:, 0:G1], AF.Sqrt, [pb_t], [tmpB_t])
        dve_ts(tmpB[:], tmpB[:], 1e-12, None, ALU.max, None, [tmpB_t], [tmpB_t])
        S.op("dve", lambda e: e.reciprocal(out=tmpB[:], in_=tmpB[:]), reads=[tmpB_t], writes=[tmpB_t])
        dve_tt(kkn[:], kkn[:], tmpB[:], ALU.mult, [kkn_t, tmpB_t], [kkn_t])
        yield
        dve_ts(tmpA[:], av[:], cv[:, 10:11], cv[:, 14:15], ALU.mult, ALU.add, [av_t, cv_t], [tmpA_t])
        dve_tt(k2[:], shk[:], tmpA[:], ALU.mult, [shk_t, tmpA_t], [k2_t])
        dve_tt(tmpA[:], shr[:], k2[:], ALU.mult, [shr_t, k2_t], [tmpA_t], eng="pool")
        dve_ts(tmpA[:], tmpA[:], cv[:, 11:12], None, ALU.mult, None, [tmpA_t, cv_t], [tmpA_t])
        pb, pb_t = scr()
        S.op("pe", mm(pb[:, 0:G1], onesblk, tmpA[:]), reads=[cst_t, tmpA_t], writes=[pb_t])
        dve_tt(bonus[:], pb[:, 0:G1], shv[:], ALU.mult, [pb_t, shv_t], [bonus_t])
        S.op("dve", lambda e: e.tensor_tensor_scan(out=cum[:], data0=rmask[:], data1=logw[:], initial=0.0, op0=ALU.mult, op1=ALU.add),
             reads=[rmask_t, logw_t], writes=[cum_t])
        yield
        act(Pm[:], cum[:], AF.Exp, [cum_t], [Pm_t])
        act(Pinv[:], cum[:], AF.Exp, [cum_t], [Pinv_t], scale=-1.0)
        dve_tt(tmpA[:], cum[:], logw[:], ALU.subtract, [cum_t, logw_t], [tmpA_t], eng="pool")
        act(Pprev[:], tmpA[:], AF.Exp, [tmpA_t], [Pprev_t])
        dve_stt(At[:], kkn[:], -1.0, Pprev[:], ALU.mult, ALU.mult, [kkn_t, Pprev_t], [At_t])
        dve_tt(tmpB[:], kkn[:], av[:], ALU.mult, [kkn_t, av_t], [tmpB_t], eng="pool")
        dve_tt(Bt[:], tmpB[:], Pinv[:], ALU.mult, [tmpB_t, Pinv_t], [Bt_t])
        dve_tt(Kt[:], k2[:], Pinv[:], ALU.mult, [k2_t, Pinv_t], [Kt_t], eng="pool")
        dve_tt(Rt[:], shr[:], Pm[:], ALU.mult, [shr_t, Pm_t], [Rt_t])
        S.op("pool", lambda e: e.tensor_copy(out=vb16[:], in_=shv[:]), reads=[shv_t], writes=[vb16_t])
        yield
        for tl in range(G1 // 128):
            cs = slice(tl * 128, (tl + 1) * 128)
            for j, (src, srct) in enumerate([(At, At_t), (Bt, Bt_t), (Kt, Kt_t), (vb16, vb16_t)]):
                S.op("pe", lambda e, j=j, src=src: e.transpose(out=psb[0][:, j * 128:(j + 1) * 128], in_=src[:, cs], identity=identb[:]),
                     reads=[srct, identb_t], writes=[psb_t[0]])
            copy_out(TOK[:].rearrange("p a b -> p (a b)"), psb[0][:, 0:512], [psb_t[0]], [TOK_t], eng="act")
            yield
            chains = [head_chain(0, cs), head_chain(1, cs)]
            while chains:
                for ch in list(chains):
                    try:
                        next(ch)
                    except StopIteration:
                        chains.remove(ch)
                yield
            dve_tt(MT[:], ps[2][:, 0:256].rearrange("p (c k) -> p c k", c=4), I64.unsqueeze(1).broadcast_to([128, 4, 64]), ALU.add,
                   [ps_t[2], cst_t], [MT_t])
            for c in range(4):
                col = tl * 128 + 32 * c + 31
                dve_ts(HP[:, c, :], ps[3][:, c * 64:(c + 1) * 64], Pm[:, col:col + 1], None, ALU.mult, None, [ps_t[3], Pm_t], [HP_t])
            yield
            for c in range(4):
                col = tl * 128 + 32 * c + 31
                sc_, sc_t = Sst[scur[0]]
                sn_, sn_t = Sst[1 - scur[0]]
                for h in range(2):
                    hs = slice(64 * h, 64 * h + 64)
                    S.op("pe", mm(ps[0][hs, 0:64], MT[hs, c, :], sc_[hs, :]), reads=[MT_t, sc_t], writes=[ps_t[0]])
                for h in range(2):
                    hs = slice(64 * h, 64 * h + 64)
                    S.op("pe", mm(ps[1][hs, 32 * c:32 * c + 32], sc_[hs, :], RhT[hs, 32 * c:32 * c + 32]), reads=[sc_t, RhT_t], writes=[ps_t[1]])
                dve_stt(sn_[:], ps[0][:, 0:64], Pm[:, col:col + 1], HP[:, c, :], ALU.mult, ALU.add, [ps_t[0], Pm_t, HP_t], [sn_t])
                scur[0] = 1 - scur[0]
                yield
            dve_tt(yT[:, cs], ps[1][:, 0:128], YhT[:], ALU.add, [ps_t[1], YhT_t], [yT_t])
            yield
        pb, pb_t = scr()
        S.op("pe", mm(pb[:, 0:G1], onesblk, yT[:]), reads=[cst_t, yT_t], writes=[pb_t])
        act(tmpA[:], pb[:, 0:G1], AF.Copy, [pb_t], [tmpA_t], scale=1.0 / 64)
        act(tmpB[:], yT[:], AF.Square, [yT_t], [tmpB_t])
        pb, pb_t = scr()
        S.op("pe", mm(pb[:, 0:G1], onesblk, tmpB[:]), reads=[cst_t, tmpB_t], writes=[pb_t])
        dve_tt(tmpB[:], tmpA[:], tmpA[:], ALU.mult, [tmpA_t], [tmpB_t], eng="pool")
        dve_stt(tmpB[:], pb[:, 0:G1], 1.0 / 64, tmpB[:], ALU.mult, ALU.subtract, [pb_t, tmpB_t], [tmpB_t])
        yield
        act(tmpB[:], tmpB[:], AF.Sqrt, [tmpB_t], [tmpB_t], bias=64e-5)
        S.op("dve", lambda e: e.reciprocal(out=tmpB[:], in_=tmpB[:]), reads=[tmpB_t], writes=[tmpB_t])
        dve_tt(yo[:], yT[:], tmpA[:], ALU.subtract, [yT_t, tmpA_t], [yo_t])
        dve_tt(yo[:], yo[:], tmpB[:], ALU.mult, [yo_t, tmpB_t], [yo_t])
        dve_ts(yo[:], yo[:], cv[:, 12:13], cv[:, 13:14], ALU.mult, ALU.add, [yo_t, cv_t], [yo_t])
        dve_tt(yo[:], yo[:], bonus[:], ALU.add, [yo_t, bonus_t], [yo_t], eng="pool")
        dve_tt(yo[:], yo[:], gg[:], ALU.mult, [yo_t, gg_t], [yo_t])
        S.dma("sp", "oya", lambda e: e.dma_start(out=yaT[:, t0:t0 + G1], in_=yo[:]), reads=[yo_t], writes=[])
        yield

    def attn_group(g):
        for m in range(2):
            ms = slice(64 * m, 64 * m + 64)
            nsh, nsh_t = nshift[m]
            act(sqq[:], qmax2[m][0][:], AF.Sqrt, [qmax2[m][1], kmax2[m][1]], [sqq_t], scale=kmax2[m][0][:, 0:1])
            dve_ts(nsh[:], sqq[:], -0.125, None, ALU.mult, None, [sqq_t], [nsh_t])

            def stage_a(j):
                P_, P_t = Pb[j % 2]
                if j < g:
                    for a in range(2):
                        kt = 2 * j + a
                        S.op("pe", mm(ps[5][:, a * 256:(a + 1) * 256], KT[ms, kt * 128:(kt + 1) * 128], QT[ms, :]), reads=[QT_t, KT_t], writes=[ps_t[5]])
                    act(P_[:], ps[5][:, :], AF.Exp, [ps_t[5], nsh_t], [P_t], scale=0.125, bias=nsh[:, 0:1])
                else:
                    kt = 2 * g
                    S.op("pe", mm(ps[5][:, 0:256], KT[ms, kt * 128:(kt + 1) * 128], QT[ms, :]), reads=[QT_t, KT_t], writes=[ps_t[5]])
                    S.op("pe", mm(ps[5][:, 384:512], KT[ms, (kt + 1) * 128:(kt + 2) * 128], QT[ms, 128:256]), reads=[QT_t, KT_t], writes=[ps_t[5]])
                    act(P_[:, 0:256], ps[5][:, 0:256], AF.Exp, [ps_t[5], nsh_t], [P_t], scale=0.125, bias=nsh[:, 0:1])
                    act(P_[:, 384:512], ps[5][:, 384:512], AF.Exp, [ps_t[5], nsh_t], [P_t], scale=0.125, bias=nsh[:, 0:1])
                    dve_tt(P_[:, 0:128], P_[:, 0:128], trib[:], ALU.mult, [P_t, trib_t], [P_t], eng="pool")
                    dve_tt(P_[:, 384:512], P_[:, 384:512], trib[:], ALU.mult, [P_t, trib_t], [P_t], eng="pool")

            def stage_b(j):
                P_, P_t = Pb[j % 2]
                if j < g:
                    for a in range(2):
                        kt = 2 * j + a
                        for tl in range(2):
                            ob = OB[tl]
                            S.op("pe", mm(ps[ob][:, 0:129], P_[:, a * 256 + tl * 128:a * 256 + (tl + 1) * 128], VA[:, kt, 0:129], kt == 0, False),
                                 reads=[P_t, VA_t], writes=[ps_t[ob]])
                else:
                    kt = 2 * g
                    S.op("pe", mm(ps[OB[0]][:, 0:129], P_[:, 0:128], VA[:, kt, 0:129], kt == 0, True), reads=[P_t, VA_t], writes=[ps_t[OB[0]]])
                    S.op("pe", mm(ps[OB[1]][:, 0:129], P_[:, 128:256], VA[:, kt, 0:129], kt == 0, False), reads=[P_t, VA_t], writes=[ps_t[OB[1]]])
                    S.op("pe", mm(ps[OB[1]][:, 0:129], P_[:, 384:512], VA[:, kt + 1, 0:129], False, True), reads=[P_t, VA_t], writes=[ps_t[OB[1]]])

            stage_a(0)
            for j in range(g + 1):
                if j + 1 <= g:
                    stage_a(j + 1)
                stage_b(j)
                yield
            for tl in range(2):
                ob = OB[tl]
                S.op("dve", lambda e, ob=ob: e.reciprocal(out=rs_[:], in_=ps[ob][:, 128:129]), reads=[ps_t[ob]], writes=[rs_t])
                dve_ts(om[tl][m][0][:], ps[ob][:, 0:128], rs_[:, 0:1], None, ALU.mult, None, [ps_t[ob], rs_t], [om[tl][m][1]])
            yield
        for tl in range(2):
            qt = 2 * g + tl
            dve_stt(attn[:], om[tl][1][0][:], neglam[:, 0:1], om[tl][0][0][:], ALU.mult, ALU.add, [om[tl][0][1], om[tl][1][1], neglam_t], [attn_t])
            S.op("act", lambda e: e.activation(out=junk2[:], in_=attn[:], func=AF.Square, accum_out=ss2[:]), reads=[attn_t], writes=[junk2_t, ss2_t])
            S.op("act", lambda e: e.activation(out=ss2[:], in_=ss2[:], func=AF.Sqrt, scale=1.0 / 128, bias=1e-5), reads=[ss2_t], writes=[ss2_t])
            S.op("dve", lambda e: e.reciprocal(out=rstd2[:], in_=ss2[:]), reads=[ss2_t], writes=[rstd2_t])
            dve_stt(ybo[:], attn[:], rstd2[:, 0:1], sgv[:], ALU.mult, ALU.mult, [attn_t, rstd2_t, sgv_t], [ybo_t])
            S.dma("sp", "oyb", lambda e, qt=qt: e.dma_start(out=yb[qt * 128:(qt + 1) * 128, :], in_=ybo[:]), reads=[ybo_t], writes=[])
            yield

    for g in range(ngroups):
        t0 = g * G1
        for tl in range(2):
            xb, xb_t = xt[tl]
            S.dma("sp", "x%d" % tl, lambda e, tl=tl, xb=xb: e.dma_start(out=xb[:], in_=x[t0 + tl * 128:t0 + (tl + 1) * 128, :]), writes=[xb_t])
            _rms_rstd(S, "act", xb[:], xb_t, junk[:], junk_t, ss[:], ss_t, rstd[:], rstd_t, 1e-6)
            dve_ts(xnb[:], xb[:], rstd[:, 0:1], None, ALU.mult, None, [xb_t, rstd_t], [xnb_t])
            for half in range(2):
                for j in range(8):
                    kc = half * 8 + j
                    S.op("pe", lambda e, kc=kc, j=j: e.transpose(out=psb[0][:, j * 128:(j + 1) * 128], in_=xnb[:, kc * 128:(kc + 1) * 128], identity=identb[:]),
                         reads=[xnb_t, identb_t], writes=[psb_t[0]])
                copy_out(xnT[:, half * 8:(half + 1) * 8, tl * 128:(tl + 1) * 128], psb[0][:].rearrange("p (k t) -> p k t", k=8), [psb_t[0]], [xnT_t])
        blocks = [(C_R, 128), (C_K, 128), (C_V, 128), (C_XW, 96), (C_XA, 96), (C_XG, 128), (C_XG + 128, 128)]
        for bi, (c0, w) in enumerate(blocks):
            pb, pb_t = scr()
            for kc in range(16):
                S.op("pe", mm(pb[0:w, 0:G1], W1[:, kc, c0:c0 + w], xnT[:, kc, :], kc == 0, kc == 15), reads=[W1_t, xnT_t], writes=[pb_t])
            copy_out(PB[bi][0][0:w, 1:G1 + 1], pb[0:w, 0:G1], [pb_t], [PB[bi][1]])
        pb, pb_t = scr()
        for kc in range(16):
            S.op("pe", mm(pb[:, 0:G1], W1[:, kc, C_QD:C_QD + 128], xnT[:, kc, :], kc == 0, kc == 15), reads=[W1_t, xnT_t], writes=[pb_t])
        act(QT[:], pb[:, 0:G1], AF.Copy, [pb_t], [QT_t])
        act(QSQ[:], pb[:, 0:G1], AF.Square, [pb_t], [QSQ_t])
        pb, pb_t = scr()
        for kc in range(16):
            S.op("pe", mm(pb[:, 0:G1], W1[:, kc, C_KD:C_KD + 128], xnT[:, kc, :], kc == 0, kc == 15), reads=[W1_t, xnT_t], writes=[pb_t])
        act(KT[:, t0:t0 + G1], pb[:, 0:G1], AF.Copy, [pb_t], [KT_t])
        act(KSQ[:], pb[:, 0:G1], AF.Square, [pb_t], [KSQ_t])
        for m in range(2):
            ms = slice(64 * m, 64 * m + 64)
            pb, pb_t = scr()
            S.op("pe", mm(pb[:, 0:G1], ones128[ms, :], KSQ[ms, :]), reads=[ones_t, KSQ_t], writes=[pb_t])
            S.op("dve", lambda e, pb=pb: e.tensor_reduce(out=kred[:], in_=pb[:, 0:G1], axis=AX.X, op=ALU.max), reads=[pb_t], writes=[kred_t])
            dve_tt(kmax2[m][0][:], kmax2[m][0][:], kred[:], ALU.max, [kred_t, kmax2[m][1]], [kmax2[m][1]])
            pb, pb_t = scr()
            S.op("pe", mm(pb[:, 0:G1], ones128[ms, :], QSQ[ms, :]), reads=[ones_t, QSQ_t], writes=[pb_t])
            S.op("dve", lambda e, pb=pb: e.tensor_reduce(out=kred[:], in_=pb[:, 0:G1], axis=AX.X, op=ALU.max), reads=[pb_t], writes=[kred_t])
            dve_tt(qmax2[m][0][:], qmax2[m][0][:], kred[:], ALU.max, [kred_t, qmax2[m][1]], [qmax2[m][1]])
        for tl in range(2):
            pb, pb_t = scr()
            for kc in range(16):
                S.op("pe", mm(pb[:, 0:128], xnT[:, kc, tl * 128:(tl + 1) * 128], W1[:, kc, C_VD:C_VD + 128], kc == 0, kc == 15),
                     reads=[W1_t, xnT_t], writes=[pb_t])
            copy_out(VA[:, 2 * g + tl, 0:128], pb[:, 0:128], [pb_t], [VA_t])
        gens = []
        if do_rwkv:
            gr = rwkv_group(g)
            if rw_stage < 9000:
                def lim(gr=gr):
                    for _ in range(rw_stage):
                        next(gr)
                        yield
                gr = lim()
            gens.append(gr)
        if do_attn:
            gens.append(attn_group(g))
        _roundrobin(gens)
    for k in S.dsem:
        if k.startswith("dma:o"):
            nc.sync.wait_ge(S.dsem[k][0], S.dsem[k][1])
    return nc, S


def _consts():
    t = np.arange(128)
    same = (t[:, None] // CHK) == (t[None, :] // CHK)
    MU = (same & (t[:, None] < t[None, :])).astype(np.float32)
    ML = MU.T.copy()
    MUI = (same & (t[:, None] <= t[None, :])).astype(np.float32)
    TRI = (t[:, None] <= t[None, :]).astype(np.float32)
    ob = ((t[:, None] // 64) == (t[None, :] // 64)).astype(np.float32)
    i64 = np.zeros((128, 128), np.float32)
    i64[t, t % 64] = 1.0
    return np.stack([np.eye(128, dtype=np.float32), MU, ML, MUI, TRI, ob, i64])


def prep_l1(inp, c, ntok=SEQ):
    w_in = inp["w_in"][0]
    hs = slice(128 * c, 128 * c + 128)
    o_d = 3520
    cols = np.concatenate([np.arange(128 * c, 128 * c + 128), 1024 + np.arange(128 * c, 128 * c + 128),
                           2048 + np.arange(128 * c, 128 * c + 128), np.arange(3072, 3520),
                           o_d + np.arange(128 * c, 128 * c + 128), o_d + 1024 + np.arange(128 * c, 128 * c + 128),
                           o_d + 2048 + np.arange(128 * c, 128 * c + 128)])
    mu = inp["shift_mu"][0]
    cvec = np.zeros((128, 20), np.float32)
    cvec[:, 0] = mu[0:1024][hs]; cvec[:, 1] = mu[1024:2048][hs]; cvec[:, 2] = mu[2048:3072][hs]
    cvec[:96, 3] = mu[3072:3168]; cvec[:96, 4] = mu[3168:3264]; cvec[:, 5] = mu[3264:3392]; cvec[:, 6] = mu[3392:3520]
    cvec[:, 7] = inp["rwkv_w0"][0][hs]; cvec[:, 8] = inp["rwkv_a0"][0][hs]; cvec[:, 9] = inp["k_k"][0][hs]
    cvec[:, 10] = inp["k_a"][0][hs]; cvec[:, 11] = inp["r_k"][0].reshape(-1)[hs]
    cvec[:, 12] = inp["lnx_g"][0][hs]; cvec[:, 13] = inp["lnx_b"][0][hs]
    for c_ in range(4):
        cvec[32 * c_:32 * c_ + 32, 16 + c_] = 1.0
    rm = np.ones((1, G1), np.float32); rm[0, ::CHK] = 0.0
    return dict(
        x=np.ascontiguousarray(inp["x"][0, :ntok]), w1=np.ascontiguousarray(w_in[:, cols]), cvec=cvec,
        wdu=np.ascontiguousarray(inp["w_decay_up"][0][:, hs]), wiu=np.ascontiguousarray(inp["w_iclr_up"][0][:, hs]),
        wgu=np.ascontiguousarray(inp["w_gate_up"][0][:, hs]),
        lamv=np.concatenate([inp["lam_q1"][0], inp["lam_k1"][0], inp["lam_q2"][0], inp["lam_k2"][0]])[None, :].astype(np.float32),
        sublng=inp["subln_g"][0][None, :].astype(np.float32), g1=inp["norm1_g"][0][None, :].astype(np.float32),
        consts=_consts(), rmask=rm)


def _blk(w, nb):
    K_ = w.shape[0]
    return np.ascontiguousarray(w.reshape(K_ // 128, 128, nb, 512).transpose(2, 1, 0, 3).reshape(nb, 128, (K_ // 128) * 512))


W_BLOCKS = (("s_wg", 8), ("s_wab", 4), ("s_wo", 4), ("s_wq", 4), ("s_u", 32), ("s_v", 32))


def l2_weight_blocks(inp):
    w_in = inp["w_in"][0]
    wgb = _blk(w_in[:, 6592:], 8)
    wabb = np.concatenate([_blk(inp["w_proj_a"][0], 4), _blk(inp["w_proj_b"][0], 4)], axis=2)
    uTb = _blk(np.ascontiguousarray(inp["peer_u"][0].T), 32)
    vtb = inp["peer_v"][0].reshape(32, 4, 128, D).transpose(0, 2, 1, 3).reshape(32, 128, 4 * D)
    allb = np.zeros((NCAST * NCORES, 128, 8192), np.float32)
    o = 0
    for a in (wgb, wabb, _blk(inp["w_out"][0], 4), _blk(inp["peer_wq"][0], 4), uTb, vtb):
        allb[o:o + a.shape[0]] = a
        o += a.shape[0]
    return allb


def l2_shared(inp, cast_all):
    m = dict(
        skT=np.ascontiguousarray(inp["peer_sub_keys"][0].reshape(16, 128, 128).transpose(0, 2, 1)),
        gv=np.stack([inp["norm1_g"][0], inp["norm2_g"][0], inp["final_g"]]).astype(np.float32),
        ident=np.eye(128, dtype=np.float32))
    o = 0
    for name, nb in W_BLOCKS:
        m[name] = np.ascontiguousarray(cast_all[o:o + nb])
        o += nb
    return m


def prep_l2(inp, c, yaT_full, ybT_full, shared):
    ts = slice(TOK * c, TOK * (c + 1))
    m = dict(shared)
    m["x"] = np.ascontiguousarray(inp["x"][0, ts])
    m["yaT"] = np.ascontiguousarray(yaT_full[:, ts])
    m["ybT"] = np.ascontiguousarray(ybT_full[:, ts])
    return m


def kernel(**inputs):
    inp = {k: np.asarray(v) for k, v in inputs.items()}
    nc1, _ = build_l1()
    allb = l2_weight_blocks(inp)
    maps1 = [prep_l1(inp, c) for c in range(NCORES)]
    for c in range(NCORES):
        maps1[c]["castin"] = allb[NCAST * c:NCAST * (c + 1)]
    r1 = run_bass_kernel_spmd(nc1, maps1, core_ids=list(range(NCORES))).results
    del maps1, allb
    cast_all = np.concatenate([r1[c]["castout"] for c in range(NCORES)], axis=0)
    yaT_full = np.concatenate([r1[c]["yaT"] for c in range(NCORES)], axis=0)
    ybT_full = np.concatenate([r1[c]["yb"].T for c in range(NCORES)], axis=0)
    w_in = inp["w_in"][0]
    shared = l2_shared(inp, cast_all)
    nc2, _ = build_l2()
    maps2 = [prep_l2(inp, c, yaT_full, ybT_full, shared) for c in range(NCORES)]
    r2 = run_bass_kernel_spmd(nc2, maps2, core_ids=list(range(NCORES))).results
    out = np.concatenate([r2[c]["y"] for c in range(NCORES)], axis=0)
    return out.reshape(1, SEQ, D).astype(np.float32)
```

```python
import numpy as np
import concourse.bass as bass
import concourse.mybir as mybir
from concourse.bass_utils import run_bass_kernel_spmd

F32 = mybir.dt.float32
BF16 = mybir.dt.bfloat16
AF = mybir.ActivationFunctionType
ALU = mybir.AluOpType
AX = mybir.AxisListType

NCORES = 8
D = 2048
SEQ = 16384
TOK = SEQ // NCORES
NEG = -1.0e30


class Tok:
    __slots__ = ("w", "r")

    def __init__(self):
        self.w = None
        self.r = []


class Sync:
    def __init__(self, nc):
        self.nc = nc
        self.engs = {"pe": nc.tensor, "act": nc.scalar, "dve": nc.vector, "pool": nc.gpsimd, "sp": nc.sync}
        self.sem = {k: nc.alloc_semaphore("s_" + k) for k in ("pe", "act", "dve", "pool")}
        self.cnt = {k: 0 for k in self.sem}
        self.waited = {e: {} for e in self.engs}
        self.dsem = {}
        self.ninst = 0

    def _deps(self, reads, writes):
        deps = {}
        for b in reads:
            if b.w is not None:
                k, v = b.w
                deps[k] = max(deps.get(k, 0), v)
        for b in writes:
            if b.w is not None:
                k, v = b.w
                deps[k] = max(deps.get(k, 0), v)
            for (k, v) in b.r:
                deps[k] = max(deps.get(k, 0), v)
        return deps

    def _wait(self, ek, deps):
        eng = self.engs[ek]
        wd = self.waited[ek]
        for k, v in deps.items():
            if k == ek and ek == "pe":
                continue
            if k.startswith("dma:"):
                v = self.dsem[k][1]
                s = self.dsem[k][0]
            else:
                s = self.sem[k]
            if wd.get(k, 0) >= v:
                continue
            eng.wait_ge(s, v)
            wd[k] = v
            self.ninst += 1

    def _mark(self, ev, reads, writes):
        for b in reads:
            b.r.append(ev)
        for b in writes:
            b.w = ev
            b.r = []

    def op(self, ek, fn, reads=(), writes=()):
        self._wait(ek, self._deps(reads, writes))
        inst = fn(self.engs[ek])
        self.cnt[ek] += 1
        inst.then_inc(self.sem[ek], 1)
        self.ninst += 1
        self._mark((ek, self.cnt[ek]), reads, writes)

    def dma(self, qk, stream, fn, reads=(), writes=()):
        self._wait(qk, self._deps(reads, writes))
        inst = fn(self.engs[qk])
        k = "dma:" + stream
        if k not in self.dsem:
            self.dsem[k] = [self.nc.alloc_semaphore("d_" + stream), 0]
        self.dsem[k][1] += 16
        inst.then_inc(self.dsem[k][0], 16)
        self.ninst += 1
        self._mark((k, self.dsem[k][1]), reads, writes)

    def finish(self, toks):
        deps = self._deps(toks, ())
        self._wait("sp", deps)


def _rms_rstd(S, ek_sq, src_ap, src_tok, junk, junk_tok, ss, ss_tok, rstd, rstd_tok, eps):
    S.op("act", lambda e: e.activation(out=junk, in_=src_ap, func=AF.Square, accum_out=ss),
         reads=[src_tok], writes=[junk_tok, ss_tok])
    S.op("act", lambda e: e.activation(out=ss, in_=ss, func=AF.Sqrt, scale=1.0 / D, bias=float(eps)),
         reads=[ss_tok], writes=[ss_tok])
    S.op("dve", lambda e: e.reciprocal(out=rstd, in_=ss), reads=[ss_tok], writes=[rstd_tok])


CH = 256
NT = CH // 128


def build_l2(nchunks=TOK // CH, peer_blocks=32, stage=99):
    nc = bass.Bass("TRN2", target_bir_lowering=False)
    ntok = nchunks * CH
    dr = lambda n, s, k="ExternalInput": nc.dram_tensor(n, s, F32, kind=k).ap()
    x = dr("x", [ntok, D])
    yaT = dr("yaT", [1024, ntok])
    ybT = dr("ybT", [1024, ntok])
    bfi = lambda n, nb: nc.dram_tensor(n, [nb, 128, 8192], BF16, kind="ExternalInput").ap()
    s_wg, s_wab, s_wo, s_wq, s_u, s_v = bfi("s_wg", 8), bfi("s_wab", 4), bfi("s_wo", 4), bfi("s_wq", 4), bfi("s_u", 32), bfi("s_v", 32)
    skT = dr("skT", [16, 128, 128])
    gv = dr("gv", [3, D])
    ident_d = dr("ident", [128, 128])
    y = dr("y", [ntok, D], "ExternalOutput")

    S = Sync(nc)
    sb = lambda n, s, dt=F32: nc.alloc_sbuf_tensor(n, s, dt)
    ps = [nc.alloc_psum_tensor("ps%d" % i, [128, 512], F32) for i in range(6)]
    psb = [nc.alloc_psum_tensor("psb%d" % i, [128, 1024], BF16) for i in range(2)]
    ps_t = [Tok() for _ in range(6)]
    psb_t = [Tok() for _ in range(2)]

    gvec = sb("gvec", [128, D]); gvec_t = Tok()
    xh = sb("xh", [128, NT, D]); xh_t = [Tok() for _ in range(NT)]
    R1 = sb("R1", [128, 16, CH], BF16); R1_t = Tok()
    R2 = sb("R2", [128, 16, CH], BF16); R2_t = Tok()
    R3 = sb("R3", [128, 16, CH], BF16); R3_t = Tok()
    WS = [sb("WS%d" % i, [128, 16, 512], BF16) for i in range(3)]; WS_t = [Tok() for _ in range(3)]
    VS = [sb("VS%d" % i, [128, 4, D], BF16) for i in range(2)]; VS_t = [Tok() for _ in range(2)]
    acc = sb("acc", [128, NT, D]); acc_t = [Tok() for _ in range(NT)]
    sc = sb("sc", [128, NT, 16, 128]); sc_t = [Tok() for _ in range(NT)]
    xnb = sb("xnb", [128, D], BF16); xnb_t = Tok()
    junk, junk_t = xnb, xnb_t
    ident = sb("identb", [128, 128], BF16); ident_t = Tok()
    identf = sb("identf", [128, 128]); identf_t = Tok()
    skb = sb("skb", [128, 16, 128], BF16); skb_t = Tok()
    ss = sb("ss", [128, 1]); ss_t = Tok()
    rstd = sb("rstd", [128, 1]); rstd_t = Tok()
    sg = [sb("sg%d" % i, [128, CH], BF16) for i in range(2)]; sg_t = [Tok() for _ in range(2)]
    mm = [sb("mm%d" % i, [128, CH], BF16) for i in range(2)]; mm_t = [Tok() for _ in range(2)]
    top = sb("top", [128, 1, 16, 16]); top_t = [Tok()] * NT
    tmpk = sb("tmpk", [128, 256]); tmpk_t = Tok()
    cand = sb("cand", [128, 256]); cand_t = Tok()
    best = sb("best", [128, NT, 8, 16]); best_t = [Tok() for _ in range(NT)]
    negmx = sb("negmx", [128, NT, 8]); negmx_t = [Tok() for _ in range(NT)]
    zz = sb("zz", [128, NT, 8]); zz_t = [Tok() for _ in range(NT)]
    nbias = sb("nbias", [128, NT, 8]); nbias_t = [Tok() for _ in range(NT)]
    ebuf = sb("ebuf", [128, 16]); ebuf_t = Tok()
    Ab = [sb("Ab%d" % i, [128, 512], BF16) for i in range(2)]; Ab_t = [Tok() for _ in range(2)]
    Wc = [sb("Wc%d" % i, [128, 512], BF16) for i in range(2)]; Wc_t = [Tok() for _ in range(2)]
    Tb = [sb("Tb%d" % i, [128, 512]) for i in range(3)]; Tb_t = [Tok() for _ in range(3)]
    Eb = [sb("Eb%d" % i, [128, 512]) for i in range(3)]; Eb_t = [Tok() for _ in range(3)]
    stg = [(sb("stg%d" % i, [128, 512]), Tok()) for i in range(2)]
    Wh = [sb("Wh%d" % i, [128, 512], BF16) for i in range(2)]; Wh_t = [Tok() for _ in range(2)]
    Wb = [sb("Wb%d" % i, [128, 512], BF16) for i in range(2)]; Wb_t = [Tok() for _ in range(2)]
    AW = [sb("AW%d" % i, [128, 512], BF16) for i in range(2)]; AW_t = [Tok() for _ in range(2)]
    AWT = [sb("AWT%d" % i, [128, 4, 128], BF16) for i in range(2)]; AWT_t = [Tok() for _ in range(2)]

    S.dma("sp", "c", lambda e: e.dma_start(out=identf[:], in_=ident_d[:, :]), writes=[identf_t])
    S.op("dve", lambda e: e.tensor_copy(out=ident[:], in_=identf[:]), reads=[identf_t], writes=[ident_t])
    S.dma("pool", "w", lambda e: e.dma_start(out=skb[:], in_=skT.rearrange("b d n -> d b n")), writes=[skb_t])

    def load_blk(dst_tile, dst_tok, name, b, scr):
        S.dma("sp", "ws_" + dst_tile.name, lambda e: e.dma_start(out=dst_tile[:].rearrange("p a b -> p (a b)"), in_=scr[b]),
              writes=[dst_tok])

    def norm_transpose(t, g_row, dstT, dstT_t, first):
        src = xh[:, t, :]
        _rms_rstd(S, "act", src, xh_t[t], junk[:], junk_t, ss[:], ss_t, rstd[:], rstd_t, 1e-6)
        S.op("dve", lambda e: e.scalar_tensor_tensor(out=xnb[:], in0=src, scalar=rstd[:, 0:1], in1=gvec[:],
                                                     op0=ALU.mult, op1=ALU.mult),
             reads=[xh_t[t], rstd_t, gvec_t], writes=[xnb_t])
        for half in range(2):
            pb = psb[half]
            for j in range(8):
                kc = half * 8 + j
                S.op("pe", lambda e, kc=kc, j=j, pb=pb: e.transpose(out=pb[:, j * 128:(j + 1) * 128],
                                                                    in_=xnb[:, kc * 128:(kc + 1) * 128], identity=ident[:]),
                     reads=[xnb_t, ident_t], writes=[psb_t[half]])
            S.op("act" if half == 0 else "dve",
                 (lambda e, half=half, pb=pb: e.activation(out=dstT[:, half * 8:(half + 1) * 8, t * 128:(t + 1) * 128],
                                                           in_=pb[:].rearrange("p (k t) -> p k t", k=8), func=AF.Copy))
                 if half == 0 else
                 (lambda e, half=half, pb=pb: e.tensor_copy(out=dstT[:, half * 8:(half + 1) * 8, t * 128:(t + 1) * 128],
                                                            in_=pb[:].rearrange("p (k t) -> p k t", k=8))),
                 reads=[psb_t[half]], writes=[dstT_t])

    for c in range(nchunks):
        t0 = c * CH
        for t in range(NT):
            S.dma("sp", "x%d" % t, lambda e, t=t: e.dma_start(out=xh[:, t, :], in_=x[t0 + t * 128:t0 + (t + 1) * 128, :]),
                  writes=[xh_t[t]])
        S.dma("sp", "g", lambda e: e.dma_start(out=gvec[:], in_=gv[0:1, :].partition_broadcast(128)), writes=[gvec_t])
        S.dma("pool", "w", lambda e: e.dma_start(out=R2[:, 0:8, :], in_=yaT[:, t0:t0 + CH].rearrange("(k p) t -> p k t", p=128)),
              writes=[R2_t])
        S.dma("pool", "w", lambda e: e.dma_start(out=R2[:, 8:16, :], in_=ybT[:, t0:t0 + CH].rearrange("(k p) t -> p k t", p=128)),
              writes=[R2_t])
        for t in range(NT):
            norm_transpose(t, 0, R1, R1_t, True)
        for cg in range(4):
            cs = slice(cg * 512, (cg + 1) * 512)
            load_blk(WS[0], WS_t[0], "wg", cg, s_wg)
            load_blk(WS[1], WS_t[1], "wg", 4 + cg, s_wg)
            load_blk(WS[2], WS_t[2], "wab", cg, s_wab)
            for cb in range(4):
                cc = slice(cb * 128, (cb + 1) * 128)
                cidx = cg * 4 + cb
                for kc in range(16):
                    S.op("pe", lambda e, kc=kc, cc=cc: e.matmul(out=ps[0][:, 0:CH], lhsT=WS[0][:, kc, cc], rhs=R1[:, kc, :],
                                                                start=(kc == 0), stop=(kc == 15)),
                         reads=[WS_t[0], R1_t], writes=[ps_t[0]])
                for kc in range(16):
                    S.op("pe", lambda e, kc=kc, cc=cc: e.matmul(out=ps[1][:, 0:CH], lhsT=WS[1][:, kc, cc], rhs=R1[:, kc, :],
                                                                start=(kc == 0), stop=(kc == 15)),
                         reads=[WS_t[1], R1_t], writes=[ps_t[1]])
                for kc in range(8):
                    S.op("pe", lambda e, kc=kc, cc=cc: e.matmul(out=ps[2][:, 0:CH], lhsT=WS[2][:, kc, cc], rhs=R2[:, kc, :],
                                                                start=(kc == 0), stop=(kc == 7)),
                         reads=[WS_t[2], R2_t], writes=[ps_t[2]])
                for kc in range(8):
                    S.op("pe", lambda e, kc=kc, cc=cc: e.matmul(out=ps[3][:, 0:CH], lhsT=WS[2][:, 8 + kc, cc], rhs=R2[:, 8 + kc, :],
                                                                start=(kc == 0), stop=(kc == 7)),
                         reads=[WS_t[2], R2_t], writes=[ps_t[3]])
                for i in range(2):
                    S.op("act", lambda e, i=i: e.activation(out=sg[i][:], in_=ps[i][:, 0:CH], func=AF.Sigmoid),
                         reads=[ps_t[i]], writes=[sg_t[i]])
                for i in range(2):
                    S.op("dve", lambda e, i=i: e.tensor_tensor(out=mm[i][:], in0=sg[i][:], in1=ps[2 + i][:, 0:CH], op=ALU.mult),
                         reads=[sg_t[i], ps_t[2 + i]], writes=[mm_t[i]])
                S.op("pool", lambda e, cidx=cidx: e.tensor_tensor(out=R3[:, cidx, :], in0=mm[0][:], in1=mm[1][:], op=ALU.add),
                     reads=[mm_t[0], mm_t[1]], writes=[R3_t])
        for db in range(4):
            slot = db % 2
            load_blk(WS[slot], WS_t[slot], "wo", db, s_wo)
            for t in range(NT):
                pbk = 4 + (t % 2)
                for kc in range(16):
                    S.op("pe", lambda e, kc=kc, t=t, pbk=pbk, slot=slot: e.matmul(out=ps[pbk][:, :], lhsT=R3[:, kc, t * 128:(t + 1) * 128],
                                                                                 rhs=WS[slot][:, kc, :], start=(kc == 0), stop=(kc == 15)),
                         reads=[R3_t, WS_t[slot]], writes=[ps_t[pbk]])
                S.op("dve", lambda e, t=t, db=db, pbk=pbk: e.tensor_tensor(out=xh[:, t, db * 512:(db + 1) * 512],
                                                                         in0=xh[:, t, db * 512:(db + 1) * 512], in1=ps[pbk][:, :], op=ALU.add),
                     reads=[ps_t[pbk], xh_t[t]], writes=[xh_t[t]])
        if stage >= 2:
            S.dma("sp", "g", lambda e: e.dma_start(out=gvec[:], in_=gv[1:2, :].partition_broadcast(128)), writes=[gvec_t])
            for t in range(NT):
                norm_transpose(t, 1, R1, R1_t, False)
            for qg in range(4):
                slot = qg % 2
                load_blk(WS[slot], WS_t[slot], "wq", qg, s_wq)
                for qb in range(4):
                    blk = qg * 4 + qb
                    pbk = blk % 2
                    for kc in range(16):
                        S.op("pe", lambda e, kc=kc, qb=qb, pbk=pbk, slot=slot: e.matmul(out=ps[pbk][:, 0:CH], lhsT=WS[slot][:, kc, qb * 128:(qb + 1) * 128],
                                                                                       rhs=R1[:, kc, :], start=(kc == 0), stop=(kc == 15)),
                             reads=[WS_t[slot], R1_t], writes=[ps_t[pbk]])
                    S.op("act", lambda e, blk=blk, pbk=pbk: e.activation(out=R2[:, blk, :], in_=ps[pbk][:, 0:CH], func=AF.Copy),
                         reads=[ps_t[pbk]], writes=[R2_t])
            for t in range(NT):
                for g4 in range(4):
                    pbk = 2 + (g4 % 2)
                    for j in range(4):
                        blk = g4 * 4 + j
                        S.op("pe", lambda e, blk=blk, j=j, pbk=pbk, t=t: e.matmul(out=ps[pbk][:, j * 128:(j + 1) * 128],
                                                                                 lhsT=R2[:, blk, t * 128:(t + 1) * 128], rhs=skb[:, blk, :],
                                                                                 start=True, stop=True),
                             reads=[R2_t, skb_t], writes=[ps_t[pbk]])
                    S.op("act", lambda e, g4=g4, pbk=pbk, t=t: e.activation(out=sc[:, t, g4 * 4:(g4 + 1) * 4, :],
                                                                           in_=ps[pbk][:].rearrange("p (b n) -> p b n", b=4), func=AF.Copy),
                         reads=[ps_t[pbk]], writes=[sc_t[t]])
                for blk in range(16):
                    S.op("dve", lambda e, blk=blk, t=t: e.max(out=top[:, 0, blk, 0:8], in_=sc[:, t, blk, :]),
                         reads=[sc_t[t]], writes=[top_t[t]])
                    S.op("dve", lambda e, blk=blk, t=t: e.match_replace(out=tmpk[:, 0:128], in_to_replace=top[:, 0, blk, 0:8],
                                                                        in_values=sc[:, t, blk, :], imm_value=NEG),
                         reads=[sc_t[t], top_t[t]], writes=[tmpk_t])
                    S.op("dve", lambda e, blk=blk, t=t: e.max(out=top[:, 0, blk, 8:16], in_=tmpk[:, 0:128]),
                         reads=[tmpk_t], writes=[top_t[t]])
                for h in range(8):
                    S.op("dve", lambda e, h=h, t=t: e.tensor_tensor(
                        out=cand[:].rearrange("p (a b) -> p a b", a=16),
                        in0=top[:, 0, 2 * h, :].unsqueeze(2).broadcast_to([128, 16, 16]),
                        in1=top[:, 0, 2 * h + 1, :].unsqueeze(1).broadcast_to([128, 16, 16]), op=ALU.add),
                         reads=[top_t[t]], writes=[cand_t])
                    S.op("dve", lambda e, h=h, t=t: e.max(out=best[:, t, h, 0:8], in_=cand[:]),
                         reads=[cand_t], writes=[best_t[t]])
                    S.op("dve", lambda e, h=h, t=t: e.match_replace(out=tmpk[:], in_to_replace=best[:, t, h, 0:8],
                                                                    in_values=cand[:], imm_value=NEG),
                         reads=[cand_t, best_t[t]], writes=[tmpk_t])
                    S.op("dve", lambda e, h=h, t=t: e.max(out=best[:, t, h, 8:16], in_=tmpk[:]),
                         reads=[tmpk_t], writes=[best_t[t]])
                S.op("dve", lambda e, t=t: e.tensor_scalar(out=negmx[:, t, :], in0=best[:, t, :, 0], scalar1=-1.0, scalar2=None, op0=ALU.mult),
                     reads=[best_t[t]], writes=[negmx_t[t]])
                for h in range(8):
                    S.op("act", lambda e, h=h, t=t: e.activation(out=ebuf[:], in_=best[:, t, h, :], func=AF.Exp,
                                                                 bias=negmx[:, t, h:h + 1], accum_out=zz[:, t, h:h + 1]),
                         reads=[best_t[t], negmx_t[t]], writes=[ebuf_t, zz_t[t]])
                S.op("act", lambda e, t=t: e.activation(out=zz[:, t, :], in_=zz[:, t, :], func=AF.Ln),
                     reads=[zz_t[t]], writes=[zz_t[t]])
                S.op("dve", lambda e, t=t: e.tensor_tensor(out=nbias[:, t, :], in0=negmx[:, t, :], in1=zz[:, t, :], op=ALU.subtract),
                     reads=[negmx_t[t], zz_t[t]], writes=[nbias_t[t]])
            its = [(eb, t) for eb in range(peer_blocks) for t in range(NT)]
            nit = len(its)

            def st1(k):
                eb, t = its[k]
                slot = eb % 2
                if t == 0:
                    load_blk(WS[slot], WS_t[slot], "u", eb, s_u)
                    load_blk(VS[slot], VS_t[slot], "v", eb, s_v)
                pa = k % 2
                for kc in range(16):
                    S.op("pe", lambda e, kc=kc, t=t, slot=slot, pa=pa: e.matmul(out=ps[pa][:, :], lhsT=R1[:, kc, t * 128:(t + 1) * 128],
                                                                               rhs=WS[slot][:, kc, :], start=(kc == 0), stop=(kc == 15)),
                         reads=[R1_t, WS_t[slot]], writes=[ps_t[pa]])

            def st1g(k):
                pa = k % 2
                S.op("act", lambda e, pa=pa: e.activation(out=Ab[pa][:], in_=ps[pa][:, :], func=AF.Gelu),
                     reads=[ps_t[pa]], writes=[Ab_t[pa]])

            def st2(k):
                eb, t = its[k]
                pa = k % 2

                def emit_T(h):
                    i = h % 3
                    S.op("dve", lambda e, h=h, i=i: e.tensor_tensor(
                        out=Tb[i][:].rearrange("p (a b) -> p a b", a=4),
                        in0=sc[:, t, 2 * h, eb * 4:(eb + 1) * 4].unsqueeze(2).broadcast_to([128, 4, 128]),
                        in1=sc[:, t, 2 * h + 1, :].unsqueeze(1).broadcast_to([128, 4, 128]), op=ALU.add),
                         reads=[sc_t[t]], writes=[Tb_t[i]])
                    S.op("act", lambda e, h=h, i=i: e.activation(out=Eb[i][:], in_=Tb[i][:], func=AF.Exp, bias=nbias[:, t, h:h + 1]),
                         reads=[Tb_t[i], nbias_t[t]], writes=[Eb_t[i]])
                emit_T(0)
                emit_T(1)
                for h in range(8):
                    i = h % 3
                    if h + 2 < 8:
                        emit_T(h + 2)
                    accb, accb_t = (Wb[pa], Wb_t[pa]) if h % 2 == 0 else (Wc[pa], Wc_t[pa])
                    dst, dst_t = (accb, accb_t) if h < 2 else (Wh[h % 2], Wh_t[h % 2])
                    S.op("dve", lambda e, h=h, i=i, dst=dst: e.scalar_tensor_tensor(
                        out=dst[:], in0=Tb[i][:], scalar=best[:, t, h, 15:16], in1=Eb[i][:], op0=ALU.is_ge, op1=ALU.mult),
                         reads=[Tb_t[i], Eb_t[i], best_t[t]], writes=[dst_t])
                    if h >= 2:
                        S.op("pool" if h % 2 == 0 else "dve", lambda e, h=h, accb=accb: e.tensor_tensor(out=accb[:], in0=accb[:], in1=Wh[h % 2][:], op=ALU.add),
                             reads=[Wh_t[h % 2], accb_t], writes=[accb_t])
                S.op("dve", lambda e, pa=pa: e.tensor_tensor(out=Wb[pa][:], in0=Wb[pa][:], in1=Wc[pa][:], op=ALU.add),
                     reads=[Wb_t[pa], Wc_t[pa]], writes=[Wb_t[pa]])
                S.op("dve", lambda e, pa=pa: e.tensor_tensor(out=AW[pa][:], in0=Ab[pa][:], in1=Wb[pa][:], op=ALU.mult),
                     reads=[Ab_t[pa], Wb_t[pa]], writes=[AW_t[pa]])

            def st3a(k):
                pa = k % 2
                for es in range(4):
                    S.op("pe", lambda e, es=es, pa=pa: e.transpose(out=psb[pa][:, es * 128:(es + 1) * 128], in_=AW[pa][:, es * 128:(es + 1) * 128],
                                                                   identity=ident[:]),
                         reads=[AW_t[pa], ident_t], writes=[psb_t[pa]])
                S.op("act", lambda e, pa=pa: e.activation(out=AWT[pa][:], in_=psb[pa][:, 0:512].rearrange("p (s t) -> p s t", s=4), func=AF.Copy),
                     reads=[psb_t[pa]], writes=[AWT_t[pa]])

            def st3b(k):
                eb, t = its[k]
                slot = eb % 2
                pa = k % 2
                for db in range(4):
                    pbk = 2 + db
                    for es in range(4):
                        S.op("pe", lambda e, es=es, db=db, pbk=pbk, slot=slot, pa=pa: e.matmul(
                            out=ps[pbk][:, :], lhsT=AWT[pa][:, es, :], rhs=VS[slot][:, es, db * 512:(db + 1) * 512],
                            start=(es == 0), stop=(es == 3)),
                             reads=[AWT_t[pa], VS_t[slot]], writes=[ps_t[pbk]])

            def st3c(k):
                eb, t = its[k]
                for db in range(4):
                    pbk = 2 + db
                    if eb == 0:
                        S.op("act", lambda e, db=db, t=t, pbk=pbk: e.activation(out=acc[:, t, db * 512:(db + 1) * 512], in_=ps[pbk][:, :], func=AF.Copy),
                             reads=[ps_t[pbk]], writes=[acc_t[t]])
                    elif db % 2 == 0:
                        S.op("dve", lambda e, db=db, t=t, pbk=pbk: e.tensor_tensor(out=acc[:, t, db * 512:(db + 1) * 512],
                                                                                 in0=acc[:, t, db * 512:(db + 1) * 512], in1=ps[pbk][:, :], op=ALU.add),
                             reads=[ps_t[pbk], acc_t[t]], writes=[acc_t[t]])
                    else:
                        sg_, sg_t = stg[db // 2]
                        S.op("act", lambda e, pbk=pbk, sg_=sg_: e.activation(out=sg_[:], in_=ps[pbk][:, :], func=AF.Copy),
                             reads=[ps_t[pbk]], writes=[sg_t])
                        S.op("pool", lambda e, db=db, t=t, sg_=sg_: e.tensor_tensor(out=acc[:, t, db * 512:(db + 1) * 512],
                                                                                   in0=acc[:, t, db * 512:(db + 1) * 512], in1=sg_[:], op=ALU.add),
                             reads=[sg_t, acc_t[t]], writes=[acc_t[t]])

            for k in range(nit + 2):
                if 0 <= k - 2 < nit:
                    st3a(k - 2)
                if k < nit:
                    st1(k)
                if 0 <= k - 2 < nit:
                    st3b(k - 2)
                if 0 <= k - 1 < nit:
                    st2(k - 1)
                if k < nit:
                    st1g(k)
                if 0 <= k - 2 < nit:
                    st3c(k - 2)
            for t in range(NT):
                S.op("pool", lambda e, t=t: e.tensor_tensor(out=xh[:, t, :], in0=xh[:, t, :], in1=acc[:, t, :], op=ALU.add),
                     reads=[acc_t[t], xh_t[t]], writes=[xh_t[t]])
        if stage >= 3:
            S.dma("sp", "g", lambda e: e.dma_start(out=gvec[:], in_=gv[2:3, :].partition_broadcast(128)), writes=[gvec_t])
        for t in range(NT):
            if stage >= 3:
                _rms_rstd(S, "act", xh[:, t, :], xh_t[t], junk[:], junk_t, ss[:], ss_t, rstd[:], rstd_t, 1e-6)
                S.op("dve", lambda e, t=t: e.scalar_tensor_tensor(out=acc[:, t, :], in0=xh[:, t, :], scalar=rstd[:, 0:1], in1=gvec[:],
                                                                 op0=ALU.mult, op1=ALU.mult),
                     reads=[xh_t[t], rstd_t, gvec_t], writes=[acc_t[t]])
            else:
                S.op("dve", lambda e, t=t: e.tensor_copy(out=acc[:, t, :], in_=xh[:, t, :]), reads=[xh_t[t]], writes=[acc_t[t]])
            S.dma("sp", "o%d" % t, lambda e, t=t: e.dma_start(out=y[t0 + t * 128:t0 + (t + 1) * 128, :], in_=acc[:, t, :]),
                  reads=[acc_t[t]], writes=[])
    for k in S.dsem:
        if k.startswith("dma:o"):
            nc.sync.wait_ge(S.dsem[k][0], S.dsem[k][1])
    return nc, S


G1 = 256
NC1 = 1216
C_R, C_K, C_V, C_XW, C_XA, C_XG, C_QD, C_KD, C_VD = 0, 128, 256, 384, 480, 576, 832, 960, 1088
CHK = 32
NCAST = 11


def _roundrobin(gens):
    gens = list(gens)
    while gens:
        for g_ in list(gens):
            try:
                next(g_)
            except StopIteration:
                gens.remove(g_)


def build_l1(ngroups=SEQ // G1, do_rwkv=True, do_attn=True, rw_stage=9999):
    nc = bass.Bass("TRN2", target_bir_lowering=False)
    ntok = ngroups * G1
    ntile = ntok // 128
    dr = lambda n, s, k="ExternalInput": nc.dram_tensor(n, s, F32, kind=k).ap()
    x = dr("x", [ntok, D])
    w1 = dr("w1", [D, NC1])
    cvec = dr("cvec", [128, 20])
    wdu_d = dr("wdu", [96, 128]); wiu_d = dr("wiu", [96, 128]); wgu_d = dr("wgu", [256, 128])
    lamv = dr("lamv", [1, 256]); sublng = dr("sublng", [1, 128]); g1d = dr("g1", [1, D])
    consts = dr("consts", [7, 128, 128])
    rmask_d = dr("rmask", [1, G1])
    yaT = dr("yaT", [128, ntok], "ExternalOutput")
    yb = dr("yb", [ntok, 128], "ExternalOutput")
    castin = dr("castin", [NCAST, 128, 8192])
    castout = nc.dram_tensor("castout", [NCAST, 128, 8192], BF16, kind="ExternalOutput").ap()

    S = Sync(nc)
    sb = lambda n, s, dt=F32: nc.alloc_sbuf_tensor(n, s, dt)
    ps = [nc.alloc_psum_tensor("ps%d" % i, [128, 512], F32) for i in range(7)]
    psb = [nc.alloc_psum_tensor("psb%d" % i, [128, 1024], BF16) for i in range(1)]
    ps_t = [Tok() for _ in range(7)]
    psb_t = [Tok() for _ in range(1)]
    OB = [4, 6]
    scr_i = [0]

    def scr():
        scr_i[0] ^= 1
        return ps[scr_i[0]], ps_t[scr_i[0]]

    def T_(name, shape, dt=F32):
        return sb(name, shape, dt), Tok()

    W1, W1_t = T_("W1", [128, 16, NC1], BF16)
    KT, KT_t = T_("KT", [128, ntok], BF16)
    VA, VA_t = T_("VA", [128, ntile, 130], BF16)
    g1c, g1c_t = T_("g1c", [128, 16])
    xt = [T_("xt%d" % i, [128, D]) for i in range(2)]
    xnb, xnb_t = T_("xnb", [128, D], BF16)
    junk, junk_t = xnb, xnb_t
    junk2, junk2_t = T_("junk2", [128, 128], BF16)
    xnT, xnT_t = T_("xnT", [128, 16, G1], BF16)
    cst, cst_t = T_("cst", [128, 7, 128])
    identb, identb_t = T_("identb", [128, 128], BF16)
    trib, trib_t = T_("trib", [128, 128], BF16)
    rmask, rmask_t = T_("rmask_s", [128, G1])
    cv, cv_t = T_("cv_s", [128, 20])
    omm, omm_t = T_("omm", [128, 8])
    wdu, wdu_t = T_("wdus", [96, 128]); wiu, wiu_t = T_("wius", [96, 128]); wgu, wgu_t = T_("wgus", [128, 2, 128])
    lacc, lacc_t = T_("lacc", [128, 4])
    neglam, neglam_t = T_("neglam", [128, 1])
    sgv, sgv_t = T_("sgv", [128, 128])
    ss, ss_t = T_("ss", [128, 1]); rstd, rstd_t = T_("rstd", [128, 1])
    ss2, ss2_t = T_("ss2", [128, 1]); rstd2, rstd2_t = T_("rstd2", [128, 1])
    ident = cst[:, 0, :]; MU = cst[:, 1, :]; ML = cst[:, 2, :]; MUI = cst[:, 3, :]; onesblk = cst[:, 5, :]; I64 = cst[:, 6, 0:64]
    PB = [T_("PB%d" % i, [128, G1 + 1]) for i in range(7)]
    SH = [T_("SH%d" % i, [128, G1]) for i in range(7)]
    tmpA, tmpA_t = T_("tmpA", [128, G1]); tmpB, tmpB_t = T_("tmpB", [128, G1])
    logw, logw_t = T_("logw", [128, G1]); av, av_t = T_("av", [128, G1]); gg, gg_t = T_("gg", [128, G1])
    kkn, kkn_t = T_("kkn", [128, G1]); k2, k2_t = T_("k2", [128, G1]); bonus, bonus_t = T_("bonus", [128, G1])
    cum, cum_t = T_("cum", [128, G1]); Pm, Pm_t = T_("Pm", [128, G1]); Pinv, Pinv_t = T_("Pinv", [128, G1]); Pprev, Pprev_t = T_("Pprev", [128, G1])
    At, At_t = T_("At", [128, G1], BF16); Bt, Bt_t = T_("Bt", [128, G1], BF16); Kt, Kt_t = T_("Kt", [128, G1], BF16); Rt, Rt_t = T_("Rt", [128, G1], BF16)
    vb16, vb16_t = T_("vb16", [128, G1], BF16)
    yT, yT_t = T_("yT", [128, G1]); yo, yo_t = T_("yo", [128, G1])
    TOK, TOK_t = T_("TOK", [128, 4, 128], BF16)
    lam_s = yT[:, 0:256].rearrange("p (a c) -> p a c", c=64); lam_t = yT_t
    lamp = yo[:, 0:128].rearrange("p (b c) -> p b c", c=64); lamp_t = yo_t
    HB = []
    for h in range(2):
        HB.append(dict(
            XT=[T_("XT%d_%d" % (h, i), [128, 128], BF16) for i in range(5)], XX=[T_("XX%d_%d" % (h, i), [128, 128], BF16) for i in range(4)],
            LakT=T_("LakT%d" % h, [128, 128], BF16), MrbT=T_("MrbT%d" % h, [128, 128], BF16), MrkT=T_("MrkT%d" % h, [128, 128], BF16),
            Z=[T_("Z%d_%d" % (h, i), [128, 128], BF16) for i in range(2)], ZF=T_("ZF%d" % h, [128, 128], BF16),
            MZ=T_("MZ%d" % h, [128, 4, 64], BF16), MB=T_("MB%d" % h, [128, 4, 64], BF16), MK=T_("MK%d" % h, [128, 4, 64], BF16)))
    RhT, RhT_t = T_("RhT", [128, 128]); YhT, YhT_t = T_("YhT", [128, 128])
    MT, MT_t = T_("MT", [128, 4, 64]); HP, HP_t = T_("HP", [128, 4, 64])
    Sst = [T_("Sst%d" % i, [128, 64]) for i in range(2)]
    QT, QT_t = T_("QT", [128, G1], BF16); QSQ, QSQ_t = T_("QSQ", [128, G1]); KSQ, KSQ_t = T_("KSQ", [128, G1])
    kmax2 = [T_("kmax2_%d" % i, [128, 1]) for i in range(2)]
    kred, kred_t = T_("kred", [128, 1]); sqq, sqq_t = T_("sqq", [128, 1])
    nshift = [T_("nshift%d" % i, [128, 1]) for i in range(2)]
    Pb = [T_("Pb%d" % i, [128, 512], BF16) for i in range(2)]
    om = [[T_("om%d_%d" % (i, j), [128, 128]) for j in range(2)] for i in range(2)]
    qmax2 = [T_("qmax2_%d" % i, [128, 1]) for i in range(2)]
    rs_, rs_t = T_("rs_", [128, 1]); attn, attn_t = T_("attn", [128, 128]); ybo, ybo_t = T_("ybo", [128, 128])
    ones128, ones_t = T_("ones128", [128, 128])

    def mm(out, lhsT, rhs, start=True, stop=True):
        return lambda e: e.matmul(out=out, lhsT=lhsT, rhs=rhs, start=start, stop=stop)

    cpy_i = [0]

    def copy_out(out, in_, reads, writes, eng=None):
        if eng is None:
            cpy_i[0] ^= 1
            eng = "act" if cpy_i[0] else "dve"
        if eng == "act":
            S.op("act", lambda e: e.activation(out=out, in_=in_, func=AF.Copy), reads=reads, writes=writes)
        else:
            S.op("dve", lambda e: e.tensor_copy(out=out, in_=in_), reads=reads, writes=writes)

    def dve_tt(out, in0, in1, op, reads, writes, eng="dve"):
        S.op(eng, lambda e: e.tensor_tensor(out=out, in0=in0, in1=in1, op=op), reads=reads, writes=writes)

    def dve_ts(out, in0, s1, s2, op0, op1, reads, writes, eng="dve"):
        if op1 is None:
            S.op(eng, lambda e: e.tensor_scalar(out=out, in0=in0, scalar1=s1, scalar2=None, op0=op0), reads=reads, writes=writes)
        else:
            S.op(eng, lambda e: e.tensor_scalar(out=out, in0=in0, scalar1=s1, scalar2=s2, op0=op0, op1=op1), reads=reads, writes=writes)

    def dve_stt(out, in0, scalar, in1, op0, op1, reads, writes):
        S.op("dve", lambda e: e.scalar_tensor_tensor(out=out, in0=in0, scalar=scalar, in1=in1, op0=op0, op1=op1), reads=reads, writes=writes)

    def act(out, in_, func, reads, writes, **kw):
        S.op("act", lambda e: e.activation(out=out, in_=in_, func=func, **kw), reads=reads, writes=writes)

    S.dma("sp", "c", lambda e: e.dma_start(out=cst[:], in_=consts.rearrange("c p n -> p c n")), writes=[cst_t])
    S.dma("sp", "c", lambda e: e.dma_start(out=cv[:], in_=cvec[:, :]), writes=[cv_t])
    S.dma("sp", "c", lambda e: e.dma_start(out=wdu[:], in_=wdu_d[:, :]), writes=[wdu_t])
    S.dma("sp", "c", lambda e: e.dma_start(out=wiu[:], in_=wiu_d[:, :]), writes=[wiu_t])
    S.dma("sp", "c", lambda e: e.dma_start(out=wgu[:], in_=wgu_d.rearrange("(k p) c -> p k c", p=128)), writes=[wgu_t])
    S.dma("sp", "c", lambda e: e.dma_start(out=yT[:, 0:256], in_=lamv[0:1, :].partition_broadcast(128)), writes=[lam_t])
    S.dma("sp", "c", lambda e: e.dma_start(out=sgv[:], in_=sublng[0:1, :].partition_broadcast(128)), writes=[sgv_t])
    S.dma("sp", "c", lambda e: e.dma_start(out=rmask[:], in_=rmask_d[0:1, :].partition_broadcast(128)), writes=[rmask_t])
    with nc.allow_non_contiguous_dma(reason="tiny gain vector"):
        S.dma("sp", "c", lambda e: e.dma_start(out=g1c[:], in_=g1d.rearrange("o (k p) -> p (o k)", p=128)), writes=[g1c_t])
    S.dma("pool", "w", lambda e: e.dma_start(out=W1[:], in_=w1.rearrange("(k p) c -> p k c", p=128)), writes=[W1_t])
    for b in range(NCAST):
        S.dma("pool", "ocast", lambda e, b=b: e.dma_start(out=castout[b].rearrange("p (s e) -> p s e", e=2048),
                                                      in_=castin[b].rearrange("p (s e) -> p s e", e=2048)))
    for kc in range(16):
        S.op("dve" if kc % 2 else "pool", lambda e, kc=kc: e.tensor_scalar(out=W1[:, kc, :], in0=W1[:, kc, :], scalar1=g1c[:, kc:kc + 1], scalar2=0.0,
                                                                            op0=ALU.mult, op1=ALU.add),
             reads=[W1_t, g1c_t], writes=[W1_t])
    S.op("dve", lambda e: e.tensor_copy(out=identb[:], in_=cst[:, 0, :]), reads=[cst_t], writes=[identb_t])
    S.op("dve", lambda e: e.tensor_copy(out=trib[:], in_=cst[:, 4, :]), reads=[cst_t], writes=[trib_t])
    dve_ts(omm[:, 0:8], cv[:, 0:8], -1.0, 1.0, ALU.mult, ALU.add, [cv_t], [omm_t])
    dve_ts(cv[:, 14:15], cv[:, 10:11], -1.0, 1.0, ALU.mult, ALU.add, [cv_t], [cv_t])
    dve_ts(sgv[:], sgv[:], 0.8, None, ALU.mult, None, [sgv_t], [sgv_t])
    dve_tt(lamp[:, 0, :], lam_s[:, 0, :], lam_s[:, 1, :], ALU.mult, [lam_t], [lamp_t])
    dve_tt(lamp[:, 1, :], lam_s[:, 2, :], lam_s[:, 3, :], ALU.mult, [lam_t], [lamp_t])
    S.op("dve", lambda e: e.tensor_reduce(out=lacc[:, 0:2], in_=lamp[:, 0:2, :], axis=AX.X, op=ALU.add), reads=[lamp_t], writes=[lacc_t])
    act(lacc[:, 2:4], lacc[:, 0:2], AF.Exp, [lacc_t], [lacc_t])
    dve_tt(neglam[:], lacc[:, 3:4], lacc[:, 2:3], ALU.subtract, [lacc_t], [neglam_t])
    dve_ts(neglam[:], neglam[:], -0.2, None, ALU.add, None, [neglam_t], [neglam_t])
    for i in range(7):
        S.op("pool", lambda e, i=i: e.memset(PB[i][0][:, 0:1], 0.0), writes=[PB[i][1]])
    for i in range(2):
        S.op("pool", lambda e, i=i: e.memset(Sst[i][0][:], 0.0), writes=[Sst[i][1]])
        S.op("pool", lambda e, i=i: e.memset(kmax2[i][0][:], 0.0), writes=[kmax2[i][1]])
        S.op("pool", lambda e, i=i: e.memset(qmax2[i][0][:], 0.0), writes=[qmax2[i][1]])
    S.op("pool", lambda e: e.memset(VA[:, :, 128:130], 1.0), writes=[VA_t])
    S.op("pool", lambda e: e.memset(ones128[:], 1.0), writes=[ones_t])

    scur = [0]

    def head_chain(h, cs):
        hb = HB[h]
        XT, XX, Z = hb["XT"], hb["XX"], hb["Z"]
        LakT, LakT_t = hb["LakT"]; MrbT, MrbT_t = hb["MrbT"]; MrkT, MrkT_t = hb["MrkT"]
        zf, zf_t = hb["ZF"]
        MZ, MZ_t = hb["MZ"]; MB, MB_t = hb["MB"]; MK, MK_t = hb["MK"]
        pb, pb_t = ps[h], ps_t[h]
        hs = slice(64 * h, 64 * h + 64)
        Ah, Bh, Kh, Rh = At[hs, cs], Bt[hs, cs], Kt[hs, cs], Rt[hs, cs]
        S.op("pe", mm(pb[:, 0:128], Bh, Ah), reads=[Bt_t, At_t], writes=[pb_t])
        dve_tt(XT[0][0][:], pb[:, 0:128], MU, ALU.mult, [pb_t, cst_t], [XT[0][1]])
        yield
        S.op("pe", mm(pb[:, 0:128], Ah, Bh), reads=[Bt_t, At_t], writes=[pb_t])
        dve_tt(XX[0][0][:], pb[:, 0:128], ML, ALU.mult, [pb_t, cst_t], [XX[0][1]])
        yield
        S.op("pe", mm(pb[:, 0:128], Kh, Ah), reads=[Kt_t, At_t], writes=[pb_t])
        dve_tt(LakT[:], pb[:, 0:128], MU, ALU.mult, [pb_t, cst_t], [LakT_t])
        yield
        S.op("pe", mm(pb[:, 0:128], Bh, Rh), reads=[Bt_t, Rt_t], writes=[pb_t])
        dve_tt(MrbT[:], pb[:, 0:128], MUI, ALU.mult, [pb_t, cst_t], [MrbT_t])
        yield
        S.op("pe", mm(pb[:, 0:128], Kh, Rh), reads=[Kt_t, Rt_t], writes=[pb_t])
        dve_tt(MrkT[:], pb[:, 0:128], MUI, ALU.mult, [pb_t, cst_t], [MrkT_t])
        yield
        S.op("pe", mm(pb[:, 0:64], LakT[:], TOK[:, 3, hs]), reads=[LakT_t, TOK_t], writes=[pb_t])
        zc, zc_t = Z[0]
        copy_out(zc[:, 64:128], pb[:, 0:64], [pb_t], [zc_t], eng="dve")
        S.op("pool", lambda e: e.tensor_copy(out=zc[:, 0:64], in_=TOK[:, 0, hs]), reads=[TOK_t], writes=[zc_t])
        yield
        for i in range(4):
            S.op("pe", mm(pb[:, 0:128], XX[i][0][:], XT[i][0][:]), reads=[XX[i][1], XT[i][1]], writes=[pb_t])
            copy_out(XT[i + 1][0][:], pb[:, 0:128], [pb_t], [XT[i + 1][1]], eng="dve")
            yield
            if i < 3:
                S.op("pe", mm(pb[:, 0:128], XT[i][0][:], XX[i][0][:]), reads=[XX[i][1], XT[i][1]], writes=[pb_t])
                copy_out(XX[i + 1][0][:], pb[:, 0:128], [pb_t], [XX[i + 1][1]], eng="act")
                yield
            zc, zc_t = Z[i % 2]
            zn, zn_t = Z[(i + 1) % 2]
            S.op("pe", mm(pb[:, 0:128], XT[i][0][:], zc[:]), reads=[XT[i][1], zc_t], writes=[pb_t])
            dve_tt(zn[:], pb[:, 0:128], zc[:], ALU.add, [pb_t, zc_t], [zn_t])
            yield
        zc, zc_t = Z[0]
        S.op("pe", mm(pb[:, 0:128], XT[4][0][:], zc[:]), reads=[XT[4][1], zc_t], writes=[pb_t])
        dve_tt(zf[:], pb[:, 0:128], zc[:], ALU.add, [pb_t, zc_t], [zf_t])
        yield
        S.op("pe", mm(pb[hs, 0:128], zf[:, 0:64], MrbT[:]), reads=[zf_t, MrbT_t], writes=[pb_t])
        dve_tt(RhT[hs, :], pb[hs, 0:128], Rh, ALU.add, [pb_t, Rt_t], [RhT_t])
        yield
        S.op("pe", mm(pb[hs, 0:128], zf[:, 64:128], MrbT[:], True, False), reads=[zf_t, MrbT_t], writes=[pb_t])
        S.op("pe", mm(pb[hs, 0:128], TOK[:, 3, hs], MrkT[:], False, True), reads=[TOK_t, MrkT_t], writes=[pb_t])
        copy_out(YhT[hs, :], pb[hs, 0:128], [pb_t], [YhT_t], eng="act")
        cmb = cv[:, 16:20].unsqueeze(2).broadcast_to([128, 4, 64])
        dve_tt(MZ[:], zf[:, 0:64].unsqueeze(1).broadcast_to([128, 4, 64]), cmb, ALU.mult, [zf_t, cv_t], [MZ_t])
        dve_tt(MB[:], TOK[:, 1, hs].unsqueeze(1).broadcast_to([128, 4, 64]), cmb, ALU.mult, [TOK_t, cv_t], [MB_t], eng="pool")
        dve_tt(MK[:], TOK[:, 2, hs].unsqueeze(1).broadcast_to([128, 4, 64]), cmb, ALU.mult, [TOK_t, cv_t], [MK_t], eng="pool")
        yield
        for c in range(4):
            S.op("pe", mm(ps[2][hs, c * 64:(c + 1) * 64], MZ[:, c, :], TOK[:, 1, hs]), reads=[MZ_t, TOK_t], writes=[ps_t[2]])
        for c in range(4):
            S.op("pe", mm(ps[3][hs, c * 64:(c + 1) * 64], MB[:, c, :], zf[:, 64:128], True, False), reads=[zf_t, MB_t], writes=[ps_t[3]])
            S.op("pe", mm(ps[3][hs, c * 64:(c + 1) * 64], MK[:, c, :], TOK[:, 3, hs], False, True), reads=[TOK_t, MK_t], writes=[ps_t[3]])
        yield

    def rwkv_group(g):
        t0 = g * G1
        r_, k_, v_, xw_, xa_, xg0_, xg1_ = range(7)
        rows = [128, 128, 128, 96, 96, 128, 128]
        for b in range(7):
            n = rows[b]
            pbuf, pbt = PB[b]
            sh, sht = SH[b]
            dve_ts(tmpA[0:n, :], pbuf[0:n, 0:G1], cv[0:n, b:b + 1], None, ALU.mult, None, [pbt, cv_t], [tmpA_t])
            dve_stt(sh[0:n, :], pbuf[0:n, 1:G1 + 1], omm[0:n, b:b + 1], tmpA[0:n, :], ALU.mult, ALU.add, [pbt, omm_t, tmpA_t], [sht])
            S.op("pool", lambda e, pbuf=pbuf, n=n: e.tensor_copy(out=pbuf[0:n, 0:1], in_=pbuf[0:n, G1:G1 + 1]), reads=[pbt], writes=[pbt])
            if b % 2:
                yield
        shr, shr_t = SH[r_]; shk, shk_t = SH[k_]; shv, shv_t = SH[v_]
        act(tmpB[0:96, :], SH[xw_][0][0:96, :], AF.Tanh, [SH[xw_][1]], [tmpB_t])
        pb, pb_t = scr()
        S.op("pe", mm(pb[:, 0:G1], wdu[:, :], tmpB[0:96, :]), reads=[wdu_t, tmpB_t], writes=[pb_t])
        act(logw[:], pb[:, 0:G1], AF.Sigmoid, [pb_t, cv_t], [logw_t], bias=cv[:, 7:8])
        dve_ts(logw[:], logw[:], -0.6065306597126334, None, ALU.mult, None, [logw_t], [logw_t])
        pb, pb_t = scr()
        S.op("pe", mm(pb[:, 0:G1], wiu[:, :], SH[xa_][0][0:96, :]), reads=[wiu_t, SH[xa_][1]], writes=[pb_t])
        act(av[:], pb[:, 0:G1], AF.Sigmoid, [pb_t, cv_t], [av_t], bias=cv[:, 8:9])
        act(SH[xg0_][0][:], SH[xg0_][0][:], AF.Sigmoid, [SH[xg0_][1]], [SH[xg0_][1]])
        act(SH[xg1_][0][:], SH[xg1_][0][:], AF.Sigmoid, [SH[xg1_][1]], [SH[xg1_][1]])
        yield
        pb, pb_t = scr()
        S.op("pe", mm(pb[:, 0:G1], wgu[:, 0, :], SH[xg0_][0][:], True, False), reads=[wgu_t, SH[xg0_][1]], writes=[pb_t])
        S.op("pe", mm(pb[:, 0:G1], wgu[:, 1, :], SH[xg1_][0][:], False, True), reads=[wgu_t, SH[xg1_][1]], writes=[pb_t])
        copy_out(gg[:], pb[:, 0:G1], [pb_t], [gg_t], eng="dve")
        dve_ts(kkn[:], shk[:], cv[:, 9:10], None, ALU.mult, None, [shk_t, cv_t], [kkn_t])
        dve_tt(tmpA[:], kkn[:], kkn[:], ALU.mult, [kkn_t], [tmpA_t], eng="pool")
        pb, pb_t = scr()
        S.op("pe", mm(pb[:, 0:G1], onesblk, tmpA[:]), reads=[cst_t, tmpA_t], writes=[pb_t])
        act(tmpB[:], pb[:, 0:G1], AF.Sqrt, [pb_t], [tmpB_t])
        dve_ts(tmpB[:], tmpB[:], 1e-12, None, ALU.max, None, [tmpB_t], [tmpB_t])
        S.op("dve", lambda e: e.reciprocal(out=tmpB[:], in_=tmpB[:]), reads=[tmpB_t], writes=[tmpB_t])
        dve_tt(kkn[:], kkn[:], tmpB[:], ALU.mult, [kkn_t, tmpB_t], [kkn_t])
        yield
        dve_ts(tmpA[:], av[:], cv[:, 10:11], cv[:, 14:15], ALU.mult, ALU.add, [av_t, cv_t], [tmpA_t])
        dve_tt(k2[:], shk[:], tmpA[:], ALU.mult, [shk_t, tmpA_t], [k2_t])
        dve_tt(tmpA[:], shr[:], k2[:], ALU.mult, [shr_t, k2_t], [tmpA_t], eng="pool")
        dve_ts(tmpA[:], tmpA[:], cv[:, 11:12], None, ALU.mult, None, [tmpA_t, cv_t], [tmpA_t])
        pb, pb_t = scr()
        S.op("pe", mm(pb[:, 0:G1], onesblk, tmpA[:]), reads=[cst_t, tmpA_t], writes=[pb_t])
        dve_tt(bonus[:], pb[:, 0:G1], shv[:], ALU.mult, [pb_t, shv_t], [bonus_t])
        S.op("dve", lambda e: e.tensor_tensor_scan(out=cum[:], data0=rmask[:], data1=logw[:], initial=0.0, op0=ALU.mult, op1=ALU.add),
             reads=[rmask_t, logw_t], writes=[cum_t])
        yield
        act(Pm[:], cum[:], AF.Exp, [cum_t], [Pm_t])
        act(Pinv[:], cum[:], AF.Exp, [cum_t], [Pinv_t], scale=-1.0)
        dve_tt(tmpA[:], cum[:], logw[:], ALU.subtract, [cum_t, logw_t], [tmpA_t], eng="pool")
        act(Pprev[:], tmpA[:], AF.Exp, [tmpA_t], [Pprev_t])
        dve_stt(At[:], kkn[:], -1.0, Pprev[:], ALU.mult, ALU.mult, [kkn_t, Pprev_t], [At_t])
        dve_tt(tmpB[:], kkn[:], av[:], ALU.mult, [kkn_t, av_t], [tmpB_t], eng="pool")
        dve_tt(Bt[:], tmpB[:], Pinv[:], ALU.mult, [tmpB_t, Pinv_t], [Bt_t])
        dve_tt(Kt[:], k2[:], Pinv[:], ALU.mult, [k2_t, Pinv_t], [Kt_t], eng="pool")
        dve_tt(Rt[:], shr[:], Pm[:], ALU.mult, [shr_t, Pm_t], [Rt_t])
        S.op("pool", lambda e: e.tensor_copy(out=vb16[:], in_=shv[:]), reads=[shv_t], writes=[vb16_t])
        yield
        for tl in range(G1 // 128):
            cs = slice(tl * 128, (tl + 1) * 128)
            for j, (src, srct) in enumerate([(At, At_t), (Bt, Bt_t), (Kt, Kt_t), (vb16, vb16_t)]):
                S.op("pe", lambda e, j=j, src=src: e.transpose(out=psb[0][:, j * 128:(j + 1) * 128], in_=src[:, cs], identity=identb[:]),
                     reads=[srct, identb_t], writes=[psb_t[0]])
            copy_out(TOK[:].rearrange("p a b -> p (a b)"), psb[0][:, 0:512], [psb_t[0]], [TOK_t], eng="act")
            yield
            chains = [head_chain(0, cs), head_chain(1, cs)]
            while chains:
                for ch in list(chains):
                    try:
                        next(ch)
                    except StopIteration:
                        chains.remove(ch)
                yield
            dve_tt(MT[:], ps[2][:, 0:256].rearrange("p (c k) -> p c k", c=4), I64.unsqueeze(1).broadcast_to([128, 4, 64]), ALU.add,
                   [ps_t[2], cst_t], [MT_t])
            for c in range(4):
                col = tl * 128 + 32 * c + 31
                dve_ts(HP[:, c, :], ps[3][:, c * 64:(c + 1) * 64], Pm[:, col:col + 1], None, ALU.mult, None, [ps_t[3], Pm_t], [HP_t])
            yield
            for c in range(4):
                col = tl * 128 + 32 * c + 31
                sc_, sc_t = Sst[scur[0]]
                sn_, sn_t = Sst[1 - scur[0]]
                for h in range(2):
                    hs = slice(64 * h, 64 * h + 64)
                    S.op("pe", mm(ps[0][hs, 0:64], MT[hs, c, :], sc_[hs, :]), reads=[MT_t, sc_t], writes=[ps_t[0]])
                for h in range(2):
                    hs = slice(64 * h, 64 * h + 64)
                    S.op("pe", mm(ps[1][hs, 32 * c:32 * c + 32], sc_[hs, :], RhT[hs, 32 * c:32 * c + 32]), reads=[sc_t, RhT_t], writes=[ps_t[1]])
                dve_stt(sn_[:], ps[0][:, 0:64], Pm[:, col:col + 1], HP[:, c, :], ALU.mult, ALU.add, [ps_t[0], Pm_t, HP_t], [sn_t])
                scur[0] = 1 - scur[0]
                yield
            dve_tt(yT[:, cs], ps[1][:, 0:128], YhT[:], ALU.add, [ps_t[1], YhT_t], [yT_t])
            yield
        pb, pb_t = scr()
        S.op("pe", mm(pb[:, 0:G1], onesblk, yT[:]), reads=[cst_t, yT_t], writes=[pb_t])
        act(tmpA[:], pb[:, 0:G1], AF.Copy, [pb_t], [tmpA_t], scale=1.0 / 64)
        act(tmpB[:], yT[:], AF.Square, [yT_t], [tmpB_t])
        pb, pb_t = scr()
        S.op("pe", mm(pb[:, 0:G1], onesblk, tmpB[:]), reads=[cst_t, tmpB_t], writes=[pb_t])
        dve_tt(tmpB[:], tmpA[:], tmpA[:], ALU.mult, [tmpA_t], [tmpB_t], eng="pool")
        dve_stt(tmpB[:], pb[:, 0:G1], 1.0 / 64, tmpB[:], ALU.mult, ALU.subtract, [pb_t, tmpB_t], [tmpB_t])
        yield
        act(tmpB[:], tmpB[:], AF.Sqrt, [tmpB_t], [tmpB_t], bias=64e-5)
        S.op("dve", lambda e: e.reciprocal(out=tmpB[:], in_=tmpB[:]), reads=[tmpB_t], writes=[tmpB_t])
        dve_tt(yo[:], yT[:], tmpA[:], ALU.subtract, [yT_t, tmpA_t], [yo_t])
        dve_tt(yo[:], yo[:], tmpB[:], ALU.mult, [yo_t, tmpB_t], [yo_t])
        dve_ts(yo[:], yo[:], cv[:, 12:13], cv[:, 13:14], ALU.mult, ALU.add, [yo_t, cv_t], [yo_t])
        dve_tt(yo[:], yo[:], bonus[:], ALU.add, [yo_t, bonus_t], [yo_t], eng="pool")
        dve_tt(yo[:], yo[:], gg[:], ALU.mult, [yo_t, gg_t], [yo_t])
        S.dma("sp", "oya", lambda e: e.dma_start(out=yaT[:, t0:t0 + G1], in_=yo[:]), reads=[yo_t], writes=[])
        yield

    def attn_group(g):
        for m in range(2):
            ms = slice(64 * m, 64 * m + 64)
            nsh, nsh_t = nshift[m]
            act(sqq[:], qmax2[m][0][:], AF.Sqrt, [qmax2[m][1], kmax2[m][1]], [sqq_t], scale=kmax2[m][0][:, 0:1])
            dve_ts(nsh[:], sqq[:], -0.125, None, ALU.mult, None, [sqq_t], [nsh_t])

            def stage_a(j):
                P_, P_t = Pb[j % 2]
                if j < g:
                    for a in range(2):
                        kt = 2 * j + a
                        S.op("pe", mm(ps[5][:, a * 256:(a + 1) * 256], KT[ms, kt * 128:(kt + 1) * 128], QT[ms, :]), reads=[QT_t, KT_t], writes=[ps_t[5]])
                    act(P_[:], ps[5][:, :], AF.Exp, [ps_t[5], nsh_t], [P_t], scale=0.125, bias=nsh[:, 0:1])
                else:
                    kt = 2 * g
                    S.op("pe", mm(ps[5][:, 0:256], KT[ms, kt * 128:(kt + 1) * 128], QT[ms, :]), reads=[QT_t, KT_t], writes=[ps_t[5]])
                    S.op("pe", mm(ps[5][:, 384:512], KT[ms, (kt + 1) * 128:(kt + 2) * 128], QT[ms, 128:256]), reads=[QT_t, KT_t], writes=[ps_t[5]])
                    act(P_[:, 0:256], ps[5][:, 0:256], AF.Exp, [ps_t[5], nsh_t], [P_t], scale=0.125, bias=nsh[:, 0:1])
                    act(P_[:, 384:512], ps[5][:, 384:512], AF.Exp, [ps_t[5], nsh_t], [P_t], scale=0.125, bias=nsh[:, 0:1])
                    dve_tt(P_[:, 0:128], P_[:, 0:128], trib[:], ALU.mult, [P_t, trib_t], [P_t], eng="pool")
                    dve_tt(P_[:, 384:512], P_[:, 384:512], trib[:], ALU.mult, [P_t, trib_t], [P_t], eng="pool")

            def stage_b(j):
                P_, P_t = Pb[j % 2]
                if j < g:
                    for a in range(2):
                        kt = 2 * j + a
                        for tl in range(2):
                            ob = OB[tl]
                            S.op("pe", mm(ps[ob][:, 0:129], P_[:, a * 256 + tl * 128:a * 256 + (tl + 1) * 128], VA[:, kt, 0:129], kt == 0, False),
                                 reads=[P_t, VA_t], writes=[ps_t[ob]])
                else:
                    kt = 2 * g
                    S.op("pe", mm(ps[OB[0]][:, 0:129], P_[:, 0:128], VA[:, kt, 0:129], kt == 0, True), reads=[P_t, VA_t], writes=[ps_t[OB[0]]])
                    S.op("pe", mm(ps[OB[1]][:, 0:129], P_[:, 128:256], VA[:, kt, 0:129], kt == 0, False), reads=[P_t, VA_t], writes=[ps_t[OB[1]]])
                    S.op("pe", mm(ps[OB[1]][:, 0:129], P_[:, 384:512], VA[:, kt + 1, 0:129], False, True), reads=[P_t, VA_t], writes=[ps_t[OB[1]]])

            stage_a(0)
            for j in range(g + 1):
                if j + 1 <= g:
                    stage_a(j + 1)
                stage_b(j)
                yield
            for tl in range(2):
                ob = OB[tl]
                S.op("dve", lambda e, ob=ob: e.reciprocal(out=rs_[:], in_=ps[ob][:, 128:129]), reads=[ps_t[ob]], writes=[rs_t])
                dve_ts(om[tl][m][0][:], ps[ob][:, 0:128], rs_[:, 0:1], None, ALU.mult, None, [ps_t[ob], rs_t], [om[tl][m][1]])
            yield
        for tl in range(2):
            qt = 2 * g + tl
            dve_stt(attn[:], om[tl][1][0][:], neglam[:, 0:1], om[tl][0][0][:], ALU.mult, ALU.add, [om[tl][0][1], om[tl][1][1], neglam_t], [attn_t])
            S.op("act", lambda e: e.activation(out=junk2[:], in_=attn[:], func=AF.Square, accum_out=ss2[:]), reads=[attn_t], writes=[junk2_t, ss2_t])
            S.op("act", lambda e: e.activation(out=ss2[:], in_=ss2[:], func=AF.Sqrt, scale=1.0 / 128, bias=1e-5), reads=[ss2_t], writes=[ss2_t])
            S.op("dve", lambda e: e.reciprocal(out=rstd2[:], in_=ss2[:]), reads=[ss2_t], writes=[rstd2_t])
            dve_stt(ybo[:], attn[:], rstd2[:, 0:1], sgv[:], ALU.mult, ALU.mult, [attn_t, rstd2_t, sgv_t], [ybo_t])
            S.dma("sp", "oyb", lambda e, qt=qt: e.dma_start(out=yb[qt * 128:(qt + 1) * 128, :], in_=ybo[:]), reads=[ybo_t], writes=[])
            yield

    def load_x(g_):
        for tl in range(2):
            xb, xb_t = xt[tl]
            S.dma("sp", "x%d" % tl, lambda e, tl=tl, xb=xb: e.dma_start(out=xb[:], in_=x[g_ * G1 + tl * 128:g_ * G1 + (tl + 1) * 128, :]), writes=[xb_t])

    load_x(0)
    for g in range(ngroups):
        t0 = g * G1
        for tl in range(2):
            xb, xb_t = xt[tl]
            _rms_rstd(S, "act", xb[:], xb_t, junk[:], junk_t, ss[:], ss_t, rstd[:], rstd_t, 1e-6)
            dve_ts(xnb[:], xb[:], rstd[:, 0:1], None, ALU.mult, None, [xb_t, rstd_t], [xnb_t])
            for half in range(2):
                for j in range(8):
                    kc = half * 8 + j
                    S.op("pe", lambda e, kc=kc, j=j: e.transpose(out=psb[0][:, j * 128:(j + 1) * 128], in_=xnb[:, kc * 128:(kc + 1) * 128], identity=identb[:]),
                         reads=[xnb_t, identb_t], writes=[psb_t[0]])
                copy_out(xnT[:, half * 8:(half + 1) * 8, tl * 128:(tl + 1) * 128], psb[0][:].rearrange("p (k t) -> p k t", k=8), [psb_t[0]], [xnT_t])
        if g + 1 < ngroups:
            load_x(g + 1)
        blocks = [(C_R, 128), (C_K, 128), (C_V, 128), (C_XW, 96), (C_XA, 96), (C_XG, 128), (C_XG + 128, 128)]
        for bi, (c0, w) in enumerate(blocks):
            pb, pb_t = scr()
            for kc in range(16):
                S.op("pe", mm(pb[0:w, 0:G1], W1[:, kc, c0:c0 + w], xnT[:, kc, :], kc == 0, kc == 15), reads=[W1_t, xnT_t], writes=[pb_t])
            copy_out(PB[bi][0][0:w, 1:G1 + 1], pb[0:w, 0:G1], [pb_t], [PB[bi][1]])
        pb, pb_t = scr()
        for kc in range(16):
            S.op("pe", mm(pb[:, 0:G1], W1[:, kc, C_QD:C_QD + 128], xnT[:, kc, :], kc == 0, kc == 15), reads=[W1_t, xnT_t], writes=[pb_t])
        act(QT[:], pb[:, 0:G1], AF.Copy, [pb_t], [QT_t])
        act(QSQ[:], pb[:, 0:G1], AF.Square, [pb_t], [QSQ_t])
        pb, pb_t = scr()
        for kc in range(16):
            S.op("pe", mm(pb[:, 0:G1], W1[:, kc, C_KD:C_KD + 128], xnT[:, kc, :], kc == 0, kc == 15), reads=[W1_t, xnT_t], writes=[pb_t])
        act(KT[:, t0:t0 + G1], pb[:, 0:G1], AF.Copy, [pb_t], [KT_t])
        act(KSQ[:], pb[:, 0:G1], AF.Square, [pb_t], [KSQ_t])
        for m in range(2):
            ms = slice(64 * m, 64 * m + 64)
            pb, pb_t = scr()
            S.op("pe", mm(pb[:, 0:G1], ones128[ms, :], KSQ[ms, :]), reads=[ones_t, KSQ_t], writes=[pb_t])
            S.op("dve", lambda e, pb=pb: e.tensor_reduce(out=kred[:], in_=pb[:, 0:G1], axis=AX.X, op=ALU.max), reads=[pb_t], writes=[kred_t])
            dve_tt(kmax2[m][0][:], kmax2[m][0][:], kred[:], ALU.max, [kred_t, kmax2[m][1]], [kmax2[m][1]])
            pb, pb_t = scr()
            S.op("pe", mm(pb[:, 0:G1], ones128[ms, :], QSQ[ms, :]), reads=[ones_t, QSQ_t], writes=[pb_t])
            S.op("dve", lambda e, pb=pb: e.tensor_reduce(out=kred[:], in_=pb[:, 0:G1], axis=AX.X, op=ALU.max), reads=[pb_t], writes=[kred_t])
            dve_tt(qmax2[m][0][:], qmax2[m][0][:], kred[:], ALU.max, [kred_t, qmax2[m][1]], [qmax2[m][1]])
        for tl in range(2):
            pb, pb_t = scr()
            for kc in range(16):
                S.op("pe", mm(pb[:, 0:128], xnT[:, kc, tl * 128:(tl + 1) * 128], W1[:, kc, C_VD:C_VD + 128], kc == 0, kc == 15),
                     reads=[W1_t, xnT_t], writes=[pb_t])
            copy_out(VA[:, 2 * g + tl, 0:128], pb[:, 0:128], [pb_t], [VA_t])
        gens = []
        if do_rwkv:
            gr = rwkv_group(g)
            if rw_stage < 9000:
                def lim(gr=gr):
                    for _ in range(rw_stage):
                        next(gr)
                        yield
                gr = lim()
            gens.append(gr)
        if do_attn:
            gens.append(attn_group(g))
        _roundrobin(gens)
    for k in S.dsem:
        if k.startswith("dma:o"):
            nc.sync.wait_ge(S.dsem[k][0], S.dsem[k][1])
    return nc, S


def _consts():
    t = np.arange(128)
    same = (t[:, None] // CHK) == (t[None, :] // CHK)
    MU = (same & (t[:, None] < t[None, :])).astype(np.float32)
    ML = MU.T.copy()
    MUI = (same & (t[:, None] <= t[None, :])).astype(np.float32)
    TRI = (t[:, None] <= t[None, :]).astype(np.float32)
    ob = ((t[:, None] // 64) == (t[None, :] // 64)).astype(np.float32)
    i64 = np.zeros((128, 128), np.float32)
    i64[t, t % 64] = 1.0
    return np.stack([np.eye(128, dtype=np.float32), MU, ML, MUI, TRI, ob, i64])


def prep_l1(inp, c, ntok=SEQ):
    w_in = inp["w_in"][0]
    hs = slice(128 * c, 128 * c + 128)
    o_d = 3520
    cols = np.concatenate([np.arange(128 * c, 128 * c + 128), 1024 + np.arange(128 * c, 128 * c + 128),
                           2048 + np.arange(128 * c, 128 * c + 128), np.arange(3072, 3520),
                           o_d + np.arange(128 * c, 128 * c + 128), o_d + 1024 + np.arange(128 * c, 128 * c + 128),
                           o_d + 2048 + np.arange(128 * c, 128 * c + 128)])
    mu = inp["shift_mu"][0]
    cvec = np.zeros((128, 20), np.float32)
    cvec[:, 0] = mu[0:1024][hs]; cvec[:, 1] = mu[1024:2048][hs]; cvec[:, 2] = mu[2048:3072][hs]
    cvec[:96, 3] = mu[3072:3168]; cvec[:96, 4] = mu[3168:3264]; cvec[:, 5] = mu[3264:3392]; cvec[:, 6] = mu[3392:3520]
    cvec[:, 7] = inp["rwkv_w0"][0][hs]; cvec[:, 8] = inp["rwkv_a0"][0][hs]; cvec[:, 9] = inp["k_k"][0][hs]
    cvec[:, 10] = inp["k_a"][0][hs]; cvec[:, 11] = inp["r_k"][0].reshape(-1)[hs]
    cvec[:, 12] = inp["lnx_g"][0][hs]; cvec[:, 13] = inp["lnx_b"][0][hs]
    for c_ in range(4):
        cvec[32 * c_:32 * c_ + 32, 16 + c_] = 1.0
    rm = np.ones((1, G1), np.float32); rm[0, ::CHK] = 0.0
    return dict(
        x=np.ascontiguousarray(inp["x"][0, :ntok]), w1=np.ascontiguousarray(w_in[:, cols]), cvec=cvec,
        wdu=np.ascontiguousarray(inp["w_decay_up"][0][:, hs]), wiu=np.ascontiguousarray(inp["w_iclr_up"][0][:, hs]),
        wgu=np.ascontiguousarray(inp["w_gate_up"][0][:, hs]),
        lamv=np.concatenate([inp["lam_q1"][0], inp["lam_k1"][0], inp["lam_q2"][0], inp["lam_k2"][0]])[None, :].astype(np.float32),
        sublng=inp["subln_g"][0][None, :].astype(np.float32), g1=inp["norm1_g"][0][None, :].astype(np.float32),
        consts=_consts(), rmask=rm)


def _blk(w, nb):
    K_ = w.shape[0]
    return np.ascontiguousarray(w.reshape(K_ // 128, 128, nb, 512).transpose(2, 1, 0, 3).reshape(nb, 128, (K_ // 128) * 512))


W_BLOCKS = (("s_wg", 8), ("s_wab", 4), ("s_wo", 4), ("s_wq", 4), ("s_u", 32), ("s_v", 32))


def l2_weight_blocks(inp):
    w_in = inp["w_in"][0]
    wgb = _blk(w_in[:, 6592:], 8)
    wabb = np.concatenate([_blk(inp["w_proj_a"][0], 4), _blk(inp["w_proj_b"][0], 4)], axis=2)
    uTb = _blk(np.ascontiguousarray(inp["peer_u"][0].T), 32)
    vtb = inp["peer_v"][0].reshape(32, 4, 128, D).transpose(0, 2, 1, 3).reshape(32, 128, 4 * D)
    allb = np.zeros((NCAST * NCORES, 128, 8192), np.float32)
    o = 0
    for a in (wgb, wabb, _blk(inp["w_out"][0], 4), _blk(inp["peer_wq"][0], 4), uTb, vtb):
        allb[o:o + a.shape[0]] = a
        o += a.shape[0]
    return allb


def l2_shared(inp, cast_all):
    m = dict(
        skT=np.ascontiguousarray(inp["peer_sub_keys"][0].reshape(16, 128, 128).transpose(0, 2, 1)),
        gv=np.stack([inp["norm1_g"][0], inp["norm2_g"][0], inp["final_g"]]).astype(np.float32),
        ident=np.eye(128, dtype=np.float32))
    o = 0
    for name, nb in W_BLOCKS:
        m[name] = np.ascontiguousarray(cast_all[o:o + nb])
        o += nb
    return m


def prep_l2(inp, c, yaT_full, ybT_full, shared):
    ts = slice(TOK * c, TOK * (c + 1))
    m = dict(shared)
    m["x"] = np.ascontiguousarray(inp["x"][0, ts])
    m["yaT"] = np.ascontiguousarray(yaT_full[:, ts])
    m["ybT"] = np.ascontiguousarray(ybT_full[:, ts])
    return m


def kernel(**inputs):
    inp = {k: np.asarray(v) for k, v in inputs.items()}
    nc1, _ = build_l1()
    allb = l2_weight_blocks(inp)
    maps1 = [prep_l1(inp, c) for c in range(NCORES)]
    for c in range(NCORES):
        maps1[c]["castin"] = allb[NCAST * c:NCAST * (c + 1)]
    r1 = run_bass_kernel_spmd(nc1, maps1, core_ids=list(range(NCORES))).results
    del maps1, allb
    cast_all = np.concatenate([r1[c]["castout"] for c in range(NCORES)], axis=0)
    yaT_full = np.concatenate([r1[c]["yaT"] for c in range(NCORES)], axis=0)
    ybT_full = np.concatenate([r1[c]["yb"].T for c in range(NCORES)], axis=0)
    w_in = inp["w_in"][0]
    shared = l2_shared(inp, cast_all)
    nc2, _ = build_l2()
    maps2 = [prep_l2(inp, c, yaT_full, ybT_full, shared) for c in range(NCORES)]
    r2 = run_bass_kernel_spmd(nc2, maps2, core_ids=list(range(NCORES))).results
    out = np.concatenate([r2[c]["y"] for c in range(NCORES)], axis=0)
    return out.reshape(1, SEQ, D).astype(np.float32)
```

```python
import numpy as np
import concourse.bass as bass
import concourse.mybir as mybir
from concourse.bass_utils import run_bass_kernel_spmd

F32 = mybir.dt.float32
BF16 = mybir.dt.bfloat16
AF = mybir.ActivationFunctionType
ALU = mybir.AluOpType
AX = mybir.AxisListType

NCORES = 8
D = 2048
SEQ = 16384
TOK = SEQ // NCORES
NEG = -1.0e30


class Tok:
    __slots__ = ("w", "r")

    def __init__(self):
        self.w = None
        self.r = []


class Sync:
    def __init__(self, nc):
        self.nc = nc
        self.engs = {"pe": nc.tensor, "act": nc.scalar, "dve": nc.vector, "pool": nc.gpsimd, "sp": nc.sync}
        self.sem = {k: nc.alloc_semaphore("s_" + k) for k in ("pe", "act", "dve", "pool")}
        self.cnt = {k: 0 for k in self.sem}
        self.waited = {e: {} for e in self.engs}
        self.dsem = {}
        self.ninst = 0

    def _deps(self, reads, writes):
        deps = {}
        for b in reads:
            if b.w is not None:
                k, v = b.w
                deps[k] = max(deps.get(k, 0), v)
        for b in writes:
            if b.w is not None:
                k, v = b.w
                deps[k] = max(deps.get(k, 0), v)
            for (k, v) in b.r:
                deps[k] = max(deps.get(k, 0), v)
        return deps

    def _wait(self, ek, deps):
        eng = self.engs[ek]
        wd = self.waited[ek]
        for k, v in deps.items():
            if k == ek and ek == "pe":
                continue
            if k.startswith("dma:"):
                v = self.dsem[k][1]
                s = self.dsem[k][0]
            else:
                s = self.sem[k]
            if wd.get(k, 0) >= v:
                continue
            eng.wait_ge(s, v)
            wd[k] = v
            self.ninst += 1

    def _mark(self, ev, reads, writes):
        for b in reads:
            b.r.append(ev)
        for b in writes:
            b.w = ev
            b.r = []

    def op(self, ek, fn, reads=(), writes=()):
        self._wait(ek, self._deps(reads, writes))
        inst = fn(self.engs[ek])
        self.cnt[ek] += 1
        inst.then_inc(self.sem[ek], 1)
        self.ninst += 1
        self._mark((ek, self.cnt[ek]), reads, writes)

    def dma(self, qk, stream, fn, reads=(), writes=()):
        self._wait(qk, self._deps(reads, writes))
        inst = fn(self.engs[qk])
        k = "dma:" + stream
        if k not in self.dsem:
            self.dsem[k] = [self.nc.alloc_semaphore("d_" + stream), 0]
        self.dsem[k][1] += 16
        inst.then_inc(self.dsem[k][0], 16)
        self.ninst += 1
        self._mark((k, self.dsem[k][1]), reads, writes)

    def finish(self, toks):
        deps = self._deps(toks, ())
        self._wait("sp", deps)


def _rms_rstd(S, ek_sq, src_ap, src_tok, junk, junk_tok, ss, ss_tok, rstd, rstd_tok, eps):
    S.op("act", lambda e: e.activation(out=junk, in_=src_ap, func=AF.Square, accum_out=ss),
         reads=[src_tok], writes=[junk_tok, ss_tok])
    S.op("act", lambda e: e.activation(out=ss, in_=ss, func=AF.Sqrt, scale=1.0 / D, bias=float(eps)),
         reads=[ss_tok], writes=[ss_tok])
    S.op("dve", lambda e: e.reciprocal(out=rstd, in_=ss), reads=[ss_tok], writes=[rstd_tok])


CH = 256
NT = CH // 128


def build_l2(nchunks=TOK // CH, peer_blocks=32, stage=99):
    nc = bass.Bass("TRN2", target_bir_lowering=False)
    ntok = nchunks * CH
    dr = lambda n, s, k="ExternalInput": nc.dram_tensor(n, s, F32, kind=k).ap()
    x = dr("x", [ntok, D])
    yaT = dr("yaT", [1024, ntok])
    ybT = dr("ybT", [1024, ntok])
    bfi = lambda n, nb: nc.dram_tensor(n, [nb, 128, 8192], BF16, kind="ExternalInput").ap()
    s_wg, s_wab, s_wo, s_wq, s_u, s_v = bfi("s_wg", 8), bfi("s_wab", 4), bfi("s_wo", 4), bfi("s_wq", 4), bfi("s_u", 32), bfi("s_v", 32)
    skT = dr("skT", [16, 128, 128])
    gv = dr("gv", [3, D])
    ident_d = dr("ident", [128, 128])
    y = dr("y", [ntok, D], "ExternalOutput")

    S = Sync(nc)
    sb = lambda n, s, dt=F32: nc.alloc_sbuf_tensor(n, s, dt)
    ps = [nc.alloc_psum_tensor("ps%d" % i, [128, 512], F32) for i in range(6)]
    psb = [nc.alloc_psum_tensor("psb%d" % i, [128, 1024], BF16) for i in range(2)]
    ps_t = [Tok() for _ in range(6)]
    psb_t = [Tok() for _ in range(2)]

    gvec = sb("gvec", [128, D]); gvec_t = Tok()
    xh = sb("xh", [128, NT, D]); xh_t = [Tok() for _ in range(NT)]
    R1 = sb("R1", [128, 16, CH], BF16); R1_t = Tok()
    R2 = sb("R2", [128, 16, CH], BF16); R2_t = Tok()
    R3 = sb("R3", [128, 16, CH], BF16); R3_t = Tok()
    WS = [sb("WS%d" % i, [128, 16, 512], BF16) for i in range(3)]; WS_t = [Tok() for _ in range(3)]
    VS = [sb("VS%d" % i, [128, 4, D], BF16) for i in range(2)]; VS_t = [Tok() for _ in range(2)]
    acc = sb("acc", [128, NT, D]); acc_t = [Tok() for _ in range(NT)]
    sc = sb("sc", [128, NT, 16, 128]); sc_t = [Tok() for _ in range(NT)]
    xnb = sb("xnb", [128, D], BF16); xnb_t = Tok()
    junk, junk_t = xnb, xnb_t
    ident = sb("identb", [128, 128], BF16); ident_t = Tok()
    identf = sb("identf", [128, 128]); identf_t = Tok()
    skb = sb("skb", [128, 16, 128], BF16); skb_t = Tok()
    ss = sb("ss", [128, 1]); ss_t = Tok()
    rstd = sb("rstd", [128, 1]); rstd_t = Tok()
    sg = [sb("sg%d" % i, [128, CH], BF16) for i in range(2)]; sg_t = [Tok() for _ in range(2)]
    mm = [sb("mm%d" % i, [128, CH], BF16) for i in range(2)]; mm_t = [Tok() for _ in range(2)]
    top = sb("top", [128, 1, 16, 16]); top_t = [Tok()] * NT
    tmpk = sb("tmpk", [128, 256]); tmpk_t = Tok()
    cand = sb("cand", [128, 256]); cand_t = Tok()
    best = sb("best", [128, NT, 8, 16]); best_t = [Tok() for _ in range(NT)]
    negmx = sb("negmx", [128, NT, 8]); negmx_t = [Tok() for _ in range(NT)]
    zz = sb("zz", [128, NT, 8]); zz_t = [Tok() for _ in range(NT)]
    nbias = sb("nbias", [128, NT, 8]); nbias_t = [Tok() for _ in range(NT)]
    ebuf = sb("ebuf", [128, 16]); ebuf_t = Tok()
    Ab = [sb("Ab%d" % i, [128, 512], BF16) for i in range(2)]; Ab_t = [Tok() for _ in range(2)]
    Wc = [sb("Wc%d" % i, [128, 512], BF16) for i in range(2)]; Wc_t = [Tok() for _ in range(2)]
    Tb = [sb("Tb%d" % i, [128, 512]) for i in range(3)]; Tb_t = [Tok() for _ in range(3)]
    Eb = [sb("Eb%d" % i, [128, 512]) for i in range(3)]; Eb_t = [Tok() for _ in range(3)]
    stg = [(sb("stg%d" % i, [128, 512]), Tok()) for i in range(2)]
    Wh = [sb("Wh%d" % i, [128, 512], BF16) for i in range(2)]; Wh_t = [Tok() for _ in range(2)]
    Wb = [sb("Wb%d" % i, [128, 512], BF16) for i in range(2)]; Wb_t = [Tok() for _ in range(2)]
    AW = [sb("AW%d" % i, [128, 512], BF16) for i in range(2)]; AW_t = [Tok() for _ in range(2)]
    AWT = [sb("AWT%d" % i, [128, 4, 128], BF16) for i in range(2)]; AWT_t = [Tok() for _ in range(2)]

    S.dma("sp", "c", lambda e: e.dma_start(out=identf[:], in_=ident_d[:, :]), writes=[identf_t])
    S.op("dve", lambda e: e.tensor_copy(out=ident[:], in_=identf[:]), reads=[identf_t], writes=[ident_t])
    S.dma("pool", "w", lambda e: e.dma_start(out=skb[:], in_=skT.rearrange("b d n -> d b n")), writes=[skb_t])

    def load_blk(dst_tile, dst_tok, name, b, scr):
        S.dma("sp", "ws_" + dst_tile.name, lambda e: e.dma_start(out=dst_tile[:].rearrange("p a b -> p (a b)"), in_=scr[b]),
              writes=[dst_tok])

    def norm_transpose(t, g_row, dstT, dstT_t, first):
        src = xh[:, t, :]
        _rms_rstd(S, "act", src, xh_t[t], junk[:], junk_t, ss[:], ss_t, rstd[:], rstd_t, 1e-6)
        S.op("dve", lambda e: e.scalar_tensor_tensor(out=xnb[:], in0=src, scalar=rstd[:, 0:1], in1=gvec[:],
                                                     op0=ALU.mult, op1=ALU.mult),
             reads=[xh_t[t], rstd_t, gvec_t], writes=[xnb_t])
        for half in range(2):
            pb = psb[half]
            for j in range(8):
                kc = half * 8 + j
                S.op("pe", lambda e, kc=kc, j=j, pb=pb: e.transpose(out=pb[:, j * 128:(j + 1) * 128],
                                                                    in_=xnb[:, kc * 128:(kc + 1) * 128], identity=ident[:]),
                     reads=[xnb_t, ident_t], writes=[psb_t[half]])
            S.op("act" if half == 0 else "dve",
                 (lambda e, half=half, pb=pb: e.activation(out=dstT[:, half * 8:(half + 1) * 8, t * 128:(t + 1) * 128],
                                                           in_=pb[:].rearrange("p (k t) -> p k t", k=8), func=AF.Copy))
                 if half == 0 else
                 (lambda e, half=half, pb=pb: e.tensor_copy(out=dstT[:, half * 8:(half + 1) * 8, t * 128:(t + 1) * 128],
                                                            in_=pb[:].rearrange("p (k t) -> p k t", k=8))),
                 reads=[psb_t[half]], writes=[dstT_t])

    def load_y(tt):
        S.dma("pool", "w", lambda e: e.dma_start(out=R2[:, 0:8, :], in_=yaT[:, tt:tt + CH].rearrange("(k p) t -> p k t", p=128)),
              writes=[R2_t])
        S.dma("pool", "w", lambda e: e.dma_start(out=R2[:, 8:16, :], in_=ybT[:, tt:tt + CH].rearrange("(k p) t -> p k t", p=128)),
              writes=[R2_t])

    for c in range(nchunks):
        t0 = c * CH
        for t in range(NT):
            S.dma("sp", "x%d" % t, lambda e, t=t: e.dma_start(out=xh[:, t, :], in_=x[t0 + t * 128:t0 + (t + 1) * 128, :]),
                  writes=[xh_t[t]])
        S.dma("sp", "g", lambda e: e.dma_start(out=gvec[:], in_=gv[0:1, :].partition_broadcast(128)), writes=[gvec_t])
        if c == 0 or stage < 2:
            load_y(t0)
        for t in range(NT):
            norm_transpose(t, 0, R1, R1_t, True)
        for cg in range(4):
            cs = slice(cg * 512, (cg + 1) * 512)
            load_blk(WS[0], WS_t[0], "wg", cg, s_wg)
            load_blk(WS[1], WS_t[1], "wg", 4 + cg, s_wg)
            load_blk(WS[2], WS_t[2], "wab", cg, s_wab)
            for cb in range(4):
                cc = slice(cb * 128, (cb + 1) * 128)
                cidx = cg * 4 + cb
                for kc in range(16):
                    S.op("pe", lambda e, kc=kc, cc=cc: e.matmul(out=ps[0][:, 0:CH], lhsT=WS[0][:, kc, cc], rhs=R1[:, kc, :],
                                                                start=(kc == 0), stop=(kc == 15)),
                         reads=[WS_t[0], R1_t], writes=[ps_t[0]])
                for kc in range(16):
                    S.op("pe", lambda e, kc=kc, cc=cc: e.matmul(out=ps[1][:, 0:CH], lhsT=WS[1][:, kc, cc], rhs=R1[:, kc, :],
                                                                start=(kc == 0), stop=(kc == 15)),
                         reads=[WS_t[1], R1_t], writes=[ps_t[1]])
                for kc in range(8):
                    S.op("pe", lambda e, kc=kc, cc=cc: e.matmul(out=ps[2][:, 0:CH], lhsT=WS[2][:, kc, cc], rhs=R2[:, kc, :],
                                                                start=(kc == 0), stop=(kc == 7)),
                         reads=[WS_t[2], R2_t], writes=[ps_t[2]])
                for kc in range(8):
                    S.op("pe", lambda e, kc=kc, cc=cc: e.matmul(out=ps[3][:, 0:CH], lhsT=WS[2][:, 8 + kc, cc], rhs=R2[:, 8 + kc, :],
                                                                start=(kc == 0), stop=(kc == 7)),
                         reads=[WS_t[2], R2_t], writes=[ps_t[3]])
                for i in range(2):
                    S.op("act", lambda e, i=i: e.activation(out=sg[i][:], in_=ps[i][:, 0:CH], func=AF.Sigmoid),
                         reads=[ps_t[i]], writes=[sg_t[i]])
                for i in range(2):
                    S.op("dve", lambda e, i=i: e.tensor_tensor(out=mm[i][:], in0=sg[i][:], in1=ps[2 + i][:, 0:CH], op=ALU.mult),
                         reads=[sg_t[i], ps_t[2 + i]], writes=[mm_t[i]])
                S.op("pool", lambda e, cidx=cidx: e.tensor_tensor(out=R3[:, cidx, :], in0=mm[0][:], in1=mm[1][:], op=ALU.add),
                     reads=[mm_t[0], mm_t[1]], writes=[R3_t])
        for db in range(4):
            slot = db % 2
            load_blk(WS[slot], WS_t[slot], "wo", db, s_wo)
            for t in range(NT):
                pbk = 4 + (t % 2)
                for kc in range(16):
                    S.op("pe", lambda e, kc=kc, t=t, pbk=pbk, slot=slot: e.matmul(out=ps[pbk][:, :], lhsT=R3[:, kc, t * 128:(t + 1) * 128],
                                                                                 rhs=WS[slot][:, kc, :], start=(kc == 0), stop=(kc == 15)),
                         reads=[R3_t, WS_t[slot]], writes=[ps_t[pbk]])
                S.op("dve", lambda e, t=t, db=db, pbk=pbk: e.tensor_tensor(out=xh[:, t, db * 512:(db + 1) * 512],
                                                                         in0=xh[:, t, db * 512:(db + 1) * 512], in1=ps[pbk][:, :], op=ALU.add),
                     reads=[ps_t[pbk], xh_t[t]], writes=[xh_t[t]])
        if stage >= 2:
            S.dma("sp", "g", lambda e: e.dma_start(out=gvec[:], in_=gv[1:2, :].partition_broadcast(128)), writes=[gvec_t])
            for t in range(NT):
                norm_transpose(t, 1, R1, R1_t, False)
            for qg in range(4):
                slot = qg % 2
                load_blk(WS[slot], WS_t[slot], "wq", qg, s_wq)
                for qb in range(4):
                    blk = qg * 4 + qb
                    pbk = blk % 2
                    for kc in range(16):
                        S.op("pe", lambda e, kc=kc, qb=qb, pbk=pbk, slot=slot: e.matmul(out=ps[pbk][:, 0:CH], lhsT=WS[slot][:, kc, qb * 128:(qb + 1) * 128],
                                                                                       rhs=R1[:, kc, :], start=(kc == 0), stop=(kc == 15)),
                             reads=[WS_t[slot], R1_t], writes=[ps_t[pbk]])
                    S.op("act", lambda e, blk=blk, pbk=pbk: e.activation(out=R2[:, blk, :], in_=ps[pbk][:, 0:CH], func=AF.Copy),
                         reads=[ps_t[pbk]], writes=[R2_t])
            for t in range(NT):
                for g4 in range(4):
                    pbk = 2 + (g4 % 2)
                    for j in range(4):
                        blk = g4 * 4 + j
                        S.op("pe", lambda e, blk=blk, j=j, pbk=pbk, t=t: e.matmul(out=ps[pbk][:, j * 128:(j + 1) * 128],
                                                                                 lhsT=R2[:, blk, t * 128:(t + 1) * 128], rhs=skb[:, blk, :],
                                                                                 start=True, stop=True),
                             reads=[R2_t, skb_t], writes=[ps_t[pbk]])
                    S.op("act", lambda e, g4=g4, pbk=pbk, t=t: e.activation(out=sc[:, t, g4 * 4:(g4 + 1) * 4, :],
                                                                           in_=ps[pbk][:].rearrange("p (b n) -> p b n", b=4), func=AF.Copy),
                         reads=[ps_t[pbk]], writes=[sc_t[t]])
                for blk in range(16):
                    S.op("dve", lambda e, blk=blk, t=t: e.max(out=top[:, 0, blk, 0:8], in_=sc[:, t, blk, :]),
                         reads=[sc_t[t]], writes=[top_t[t]])
                    S.op("dve", lambda e, blk=blk, t=t: e.match_replace(out=tmpk[:, 0:128], in_to_replace=top[:, 0, blk, 0:8],
                                                                        in_values=sc[:, t, blk, :], imm_value=NEG),
                         reads=[sc_t[t], top_t[t]], writes=[tmpk_t])
                    S.op("dve", lambda e, blk=blk, t=t: e.max(out=top[:, 0, blk, 8:16], in_=tmpk[:, 0:128]),
                         reads=[tmpk_t], writes=[top_t[t]])
                for h in range(8):
                    S.op("dve", lambda e, h=h, t=t: e.tensor_tensor(
                        out=cand[:].rearrange("p (a b) -> p a b", a=16),
                        in0=top[:, 0, 2 * h, :].unsqueeze(2).broadcast_to([128, 16, 16]),
                        in1=top[:, 0, 2 * h + 1, :].unsqueeze(1).broadcast_to([128, 16, 16]), op=ALU.add),
                         reads=[top_t[t]], writes=[cand_t])
                    S.op("dve", lambda e, h=h, t=t: e.max(out=best[:, t, h, 0:8], in_=cand[:]),
                         reads=[cand_t], writes=[best_t[t]])
                    S.op("dve", lambda e, h=h, t=t: e.match_replace(out=tmpk[:], in_to_replace=best[:, t, h, 0:8],
                                                                    in_values=cand[:], imm_value=NEG),
                         reads=[cand_t, best_t[t]], writes=[tmpk_t])
                    S.op("dve", lambda e, h=h, t=t: e.max(out=best[:, t, h, 8:16], in_=tmpk[:]),
                         reads=[tmpk_t], writes=[best_t[t]])
                S.op("dve", lambda e, t=t: e.tensor_scalar(out=negmx[:, t, :], in0=best[:, t, :, 0], scalar1=-1.0, scalar2=None, op0=ALU.mult),
                     reads=[best_t[t]], writes=[negmx_t[t]])
                for h in range(8):
                    S.op("act", lambda e, h=h, t=t: e.activation(out=ebuf[:], in_=best[:, t, h, :], func=AF.Exp,
                                                                 bias=negmx[:, t, h:h + 1], accum_out=zz[:, t, h:h + 1]),
                         reads=[best_t[t], negmx_t[t]], writes=[ebuf_t, zz_t[t]])
                S.op("act", lambda e, t=t: e.activation(out=zz[:, t, :], in_=zz[:, t, :], func=AF.Ln),
                     reads=[zz_t[t]], writes=[zz_t[t]])
                S.op("dve", lambda e, t=t: e.tensor_tensor(out=nbias[:, t, :], in0=negmx[:, t, :], in1=zz[:, t, :], op=ALU.subtract),
                     reads=[negmx_t[t], zz_t[t]], writes=[nbias_t[t]])
            if c + 1 < nchunks:
                load_y(t0 + CH)
            its = [(eb, t) for eb in range(peer_blocks) for t in range(NT)]
            nit = len(its)

            def st1(k):
                eb, t = its[k]
                slot = eb % 2
                if t == 0:
                    load_blk(WS[slot], WS_t[slot], "u", eb, s_u)
                    load_blk(VS[slot], VS_t[slot], "v", eb, s_v)
                pa = k % 2
                for kc in range(16):
                    S.op("pe", lambda e, kc=kc, t=t, slot=slot, pa=pa: e.matmul(out=ps[pa][:, :], lhsT=R1[:, kc, t * 128:(t + 1) * 128],
                                                                               rhs=WS[slot][:, kc, :], start=(kc == 0), stop=(kc == 15)),
                         reads=[R1_t, WS_t[slot]], writes=[ps_t[pa]])

            def st1g(k):
                pa = k % 2
                S.op("act", lambda e, pa=pa: e.activation(out=Ab[pa][:], in_=ps[pa][:, :], func=AF.Gelu),
                     reads=[ps_t[pa]], writes=[Ab_t[pa]])

            def st2(k):
                eb, t = its[k]
                pa = k % 2

                def emit_T(h):
                    i = h % 3
                    S.op("dve", lambda e, h=h, i=i: e.tensor_tensor(
                        out=Tb[i][:].rearrange("p (a b) -> p a b", a=4),
                        in0=sc[:, t, 2 * h, eb * 4:(eb + 1) * 4].unsqueeze(2).broadcast_to([128, 4, 128]),
                        in1=sc[:, t, 2 * h + 1, :].unsqueeze(1).broadcast_to([128, 4, 128]), op=ALU.add),
                         reads=[sc_t[t]], writes=[Tb_t[i]])
                    S.op("act", lambda e, h=h, i=i: e.activation(out=Eb[i][:], in_=Tb[i][:], func=AF.Exp, bias=nbias[:, t, h:h + 1]),
                         reads=[Tb_t[i], nbias_t[t]], writes=[Eb_t[i]])
                emit_T(0)
                emit_T(1)
                for h in range(8):
                    i = h % 3
                    if h + 2 < 8:
                        emit_T(h + 2)
                    accb, accb_t = (Wb[pa], Wb_t[pa]) if h % 2 == 0 else (Wc[pa], Wc_t[pa])
                    dst, dst_t = (accb, accb_t) if h < 2 else (Wh[h % 2], Wh_t[h % 2])
                    S.op("dve", lambda e, h=h, i=i, dst=dst: e.scalar_tensor_tensor(
                        out=dst[:], in0=Tb[i][:], scalar=best[:, t, h, 15:16], in1=Eb[i][:], op0=ALU.is_ge, op1=ALU.mult),
                         reads=[Tb_t[i], Eb_t[i], best_t[t]], writes=[dst_t])
                    if h >= 2:
                        S.op("pool" if h % 2 == 0 else "dve", lambda e, h=h, accb=accb: e.tensor_tensor(out=accb[:], in0=accb[:], in1=Wh[h % 2][:], op=ALU.add),
                             reads=[Wh_t[h % 2], accb_t], writes=[accb_t])
                S.op("dve", lambda e, pa=pa: e.tensor_tensor(out=Wb[pa][:], in0=Wb[pa][:], in1=Wc[pa][:], op=ALU.add),
                     reads=[Wb_t[pa], Wc_t[pa]], writes=[Wb_t[pa]])
                S.op("dve", lambda e, pa=pa: e.tensor_tensor(out=AW[pa][:], in0=Ab[pa][:], in1=Wb[pa][:], op=ALU.mult),
                     reads=[Ab_t[pa], Wb_t[pa]], writes=[AW_t[pa]])

            def st3a(k):
                pa = k % 2
                for es in range(4):
                    S.op("pe", lambda e, es=es, pa=pa: e.transpose(out=psb[pa][:, es * 128:(es + 1) * 128], in_=AW[pa][:, es * 128:(es + 1) * 128],
                                                                   identity=ident[:]),
                         reads=[AW_t[pa], ident_t], writes=[psb_t[pa]])
                S.op("act", lambda e, pa=pa: e.activation(out=AWT[pa][:], in_=psb[pa][:, 0:512].rearrange("p (s t) -> p s t", s=4), func=AF.Copy),
                     reads=[psb_t[pa]], writes=[AWT_t[pa]])

            def st3b(k):
                eb, t = its[k]
                slot = eb % 2
                pa = k % 2
                for db in range(4):
                    pbk = 2 + db
                    for es in range(4):
                        S.op("pe", lambda e, es=es, db=db, pbk=pbk, slot=slot, pa=pa: e.matmul(
                            out=ps[pbk][:, :], lhsT=AWT[pa][:, es, :], rhs=VS[slot][:, es, db * 512:(db + 1) * 512],
                            start=(es == 0), stop=(es == 3)),
                             reads=[AWT_t[pa], VS_t[slot]], writes=[ps_t[pbk]])

            def st3c(k):
                eb, t = its[k]
                for db in range(4):
                    pbk = 2 + db
                    if eb == 0:
                        S.op("act", lambda e, db=db, t=t, pbk=pbk: e.activation(out=acc[:, t, db * 512:(db + 1) * 512], in_=ps[pbk][:, :], func=AF.Copy),
                             reads=[ps_t[pbk]], writes=[acc_t[t]])
                    elif db % 2 == 0:
                        S.op("dve", lambda e, db=db, t=t, pbk=pbk: e.tensor_tensor(out=acc[:, t, db * 512:(db + 1) * 512],
                                                                                 in0=acc[:, t, db * 512:(db + 1) * 512], in1=ps[pbk][:, :], op=ALU.add),
                             reads=[ps_t[pbk], acc_t[t]], writes=[acc_t[t]])
                    else:
                        sg_, sg_t = stg[db // 2]
                        S.op("act", lambda e, pbk=pbk, sg_=sg_: e.activation(out=sg_[:], in_=ps[pbk][:, :], func=AF.Copy),
                             reads=[ps_t[pbk]], writes=[sg_t])
                        S.op("pool", lambda e, db=db, t=t, sg_=sg_: e.tensor_tensor(out=acc[:, t, db * 512:(db + 1) * 512],
                                                                                   in0=acc[:, t, db * 512:(db + 1) * 512], in1=sg_[:], op=ALU.add),
                             reads=[sg_t, acc_t[t]], writes=[acc_t[t]])

            for k in range(nit + 2):
                if 0 <= k - 2 < nit:
                    st3a(k - 2)
                if k < nit:
                    st1(k)
                if 0 <= k - 2 < nit:
                    st3b(k - 2)
                if 0 <= k - 1 < nit:
                    st2(k - 1)
                if k < nit:
                    st1g(k)
                if 0 <= k - 2 < nit:
                    st3c(k - 2)
            for t in range(NT):
                S.op("pool", lambda e, t=t: e.tensor_tensor(out=xh[:, t, :], in0=xh[:, t, :], in1=acc[:, t, :], op=ALU.add),
                     reads=[acc_t[t], xh_t[t]], writes=[xh_t[t]])
        if stage >= 3:
            S.dma("sp", "g", lambda e: e.dma_start(out=gvec[:], in_=gv[2:3, :].partition_broadcast(128)), writes=[gvec_t])
        for t in range(NT):
            if stage >= 3:
                _rms_rstd(S, "act", xh[:, t, :], xh_t[t], junk[:], junk_t, ss[:], ss_t, rstd[:], rstd_t, 1e-6)
                S.op("dve", lambda e, t=t: e.scalar_tensor_tensor(out=acc[:, t, :], in0=xh[:, t, :], scalar=rstd[:, 0:1], in1=gvec[:],
                                                                 op0=ALU.mult, op1=ALU.mult),
                     reads=[xh_t[t], rstd_t, gvec_t], writes=[acc_t[t]])
            else:
                S.op("dve", lambda e, t=t: e.tensor_copy(out=acc[:, t, :], in_=xh[:, t, :]), reads=[xh_t[t]], writes=[acc_t[t]])
            S.dma("sp", "o%d" % t, lambda e, t=t: e.dma_start(out=y[t0 + t * 128:t0 + (t + 1) * 128, :], in_=acc[:, t, :]),
                  reads=[acc_t[t]], writes=[])
    for k in S.dsem:
        if k.startswith("dma:o"):
            nc.sync.wait_ge(S.dsem[k][0], S.dsem[k][1])
    return nc, S


G1 = 256
NC1 = 1216
C_R, C_K, C_V, C_XW, C_XA, C_XG, C_QD, C_KD, C_VD = 0, 128, 256, 384, 480, 576, 832, 960, 1088
CHK = 32
NCAST = 11


def _roundrobin(gens):
    gens = list(gens)
    while gens:
        for g_ in list(gens):
            try:
                next(g_)
            except StopIteration:
                gens.remove(g_)


def build_l1(ngroups=SEQ // G1, do_rwkv=True, do_attn=True, rw_stage=9999):
    nc = bass.Bass("TRN2", target_bir_lowering=False)
    ntok = ngroups * G1
    ntile = ntok // 128
    dr = lambda n, s, k="ExternalInput": nc.dram_tensor(n, s, F32, kind=k).ap()
    x = dr("x", [ntok, D])
    w1 = dr("w1", [D, NC1])
    cvec = dr("cvec", [128, 20])
    wdu_d = dr("wdu", [96, 128]); wiu_d = dr("wiu", [96, 128]); wgu_d = dr("wgu", [256, 128])
    lamv = dr("lamv", [1, 256]); sublng = dr("sublng", [1, 128]); g1d = dr("g1", [1, D])
    consts = dr("consts", [7, 128, 128])
    rmask_d = dr("rmask", [1, G1])
    yaT = dr("yaT", [128, ntok], "ExternalOutput")
    yb = dr("yb", [ntok, 128], "ExternalOutput")
    castin = dr("castin", [NCAST, 128, 8192])
    castout = nc.dram_tensor("castout", [NCAST, 128, 8192], BF16, kind="ExternalOutput").ap()

    S = Sync(nc)
    sb = lambda n, s, dt=F32: nc.alloc_sbuf_tensor(n, s, dt)
    ps = [nc.alloc_psum_tensor("ps%d" % i, [128, 512], F32) for i in range(7)]
    psb = [nc.alloc_psum_tensor("psb%d" % i, [128, 1024], BF16) for i in range(1)]
    ps_t = [Tok() for _ in range(7)]
    psb_t = [Tok() for _ in range(1)]
    OB = [4, 6]
    scr_i = [0]

    def scr():
        scr_i[0] ^= 1
        return ps[scr_i[0]], ps_t[scr_i[0]]

    def T_(name, shape, dt=F32):
        return sb(name, shape, dt), Tok()

    W1, W1_t = T_("W1", [128, 16, NC1], BF16)
    KT, KT_t = T_("KT", [128, ntok], BF16)
    VA, VA_t = T_("VA", [128, ntile, 130], BF16)
    g1c, g1c_t = T_("g1c", [128, 16])
    xt = [T_("xt%d" % i, [128, D]) for i in range(2)]
    xnb, xnb_t = T_("xnb", [128, D], BF16)
    junk, junk_t = xnb, xnb_t
    junk2, junk2_t = T_("junk2", [128, 128], BF16)
    xnT, xnT_t = T_("xnT", [128, 16, G1], BF16)
    cst, cst_t = T_("cst", [128, 7, 128])
    identb, identb_t = T_("identb", [128, 128], BF16)
    trib, trib_t = T_("trib", [128, 128], BF16)
    rmask, rmask_t = T_("rmask_s", [128, G1])
    cv, cv_t = T_("cv_s", [128, 20])
    omm, omm_t = T_("omm", [128, 8])
    wdu, wdu_t = T_("wdus", [96, 128]); wiu, wiu_t = T_("wius", [96, 128]); wgu, wgu_t = T_("wgus", [128, 2, 128])
    lacc, lacc_t = T_("lacc", [128, 4])
    neglam, neglam_t = T_("neglam", [128, 1])
    sgv, sgv_t = T_("sgv", [128, 128])
    ss, ss_t = T_("ss", [128, 1]); rstd, rstd_t = T_("rstd", [128, 1])
    ss2, ss2_t = T_("ss2", [128, 1]); rstd2, rstd2_t = T_("rstd2", [128, 1])
    ident = cst[:, 0, :]; MU = cst[:, 1, :]; ML = cst[:, 2, :]; MUI = cst[:, 3, :]; onesblk = cst[:, 5, :]; I64 = cst[:, 6, 0:64]
    PB = [T_("PB%d" % i, [128, G1 + 1]) for i in range(7)]
    SH = [T_("SH%d" % i, [128, G1]) for i in range(7)]
    tmpA, tmpA_t = T_("tmpA", [128, G1]); tmpB, tmpB_t = T_("tmpB", [128, G1])
    logw, logw_t = T_("logw", [128, G1]); av, av_t = T_("av", [128, G1]); gg, gg_t = T_("gg", [128, G1])
    kkn, kkn_t = T_("kkn", [128, G1]); k2, k2_t = T_("k2", [128, G1]); bonus, bonus_t = T_("bonus", [128, G1])
    cum, cum_t = T_("cum", [128, G1]); Pm, Pm_t = T_("Pm", [128, G1]); Pinv, Pinv_t = T_("Pinv", [128, G1]); Pprev, Pprev_t = T_("Pprev", [128, G1])
    At, At_t = T_("At", [128, G1], BF16); Bt, Bt_t = T_("Bt", [128, G1], BF16); Kt, Kt_t = T_("Kt", [128, G1], BF16); Rt, Rt_t = T_("Rt", [128, G1], BF16)
    vb16, vb16_t = T_("vb16", [128, G1], BF16)
    yT, yT_t = T_("yT", [128, G1]); yo, yo_t = T_("yo", [128, G1])
    TOK, TOK_t = T_("TOK", [128, 4, 128], BF16)
    lam_s = yT[:, 0:256].rearrange("p (a c) -> p a c", c=64); lam_t = yT_t
    lamp = yo[:, 0:128].rearrange("p (b c) -> p b c", c=64); lamp_t = yo_t
    HB = []
    for h in range(2):
        HB.append(dict(
            XT=[T_("XT%d_%d" % (h, i), [128, 128], BF16) for i in range(5)], XX=[T_("XX%d_%d" % (h, i), [128, 128], BF16) for i in range(4)],
            LakT=T_("LakT%d" % h, [128, 128], BF16), MrbT=T_("MrbT%d" % h, [128, 128], BF16), MrkT=T_("MrkT%d" % h, [128, 128], BF16),
            Z=[T_("Z%d_%d" % (h, i), [128, 128], BF16) for i in range(2)], ZF=T_("ZF%d" % h, [128, 128], BF16),
            MZ=T_("MZ%d" % h, [128, 4, 64], BF16), MB=T_("MB%d" % h, [128, 4, 64], BF16), MK=T_("MK%d" % h, [128, 4, 64], BF16)))
    RhT, RhT_t = T_("RhT", [128, 128]); YhT, YhT_t = T_("YhT", [128, 128])
    MT, MT_t = T_("MT", [128, 4, 64]); HP, HP_t = T_("HP", [128, 4, 64])
    Sst = [T_("Sst%d" % i, [128, 64]) for i in range(2)]
    QT, QT_t = T_("QT", [128, G1], BF16); QSQ, QSQ_t = T_("QSQ", [128, G1]); KSQ, KSQ_t = T_("KSQ", [128, G1])
    kmax2 = [T_("kmax2_%d" % i, [128, 1]) for i in range(2)]
    kred, kred_t = T_("kred", [128, 1]); sqq, sqq_t = T_("sqq", [128, 1])
    nshift = [T_("nshift%d" % i, [128, 1]) for i in range(2)]
    Pb = [T_("Pb%d" % i, [128, 512], BF16) for i in range(2)]
    om = [[T_("om%d_%d" % (i, j), [128, 128]) for j in range(2)] for i in range(2)]
    qmax2 = [T_("qmax2_%d" % i, [128, 1]) for i in range(2)]
    rs_, rs_t = T_("rs_", [128, 1]); attn, attn_t = T_("attn", [128, 128]); ybo, ybo_t = T_("ybo", [128, 128])
    ones128, ones_t = T_("ones128", [128, 128])

    def mm(out, lhsT, rhs, start=True, stop=True):
        return lambda e: e.matmul(out=out, lhsT=lhsT, rhs=rhs, start=start, stop=stop)

    cpy_i = [0]

    def copy_out(out, in_, reads, writes, eng=None):
        if eng is None:
            cpy_i[0] ^= 1
            eng = "act" if cpy_i[0] else "dve"
        if eng == "act":
            S.op("act", lambda e: e.activation(out=out, in_=in_, func=AF.Copy), reads=reads, writes=writes)
        else:
            S.op("dve", lambda e: e.tensor_copy(out=out, in_=in_), reads=reads, writes=writes)

    def dve_tt(out, in0, in1, op, reads, writes, eng="dve"):
        S.op(eng, lambda e: e.tensor_tensor(out=out, in0=in0, in1=in1, op=op), reads=reads, writes=writes)

    def dve_ts(out, in0, s1, s2, op0, op1, reads, writes, eng="dve"):
        if op1 is None:
            S.op(eng, lambda e: e.tensor_scalar(out=out, in0=in0, scalar1=s1, scalar2=None, op0=op0), reads=reads, writes=writes)
        else:
            S.op(eng, lambda e: e.tensor_scalar(out=out, in0=in0, scalar1=s1, scalar2=s2, op0=op0, op1=op1), reads=reads, writes=writes)

    def dve_stt(out, in0, scalar, in1, op0, op1, reads, writes):
        S.op("dve", lambda e: e.scalar_tensor_tensor(out=out, in0=in0, scalar=scalar, in1=in1, op0=op0, op1=op1), reads=reads, writes=writes)

    def act(out, in_, func, reads, writes, **kw):
        S.op("act", lambda e: e.activation(out=out, in_=in_, func=func, **kw), reads=reads, writes=writes)

    S.dma("sp", "c", lambda e: e.dma_start(out=cst[:], in_=consts.rearrange("c p n -> p c n")), writes=[cst_t])
    S.dma("sp", "c", lambda e: e.dma_start(out=cv[:], in_=cvec[:, :]), writes=[cv_t])
    S.dma("sp", "c", lambda e: e.dma_start(out=wdu[:], in_=wdu_d[:, :]), writes=[wdu_t])
    S.dma("sp", "c", lambda e: e.dma_start(out=wiu[:], in_=wiu_d[:, :]), writes=[wiu_t])
    S.dma("sp", "c", lambda e: e.dma_start(out=wgu[:], in_=wgu_d.rearrange("(k p) c -> p k c", p=128)), writes=[wgu_t])
    S.dma("sp", "c", lambda e: e.dma_start(out=yT[:, 0:256], in_=lamv[0:1, :].partition_broadcast(128)), writes=[lam_t])
    S.dma("sp", "c", lambda e: e.dma_start(out=sgv[:], in_=sublng[0:1, :].partition_broadcast(128)), writes=[sgv_t])
    S.dma("sp", "c", lambda e: e.dma_start(out=rmask[:], in_=rmask_d[0:1, :].partition_broadcast(128)), writes=[rmask_t])
    with nc.allow_non_contiguous_dma(reason="tiny gain vector"):
        S.dma("sp", "c", lambda e: e.dma_start(out=g1c[:], in_=g1d.rearrange("o (k p) -> p (o k)", p=128)), writes=[g1c_t])
    S.dma("pool", "w", lambda e: e.dma_start(out=W1[:], in_=w1.rearrange("(k p) c -> p k c", p=128)), writes=[W1_t])
    for b in range(NCAST):
        S.dma("pool", "ocast", lambda e, b=b: e.dma_start(out=castout[b].rearrange("p (s e) -> p s e", e=2048),
                                                      in_=castin[b].rearrange("p (s e) -> p s e", e=2048)))
    for kc in range(16):
        S.op("dve" if kc % 2 else "pool", lambda e, kc=kc: e.tensor_scalar(out=W1[:, kc, :], in0=W1[:, kc, :], scalar1=g1c[:, kc:kc + 1], scalar2=0.0,
                                                                            op0=ALU.mult, op1=ALU.add),
             reads=[W1_t, g1c_t], writes=[W1_t])
    S.op("dve", lambda e: e.tensor_copy(out=identb[:], in_=cst[:, 0, :]), reads=[cst_t], writes=[identb_t])
    S.op("dve", lambda e: e.tensor_copy(out=trib[:], in_=cst[:, 4, :]), reads=[cst_t], writes=[trib_t])
    dve_ts(omm[:, 0:8], cv[:, 0:8], -1.0, 1.0, ALU.mult, ALU.add, [cv_t], [omm_t])
    dve_ts(cv[:, 14:15], cv[:, 10:11], -1.0, 1.0, ALU.mult, ALU.add, [cv_t], [cv_t])
    dve_ts(sgv[:], sgv[:], 0.8, None, ALU.mult, None, [sgv_t], [sgv_t])
    dve_tt(lamp[:, 0, :], lam_s[:, 0, :], lam_s[:, 1, :], ALU.mult, [lam_t], [lamp_t])
    dve_tt(lamp[:, 1, :], lam_s[:, 2, :], lam_s[:, 3, :], ALU.mult, [lam_t], [lamp_t])
    S.op("dve", lambda e: e.tensor_reduce(out=lacc[:, 0:2], in_=lamp[:, 0:2, :], axis=AX.X, op=ALU.add), reads=[lamp_t], writes=[lacc_t])
    act(lacc[:, 2:4], lacc[:, 0:2], AF.Exp, [lacc_t], [lacc_t])
    dve_tt(neglam[:], lacc[:, 3:4], lacc[:, 2:3], ALU.subtract, [lacc_t], [neglam_t])
    dve_ts(neglam[:], neglam[:], -0.2, None, ALU.add, None, [neglam_t], [neglam_t])
    for i in range(7):
        S.op("pool", lambda e, i=i: e.memset(PB[i][0][:, 0:1], 0.0), writes=[PB[i][1]])
    for i in range(2):
        S.op("pool", lambda e, i=i: e.memset(Sst[i][0][:], 0.0), writes=[Sst[i][1]])
        S.op("pool", lambda e, i=i: e.memset(kmax2[i][0][:], 0.0), writes=[kmax2[i][1]])
        S.op("pool", lambda e, i=i: e.memset(qmax2[i][0][:], 0.0), writes=[qmax2[i][1]])
    S.op("pool", lambda e: e.memset(VA[:, :, 128:130], 1.0), writes=[VA_t])
    S.op("pool", lambda e: e.memset(ones128[:], 1.0), writes=[ones_t])

    scur = [0]

    def head_chain(h, cs):
        hb = HB[h]
        XT, XX, Z = hb["XT"], hb["XX"], hb["Z"]
        LakT, LakT_t = hb["LakT"]; MrbT, MrbT_t = hb["MrbT"]; MrkT, MrkT_t = hb["MrkT"]
        zf, zf_t = hb["ZF"]
        MZ, MZ_t = hb["MZ"]; MB, MB_t = hb["MB"]; MK, MK_t = hb["MK"]
        pb, pb_t = ps[h], ps_t[h]
        hs = slice(64 * h, 64 * h + 64)
        Ah, Bh, Kh, Rh = At[hs, cs], Bt[hs, cs], Kt[hs, cs], Rt[hs, cs]
        S.op("pe", mm(pb[:, 0:128], Bh, Ah), reads=[Bt_t, At_t], writes=[pb_t])
        dve_tt(XT[0][0][:], pb[:, 0:128], MU, ALU.mult, [pb_t, cst_t], [XT[0][1]])
        yield
        S.op("pe", mm(pb[:, 0:128], Ah, Bh), reads=[Bt_t, At_t], writes=[pb_t])
        dve_tt(XX[0][0][:], pb[:, 0:128], ML, ALU.mult, [pb_t, cst_t], [XX[0][1]])
        yield
        S.op("pe", mm(pb[:, 0:128], Kh, Ah), reads=[Kt_t, At_t], writes=[pb_t])
        dve_tt(LakT[:], pb[:, 0:128], MU, ALU.mult, [pb_t, cst_t], [LakT_t])
        yield
        S.op("pe", mm(pb[:, 0:128], Bh, Rh), reads=[Bt_t, Rt_t], writes=[pb_t])
        dve_tt(MrbT[:], pb[:, 0:128], MUI, ALU.mult, [pb_t, cst_t], [MrbT_t])
        yield
        S.op("pe", mm(pb[:, 0:128], Kh, Rh), reads=[Kt_t, Rt_t], writes=[pb_t])
        dve_tt(MrkT[:], pb[:, 0:128], MUI, ALU.mult, [pb_t, cst_t], [MrkT_t])
        yield
        S.op("pe", mm(pb[:, 0:64], LakT[:], TOK[:, 3, hs]), reads=[LakT_t, TOK_t], writes=[pb_t])
        zc, zc_t = Z[0]
        copy_out(zc[:, 64:128], pb[:, 0:64], [pb_t], [zc_t], eng="dve")
        S.op("pool", lambda e: e.tensor_copy(out=zc[:, 0:64], in_=TOK[:, 0, hs]), reads=[TOK_t], writes=[zc_t])
        yield
        for i in range(4):
            S.op("pe", mm(pb[:, 0:128], XX[i][0][:], XT[i][0][:]), reads=[XX[i][1], XT[i][1]], writes=[pb_t])
            copy_out(XT[i + 1][0][:], pb[:, 0:128], [pb_t], [XT[i + 1][1]], eng="dve")
            yield
            if i < 3:
                S.op("pe", mm(pb[:, 0:128], XT[i][0][:], XX[i][0][:]), reads=[XX[i][1], XT[i][1]], writes=[pb_t])
                copy_out(XX[i + 1][0][:], pb[:, 0:128], [pb_t], [XX[i + 1][1]], eng="act")
                yield
            zc, zc_t = Z[i % 2]
            zn, zn_t = Z[(i + 1) % 2]
            S.op("pe", mm(pb[:, 0:128], XT[i][0][:], zc[:]), reads=[XT[i][1], zc_t], writes=[pb_t])
            dve_tt(zn[:], pb[:, 0:128], zc[:], ALU.add, [pb_t, zc_t], [zn_t])
            yield
        zc, zc_t = Z[0]
        S.op("pe", mm(pb[:, 0:128], XT[4][0][:], zc[:]), reads=[XT[4][1], zc_t], writes=[pb_t])
        dve_tt(zf[:], pb[:, 0:128], zc[:], ALU.add, [pb_t, zc_t], [zf_t])
        yield
        S.op("pe", mm(pb[hs, 0:128], zf[:, 0:64], MrbT[:]), reads=[zf_t, MrbT_t], writes=[pb_t])
        dve_tt(RhT[hs, :], pb[hs, 0:128], Rh, ALU.add, [pb_t, Rt_t], [RhT_t])
        yield
        S.op("pe", mm(pb[hs, 0:128], zf[:, 64:128], MrbT[:], True, False), reads=[zf_t, MrbT_t], writes=[pb_t])
        S.op("pe", mm(pb[hs, 0:128], TOK[:, 3, hs], MrkT[:], False, True), reads=[TOK_t, MrkT_t], writes=[pb_t])
        copy_out(YhT[hs, :], pb[hs, 0:128], [pb_t], [YhT_t], eng="act")
        cmb = cv[:, 16:20].unsqueeze(2).broadcast_to([128, 4, 64])
        dve_tt(MZ[:], zf[:, 0:64].unsqueeze(1).broadcast_to([128, 4, 64]), cmb, ALU.mult, [zf_t, cv_t], [MZ_t])
        dve_tt(MB[:], TOK[:, 1, hs].unsqueeze(1).broadcast_to([128, 4, 64]), cmb, ALU.mult, [TOK_t, cv_t], [MB_t], eng="pool")
        dve_tt(MK[:], TOK[:, 2, hs].unsqueeze(1).broadcast_to([128, 4, 64]), cmb, ALU.mult, [TOK_t, cv_t], [MK_t], eng="pool")
        yield
        for c in range(4):
            S.op("pe", mm(ps[2][hs, c * 64:(c + 1) * 64], MZ[:, c, :], TOK[:, 1, hs]), reads=[MZ_t, TOK_t], writes=[ps_t[2]])
        for c in range(4):
            S.op("pe", mm(ps[3][hs, c * 64:(c + 1) * 64], MB[:, c, :], zf[:, 64:128], True, False), reads=[zf_t, MB_t], writes=[ps_t[3]])
            S.op("pe", mm(ps[3][hs, c * 64:(c + 1) * 64], MK[:, c, :], TOK[:, 3, hs], False, True), reads=[TOK_t, MK_t], writes=[ps_t[3]])
        yield

    def rwkv_group(g):
        t0 = g * G1
        r_, k_, v_, xw_, xa_, xg0_, xg1_ = range(7)
        rows = [128, 128, 128, 96, 96, 128, 128]
        for b in range(7):
            n = rows[b]
            pbuf, pbt = PB[b]
            sh, sht = SH[b]
            dve_ts(tmpA[0:n, :], pbuf[0:n, 0:G1], cv[0:n, b:b + 1], None, ALU.mult, None, [pbt, cv_t], [tmpA_t])
            dve_stt(sh[0:n, :], pbuf[0:n, 1:G1 + 1], omm[0:n, b:b + 1], tmpA[0:n, :], ALU.mult, ALU.add, [pbt, omm_t, tmpA_t], [sht])
            S.op("pool", lambda e, pbuf=pbuf, n=n: e.tensor_copy(out=pbuf[0:n, 0:1], in_=pbuf[0:n, G1:G1 + 1]), reads=[pbt], writes=[pbt])
            if b % 2:
                yield
        shr, shr_t = SH[r_]; shk, shk_t = SH[k_]; shv, shv_t = SH[v_]
        act(tmpB[0:96, :], SH[xw_][0][0:96, :], AF.Tanh, [SH[xw_][1]], [tmpB_t])
        pb, pb_t = scr()
        S.op("pe", mm(pb[:, 0:G1], wdu[:, :], tmpB[0:96, :]), reads=[wdu_t, tmpB_t], writes=[pb_t])
        act(logw[:], pb[:, 0:G1], AF.Sigmoid, [pb_t, cv_t], [logw_t], bias=cv[:, 7:8])
        dve_ts(logw[:], logw[:], -0.6065306597126334, None, ALU.mult, None, [logw_t], [logw_t])
        pb, pb_t = scr()
        S.op("pe", mm(pb[:, 0:G1], wiu[:, :], SH[xa_][0][0:96, :]), reads=[wiu_t, SH[xa_][1]], writes=[pb_t])
        act(av[:], pb[:, 0:G1], AF.Sigmoid, [pb_t, cv_t], [av_t], bias=cv[:, 8:9])
        act(SH[xg0_][0][:], SH[xg0_][0][:], AF.Sigmoid, [SH[xg0_][1]], [SH[xg0_][1]])
        act(SH[xg1_][0][:], SH[xg1_][0][:], AF.Sigmoid, [SH[xg1_][1]], [SH[xg1_][1]])
        yield
        pb, pb_t = scr()
        S.op("pe", mm(pb[:, 0:G1], wgu[:, 0, :], SH[xg0_][0][:], True, False), reads=[wgu_t, SH[xg0_][1]], writes=[pb_t])
        S.op("pe", mm(pb[:, 0:G1], wgu[:, 1, :], SH[xg1_][0][:], False, True), reads=[wgu_t, SH[xg1_][1]], writes=[pb_t])
        copy_out(gg[:], pb[:, 0:G1], [pb_t], [gg_t], eng="dve")
        dve_ts(kkn[:], shk[:], cv[:, 9:10], None, ALU.mult, None, [shk_t, cv_t], [kkn_t])
        dve_tt(tmpA[:], kkn[:], kkn[:], ALU.mult, [kkn_t], [tmpA_t], eng="pool")
        pb, pb_t = scr()
        S.op("pe", mm(pb[:, 0:G1], onesblk, tmpA[:]), reads=[cst_t, tmpA_t], writes=[pb_t])
        act(tmpB[:], pb[:, 0:G1], AF.Sqrt, [pb_t], [tmpB_t])
        dve_ts(tmpB[:], tmpB[:], 1e-12, None, ALU.max, None, [tmpB_t], [tmpB_t])
        S.op("dve", lambda e: e.reciprocal(out=tmpB[:], in_=tmpB[:]), reads=[tmpB_t], writes=[tmpB_t])
        dve_tt(kkn[:], kkn[:], tmpB[:], ALU.mult, [kkn_t, tmpB_t], [kkn_t])
        yield
        dve_ts(tmpA[:], av[:], cv[:, 10:11], cv[:, 14:15], ALU.mult, ALU.add, [av_t, cv_t], [tmpA_t])
        dve_tt(k2[:], shk[:], tmpA[:], ALU.mult, [shk_t, tmpA_t], [k2_t])
        dve_tt(tmpA[:], shr[:], k2[:], ALU.mult, [shr_t, k2_t], [tmpA_t], eng="pool")
        dve_ts(tmpA[:], tmpA[:], cv[:, 11:12], None, ALU.mult, None, [tmpA_t, cv_t], [tmpA_t])
        pb, pb_t = scr()
        S.op("pe", mm(pb[:, 0:G1], onesblk, tmpA[:]), reads=[cst_t, tmpA_t], writes=[pb_t])
        dve_tt(bonus[:], pb[:, 0:G1], shv[:], ALU.mult, [pb_t, shv_t], [bonus_t])
        S.op("dve", lambda e: e.tensor_tensor_scan(out=cum[:], data0=rmask[:], data1=logw[:], initial=0.0, op0=ALU.mult, op1=ALU.add),
             reads=[rmask_t, logw_t], writes=[cum_t])
        yield
        act(Pm[:], cum[:], AF.Exp, [cum_t], [Pm_t])
        act(Pinv[:], cum[:], AF.Exp, [cum_t], [Pinv_t], scale=-1.0)
        dve_tt(tmpA[:], cum[:], logw[:], ALU.subtract, [cum_t, logw_t], [tmpA_t], eng="pool")
        act(Pprev[:], tmpA[:], AF.Exp, [tmpA_t], [Pprev_t])
        dve_stt(At[:], kkn[:], -1.0, Pprev[:], ALU.mult, ALU.mult, [kkn_t, Pprev_t], [At_t])
        dve_tt(tmpB[:], kkn[:], av[:], ALU.mult, [kkn_t, av_t], [tmpB_t], eng="pool")
        dve_tt(Bt[:], tmpB[:], Pinv[:], ALU.mult, [tmpB_t, Pinv_t], [Bt_t])
        dve_tt(Kt[:], k2[:], Pinv[:], ALU.mult, [k2_t, Pinv_t], [Kt_t], eng="pool")
        dve_tt(Rt[:], shr[:], Pm[:], ALU.mult, [shr_t, Pm_t], [Rt_t])
        S.op("pool", lambda e: e.tensor_copy(out=vb16[:], in_=shv[:]), reads=[shv_t], writes=[vb16_t])
        yield
        for tl in range(G1 // 128):
            cs = slice(tl * 128, (tl + 1) * 128)
            for j, (src, srct) in enumerate([(At, At_t), (Bt, Bt_t), (Kt, Kt_t), (vb16, vb16_t)]):
                S.op("pe", lambda e, j=j, src=src: e.transpose(out=psb[0][:, j * 128:(j + 1) * 128], in_=src[:, cs], identity=identb[:]),
                     reads=[srct, identb_t], writes=[psb_t[0]])
            copy_out(TOK[:].rearrange("p a b -> p (a b)"), psb[0][:, 0:512], [psb_t[0]], [TOK_t], eng="act")
            yield
            chains = [head_chain(0, cs), head_chain(1, cs)]
            while chains:
                for ch in list(chains):
                    try:
                        next(ch)
                    except StopIteration:
                        chains.remove(ch)
                yield
            dve_tt(MT[:], ps[2][:, 0:256].rearrange("p (c k) -> p c k", c=4), I64.unsqueeze(1).broadcast_to([128, 4, 64]), ALU.add,
                   [ps_t[2], cst_t], [MT_t])
            for c in range(4):
                col = tl * 128 + 32 * c + 31
                dve_ts(HP[:, c, :], ps[3][:, c * 64:(c + 1) * 64], Pm[:, col:col + 1], None, ALU.mult, None, [ps_t[3], Pm_t], [HP_t])
            yield
            for c in range(4):
                col = tl * 128 + 32 * c + 31
                sc_, sc_t = Sst[scur[0]]
                sn_, sn_t = Sst[1 - scur[0]]
                for h in range(2):
                    hs = slice(64 * h, 64 * h + 64)
                    S.op("pe", mm(ps[0][hs, 0:64], MT[hs, c, :], sc_[hs, :]), reads=[MT_t, sc_t], writes=[ps_t[0]])
                for h in range(2):
                    hs = slice(64 * h, 64 * h + 64)
                    S.op("pe", mm(ps[1][hs, 32 * c:32 * c + 32], sc_[hs, :], RhT[hs, 32 * c:32 * c + 32]), reads=[sc_t, RhT_t], writes=[ps_t[1]])
                dve_stt(sn_[:], ps[0][:, 0:64], Pm[:, col:col + 1], HP[:, c, :], ALU.mult, ALU.add, [ps_t[0], Pm_t, HP_t], [sn_t])
                scur[0] = 1 - scur[0]
                yield
            dve_tt(yT[:, cs], ps[1][:, 0:128], YhT[:], ALU.add, [ps_t[1], YhT_t], [yT_t])
            yield
        pb, pb_t = scr()
        S.op("pe", mm(pb[:, 0:G1], onesblk, yT[:]), reads=[cst_t, yT_t], writes=[pb_t])
        act(tmpA[:], pb[:, 0:G1], AF.Copy, [pb_t], [tmpA_t], scale=1.0 / 64)
        act(tmpB[:], yT[:], AF.Square, [yT_t], [tmpB_t])
        pb, pb_t = scr()
        S.op("pe", mm(pb[:, 0:G1], onesblk, tmpB[:]), reads=[cst_t, tmpB_t], writes=[pb_t])
        dve_tt(tmpB[:], tmpA[:], tmpA[:], ALU.mult, [tmpA_t], [tmpB_t], eng="pool")
        dve_stt(tmpB[:], pb[:, 0:G1], 1.0 / 64, tmpB[:], ALU.mult, ALU.subtract, [pb_t, tmpB_t], [tmpB_t])
        yield
        act(tmpB[:], tmpB[:], AF.Sqrt, [tmpB_t], [tmpB_t], bias=64e-5)
        S.op("dve", lambda e: e.reciprocal(out=tmpB[:], in_=tmpB[:]), reads=[tmpB_t], writes=[tmpB_t])
        dve_tt(yo[:], yT[:], tmpA[:], ALU.subtract, [yT_t, tmpA_t], [yo_t])
        dve_tt(yo[:], yo[:], tmpB[:], ALU.mult, [yo_t, tmpB_t], [yo_t])
        dve_ts(yo[:], yo[:], cv[:, 12:13], cv[:, 13:14], ALU.mult, ALU.add, [yo_t, cv_t], [yo_t])
        dve_tt(yo[:], yo[:], bonus[:], ALU.add, [yo_t, bonus_t], [yo_t], eng="pool")
        dve_tt(yo[:], yo[:], gg[:], ALU.mult, [yo_t, gg_t], [yo_t])
        S.dma("sp", "oya", lambda e: e.dma_start(out=yaT[:, t0:t0 + G1], in_=yo[:]), reads=[yo_t], writes=[])
        yield

    def attn_group(g):
        for m in range(2):
            ms = slice(64 * m, 64 * m + 64)
            nsh, nsh_t = nshift[m]
            act(sqq[:], qmax2[m][0][:], AF.Sqrt, [qmax2[m][1], kmax2[m][1]], [sqq_t], scale=kmax2[m][0][:, 0:1])
            dve_ts(nsh[:], sqq[:], -0.125, None, ALU.mult, None, [sqq_t], [nsh_t])

            def stage_a(j):
                P_, P_t = Pb[j % 2]
                if j < g:
                    for a in range(2):
                        kt = 2 * j + a
                        S.op("pe", mm(ps[5][:, a * 256:(a + 1) * 256], KT[ms, kt * 128:(kt + 1) * 128], QT[ms, :]), reads=[QT_t, KT_t], writes=[ps_t[5]])
                    act(P_[:], ps[5][:, :], AF.Exp, [ps_t[5], nsh_t], [P_t], scale=0.125, bias=nsh[:, 0:1])
                else:
                    kt = 2 * g
                    S.op("pe", mm(ps[5][:, 0:256], KT[ms, kt * 128:(kt + 1) * 128], QT[ms, :]), reads=[QT_t, KT_t], writes=[ps_t[5]])
                    S.op("pe", mm(ps[5][:, 384:512], KT[ms, (kt + 1) * 128:(kt + 2) * 128], QT[ms, 128:256]), reads=[QT_t, KT_t], writes=[ps_t[5]])
                    act(P_[:, 0:256], ps[5][:, 0:256], AF.Exp, [ps_t[5], nsh_t], [P_t], scale=0.125, bias=nsh[:, 0:1])
                    act(P_[:, 384:512], ps[5][:, 384:512], AF.Exp, [ps_t[5], nsh_t], [P_t], scale=0.125, bias=nsh[:, 0:1])
                    dve_tt(P_[:, 0:128], P_[:, 0:128], trib[:], ALU.mult, [P_t, trib_t], [P_t], eng="pool")
                    dve_tt(P_[:, 384:512], P_[:, 384:512], trib[:], ALU.mult, [P_t, trib_t], [P_t], eng="pool")

            def stage_b(j):
                P_, P_t = Pb[j % 2]
                if j < g:
                    for a in range(2):
                        kt = 2 * j + a
                        for tl in range(2):
                            ob = OB[tl]
                            S.op("pe", mm(ps[ob][:, 0:129], P_[:, a * 256 + tl * 128:a * 256 + (tl + 1) * 128], VA[:, kt, 0:129], kt == 0, False),
                                 reads=[P_t, VA_t], writes=[ps_t[ob]])
                else:
                    kt = 2 * g
                    S.op("pe", mm(ps[OB[0]][:, 0:129], P_[:, 0:128], VA[:, kt, 0:129], kt == 0, True), reads=[P_t, VA_t], writes=[ps_t[OB[0]]])
                    S.op("pe", mm(ps[OB[1]][:, 0:129], P_[:, 128:256], VA[:, kt, 0:129], kt == 0, False), reads=[P_t, VA_t], writes=[ps_t[OB[1]]])
                    S.op("pe", mm(ps[OB[1]][:, 0:129], P_[:, 384:512], VA[:, kt + 1, 0:129], False, True), reads=[P_t, VA_t], writes=[ps_t[OB[1]]])

            stage_a(0)
            for j in range(g + 1):
                if j + 1 <= g:
                    stage_a(j + 1)
                stage_b(j)
                yield
            for tl in range(2):
                ob = OB[tl]
                S.op("dve", lambda e, ob=ob: e.reciprocal(out=rs_[:], in_=ps[ob][:, 128:129]), reads=[ps_t[ob]], writes=[rs_t])
                dve_ts(om[tl][m][0][:], ps[ob][:, 0:128], rs_[:, 0:1], None, ALU.mult, None, [ps_t[ob], rs_t], [om[tl][m][1]])
            yield
        for tl in range(2):
            qt = 2 * g + tl
            dve_stt(attn[:], om[tl][1][0][:], neglam[:, 0:1], om[tl][0][0][:], ALU.mult, ALU.add, [om[tl][0][1], om[tl][1][1], neglam_t], [attn_t])
            S.op("act", lambda e: e.activation(out=junk2[:], in_=attn[:], func=AF.Square, accum_out=ss2[:]), reads=[attn_t], writes=[junk2_t, ss2_t])
            S.op("act", lambda e: e.activation(out=ss2[:], in_=ss2[:], func=AF.Sqrt, scale=1.0 / 128, bias=1e-5), reads=[ss2_t], writes=[ss2_t])
            S.op("dve", lambda e: e.reciprocal(out=rstd2[:], in_=ss2[:]), reads=[ss2_t], writes=[rstd2_t])
            dve_stt(ybo[:], attn[:], rstd2[:, 0:1], sgv[:], ALU.mult, ALU.mult, [attn_t, rstd2_t, sgv_t], [ybo_t])
            S.dma("sp", "oyb", lambda e, qt=qt: e.dma_start(out=yb[qt * 128:(qt + 1) * 128, :], in_=ybo[:]), reads=[ybo_t], writes=[])
            yield

    def load_x(g_):
        for tl in range(2):
            xb, xb_t = xt[tl]
            S.dma("sp", "x%d" % tl, lambda e, tl=tl, xb=xb: e.dma_start(out=xb[:], in_=x[g_ * G1 + tl * 128:g_ * G1 + (tl + 1) * 128, :]), writes=[xb_t])

    load_x(0)
    for g in range(ngroups):
        t0 = g * G1
        for tl in range(2):
            xb, xb_t = xt[tl]
            _rms_rstd(S, "act", xb[:], xb_t, junk[:], junk_t, ss[:], ss_t, rstd[:], rstd_t, 1e-6)
            dve_ts(xnb[:], xb[:], rstd[:, 0:1], None, ALU.mult, None, [xb_t, rstd_t], [xnb_t])
            for half in range(2):
                for j in range(8):
                    kc = half * 8 + j
                    S.op("pe", lambda e, kc=kc, j=j: e.transpose(out=psb[0][:, j * 128:(j + 1) * 128], in_=xnb[:, kc * 128:(kc + 1) * 128], identity=identb[:]),
                         reads=[xnb_t, identb_t], writes=[psb_t[0]])
                copy_out(xnT[:, half * 8:(half + 1) * 8, tl * 128:(tl + 1) * 128], psb[0][:].rearrange("p (k t) -> p k t", k=8), [psb_t[0]], [xnT_t])
        if g + 1 < ngroups:
            load_x(g + 1)
        blocks = [(C_R, 128), (C_K, 128), (C_V, 128), (C_XW, 96), (C_XA, 96), (C_XG, 128), (C_XG + 128, 128)]
        for bi, (c0, w) in enumerate(blocks):
            pb, pb_t = scr()
            for kc in range(16):
                S.op("pe", mm(pb[0:w, 0:G1], W1[:, kc, c0:c0 + w], xnT[:, kc, :], kc == 0, kc == 15), reads=[W1_t, xnT_t], writes=[pb_t])
            copy_out(PB[bi][0][0:w, 1:G1 + 1], pb[0:w, 0:G1], [pb_t], [PB[bi][1]])
        pb, pb_t = scr()
        for kc in range(16):
            S.op("pe", mm(pb[:, 0:G1], W1[:, kc, C_QD:C_QD + 128], xnT[:, kc, :], kc == 0, kc == 15), reads=[W1_t, xnT_t], writes=[pb_t])
        act(QT[:], pb[:, 0:G1], AF.Copy, [pb_t], [QT_t])
        act(QSQ[:], pb[:, 0:G1], AF.Square, [pb_t], [QSQ_t])
        pb, pb_t = scr()
        for kc in range(16):
            S.op("pe", mm(pb[:, 0:G1], W1[:, kc, C_KD:C_KD + 128], xnT[:, kc, :], kc == 0, kc == 15), reads=[W1_t, xnT_t], writes=[pb_t])
        act(KT[:, t0:t0 + G1], pb[:, 0:G1], AF.Copy, [pb_t], [KT_t])
        act(KSQ[:], pb[:, 0:G1], AF.Square, [pb_t], [KSQ_t])
        for m in range(2):
            ms = slice(64 * m, 64 * m + 64)
            pb, pb_t = scr()
            S.op("pe", mm(pb[:, 0:G1], ones128[ms, :], KSQ[ms, :]), reads=[ones_t, KSQ_t], writes=[pb_t])
            S.op("dve", lambda e, pb=pb: e.tensor_reduce(out=kred[:], in_=pb[:, 0:G1], axis=AX.X, op=ALU.max), reads=[pb_t], writes=[kred_t])
            dve_tt(kmax2[m][0][:], kmax2[m][0][:], kred[:], ALU.max, [kred_t, kmax2[m][1]], [kmax2[m][1]])
            pb, pb_t = scr()
            S.op("pe", mm(pb[:, 0:G1], ones128[ms, :], QSQ[ms, :]), reads=[ones_t, QSQ_t], writes=[pb_t])
            S.op("dve", lambda e, pb=pb: e.tensor_reduce(out=kred[:], in_=pb[:, 0:G1], axis=AX.X, op=ALU.max), reads=[pb_t], writes=[kred_t])
            dve_tt(qmax2[m][0][:], qmax2[m][0][:], kred[:], ALU.max, [kred_t, qmax2[m][1]], [qmax2[m][1]])
        for tl in range(2):
            pb, pb_t = scr()
            for kc in range(16):
                S.op("pe", mm(pb[:, 0:128], xnT[:, kc, tl * 128:(tl + 1) * 128], W1[:, kc, C_VD:C_VD + 128], kc == 0, kc == 15),
                     reads=[W1_t, xnT_t], writes=[pb_t])
            copy_out(VA[:, 2 * g + tl, 0:128], pb[:, 0:128], [pb_t], [VA_t])
        gens = []
        if do_rwkv:
            gr = rwkv_group(g)
            if rw_stage < 9000:
                def lim(gr=gr):
                    for _ in range(rw_stage):
                        next(gr)
                        yield
                gr = lim()
            gens.append(gr)
        if do_attn:
            gens.append(attn_group(g))
        _roundrobin(gens)
    for k in S.dsem:
        if k.startswith("dma:o"):
            nc.sync.wait_ge(S.dsem[k][0], S.dsem[k][1])
    return nc, S


def _consts():
    t = np.arange(128)
    same = (t[:, None] // CHK) == (t[None, :] // CHK)
    MU = (same & (t[:, None] < t[None, :])).astype(np.float32)
    ML = MU.T.copy()
    MUI = (same & (t[:, None] <= t[None, :])).astype(np.float32)
    TRI = (t[:, None] <= t[None, :]).astype(np.float32)
    ob = ((t[:, None] // 64) == (t[None, :] // 64)).astype(np.float32)
    i64 = np.zeros((128, 128), np.float32)
    i64[t, t % 64] = 1.0
    return np.stack([np.eye(128, dtype=np.float32), MU, ML, MUI, TRI, ob, i64])


def prep_l1(inp, c, ntok=SEQ):
    w_in = inp["w_in"][0]
    hs = slice(128 * c, 128 * c + 128)
    o_d = 3520
    cols = np.concatenate([np.arange(128 * c, 128 * c + 128), 1024 + np.arange(128 * c, 128 * c + 128),
                           2048 + np.arange(128 * c, 128 * c + 128), np.arange(3072, 3520),
                           o_d + np.arange(128 * c, 128 * c + 128), o_d + 1024 + np.arange(128 * c, 128 * c + 128),
                           o_d + 2048 + np.arange(128 * c, 128 * c + 128)])
    mu = inp["shift_mu"][0]
    cvec = np.zeros((128, 20), np.float32)
    cvec[:, 0] = mu[0:1024][hs]; cvec[:, 1] = mu[1024:2048][hs]; cvec[:, 2] = mu[2048:3072][hs]
    cvec[:96, 3] = mu[3072:3168]; cvec[:96, 4] = mu[3168:3264]; cvec[:, 5] = mu[3264:3392]; cvec[:, 6] = mu[3392:3520]
    cvec[:, 7] = inp["rwkv_w0"][0][hs]; cvec[:, 8] = inp["rwkv_a0"][0][hs]; cvec[:, 9] = inp["k_k"][0][hs]
    cvec[:, 10] = inp["k_a"][0][hs]; cvec[:, 11] = inp["r_k"][0].reshape(-1)[hs]
    cvec[:, 12] = inp["lnx_g"][0][hs]; cvec[:, 13] = inp["lnx_b"][0][hs]
    for c_ in range(4):
        cvec[32 * c_:32 * c_ + 32, 16 + c_] = 1.0
    rm = np.ones((1, G1), np.float32); rm[0, ::CHK] = 0.0
    return dict(
        x=np.ascontiguousarray(inp["x"][0, :ntok]), w1=np.ascontiguousarray(w_in[:, cols]), cvec=cvec,
        wdu=np.ascontiguousarray(inp["w_decay_up"][0][:, hs]), wiu=np.ascontiguousarray(inp["w_iclr_up"][0][:, hs]),
        wgu=np.ascontiguousarray(inp["w_gate_up"][0][:, hs]),
        lamv=np.concatenate([inp["lam_q1"][0], inp["lam_k1"][0], inp["lam_q2"][0], inp["lam_k2"][0]])[None, :].astype(np.float32),
        sublng=inp["subln_g"][0][None, :].astype(np.float32), g1=inp["norm1_g"][0][None, :].astype(np.float32),
        consts=_consts(), rmask=rm)


def _blk(w, nb):
    K_ = w.shape[0]
    return np.ascontiguousarray(w.reshape(K_ // 128, 128, nb, 512).transpose(2, 1, 0, 3).reshape(nb, 128, (K_ // 128) * 512))


W_BLOCKS = (("s_wg", 8), ("s_wab", 4), ("s_wo", 4), ("s_wq", 4), ("s_u", 32), ("s_v", 32))


def l2_weight_blocks(inp):
    w_in = inp["w_in"][0]
    wgb = _blk(w_in[:, 6592:], 8)
    wabb = np.concatenate([_blk(inp["w_proj_a"][0], 4), _blk(inp["w_proj_b"][0], 4)], axis=2)
    uTb = _blk(np.ascontiguousarray(inp["peer_u"][0].T), 32)
    vtb = inp["peer_v"][0].reshape(32, 4, 128, D).transpose(0, 2, 1, 3).reshape(32, 128, 4 * D)
    allb = np.zeros((NCAST * NCORES, 128, 8192), np.float32)
    o = 0
    for a in (wgb, wabb, _blk(inp["w_out"][0], 4), _blk(inp["peer_wq"][0], 4), uTb, vtb):
        allb[o:o + a.shape[0]] = a
        o += a.shape[0]
    return allb


def l2_shared(inp, cast_all):
    m = dict(
        skT=np.ascontiguousarray(inp["peer_sub_keys"][0].reshape(16, 128, 128).transpose(0, 2, 1)),
        gv=np.stack([inp["norm1_g"][0], inp["norm2_g"][0], inp["final_g"]]).astype(np.float32),
        ident=np.eye(128, dtype=np.float32))
    o = 0
    for name, nb in W_BLOCKS:
        m[name] = np.ascontiguousarray(cast_all[o:o + nb])
        o += nb
    return m


def prep_l2(inp, c, yaT_full, ybT_full, shared):
    ts = slice(TOK * c, TOK * (c + 1))
    m = dict(shared)
    m["x"] = np.ascontiguousarray(inp["x"][0, ts])
    m["yaT"] = np.ascontiguousarray(yaT_full[:, ts])
    m["ybT"] = np.ascontiguousarray(ybT_full[:, ts])
    return m


def kernel(**inputs):
    inp = {k: np.asarray(v) for k, v in inputs.items()}
    nc1, _ = build_l1()
    allb = l2_weight_blocks(inp)
    maps1 = [prep_l1(inp, c) for c in range(NCORES)]
    for c in range(NCORES):
        maps1[c]["castin"] = allb[NCAST * c:NCAST * (c + 1)]
    r1 = run_bass_kernel_spmd(nc1, maps1, core_ids=list(range(NCORES))).results
    del maps1, allb
    cast_all = np.concatenate([r1[c]["castout"] for c in range(NCORES)], axis=0)
    yaT_full = np.concatenate([r1[c]["yaT"] for c in range(NCORES)], axis=0)
    ybT_full = np.concatenate([r1[c]["yb"].T for c in range(NCORES)], axis=0)
    w_in = inp["w_in"][0]
    shared = l2_shared(inp, cast_all)
    nc2, _ = build_l2()
    maps2 = [prep_l2(inp, c, yaT_full, ybT_full, shared) for c in range(NCORES)]
    r2 = run_bass_kernel_spmd(nc2, maps2, core_ids=list(range(NCORES))).results
    out = np.concatenate([r2[c]["y"] for c in range(NCORES)], axis=0)
    return out.reshape(1, SEQ, D).astype(np.float32)
```
